# Optimizing a Trainium2 kernel written in Bass

```python
import math
import jax, jax.numpy as jnp
from jax import lax
import numpy as np

D_MODEL = 1024
BATCH = 4
SEQ = 4096
DEPTH = 4

EPS = 1e-6
CHUNK = 64
CONV_K = 4
GDN_HEADS = 4
GDN_HEAD_DIM = 128
GDN_DIM = GDN_HEADS * GDN_HEAD_DIM
SSM_HEADS = 8
SSM_HEAD_DIM = 64
SSM_INNER = SSM_HEADS * SSM_HEAD_DIM
SSM_GROUPS = 2
SSM_STATE = 128
SSM_CONV_DIM = SSM_INNER + 2 * SSM_GROUPS * SSM_STATE
MLA_HEADS = 4
MLA_Q_LORA = 512
MLA_KV_LORA = 256
MLA_NOPE = 128
MLA_ROPE = 64
MLA_V = 128
MLA_DIM = MLA_HEADS * MLA_V
ROPE_THETA = 10000.0
Q_BLOCK = 128
N_BRANCH = 3
FFN_DIM = 2816
N_EXPERTS = 8
TOP_K = 2
EXPERT_DIM = 3584
MOE_BLOCK = 256
N_DENSE = (DEPTH + 1) // 2
N_MOE = DEPTH // 2

IN_SPLITS = (
    3 * GDN_DIM,
    GDN_DIM,
    GDN_HEADS,
    GDN_HEADS,
    SSM_INNER,
    SSM_CONV_DIM,
    SSM_HEADS,
    MLA_Q_LORA,
    MLA_KV_LORA,
    MLA_ROPE,
    N_BRANCH * D_MODEL,
)
IN_DIM = sum(IN_SPLITS)

kernel_name = 'hybrid_gdn_ssd_mla_moe_block'


def rms_norm(x, w=None):
    xf = x.astype(jnp.float32)
    y = xf * lax.rsqrt(jnp.mean(xf * xf, axis=-1, keepdims=True) + EPS)
    if w is not None:
        y = y * w.astype(jnp.float32)
    return y.astype(x.dtype)


def l2_normalize(x):
    return x * lax.rsqrt(jnp.sum(x * x, axis=-1, keepdims=True) + EPS)


def split_cols(y, sizes):
    idx = [int(i) for i in np.cumsum(sizes)[:-1]]
    return jnp.split(y, idx, axis=-1)


def causal_conv(x, w, b=None):
    k = w.shape[0]
    y = lax.conv_general_dilated(x, w[:, None, :].astype(x.dtype), window_strides=(1,),
                                 padding=[(k - 1, 0)], dimension_numbers=('NWC', 'WIO', 'NWC'),
                                 feature_group_count=x.shape[-1])
    if b is not None:
        y = y + b
    return y


def swiglu(x, w_gate, w_up, w_down):
    return (jax.nn.silu(x @ w_gate) * (x @ w_up)) @ w_down


def gated_delta_net(qkv, z, a, b, conv_w, a_log, dt_bias, norm_w):
    bsz, s, _ = qkv.shape
    H, Dh, C = GDN_HEADS, GDN_HEAD_DIM, CHUNK
    nc = s // C
    f32 = jnp.float32
    qkv = jax.nn.silu(causal_conv(qkv, conv_w)).astype(f32)
    q, k, v = jnp.split(qkv, 3, axis=-1)
    q = l2_normalize(q.reshape(bsz, s, H, Dh)) * (Dh ** -0.5)
    k = l2_normalize(k.reshape(bsz, s, H, Dh))
    v = v.reshape(bsz, s, H, Dh)
    beta = jax.nn.sigmoid(b.astype(f32))
    g = -jnp.exp(a_log.astype(f32)) * jax.nn.softplus(a.astype(f32) + dt_bias.astype(f32))

    def to_chunks(t):
        return t.reshape(bsz, nc, C, H, -1).transpose(0, 3, 1, 2, 4)
    q, k, v = to_chunks(q), to_chunks(k), to_chunks(v)
    beta = beta.reshape(bsz, nc, C, H).transpose(0, 3, 1, 2)
    g = g.reshape(bsz, nc, C, H).transpose(0, 3, 1, 2)
    G = jnp.cumsum(g, axis=-1)
    incl = jnp.tril(jnp.ones((C, C), dtype=bool))
    strict = jnp.tril(jnp.ones((C, C), dtype=bool), -1)
    decay = jnp.exp(jnp.where(incl, G[..., :, None] - G[..., None, :], -jnp.inf))
    kk = jnp.einsum('bhnid,bhnjd->bhnij', k, k)
    ia = jnp.eye(C, dtype=f32) + jnp.where(strict, beta[..., :, None] * kk * decay, 0.0)
    rhs = jnp.concatenate([v * beta[..., None], k * (beta * jnp.exp(G))[..., None]], axis=-1)
    sol = lax.linalg.triangular_solve(ia, rhs, left_side=True, lower=True, unit_diagonal=True)
    u, w = jnp.split(sol, 2, axis=-1)
    qk = jnp.einsum('bhnid,bhnjd->bhnij', q, k) * decay
    q_dec = q * jnp.exp(G)[..., None]
    k_dec = k * jnp.exp(G[..., -1:] - G)[..., None]
    g_last = jnp.exp(G[..., -1])

    def step(state, inp):
        qd, kd, uc, wc, qkc, gl = inp
        v_new = uc - jnp.einsum('bhcd,bhde->bhce', wc, state)
        o = jnp.einsum('bhcd,bhde->bhce', qd, state) + jnp.einsum('bhij,bhje->bhie', qkc, v_new)
        state = state * gl[..., None, None] + jnp.einsum('bhcd,bhce->bhde', kd, v_new)
        return state, o

    xs = tuple(jnp.moveaxis(t, 2, 0) for t in (q_dec, k_dec, u, w, qk, g_last))
    _, o = lax.scan(step, jnp.zeros((bsz, H, Dh, Dh), f32), xs)
    o = o.transpose(1, 0, 3, 2, 4).reshape(bsz, s, H, Dh)
    o = rms_norm(o, norm_w) * jax.nn.silu(z.astype(f32).reshape(bsz, s, H, Dh))
    return o.reshape(bsz, s, GDN_DIM).astype(z.dtype)


def mamba2_ssd(xbc, z, dt, conv_w, conv_b, a_log, dt_bias, d_skip, norm_w):
    bsz, s, _ = xbc.shape
    H, P, Gn, N, C = SSM_HEADS, SSM_HEAD_DIM, SSM_GROUPS, SSM_STATE, CHUNK
    E = H // Gn
    nc = s // C
    f32 = jnp.float32
    xbc = jax.nn.silu(causal_conv(xbc, conv_w, conv_b)).astype(f32)
    xs, bm, cm = jnp.split(xbc, [SSM_INNER, SSM_INNER + Gn * N], axis=-1)
    x = xs.reshape(bsz, s, H, P)
    dt = jax.nn.softplus(dt.astype(f32) + dt_bias.astype(f32))
    A = -jnp.exp(a_log.astype(f32))
    X = (x * dt[..., None]).reshape(bsz, nc, C, Gn, E, P)
    ad = (dt * A).reshape(bsz, nc, C, Gn, E).transpose(0, 3, 4, 1, 2)
    bm = bm.reshape(bsz, nc, C, Gn, N)
    cm = cm.reshape(bsz, nc, C, Gn, N)
    acs = jnp.cumsum(ad, axis=-1)
    incl = jnp.tril(jnp.ones((C, C), dtype=bool))
    L = jnp.exp(jnp.where(incl, acs[..., :, None] - acs[..., None, :], -jnp.inf))
    cb = jnp.einsum('bclgn,bcsgn->bcgls', cm, bm)
    y_diag = jnp.einsum('bcgls,bgecls,bcsgep->bclgep', cb, L, X)
    decay_states = jnp.exp(acs[..., -1:] - acs)
    states = jnp.einsum('bclgn,bgecl,bclgep->cbgepn', bm, decay_states, X)
    chunk_decay = jnp.moveaxis(jnp.exp(acs[..., -1]), -1, 0)

    def step(h, inp):
        s_c, d_c = inp
        return h * d_c[..., None, None] + s_c, h

    _, prev = lax.scan(step, jnp.zeros(states.shape[1:], f32), (states, chunk_decay))
    y_off = jnp.einsum('bclgn,cbgepn,bgecl->bclgep', cm, prev, jnp.exp(acs))
    y = (y_diag + y_off).reshape(bsz, s, H, P) + d_skip.astype(f32)[:, None] * x
    y = y.reshape(bsz, s, Gn, SSM_INNER // Gn) * jax.nn.silu(z.astype(f32).reshape(bsz, s, Gn, -1))
    y = rms_norm(y, norm_w.reshape(Gn, -1))
    return y.reshape(bsz, s, SSM_INNER).astype(z.dtype)


def apply_rope(x, cos, sin):
    x1, x2 = jnp.split(x, 2, axis=-1)
    return jnp.concatenate([x1 * cos - x2 * sin, x2 * cos + x1 * sin], axis=-1)


def mla_attention(c_q, c_kv, k_r, positions, q_norm_w, w_uq, kv_norm_w, w_uk, w_uv):
    bsz, s, _ = c_q.shape
    H = MLA_HEADS
    f32 = jnp.float32
    q = (rms_norm(c_q, q_norm_w) @ w_uq).reshape(bsz, s, H, MLA_NOPE + MLA_ROPE)
    q_nope, q_rope = q[..., :MLA_NOPE], q[..., MLA_NOPE:]
    ckv = rms_norm(c_kv, kv_norm_w)
    k_nope = (ckv @ w_uk).reshape(bsz, s, H, MLA_NOPE)
    v = (ckv @ w_uv).reshape(bsz, s, H, MLA_V)
    inv_freq = ROPE_THETA ** (-jnp.arange(0, MLA_ROPE, 2, dtype=f32) / MLA_ROPE)
    ang = positions.astype(f32)[..., None] * inv_freq
    cos, sin = jnp.cos(ang), jnp.sin(ang)
    q_rope = apply_rope(q_rope.astype(f32), cos[:, :, None], sin[:, :, None])
    k_rope = apply_rope(k_r.astype(f32), cos, sin)
    scale = (MLA_NOPE + MLA_ROPE) ** -0.5
    qn = q_nope.transpose(0, 2, 1, 3)
    qr = q_rope.transpose(0, 2, 1, 3)
    kn = k_nope.transpose(0, 2, 1, 3)
    vh = v.transpose(0, 2, 1, 3)
    key_idx = jnp.arange(s)

    def attend_block(i):
        start = i * Q_BLOCK
        qn_b = lax.dynamic_slice_in_dim(qn, start, Q_BLOCK, axis=2)
        qr_b = lax.dynamic_slice_in_dim(qr, start, Q_BLOCK, axis=2)
        sc = (jnp.einsum('bhqd,bhkd->bhqk', qn_b, kn).astype(f32)
              + jnp.einsum('bhqr,bkr->bhqk', qr_b, k_rope)) * scale
        causal = (start + jnp.arange(Q_BLOCK))[:, None] >= key_idx[None, :]
        p = jax.nn.softmax(jnp.where(causal, sc, -jnp.inf), axis=-1)
        return jnp.einsum('bhqk,bhkd->bhqd', p.astype(vh.dtype), vh)

    o = lax.map(attend_block, jnp.arange(s // Q_BLOCK))
    return o.transpose(1, 0, 3, 2, 4).reshape(bsz, s, MLA_DIM)


def token_mixer(h, positions, w_in, gdn_conv_w, gdn_a_log, gdn_dt_bias, gdn_norm_w,
                ssm_conv_w, ssm_conv_b, ssm_a_log, ssm_dt_bias, ssm_d, ssm_norm_w,
                mla_q_norm_w, mla_w_uq, mla_kv_norm_w, mla_w_uk, mla_w_uv,
                w_branch_a, w_branch_b, w_branch_c, w_out):
    (gdn_qkv, gdn_z, gdn_a, gdn_b, ssm_z, ssm_xbc, ssm_dt,
     mla_cq, mla_ckv, mla_kr, gates) = split_cols(h @ w_in, IN_SPLITS)
    y_a = gated_delta_net(gdn_qkv, gdn_z, gdn_a, gdn_b, gdn_conv_w, gdn_a_log, gdn_dt_bias, gdn_norm_w)
    y_b = mamba2_ssd(ssm_xbc, ssm_z, ssm_dt, ssm_conv_w, ssm_conv_b, ssm_a_log, ssm_dt_bias, ssm_d, ssm_norm_w)
    y_c = mla_attention(mla_cq, mla_ckv, mla_kr, positions, mla_q_norm_w, mla_w_uq,
                        mla_kv_norm_w, mla_w_uk, mla_w_uv)
    g_a, g_b, g_c = jnp.split(jax.nn.sigmoid(gates), N_BRANCH, axis=-1)
    merged = g_a * (y_a @ w_branch_a) + g_b * (y_b @ w_branch_b) + g_c * (y_c @ w_branch_c)
    return merged @ w_out


def moe_ffn(h, router, w_gate, w_up, w_down):
    bsz, s, d = h.shape
    T = bsz * s
    f32 = jnp.float32
    xt = h.reshape(T, d)
    logits = (xt @ router).astype(f32)
    top_logits, top_idx = lax.top_k(logits, TOP_K)
    top_w = jax.nn.softmax(top_logits, axis=-1)
    n_assign = T * TOP_K
    n_blocks = (n_assign + N_EXPERTS * (MOE_BLOCK - 1)) // MOE_BLOCK
    slots = n_blocks * MOE_BLOCK
    flat_e = top_idx.reshape(-1)
    flat_tok = jnp.repeat(jnp.arange(T, dtype=jnp.int32), TOP_K)
    flat_w = top_w.reshape(-1)
    order = jnp.argsort(flat_e)
    sorted_e = flat_e[order]
    counts = jnp.bincount(flat_e, length=N_EXPERTS)
    padded = (counts + MOE_BLOCK - 1) // MOE_BLOCK * MOE_BLOCK
    start_sorted = jnp.cumsum(counts) - counts
    pad_end = jnp.cumsum(padded)
    start_padded = pad_end - padded
    dest = start_padded[sorted_e] + jnp.arange(n_assign) - start_sorted[sorted_e]
    slot_tok = jnp.full((slots,), T, jnp.int32).at[dest].set(flat_tok[order])
    slot_w = jnp.zeros((slots,), f32).at[dest].set(flat_w[order])
    block_e = jnp.minimum(jnp.searchsorted(pad_end, jnp.arange(n_blocks) * MOE_BLOCK, side='right'),
                          N_EXPERTS - 1)
    x_slots = jnp.take(xt, slot_tok, axis=0, mode='fill', fill_value=0).reshape(n_blocks, MOE_BLOCK, d)

    def expert_block(args):
        xb, e = args
        return swiglu(xb, w_gate[e], w_up[e], w_down[e])

    y_slots = lax.map(expert_block, (x_slots, block_e)).reshape(slots, d)
    out = jnp.zeros((T, d), f32).at[slot_tok].add(y_slots.astype(f32) * slot_w[:, None], mode='drop')
    return out.astype(h.dtype).reshape(bsz, s, d)


def setup_inputs(seed: int = 0) -> dict:
    key = jax.random.key(seed)
    ks = list(jax.random.split(key, 48))
    f32 = jnp.float32
    L = DEPTH

    def normal(shape, scale):
        return jax.random.normal(ks.pop(), shape, f32) * scale

    def uniform(shape, lo, hi):
        return jax.random.uniform(ks.pop(), shape, f32, lo, hi)

    def dt_bias_init(shape):
        dt = jnp.exp(uniform(shape, math.log(1e-3), math.log(1e-1)))
        return jnp.log(jnp.expm1(dt))

    x = normal((BATCH, SEQ, D_MODEL), 1.0)
    c = normal((BATCH, D_MODEL), 1.0)
    positions = (jax.random.randint(ks.pop(), (BATCH, 1), 0, 1024, dtype=jnp.int32)
                 + jnp.arange(SEQ, dtype=jnp.int32)[None, :])
    return {
        'x': x,
        'c': c,
        'positions': positions,
        'w_ada': normal((L, D_MODEL, 6 * D_MODEL), 0.5 * D_MODEL ** -0.5),
        'b_ada': normal((L, 6 * D_MODEL), 0.02),
        'w_in': normal((L, D_MODEL, IN_DIM), D_MODEL ** -0.5),
        'gdn_conv_w': normal((L, CONV_K, 3 * GDN_DIM), CONV_K ** -0.5),
        'gdn_a_log': jnp.log(uniform((L, GDN_HEADS), 1.0, 16.0)),
        'gdn_dt_bias': dt_bias_init((L, GDN_HEADS)),
        'gdn_norm_w': 1.0 + normal((L, GDN_HEAD_DIM), 0.02),
        'ssm_conv_w': normal((L, CONV_K, SSM_CONV_DIM), CONV_K ** -0.5),
        'ssm_conv_b': normal((L, SSM_CONV_DIM), 0.02),
        'ssm_a_log': jnp.log(uniform((L, SSM_HEADS), 1.0, 16.0)),
        'ssm_dt_bias': dt_bias_init((L, SSM_HEADS)),
        'ssm_d': 1.0 + normal((L, SSM_HEADS), 0.1),
        'ssm_norm_w': 1.0 + normal((L, SSM_INNER), 0.02),
        'mla_q_norm_w': 1.0 + normal((L, MLA_Q_LORA), 0.02),
        'mla_w_uq': normal((L, MLA_Q_LORA, MLA_HEADS * (MLA_NOPE + MLA_ROPE)), MLA_Q_LORA ** -0.5),
        'mla_kv_norm_w': 1.0 + normal((L, MLA_KV_LORA), 0.02),
        'mla_w_uk': normal((L, MLA_KV_LORA, MLA_HEADS * MLA_NOPE), MLA_KV_LORA ** -0.5),
        'mla_w_uv': normal((L, MLA_KV_LORA, MLA_HEADS * MLA_V), MLA_KV_LORA ** -0.5),
        'w_branch_a': normal((L, GDN_DIM, D_MODEL), GDN_DIM ** -0.5),
        'w_branch_b': normal((L, SSM_INNER, D_MODEL), SSM_INNER ** -0.5),
        'w_branch_c': normal((L, MLA_DIM, D_MODEL), MLA_DIM ** -0.5),
        'w_out': normal((L, D_MODEL, D_MODEL), D_MODEL ** -0.5),
        'ffn_w_gate': normal((N_DENSE, D_MODEL, FFN_DIM), D_MODEL ** -0.5),
        'ffn_w_up': normal((N_DENSE, D_MODEL, FFN_DIM), D_MODEL ** -0.5),
        'ffn_w_down': normal((N_DENSE, FFN_DIM, D_MODEL), FFN_DIM ** -0.5),
        'moe_router': normal((N_MOE, D_MODEL, N_EXPERTS), D_MODEL ** -0.5),
        'moe_w_gate': normal((N_MOE, N_EXPERTS, D_MODEL, EXPERT_DIM), D_MODEL ** -0.5),
        'moe_w_up': normal((N_MOE, N_EXPERTS, D_MODEL, EXPERT_DIM), D_MODEL ** -0.5),
        'moe_w_down': normal((N_MOE, N_EXPERTS, EXPERT_DIM, D_MODEL), EXPERT_DIM ** -0.5),
        'final_norm_w': 1.0 + normal((D_MODEL,), 0.02),
    }


def reference(x, c, positions, w_ada, b_ada, w_in, gdn_conv_w, gdn_a_log, gdn_dt_bias, gdn_norm_w,
              ssm_conv_w, ssm_conv_b, ssm_a_log, ssm_dt_bias, ssm_d, ssm_norm_w,
              mla_q_norm_w, mla_w_uq, mla_kv_norm_w, mla_w_uk, mla_w_uv,
              w_branch_a, w_branch_b, w_branch_c, w_out,
              ffn_w_gate, ffn_w_up, ffn_w_down,
              moe_router, moe_w_gate, moe_w_up, moe_w_down, final_norm_w):
    cond = jax.nn.silu(c)
    for l in range(DEPTH):
        mod = cond @ w_ada[l] + b_ada[l]
        sh1, sc1, g1, sh2, sc2, g2 = [m[:, None, :] for m in jnp.split(mod, 6, axis=-1)]
        h = rms_norm(x) * (1 + sc1) + sh1
        x = x + g1 * token_mixer(h, positions, w_in[l], gdn_conv_w[l], gdn_a_log[l], gdn_dt_bias[l],
                                 gdn_norm_w[l], ssm_conv_w[l], ssm_conv_b[l], ssm_a_log[l],
                                 ssm_dt_bias[l], ssm_d[l], ssm_norm_w[l], mla_q_norm_w[l],
                                 mla_w_uq[l], mla_kv_norm_w[l], mla_w_uk[l], mla_w_uv[l],
                                 w_branch_a[l], w_branch_b[l], w_branch_c[l], w_out[l])
        h = rms_norm(x) * (1 + sc2) + sh2
        i = l // 2
        if l % 2 == 0:
            f = swiglu(h, ffn_w_gate[i], ffn_w_up[i], ffn_w_down[i])
        else:
            f = moe_ffn(h, moe_router[i], moe_w_gate[i], moe_w_up[i], moe_w_down[i])
        x = x + g2 * f
    return rms_norm(x, final_norm_w)
```

```python
import numpy as np
from contextlib import ExitStack
import concourse.bass as bass
import concourse.mybir as mybir
from concourse.bass_utils import run_bass_kernel_spmd

F32, BF16, I32 = mybir.dt.float32, mybir.dt.bfloat16, mybir.dt.int32
AF = mybir.ActivationFunctionType
ALU = mybir.AluOpType
AX = mybir.AxisListType

D = 1024
DEPTH = 4
EPS = 1e-6
IN_DIM = 7504
FFN_DIM = 2816
EXPERT_DIM = 3584
NEXP = 8
SAME_SYNC = True

C_QKV, C_GZ, C_A, C_B, C_SZ, C_XBC, C_DT, C_CQ, C_CKV, C_KR, C_GATES = (
    0, 1536, 2048, 2052, 2056, 2568, 3592, 3600, 4112, 4368, 4432)


class Buf:
    def __init__(self, t, multi=False):
        self.t = t
        self.multi = multi
        self.w = {}
        self.r = {}

    def __getitem__(self, key):
        return self.t[key]


def _merge(d, tok):
    k_, v = tok
    if d.get(k_, 0) < v:
        d[k_] = v


class Ring:
    def __init__(self, bufs):
        self.bufs = bufs
        self.i = 0

    def get(self):
        b = self.bufs[self.i % len(self.bufs)]
        self.i += 1
        return b


class KB:
    ENG = ('pe', 'act', 'dve', 'pool', 'sp')

    def __init__(self, nc, es):
        self.nc = nc
        self.es = es
        self.e = {'pe': nc.tensor, 'act': nc.scalar, 'dve': nc.vector, 'pool': nc.gpsimd, 'sp': nc.sync}
        self.sem = {}
        self.cnt = {}
        for e in self.ENG:
            self.sem[('e', e)] = es.enter_context(nc.semaphore('se_' + e))
            self.cnt[('e', e)] = 0
        self.NS = 8
        self.dma_i = {}
        for q in ('sp', 'pool'):
            self.dma_i[q] = 0
            for j in range(self.NS):
                self.sem[('d', q, j)] = es.enter_context(nc.semaphore('sd_%s%d' % (q, j)))
                self.cnt[('d', q, j)] = 0
        self.seen = {e: {} for e in self.ENG}
        self.ninst = 0

    def _wait(self, eng, deps):
        for key, v in deps.items():
            if key == ('e', eng) and (eng == 'pe' or eng == 'sp' or not SAME_SYNC):
                continue
            if self.seen[eng].get(key, 0) >= v:
                continue
            self.e[eng].wait_ge(self.sem[key], v)
            self.seen[eng][key] = v
            self.ninst += 1

    def _deps(self, R, W):
        deps = {}
        for b in R:
            for t in b.w.items():
                _merge(deps, t)
        for b in W:
            for t in b.r.items():
                _merge(deps, t)
            if not b.multi:
                for t in b.w.items():
                    _merge(deps, t)
        return deps

    def _post(self, tok, R, W):
        for b in R:
            _merge(b.r, tok)
        for b in W:
            if b.multi:
                _merge(b.w, tok)
            else:
                b.w = {tok[0]: tok[1]}
                b.r = {}

    def op(self, eng, fn, R=(), W=()):
        self._wait(eng, self._deps(R, W))
        ins = fn(self.e[eng])
        key = ('e', eng)
        self.cnt[key] += 1
        ins.then_inc(self.sem[key], 1)
        self.ninst += 1
        self._post((key, self.cnt[key]), R, W)

    def dma(self, q, out, in_, R=(), W=()):
        self._wait(q, self._deps(R, W))
        key = ('d', q, self.dma_i[q] % self.NS)
        self.dma_i[q] += 1
        if self.cnt[key] > 0:
            self._wait(q, {key: self.cnt[key]})
        ins = self.e[q].dma_start(out=out, in_=in_)
        self.cnt[key] += 16
        ins.then_inc(self.sem[key], 16)
        self.ninst += 1
        self._post((key, self.cnt[key]), R, W)

    def barrier(self):
        for e in self.ENG:
            deps = {key: v for key, v in self.cnt.items() if v > 0 and key != ('e', e)}
            self._wait(e, deps)

    def mm(self, ps, out, lhsT, rhs, R, start=True, stop=True):
        self.op('pe', lambda e: e.matmul(out, lhsT, rhs, start=start, stop=stop), R=R, W=[ps])

    def tr(self, ps, out, in_, ident, R):
        self.op('pe', lambda e: e.transpose(out, in_, ident), R=R, W=[ps])

    def act(self, out, in_, func, R, W, bias=None, scale=None, accum_out=None, eng='act'):
        kw = {}
        if bias is not None:
            kw['bias'] = bias
        if scale is not None:
            kw['scale'] = scale
        if accum_out is not None:
            kw['accum_out'] = accum_out
        self.op('act', lambda e: e.activation(out=out, in_=in_, func=func, **kw), R=R, W=W)

    def tt(self, eng, out, in0, in1, op, R, W):
        self.op(eng, lambda e: e.tensor_tensor(out, in0, in1, op), R=R, W=W)

    def ts(self, eng, out, in0, s1, s2, op0, op1=None, R=(), W=()):
        if op1 is None:
            self.op(eng, lambda e: e.tensor_scalar(out, in0, s1, None, op0), R=R, W=W)
        else:
            self.op(eng, lambda e: e.tensor_scalar(out, in0, s1, s2, op0, op1), R=R, W=W)

    def stt(self, eng, out, in0, scalar, in1, op0, op1, R, W):
        self.op(eng, lambda e: e.scalar_tensor_tensor(out, in0, scalar, in1, op0, op1), R=R, W=W)

    def copy(self, eng, out, in_, R, W):
        if eng == 'act':
            self.op('act', lambda e: e.activation(out=out, in_=in_, func=AF.Copy), R=R, W=W)
        else:
            self.op(eng, lambda e: e.tensor_copy(out, in_), R=R, W=W)


def build_program(T, layers, debug=False, stages=('mix', 'ffn'), mixers=('mla', 'ssd', 'gdn')):
    nc = bass.Bass("TRN2", target_bir_lowering=False)
    L = DEPTH
    NT = T // 512
    NQ = T // 128
    NCH = T // 64

    def din(name, shape, dt=F32):
        return nc.dram_tensor(name, list(shape), dt, kind="ExternalInput").ap()

    def dscr(name, shape, dt, out=False):
        kind = "ExternalOutput" if out else "Internal"
        return nc.dram_tensor(name, list(shape), dt, kind=kind).ap()

    xT_in = din("xT", [D, T])
    cT_in = din("cT", [128, 8])
    pos_in = din("pos", [1, T], I32)
    w_ada = din("w_ada", [L, D, 6 * D])
    b_adaT = din("b_adaT", [L, 128, 48])
    w_in = din("w_in", [L, D, IN_DIM])
    gdn_convT = din("gdn_convT", [L, 128, 12, 4])
    gdn_alog = din("gdn_alog", [L, 128, 4])
    gdn_dtb = din("gdn_dtb", [L, 128, 4])
    gdn_nw = din("gdn_nw", [L, 128, 1])
    ssm_convT = din("ssm_convT", [L, 128, 8, 4])
    ssm_convb = din("ssm_convb", [L, 128, 8])
    ssm_alog = din("ssm_alog", [L, 128, 8])
    ssm_dtb = din("ssm_dtb", [L, 128, 8])
    ssm_dexp = din("ssm_dexp", [L, 128, 4])
    ssm_nw = din("ssm_nw", [L, 128, 4])
    mla_qnw = din("mla_qnw", [L, 128, 4])
    mla_wuq = din("mla_wuq", [L, 512, 768])
    mla_kvnw = din("mla_kvnw", [L, 128, 2])
    mla_wuk = din("mla_wuk", [L, 256, 512])
    mla_wuv = din("mla_wuv", [L, 256, 512])
    w_br = [din("w_branch_a", [L, 512, D]), din("w_branch_b", [L, 512, D]), din("w_branch_c", [L, 512, D])]
    w_out = din("w_out", [L, D, D])
    ffn_wg = din("ffn_w_gate", [2, D, FFN_DIM])
    ffn_wu = din("ffn_w_up", [2, D, FFN_DIM])
    ffn_wd = din("ffn_w_down", [2, FFN_DIM, D])
    moe_router = din("moe_router", [2, D, NEXP])
    moe_wg = din("moe_w_gate", [2, NEXP, D, EXPERT_DIM])
    moe_wu = din("moe_w_up", [2, NEXP, D, EXPERT_DIM])
    moe_wd = din("moe_w_down", [2, NEXP, EXPERT_DIM, D])
    fnwT = din("fnwT", [128, 8])
    consts = din("consts", [128, 8, 128])
    invf_in = din("invf", [128, 1])

    outT = dscr("outT", [D, T], F32, out=True)
    x_a = dscr("x_a", [D, T], F32, out=debug)
    x_b = dscr("x_b", [D, T], F32, out=debug)
    proj = dscr("proj", [IN_DIM, T], BF16, out=debug)
    abdt = dscr("abdt", [T, 16], F32, out=debug)
    y_d = [dscr("y_a", [512, T], BF16, out=debug), dscr("y_b", [512, T], BF16, out=debug),
           dscr("y_c", [512, T], BF16, out=debug)]

    es = ExitStack()
    with es:
        k = KB(nc, es)

        uid = [0]

        def sb(name, shape, dt, multi=False, stack=es):
            uid[0] += 1
            return Buf(stack.enter_context(nc.sbuf_tensor("%s_%d" % (name, uid[0]), list(shape), dt)), multi=multi)

        Dx_a, Dx_b, Dproj, Dabdt = Buf(x_a, True), Buf(x_b, True), Buf(proj, True), Buf(abdt, True)
        Dy = [Buf(y, True) for y in y_d]
        Dout = Buf(outT, True)
        Dw = Buf(None)
        Dxin = Buf(xT_in)

        psum = Ring([Buf(es.enter_context(nc.psum_tensor("ps%d" % i, [128, 512], F32))) for i in range(6)])
        psacc = Ring([Buf(es.enter_context(nc.psum_tensor("pa%d" % i, [128, 512], F32))) for i in range(2)])

        cst = sb("cst", [128, 8, 128], F32)
        k.dma('sp', cst[:], consts, R=[Dw], W=[cst])
        ident_f = cst[:, 0, :]
        ones_f = cst[:, 1, :]
        cst_b = sb("cst_b", [128, 2, 128], BF16)
        k.copy('dve', cst_b[:], cst[:, 0:2, :], R=[cst], W=[cst_b])
        ident_b = cst_b[:, 0, :]
        condT = sb("condT", [128, 8], F32)
        k.dma('sp', condT[:], cT_in, R=[Dw], W=[condT])
        k.act(condT[:], condT[:], AF.Silu, R=[condT], W=[condT])
        modT = sb("modT", [128, 48], F32)
        fnw = sb("fnw", [128, 8], F32)
        k.dma('sp', fnw[:], fnwT, R=[Dw], W=[fnw])

        def phase_ada(l):
            with ExitStack() as ps_:
                wr = Ring([sb("wada%d" % i, [128, 8, 768], F32, stack=ps_) for i in range(2)])
                bt = sb("badat", [128, 48], F32, stack=ps_)
                k.dma('sp', bt[:], b_adaT[l], R=[Dw], W=[bt])
                ps = psum.get()
                for g in range(8):
                    wb = wr.get()
                    k.dma('sp', wb[:], w_ada[l][:, g * 768:(g + 1) * 768].rearrange('(kc p) n -> p kc n', p=128),
                          R=[Dw], W=[wb])
                    for jj in range(6):
                        j = g * 6 + jj
                        for kc in range(8):
                            k.mm(ps, ps[:, j:j + 1], wb[:, kc, jj * 128:(jj + 1) * 128], condT[:, kc:kc + 1],
                                 R=[wb, condT], start=(kc == 0), stop=(kc == 7))
                k.tt('dve', modT[:], ps[:, 0:48], bt[:], ALU.add, R=[ps, bt], W=[modT])
                k.ts('dve', modT[:, 8:16], modT[:, 8:16], 1.0, None, ALU.add, R=[modT], W=[modT])
                k.ts('dve', modT[:, 32:40], modT[:, 32:40], 1.0, None, ALU.add, R=[modT], W=[modT])
                k.barrier()

        def norm_tile(xt, xap, sq, hdst, hbuf, sc_off, sh_off, hf=None):
            k.act(sq[:], xap, AF.Square, R=[xt], W=[sq])
            ps = psum.get()
            for kc in range(8):
                k.mm(ps, ps[:, :], ones_f, sq[:, kc, :], R=[sq, cst], start=(kc == 0), stop=(kc == 7))
            rstd = rstd_ring.get()
            rsqrt(rstd, rstd[:], ps, ps[:, :], 1.0 / D)
            for kc in range(8):
                k.tt('pool' if kc % 2 else 'dve', sq[:, kc, :], xap[:, kc, :], rstd[:], ALU.mult,
                     R=[xt, rstd], W=[sq])
            for kc in range(8):
                if sc_off is None:
                    k.ts('dve', hdst(kc), sq[:, kc, :], fnw[:, kc:kc + 1], None, ALU.mult, R=[sq, fnw], W=[hbuf])
                else:
                    k.ts('dve', hdst(kc), sq[:, kc, :], modT[:, sc_off + kc:sc_off + kc + 1],
                         modT[:, sh_off + kc:sh_off + kc + 1], ALU.mult, ALU.add, R=[sq, modT], W=[hbuf])
                    if hf is not None:
                        k.ts('pool', hf[0][:, kc, :], sq[:, kc, :], modT[:, sc_off + kc:sc_off + kc + 1],
                             modT[:, sh_off + kc:sh_off + kc + 1], ALU.mult, ALU.add, R=[sq, modT], W=[hf[0]])

        epsT = sb("epsT", [128, 1], F32)
        k.op('dve', lambda e: e.memset(epsT[:], EPS), W=[epsT])

        def rsqrt(ob, out, ib, in_, scale):
            k.act(out, in_, AF.Sqrt, R=[ib, epsT], W=[ob], bias=epsT[:out.shape[0], 0:1], scale=scale)
            k.op('dve', lambda e: e.reciprocal(out, out), R=[ob], W=[ob])

        rstd_ring = Ring([sb("rstd%d" % i, [128, 512], F32) for i in range(2)])

        def phase_inproj(l, xsrc, Dxsrc):
            with ExitStack() as ps_:
                h1 = sb("h1", [128, 8, T], BF16, multi=True, stack=ps_)
                xr = Ring([sb("xt%d" % i, [128, 8, 512], F32, stack=ps_) for i in range(2)])
                sq = sb("sq", [128, 8, 512], F32, stack=ps_)
                hf = sb("hf", [128, 8, 512], F32, stack=ps_)
                wsm = sb("wsm", [128, 8, 16], F32, stack=ps_)
                sm_st = Ring([sb("smst%d" % i, [128, 16], F32, stack=ps_) for i in range(2)])
                k.dma('sp', wsm[:, :, 0:8], w_in[l][:, C_A:C_A + 8].rearrange('(kc p) n -> p kc n', p=128),
                      R=[Dw], W=[wsm])
                k.dma('sp', wsm[:, :, 8:16], w_in[l][:, C_DT:C_DT + 8].rearrange('(kc p) n -> p kc n', p=128),
                      R=[Dw], W=[wsm])
                for tt in range(NT):
                    xt = xr.get()
                    k.dma('sp', xt[:], xsrc[:, tt * 512:(tt + 1) * 512].rearrange('(kc p) t -> p kc t', p=128),
                          R=[Dxsrc], W=[xt])
                    norm_tile(xt, xt[:], sq, lambda kc: h1[:, kc, tt * 512:(tt + 1) * 512], h1, 8, 0, hf=[hf])
                    for q in range(4):
                        ps = psum.get()
                        for kc in range(8):
                            k.mm(ps, ps[:, 0:16], hf[:, kc, q * 128:(q + 1) * 128], wsm[:, kc, :], R=[hf, wsm],
                                 start=(kc == 0), stop=(kc == 7))
                        st = sm_st.get()
                        k.copy('act', st[:], ps[:, 0:16], R=[ps], W=[st])
                        t0 = tt * 512 + q * 128
                        k.dma('sp', abdt[t0:t0 + 128, :], st[:], R=[st], W=[Dabdt])
                groups = [(C_QKV, 1536, 'copy'), (C_GZ, 512, 'silu'), (C_SZ, 512, 'silu'), (C_XBC, 1024, 'copy'),
                          (C_CQ, 512, 'copy'), (C_CKV, 256, 'copy'), (C_KR, 64, 'copy'), (C_GATES, 3072, 'sig')]
                wr = Ring([sb("win%d" % i, [128, 8, 512], BF16, stack=ps_) for i in range(2)])
                stg = Ring([sb("stg%d" % i, [128, 512], BF16, stack=ps_) for i in range(4)])
                ecnt = 0
                for (c0, ncols, post) in groups:
                    for blk in range(0, ncols, 512):
                        bw = min(512, ncols - blk)
                        wt = wr.get()
                        k.dma('pool', wt[:, :, :bw],
                              w_in[l][:, c0 + blk:c0 + blk + bw].rearrange('(kc p) n -> p kc n', p=128),
                              R=[Dw], W=[wt])
                        for tt in range(NT):
                            for ct in range(0, bw, 128):
                                m = min(128, bw - ct)
                                ps = psum.get()
                                for kc in range(8):
                                    k.mm(ps, ps[:m, :], wt[:, kc, ct:ct + m], h1[:, kc, tt * 512:(tt + 1) * 512],
                                         R=[wt, h1], start=(kc == 0), stop=(kc == 7))
                                st = stg.get()
                                if post == 'silu':
                                    k.act(st[:m, :], ps[:m, :], AF.Silu, R=[ps], W=[st])
                                elif post == 'sig':
                                    k.act(st[:m, :], ps[:m, :], AF.Sigmoid, R=[ps], W=[st])
                                else:
                                    ecnt += 1
                                    k.copy('dve' if ecnt % 2 else 'act', st[:m, :], ps[:m, :], R=[ps], W=[st])
                                r0 = c0 + blk + ct
                                k.dma('sp', proj[r0:r0 + m, tt * 512:(tt + 1) * 512], st[:m, :], R=[st], W=[Dproj])
                k.barrier()


        def dump(name, b, ap=None, dt=None):
            if not debug:
                return
            ap = b[:] if ap is None else ap
            uid[0] += 1
            t = nc.dram_tensor("dbg_%s_%d" % (name, uid[0]), list(ap.shape), dt or ap.dtype, kind="ExternalOutput").ap()
            k.dma('sp', t, ap, R=[b], W=[Buf(None, True)])

        cols = sb("cols", [128, 4], F32)
        k.op('dve', lambda e: e.memset(cols[:, 0:1], 1.0), W=[cols])
        k.op('dve', lambda e: e.memset(cols[:, 1:2], -np.pi), W=[cols])
        invf = sb("invf", [128, 1], F32)
        k.dma('sp', invf[:], invf_in, R=[Dw], W=[invf])
        ATT_SCALE = 192.0 ** -0.5

        def phase_mla(l):
            with ExitStack() as ps_:
                wuq = sb("wuq", [128, 4, 768], BF16, stack=ps_)
                wuqr = sb("wuqr", [128, 4, 2, 128], BF16, stack=ps_)
                wuk = sb("wuk", [128, 2, 512], BF16, stack=ps_)
                wuv = sb("wuv", [128, 2, 512], BF16, stack=ps_)
                qnw = sb("qnw", [128, 4], F32, stack=ps_)
                kvnw = sb("kvnw", [128, 2], F32, stack=ps_)
                k.dma('pool', wuq[:], mla_wuq[l].rearrange('(kc p) n -> p kc n', p=128), R=[Dw], W=[wuq])
                for h in range(4):
                    k.dma('pool', wuqr[:, :, h // 2, (h % 2) * 64:(h % 2) * 64 + 64],
                          mla_wuq[l][:, h * 192 + 128:h * 192 + 192].rearrange('(kc p) n -> p kc n', p=128),
                          R=[Dw], W=[wuqr])
                k.dma('pool', wuk[:], mla_wuk[l].rearrange('(kc p) n -> p kc n', p=128), R=[Dw], W=[wuk])
                k.dma('pool', wuv[:], mla_wuv[l].rearrange('(kc p) n -> p kc n', p=128), R=[Dw], W=[wuv])
                k.dma('sp', qnw[:], mla_qnw[l], R=[Dw], W=[qnw])
                k.dma('sp', kvnw[:], mla_kvnw[l], R=[Dw], W=[kvnw])
                qn = sb("qn", [128, 4, T], BF16, multi=True, stack=ps_)
                qr = sb("qr", [128, 2, T], BF16, multi=True, stack=ps_)
                kn = sb("kn", [128, 4, T], BF16, multi=True, stack=ps_)
                krp = sb("krp", [128, T], BF16, multi=True, stack=ps_)
                vtm = sb("vtm", [128, NQ, 512], BF16, multi=True, stack=ps_)
                with ExitStack() as p1:
                    cin = Ring([sb("mcin%d" % i, [128, 7, 512], BF16, stack=p1) for i in range(2)])
                    sqm = sb("msq", [128, 4, 512], F32, stack=p1)
                    cqn = sb("cqn", [128, 4, 512], BF16, stack=p1)
                    ckvn = sb("ckvn", [128, 2, 512], BF16, stack=p1)
                    posi = sb("posi", [128, 512], I32, stack=p1)
                    posf = sb("posf", [128, 512], F32, stack=p1)
                    frac = sb("frac", [128, 512], F32, stack=p1)
                    fint = sb("fint", [128, 512], I32, stack=p1)
                    ftmp = sb("ftmp", [128, 512], F32, stack=p1)
                    sinT = sb("sinT", [128, 512], F32, stack=p1)
                    cosT = sb("cosT", [128, 512], F32, stack=p1)
                    rf = sb("rf", [128, 512], F32, stack=p1)
                    r1 = sb("r1", [128, 512], F32, stack=p1)
                    r2 = sb("r2", [128, 512], F32, stack=p1)
                    rstd = sb("mrstd", [128, 512], F32, stack=p1)
                    rot2 = cst[:, 5, :]

                    def rope(src_b, src_ap, dst_b, dst_ap, scale):
                        k.copy('act', rf[:], src_ap, R=[src_b], W=[rf])
                        ps = psum.get()
                        k.mm(ps, ps[:, :], rot2, rf[:], R=[cst, rf])
                        k.stt('dve', r1[:], rf[:], scale, cosT[:], ALU.mult, ALU.mult, R=[rf, cosT], W=[r1])
                        k.stt('dve', r2[:], ps[:, :], scale, sinT[:], ALU.mult, ALU.mult, R=[ps, sinT], W=[r2])
                        k.tt('dve', dst_ap, r1[:], r2[:], ALU.add, R=[r1, r2], W=[dst_b])

                    for tt in range(NT):
                        ts_ = slice(tt * 512, (tt + 1) * 512)
                        ci = cin.get()
                        k.dma('sp', ci[:, 0:6, :], proj[C_CQ:C_CQ + 768, ts_].rearrange('(kc p) t -> p kc t', p=128),
                              R=[Dproj], W=[ci])
                        k.dma('sp', ci[0:64, 6, :], proj[C_KR:C_KR + 64, ts_], R=[Dproj], W=[ci])
                        k.dma('sp', ci[64:128, 6, :], proj[C_KR:C_KR + 64, ts_], R=[Dproj], W=[ci])
                        k.dma('sp', posi[:], pos_in[0:1, ts_].partition_broadcast(128), R=[Dw], W=[posi])
                        k.copy('dve', posf[:], posi[:], R=[posi], W=[posf])
                        for (off, dst) in ((0.5, sinT), (0.75, cosT)):
                            k.ts('dve', frac[:], posf[:], invf[:, 0:1], 1.0 / (2 * np.pi), ALU.mult, ALU.mult,
                                 R=[posf, invf], W=[frac])
                            k.ts('dve', frac[:], frac[:], off, None, ALU.add, R=[frac], W=[frac])
                            k.copy('dve', fint[:], frac[:], R=[frac], W=[fint])
                            k.copy('dve', ftmp[:], fint[:], R=[fint], W=[ftmp])
                            k.tt('dve', frac[:], frac[:], ftmp[:], ALU.subtract, R=[frac, ftmp], W=[frac])
                            k.ts('dve', ftmp[:], frac[:], 0.0, None, ALU.is_lt, R=[frac], W=[ftmp])
                            k.tt('dve', frac[:], frac[:], ftmp[:], ALU.add, R=[frac, ftmp], W=[frac])
                            k.act(dst[:], frac[:], AF.Sin, R=[frac, cols], W=[dst], bias=cols[:, 1:2],
                                  scale=2 * np.pi)
                        for (c0, nk_, wv, dstb) in ((0, 4, qnw, cqn), (4, 2, kvnw, ckvn)):
                            k.act(sqm[:, 0:nk_, :], ci[:, c0:c0 + nk_, :], AF.Square, R=[ci], W=[sqm])
                            ps = psum.get()
                            for kc in range(nk_):
                                k.mm(ps, ps[:, :], ones_f, sqm[:, kc, :], R=[cst, sqm], start=(kc == 0),
                                     stop=(kc == nk_ - 1))
                            rsqrt(rstd, rstd[:], ps, ps[:, :], 1.0 / (nk_ * 128))
                            for kc in range(nk_):
                                k.stt('dve', dstb[:, kc, :], ci[:, c0 + kc, :], wv[:, kc:kc + 1], rstd[:], ALU.mult,
                                      ALU.mult, R=[ci, wv, rstd], W=[dstb])
                        for h in range(4):
                            ps = psum.get()
                            for kc in range(4):
                                k.mm(ps, ps[:, :], wuq[:, kc, h * 192:h * 192 + 128], cqn[:, kc, :], R=[wuq, cqn],
                                     start=(kc == 0), stop=(kc == 3))
                            k.act(qn[:, h, ts_], ps[:, :], AF.Copy, R=[ps], W=[qn], scale=ATT_SCALE)
                            ps = psum.get()
                            for kc in range(2):
                                k.mm(ps, ps[:, :], wuk[:, kc, h * 128:(h + 1) * 128], ckvn[:, kc, :], R=[wuk, ckvn],
                                     start=(kc == 0), stop=(kc == 1))
                            k.copy('dve', kn[:, h, ts_], ps[:, :], R=[ps], W=[kn])
                        for hp in range(2):
                            ps = psum.get()
                            for kc in range(4):
                                k.mm(ps, ps[:, :], wuqr[:, kc, hp, :], cqn[:, kc, :], R=[wuqr, cqn],
                                     start=(kc == 0), stop=(kc == 3))
                            rope(ps, ps[:, :], qr, qr[:, hp, ts_], ATT_SCALE)
                        rope(ci, ci[:, 6, :], krp, krp[:, ts_], 1.0)
                        for q in range(4):
                            ps = psum.get()
                            for kc in range(2):
                                k.mm(ps, ps[:, :], ckvn[:, kc, q * 128:(q + 1) * 128], wuv[:, kc, :], R=[ckvn, wuv],
                                     start=(kc == 0), stop=(kc == 1))
                            k.copy('act', vtm[:, tt * 4 + q, :], ps[:, :], R=[ps], W=[vtm])
                dump("qn", qn); dump("qr", qr); dump("kn", kn); dump("krp", krp); dump("vtm", vtm)
                with ExitStack() as p2:
                    Ssb = sb("Ssb", [128, T], F32, stack=p2)
                    Psb = sb("Psb", [128, T], BF16, stack=p2)
                    ptr = Ring([sb("pt%d" % i, [128, 4, 128], BF16, stack=p2) for i in range(3)])
                    sc = Ring([sb("asc%d" % i, [128, 4], F32, stack=p2) for i in range(2)])
                    dgr = Ring([sb("adg%d" % i, [128, 128], BF16, stack=p2) for i in range(2)])
                    ost = Ring([sb("aost%d" % i, [128, 4, 128], BF16, multi=True, stack=p2) for i in range(2)])
                    for qi in range(NQ):
                        nk = (qi + 1) * 128
                        qs = slice(qi * 128, (qi + 1) * 128)
                        ot = ost.get()
                        for h in range(4):
                            hp, ho = h // 2, (h % 2) * 64
                            for kb in range(0, nk, 512):
                                w = min(512, nk - kb)
                                ps = psum.get()
                                k.mm(ps, ps[:, :w], qn[:, h, qs], kn[:, h, kb:kb + w], R=[qn, kn], start=True,
                                     stop=False)
                                k.mm(ps, ps[:, :w], qr[ho:ho + 64, hp, qs], krp[ho:ho + 64, kb:kb + w], R=[qr, krp],
                                     start=False, stop=True)
                                if kb + w == nk:
                                    if w > 128:
                                        k.copy('act', Ssb[:, kb:nk - 128], ps[:, :w - 128], R=[ps], W=[Ssb])
                                    k.tt('dve', Ssb[:, nk - 128:nk], ps[:, w - 128:w], cst[:, 3, :], ALU.subtract,
                                         R=[ps, cst], W=[Ssb])
                                else:
                                    k.copy('act', Ssb[:, kb:kb + w], ps[:, :w], R=[ps], W=[Ssb])
                            s_ = sc.get()
                            k.op('dve', lambda e: e.reduce_max(s_[:, 0:1], Ssb[:, :nk], AX.X), R=[Ssb], W=[s_])
                            k.ts('dve', s_[:, 1:2], s_[:, 0:1], -1.0, None, ALU.mult, R=[s_], W=[s_])
                            k.act(Psb[:, :nk], Ssb[:, :nk], AF.Exp, R=[Ssb, s_], W=[Psb, s_], bias=s_[:, 1:2],
                                  scale=1.0, accum_out=s_[:, 2:3])
                            k.op('dve', lambda e: e.reciprocal(s_[:, 3:4], s_[:, 2:3]), R=[s_], W=[s_])
                            if qi == 1 and h == 0:
                                dump("S", Ssb, Ssb[:, :nk]); dump("P", Psb, Psb[:, :nk]); dump("sc", s_)
                            dg = dgr.get()
                            k.ts('dve', dg[:], ident_b, s_[:, 3:4], None, ALU.mult, R=[cst_b, s_], W=[dg])
                            po = psacc.get()
                            nb = nk // 128
                            for b4 in range(0, nb, 4):
                                n4 = min(4, nb - b4)
                                ps = psum.get()
                                for j in range(n4):
                                    kb2 = (b4 + j) * 128
                                    k.mm(ps, ps[:, j * 128:(j + 1) * 128], Psb[:, kb2:kb2 + 128], dg[:], R=[Psb, dg])
                                pt = ptr.get()
                                k.copy('dve' if (b4 // 4) % 2 else 'act', pt[:, 0:n4, :],
                                       ps[:, 0:n4 * 128].rearrange('p (j t) -> p j t', j=n4), R=[ps], W=[pt])
                                for j in range(n4):
                                    kblk = b4 + j
                                    k.mm(po, po[:, 0:128], vtm[:, kblk, h * 128:(h + 1) * 128], pt[:, j, :],
                                         R=[vtm, pt], start=(kblk == 0), stop=(kblk == nb - 1))
                            k.copy('act', ot[:, h, :], po[:, 0:128], R=[po], W=[ot])
                        k.dma('sp', y_d[2][:, qs].rearrange('(h p) t -> p h t', p=128), ot[:], R=[ot], W=[Dy[2]])
                k.barrier()


        def bc(ap, shape):
            return ap.to_broadcast(list(shape))

        def conv_silu(cin, cw, nchan, dst, dstb, tmpb, bias=None):
            for c in range(nchan):
                eng = 'dve'
                k.ts(eng, tmpb[:, c, :], cin[:, c, 0:512], cw[:, c, 0:1], None, ALU.mult, R=[cin, cw], W=[tmpb])
                for kk in range(1, 4):
                    k.stt(eng, tmpb[:, c, :], cin[:, c, kk:kk + 512], cw[:, c, kk:kk + 1], tmpb[:, c, :], ALU.mult,
                          ALU.add, R=[cin, cw, tmpb], W=[tmpb])
                if bias is None:
                    k.act(dst[:, c, :], tmpb[:, c, :], AF.Silu, R=[tmpb], W=[dstb])
                else:
                    k.act(dst[:, c, :], tmpb[:, c, :], AF.Silu, R=[tmpb, bias], W=[dstb], bias=bias[:, c:c + 1])

        def load_halo(cin, row0, nrows, tt):
            if tt == 0:
                k.op('dve', lambda e: e.memset(cin[:, :, 0:3], 0.0), W=[cin])
                k.dma('sp', cin[:, :, 3:515], proj[row0:row0 + nrows, 0:512].rearrange('(c p) t -> p c t', p=128),
                      R=[Dproj], W=[cin])
            else:
                k.dma('sp', cin[:, :, :],
                      proj[row0:row0 + nrows, tt * 512 - 3:tt * 512 + 512].rearrange('(c p) t -> p c t', p=128),
                      R=[Dproj], W=[cin])

        tri64 = cst[0:64, 2, 0:64]
        ones64 = cst[0:64, 1, 0:64]
        ones64w = cst[0:64, 1, :]
        posm64 = cst[0:64, 3, 0:64]
        strict64 = cst[0:64, 4, 0:64]
        lowm64 = cst[0:64, 6, 0:64]
        id64 = cst[0:64, 0, 0:64]

        def phase_ssd(l):
            with ExitStack() as ps_:
                def t_(name, shape, dt=F32, multi=False):
                    return sb(name, shape, dt, multi=multi, stack=ps_)
                cw = t_("scw", [128, 8, 4]); cb = t_("scb", [128, 8]); alog = t_("salog", [128, 8])
                dtb = t_("sdtb", [128, 8]); dexp = t_("sdexp", [128, 4]); nw = t_("snw", [128, 4])
                for (d_, s_) in ((cw, ssm_convT), (cb, ssm_convb), (alog, ssm_alog), (dtb, ssm_dtb), (dexp, ssm_dexp),
                                 (nw, ssm_nw)):
                    k.dma('sp', d_[:], s_[l], R=[Dw], W=[d_])
                k.act(alog[:], alog[:], AF.Exp, R=[alog], W=[alog])
                k.ts('dve', alog[:], alog[:], -1.0, None, ALU.mult, R=[alog], W=[alog])
                raw = t_("sraw", [64, NCH, 16])
                k.dma('sp', raw[:], abdt.rearrange('(c p) k -> p c k', p=64), R=[Dabdt], W=[raw])
                dt = t_("sdt", [64, NCH, 8]); ad = t_("sad", [64, NCH, 8]); acs = t_("sacs", [64, NCH, 8])
                acl = t_("sacl", [128, NCH, 8]); cd = t_("scd", [128, NCH, 8]); ds = t_("sds", [64, NCH, 8])
                eacs = t_("seacs", [64, NCH, 8])
                k.tt('dve', dt[:], raw[:, :, 8:16], bc(dtb[0:64, :].unsqueeze(1), [64, NCH, 8]), ALU.add,
                     R=[raw, dtb], W=[dt])
                k.act(dt[:], dt[:], AF.Exp, R=[dt], W=[dt])
                k.act(dt[:], dt[:], AF.Ln, R=[dt, cols], W=[dt], bias=cols[0:64, 0:1])
                dump("sdt", dt)
                k.tt('dve', ad[:], dt[:], bc(alog[0:64, :].unsqueeze(1), [64, NCH, 8]), ALU.mult, R=[dt, alog], W=[ad])
                adf = ad[:].rearrange('p c h -> p (c h)')
                ps = psum.get()
                k.mm(ps, ps[:64, :NCH * 8], tri64, adf, R=[cst, ad])
                k.copy('dve', acs[:].rearrange('p c h -> p (c h)'), ps[:64, :NCH * 8], R=[ps], W=[acs])
                ps = psum.get()
                k.mm(ps, ps[:, :NCH * 8], ones64w, adf, R=[cst, ad])
                k.copy('dve', acl[:].rearrange('p c h -> p (c h)'), ps[:, :NCH * 8], R=[ps], W=[acl])
                k.act(cd[:], acl[:], AF.Exp, R=[acl], W=[cd])
                k.tt('dve', ds[:], acl[0:64], acs[:], ALU.subtract, R=[acl, acs], W=[ds])
                k.act(ds[:], ds[:], AF.Exp, R=[ds], W=[ds])
                k.act(eacs[:], acs[:], AF.Exp, R=[acs], W=[eacs])
                state = t_("sstate", [128, 8, 64])
                k.op('dve', lambda e: e.memset(state[:], 0.0), W=[state])
                cinr = Ring([t_("scin%d" % i, [128, 8, 515], BF16) for i in range(2)])
                szr = Ring([t_("ssz%d" % i, [128, 4, 512], BF16) for i in range(2)])
                xf = t_("sxf", [128, 8, 512]); ctmp = t_("sctmp", [128, 8, 512])
                ystr = Ring([t_("syst%d" % i, [128, 4, 512], BF16, multi=True) for i in range(2)])
                mk = lambda n, sh, cnt=2: Ring([t_("%s%d" % (n, i), sh) for i in range(cnt)])
                trig_r = mk("strig", [64, 8, 64]); t1_r = mk("st1", [64, 8, 64]); MT_r = mk("sMT", [64, 8, 64])
                X_r = mk("sX", [64, 8, 64]); Xds_r = mk("sXds", [64, 8, 64]); Btm_r = mk("sBtm", [64, 2, 128])
                yt_r = mk("syt", [64, 8, 64]); ytm_r = mk("sytm", [64, 8, 64]); yfm_r = mk("syfm", [128, 4, 64])
                tmp_r = mk("stmp", [128, 4, 64]); sq_r = mk("ssq", [128, 4, 64]); rs_r = mk("srs", [128, 2, 64])
                for tt in range(NT):
                    cin = cinr.get(); szt = szr.get(); yst = ystr.get()
                    load_halo(cin, C_XBC, 1024, tt)
                    k.dma('sp', szt[:], proj[C_SZ:C_SZ + 512, tt * 512:(tt + 1) * 512].rearrange(
                        '(c p) t -> p c t', p=128), R=[Dproj], W=[szt])
                    conv_silu(cin, cw, 8, xf, xf, ctmp, bias=cb)
                    if tt == 0:
                        dump("sxf", xf); dump("sacs", acs); dump("scd", cd); dump("sds", ds)
                    for cc in range(8):
                        ch = tt * 8 + cc
                        cs = slice(cc * 64, cc * 64 + 64)
                        ps_cb = psum.get()
                        for g in range(2):
                            k.mm(ps_cb, ps_cb[:64, g * 64:(g + 1) * 64], xf[:, 4 + g, cs], xf[:, 6 + g, cs], R=[xf])
                        trig = trig_r.get()
                        k.tt('pool', trig[:], bc(tri64.unsqueeze(1), [64, 8, 64]),
                             bc(ad[:, ch, :].unsqueeze(2), [64, 8, 64]), ALU.mult, R=[cst, ad], W=[trig])
                        ps_r = psum.get()
                        k.mm(ps_r, ps_r[:64, :], ones64, trig[:].rearrange('p h l -> p (h l)'), R=[cst, trig])
                        t1 = t1_r.get()
                        k.tt('dve', t1[:], ps_r[:64, :].rearrange('p (h l) -> p h l', h=8),
                             bc(lowm64.unsqueeze(1), [64, 8, 64]), ALU.subtract, R=[ps_r, cst], W=[t1])
                        k.tt('dve', t1[:], t1[:], bc(acs[:, ch, :].unsqueeze(2), [64, 8, 64]), ALU.subtract,
                             R=[t1, acs], W=[t1])
                        k.act(t1[:], t1[:], AF.Exp, R=[t1], W=[t1])
                        MT = MT_r.get()
                        k.tt('dve', MT[:].rearrange('p (g e) l -> p g e l', g=2),
                             t1[:].rearrange('p (g e) l -> p g e l', g=2),
                             bc(ps_cb[:64, 0:128].rearrange('p (g l) -> p g l', g=2).unsqueeze(2), [64, 2, 4, 64]),
                             ALU.mult, R=[t1, ps_cb], W=[MT])
                        ps_x = psum.get()
                        for kc in range(4):
                            k.tr(ps_x, ps_x[:64, kc * 128:(kc + 1) * 128], xf[:, kc, cs], ident_f, R=[xf, cst])
                        X = X_r.get(); Xds = Xds_r.get()
                        k.tt('dve', X[:], ps_x[:64, :].rearrange('p (h q) -> p h q', h=8),
                             bc(dt[:, ch, :].unsqueeze(2), [64, 8, 64]), ALU.mult, R=[ps_x, dt], W=[X])
                        k.tt('pool', Xds[:], X[:], bc(ds[:, ch, :].unsqueeze(2), [64, 8, 64]), ALU.mult,
                             R=[X, ds], W=[Xds])
                        ps_b = psum.get()
                        for g in range(2):
                            k.tr(ps_b, ps_b[:64, g * 128:(g + 1) * 128], xf[:, 4 + g, cs], ident_f, R=[xf, cst])
                        Btm = Btm_r.get()
                        k.copy('act', Btm[:].rearrange('p g n -> p (g n)'), ps_b[:64, 0:256], R=[ps_b], W=[Btm])
                        ps_y1 = psum.get()
                        for h in range(8):
                            k.mm(ps_y1, ps_y1[:64, h * 64:(h + 1) * 64], MT[:, h, :], X[:, h, :], R=[MT, X])
                        ps_y2 = psum.get()
                        for h in range(8):
                            k.mm(ps_y2, ps_y2[:64, h * 64:(h + 1) * 64], xf[:, 6 + h // 4, cs], state[:, h, :],
                                 R=[xf, state])
                        yt = yt_r.get(); ytm = ytm_r.get()
                        k.tt('dve', yt[:], ps_y2[:64, :].rearrange('p (h q) -> p h q', h=8),
                             bc(eacs[:, ch, :].unsqueeze(2), [64, 8, 64]), ALU.mult, R=[ps_y2, eacs], W=[yt])
                        k.tt('dve', ytm[:], yt[:], ps_y1[:64, :].rearrange('p (h q) -> p h q', h=8), ALU.add,
                             R=[yt, ps_y1], W=[ytm])
                        ps_s = psum.get()
                        for h in range(8):
                            k.mm(ps_s, ps_s[:, h * 64:(h + 1) * 64], Btm[:, h // 4, :], Xds[:, h, :], R=[Btm, Xds])
                        k.tt('dve', state[:], state[:], bc(cd[:, ch, :].unsqueeze(2), [128, 8, 64]), ALU.mult,
                             R=[state, cd], W=[state])
                        k.tt('dve', state[:], state[:], ps_s[:, :].rearrange('p (h q) -> p h q', h=8), ALU.add,
                             R=[state, ps_s], W=[state])
                        ps_t = psum.get()
                        ytf = ytm[:].rearrange('p h q -> p (h q)')
                        for kc in range(4):
                            k.tr(ps_t, ps_t[:, kc * 64:(kc + 1) * 64], ytf[:, kc * 128:(kc + 1) * 128], id64,
                                 R=[ytm, cst])
                        tmp = tmp_r.get(); yfm = yfm_r.get(); sq = sq_r.get(); rs = rs_r.get()
                        k.tt('pool', tmp[:], xf[:, 0:4, cs], bc(dexp[:, :].unsqueeze(2), [128, 4, 64]), ALU.mult,
                             R=[xf, dexp], W=[tmp])
                        k.tt('dve', yfm[:], tmp[:], ps_t[:, 0:256].rearrange('p (c q) -> p c q', c=4), ALU.add,
                             R=[tmp, ps_t], W=[yfm])
                        k.tt('dve', yfm[:], yfm[:], szt[:, :, cs], ALU.mult, R=[yfm, szt], W=[yfm])
                        k.act(sq[:], yfm[:], AF.Square, R=[yfm], W=[sq])
                        ps_n = psum.get()
                        for g in range(2):
                            for k2 in range(2):
                                k.mm(ps_n, ps_n[:, g * 64:(g + 1) * 64], ones_f, sq[:, g * 2 + k2, :], R=[cst, sq],
                                     start=(k2 == 0), stop=(k2 == 1))
                        rsqrt(rs, rs[:].rearrange('p g q -> p (g q)'), ps_n, ps_n[:, 0:128], 1.0 / 256)
                        for kc in range(4):
                            k.stt('dve', yst[:, kc, cs], yfm[:, kc, :], nw[:, kc:kc + 1], rs[:, kc // 2, :], ALU.mult,
                                  ALU.mult, R=[yfm, nw, rs], W=[yst])
                    k.dma('sp', y_d[1][:, tt * 512:(tt + 1) * 512].rearrange('(c p) t -> p c t', p=128), yst[:],
                          R=[yst], W=[Dy[1]])
                k.barrier()

        def phase_gdn(l):
            with ExitStack() as ps_:
                def t_(name, shape, dt=F32, multi=False):
                    return sb(name, shape, dt, multi=multi, stack=ps_)
                cw = t_("gcw", [128, 12, 4]); alog = t_("galog", [128, 4]); dtb = t_("gdtb", [128, 4])
                nw = t_("gnw", [128, 1])
                for (d_, s_) in ((cw, gdn_convT), (alog, gdn_alog), (dtb, gdn_dtb), (nw, gdn_nw)):
                    k.dma('sp', d_[:], s_[l], R=[Dw], W=[d_])
                k.act(alog[:], alog[:], AF.Exp, R=[alog], W=[alog])
                k.ts('dve', alog[:], alog[:], -1.0, None, ALU.mult, R=[alog], W=[alog])
                raw = t_("graw", [64, NCH, 16])
                k.dma('sp', raw[:], abdt.rearrange('(c p) k -> p c k', p=64), R=[Dabdt], W=[raw])
                beta = t_("gbeta", [64, NCH, 4]); nbeta = t_("gnbeta", [64, NCH, 4]); g = t_("gg", [64, NCH, 4])
                G = t_("gG", [64, NCH, 4]); Glb = t_("gGlb", [128, NCH, 4]); gl = t_("ggl", [128, NCH, 4])
                eG = t_("geG", [64, NCH, 4]); kdsc = t_("gkdsc", [64, NCH, 4]); bexpG = t_("gbexpG", [64, NCH, 4])
                k.act(beta[:], raw[:, :, 4:8], AF.Sigmoid, R=[raw], W=[beta])
                k.ts('dve', nbeta[:], beta[:], -1.0, None, ALU.mult, R=[beta], W=[nbeta])
                k.tt('dve', g[:], raw[:, :, 0:4], bc(dtb[0:64, :].unsqueeze(1), [64, NCH, 4]), ALU.add,
                     R=[raw, dtb], W=[g])
                k.act(g[:], g[:], AF.Exp, R=[g], W=[g])
                k.act(g[:], g[:], AF.Ln, R=[g, cols], W=[g], bias=cols[0:64, 0:1])
                k.tt('dve', g[:], g[:], bc(alog[0:64, :].unsqueeze(1), [64, NCH, 4]), ALU.mult, R=[g, alog], W=[g])
                gf = g[:].rearrange('p c h -> p (c h)')
                ps = psum.get()
                k.mm(ps, ps[:64, :NCH * 4], tri64, gf, R=[cst, g])
                k.copy('dve', G[:].rearrange('p c h -> p (c h)'), ps[:64, :NCH * 4], R=[ps], W=[G])
                ps = psum.get()
                k.mm(ps, ps[:, :NCH * 4], ones64w, gf, R=[cst, g])
                k.copy('dve', Glb[:].rearrange('p c h -> p (c h)'), ps[:, :NCH * 4], R=[ps], W=[Glb])
                k.act(gl[:], Glb[:], AF.Exp, R=[Glb], W=[gl])
                k.act(eG[:], G[:], AF.Exp, R=[G], W=[eG])
                k.tt('dve', kdsc[:], Glb[0:64], G[:], ALU.subtract, R=[Glb, G], W=[kdsc])
                k.act(kdsc[:], kdsc[:], AF.Exp, R=[kdsc], W=[kdsc])
                k.tt('dve', bexpG[:], beta[:], eG[:], ALU.mult, R=[beta, eG], W=[bexpG])
                S = t_("gS", [128, 4, 128])
                k.op('dve', lambda e: e.memset(S[:], 0.0), W=[S])
                cinr = Ring([t_("gcin%d" % i, [128, 12, 515], BF16) for i in range(2)])
                gzr = Ring([t_("ggz%d" % i, [128, 4, 512], BF16) for i in range(2)])
                qkv = t_("gqkv", [128, 12, 512]); ctmp = t_("gctmp", [128, 12, 512])
                rsn = t_("grsn", [128, 512])
                ystr = Ring([t_("gyst%d" % i, [128, 4, 512], BF16, multi=True) for i in range(2)])
                mk = lambda n, sh, cnt=2: Ring([t_("%s%d" % (n, i), sh) for i in range(cnt)])
                ktm_r = mk("gktm", [64, 4, 128]); vtm_r = mk("gvtm", [64, 4, 128]); trig_r = mk("gtrig", [64, 4, 64])
                t1_r = mk("gt1", [64, 4, 64]); dec_r = mk("gdec", [64, 4, 64]); n_r = mk("gn", [64, 4, 64])
                X_r = mk("gX", [64, 4, 64], 3); Y_r = mk("gY", [64, 4, 64], 3); RT_r = mk("gRT", [64, 4, 64], 3)
                qkd_r = mk("gqkd", [64, 4, 64]); qkT_r = mk("gqkT", [64, 4, 64])
                vb_r = mk("gvb", [64, 4, 128]); kbg_r = mk("gkbg", [64, 4, 128]); u_r = mk("gu", [64, 4, 128])
                w_r = mk("gw", [128, 4, 64]); kdec_r = mk("gkdec", [64, 4, 128]); vn_r = mk("gvn", [64, 4, 128])
                o_r = mk("go", [64, 4, 128]); sq_r = mk("gsq", [64, 4, 128]); ss_r = mk("gss", [64, 4])
                on_r = mk("gon", [64, 4, 128])

                def v4(ps, w):
                    return ps[:64, 0:4 * w].rearrange('p (h x) -> p h x', h=4)

                for tt in range(NT):
                    cin = cinr.get(); gz = gzr.get(); yst = ystr.get()
                    load_halo(cin, C_QKV, 1536, tt)
                    k.dma('sp', gz[:], proj[C_GZ:C_GZ + 512, tt * 512:(tt + 1) * 512].rearrange(
                        '(c p) t -> p c t', p=128), R=[Dproj], W=[gz])
                    conv_silu(cin, cw, 12, qkv, qkv, ctmp)
                    for c in range(8):
                        k.act(ctmp[:, c, :], qkv[:, c, :], AF.Square, R=[qkv], W=[ctmp])
                        ps = psum.get()
                        k.mm(ps, ps[:, :], ones_f, ctmp[:, c, :], R=[cst, ctmp])
                        rsqrt(rsn, rsn[:], ps, ps[:, :], 1.0)
                        if c < 4:
                            k.stt('dve', qkv[:, c, :], qkv[:, c, :], 128.0 ** -0.5, rsn[:], ALU.mult, ALU.mult,
                                  R=[qkv, rsn], W=[qkv])
                        else:
                            k.tt('dve', qkv[:, c, :], qkv[:, c, :], rsn[:], ALU.mult, R=[qkv, rsn], W=[qkv])
                    for cc in range(8):
                        ch = tt * 8 + cc
                        cs = slice(cc * 64, cc * 64 + 64)
                        b4 = lambda t, w: bc(t[:, ch, :].unsqueeze(2), [t[:, ch, :].shape[0], 4, w])
                        ps_k = psum.get()
                        for h in range(4):
                            k.tr(ps_k, ps_k[:64, h * 128:(h + 1) * 128], qkv[:, 4 + h, cs], ident_f, R=[qkv, cst])
                        ktm = ktm_r.get()
                        k.copy('act', ktm[:], v4(ps_k, 128), R=[ps_k], W=[ktm])
                        ps_v = psum.get()
                        for h in range(4):
                            k.tr(ps_v, ps_v[:64, h * 128:(h + 1) * 128], qkv[:, 8 + h, cs], ident_f, R=[qkv, cst])
                        vtm = vtm_r.get()
                        k.copy('act', vtm[:], v4(ps_v, 128), R=[ps_v], W=[vtm])
                        ps_kk = psum.get()
                        for h in range(4):
                            k.mm(ps_kk, ps_kk[:64, h * 64:(h + 1) * 64], qkv[:, 4 + h, cs], qkv[:, 4 + h, cs], R=[qkv])
                        ps_qk = psum.get()
                        for h in range(4):
                            k.mm(ps_qk, ps_qk[:64, h * 64:(h + 1) * 64], qkv[:, h, cs], qkv[:, 4 + h, cs], R=[qkv])
                        trig = trig_r.get()
                        k.tt('pool', trig[:], bc(tri64.unsqueeze(1), [64, 4, 64]), b4(g, 64), ALU.mult,
                             R=[cst, g], W=[trig])
                        ps_g = psum.get()
                        k.mm(ps_g, ps_g[:64, 0:256], ones64, trig[:].rearrange('p h l -> p (h l)'), R=[cst, trig])
                        t1 = t1_r.get(); dec = dec_r.get()
                        k.tt('dve', t1[:], v4(ps_g, 64), bc(posm64.unsqueeze(1), [64, 4, 64]), ALU.add,
                             R=[ps_g, cst], W=[t1])
                        k.tt('dve', t1[:], b4(G, 64), t1[:], ALU.subtract, R=[G, t1], W=[t1])
                        k.act(dec[:], t1[:], AF.Exp, R=[t1], W=[dec])
                        n1 = n_r.get(); X0 = X_r.get(); qkd = qkd_r.get()
                        k.tt('dve', n1[:], v4(ps_kk, 64), dec[:], ALU.mult, R=[ps_kk, dec], W=[n1])
                        k.tt('dve', n1[:], n1[:], b4(nbeta, 64), ALU.mult, R=[n1, nbeta], W=[n1])
                        k.tt('pool', X0[:], n1[:], bc(strict64.unsqueeze(1), [64, 4, 64]), ALU.mult,
                             R=[n1, cst], W=[X0])
                        k.tt('dve', qkd[:], v4(ps_qk, 64), dec[:], ALU.mult, R=[ps_qk, dec], W=[qkd])
                        ps_y = psum.get()
                        for h in range(4):
                            k.tr(ps_y, ps_y[:64, h * 64:(h + 1) * 64], X0[:, h, :], id64, R=[X0, cst])
                        Y0 = Y_r.get()
                        k.copy('act', Y0[:], v4(ps_y, 64), R=[ps_y], W=[Y0])
                        ps_q = psum.get()
                        for h in range(4):
                            k.tr(ps_q, ps_q[:64, h * 64:(h + 1) * 64], qkd[:, h, :], id64, R=[qkd, cst])
                        qkT = qkT_r.get()
                        k.copy('act', qkT[:], v4(ps_q, 64), R=[ps_q], W=[qkT])
                        RT = RT_r.get()
                        k.tt('pool', RT[:], Y0[:], bc(id64.unsqueeze(1), [64, 4, 64]), ALU.add, R=[Y0, cst], W=[RT])
                        Xp, Yp = X0, Y0
                        for kk in range(1, 6):
                            ps_a = psum.get()
                            for h in range(4):
                                k.mm(ps_a, ps_a[:64, h * 64:(h + 1) * 64], Yp[:, h, :], Xp[:, h, :], R=[Yp, Xp])
                            Xn = X_r.get()
                            k.copy('act', Xn[:], v4(ps_a, 64), R=[ps_a], W=[Xn])
                            Yn = None
                            if kk <= 4:
                                ps_b = psum.get()
                                for h in range(4):
                                    k.mm(ps_b, ps_b[:64, h * 64:(h + 1) * 64], Xp[:, h, :], Yp[:, h, :], R=[Yp, Xp])
                                Yn = Y_r.get()
                                k.copy('dve', Yn[:], v4(ps_b, 64), R=[ps_b], W=[Yn])
                            ps_c = psum.get()
                            for h in range(4):
                                k.mm(ps_c, ps_c[:64, h * 64:(h + 1) * 64], Xn[:, h, :], RT[:, h, :], R=[Xn, RT])
                            RTn = RT_r.get()
                            k.tt('dve', RTn[:], RT[:], v4(ps_c, 64), ALU.add, R=[RT, ps_c], W=[RTn])
                            Xp, Yp, RT = Xn, Yn, RTn
                        vb = vb_r.get(); kbg = kbg_r.get(); kdec = kdec_r.get()
                        k.tt('pool', vb[:], vtm[:], b4(beta, 128), ALU.mult, R=[vtm, beta], W=[vb])
                        k.tt('pool', kbg[:], ktm[:], b4(bexpG, 128), ALU.mult, R=[ktm, bexpG], W=[kbg])
                        k.tt('pool', kdec[:], ktm[:], b4(kdsc, 128), ALU.mult, R=[ktm, kdsc], W=[kdec])
                        ps_u = psum.get()
                        for h in range(4):
                            k.mm(ps_u, ps_u[:64, h * 128:(h + 1) * 128], RT[:, h, :], vb[:, h, :], R=[RT, vb])
                        u = u_r.get()
                        k.copy('act', u[:], v4(ps_u, 128), R=[ps_u], W=[u])
                        ps_w = psum.get()
                        for h in range(4):
                            k.mm(ps_w, ps_w[:, h * 64:(h + 1) * 64], kbg[:, h, :], RT[:, h, :], R=[kbg, RT])
                        wf = w_r.get()
                        k.copy('act', wf[:], ps_w[:, 0:256].rearrange('p (h x) -> p h x', h=4), R=[ps_w], W=[wf])
                        ps_ws = psum.get()
                        for h in range(4):
                            k.mm(ps_ws, ps_ws[:64, h * 128:(h + 1) * 128], wf[:, h, :], S[:, h, :], R=[wf, S])
                        vn = vn_r.get()
                        k.tt('dve', vn[:], u[:], v4(ps_ws, 128), ALU.subtract, R=[u, ps_ws], W=[vn])
                        ps_o1 = psum.get()
                        for h in range(4):
                            k.mm(ps_o1, ps_o1[:64, h * 128:(h + 1) * 128], qkv[:, h, cs], S[:, h, :], R=[qkv, S])
                        ps_o2 = psum.get()
                        for h in range(4):
                            k.mm(ps_o2, ps_o2[:64, h * 128:(h + 1) * 128], qkT[:, h, :], vn[:, h, :], R=[qkT, vn])
                        o = o_r.get()
                        k.tt('dve', o[:], v4(ps_o1, 128), b4(eG, 128), ALU.mult, R=[ps_o1, eG], W=[o])
                        k.tt('dve', o[:], o[:], v4(ps_o2, 128), ALU.add, R=[o, ps_o2], W=[o])
                        ps_ds = psum.get()
                        for h in range(4):
                            k.mm(ps_ds, ps_ds[:, h * 128:(h + 1) * 128], kdec[:, h, :], vn[:, h, :], R=[kdec, vn])
                        k.tt('dve', S[:], S[:], bc(gl[:, ch, :].unsqueeze(2), [128, 4, 128]), ALU.mult,
                             R=[S, gl], W=[S])
                        k.tt('dve', S[:], S[:], ps_ds[:, :].rearrange('p (h x) -> p h x', h=4), ALU.add,
                             R=[S, ps_ds], W=[S])
                        sq = sq_r.get(); ss = ss_r.get(); on = on_r.get()
                        k.tt('pool', sq[:], o[:], o[:], ALU.mult, R=[o], W=[sq])
                        k.op('dve', lambda e: e.reduce_sum(ss[:], sq[:], AX.X), R=[sq], W=[ss])
                        rsqrt(ss, ss[:], ss, ss[:], 1.0 / 128)
                        k.tt('dve', on[:], o[:], bc(ss[:, :].unsqueeze(2), [64, 4, 128]), ALU.mult, R=[o, ss], W=[on])
                        ps_t = psum.get()
                        for h in range(4):
                            k.tr(ps_t, ps_t[:, h * 64:(h + 1) * 64], on[:, h, :], id64, R=[on, cst])
                        k.stt('dve', yst[:, :, cs], ps_t[:, 0:256].rearrange('p (h x) -> p h x', h=4), nw[:, 0:1],
                              gz[:, :, cs], ALU.mult, ALU.mult, R=[ps_t, nw, gz], W=[yst])
                    k.dma('sp', y_d[0][:, tt * 512:(tt + 1) * 512].rearrange('(c p) t -> p c t', p=128), yst[:],
                          R=[yst], W=[Dy[0]])
                k.barrier()

        def phase_mixers(l):
            if 'mla' in mixers:
                phase_mla(l)
            if 'ssd' in mixers:
                phase_ssd(l)
            if 'gdn' in mixers:
                phase_gdn(l)

        def phase_merge(l, xsrc, Dxsrc, xdst, Dxdst):
            with ExitStack() as ps_:
                wb = [sb("wbr%d" % i, [128, 4, 1024], BF16, stack=ps_) for i in range(3)]
                wo = sb("wo", [128, 8, 1024], BF16, stack=ps_)
                for i in range(3):
                    k.dma('pool', wb[i][:], w_br[i][l].rearrange('(kc p) n -> p kc n', p=128), R=[Dw], W=[wb[i]])
                k.dma('pool', wo[:], w_out[l].rearrange('(kc p) n -> p kc n', p=128), R=[Dw], W=[wo])
                yr = Ring([sb("ym%d" % i, [128, 3, 4, 512], BF16, stack=ps_) for i in range(2)])
                gr = Ring([sb("gm%d" % i, [128, 24, 512], BF16, stack=ps_) for i in range(2)])
                xr = Ring([sb("xm%d" % i, [128, 8, 512], F32, stack=ps_) for i in range(2)])
                mg = Ring([sb("mg%d" % i, [128, 8, 512], BF16, stack=ps_) for i in range(2)])
                tmp = Ring([sb("mt%d" % i, [128, 512], F32, stack=ps_) for i in range(3)])
                for tt in range(NT):
                    ts_ = slice(tt * 512, (tt + 1) * 512)
                    y = yr.get()
                    g = gr.get()
                    xt = xr.get()
                    m_ = mg.get()
                    for i in range(3):
                        k.dma('sp', y[:, i], y_d[i][:, ts_].rearrange('(kc p) t -> p kc t', p=128), R=[Dy[i]], W=[y])
                    k.dma('sp', g[:], proj[C_GATES:C_GATES + 3072, ts_].rearrange('(kc p) t -> p kc t', p=128),
                          R=[Dproj], W=[g])
                    k.dma('sp', xt[:], xsrc[:, ts_].rearrange('(kc p) t -> p kc t', p=128), R=[Dxsrc], W=[xt])
                    for o in range(8):
                        tl = []
                        for i in range(3):
                            ps = psum.get()
                            for kc in range(4):
                                k.mm(ps, ps[:, :], wb[i][:, kc, o * 128:(o + 1) * 128], y[:, i, kc, :], R=[wb[i], y],
                                     start=(kc == 0), stop=(kc == 3))
                            t_ = tmp.get()
                            k.tt('dve', t_[:], ps[:, :], g[:, i * 8 + o, :], ALU.mult, R=[ps, g], W=[t_])
                            tl.append(t_)
                        k.tt('pool', tl[0][:], tl[0][:], tl[1][:], ALU.add, R=[tl[0], tl[1]], W=[tl[0]])
                        k.tt('pool', m_[:, o, :], tl[0][:], tl[2][:], ALU.add, R=[tl[0], tl[2]], W=[m_])
                    for o in range(8):
                        ps = psum.get()
                        for kc in range(8):
                            k.mm(ps, ps[:, :], wo[:, kc, o * 128:(o + 1) * 128], m_[:, kc, :], R=[wo, m_],
                                 start=(kc == 0), stop=(kc == 7))
                        k.stt('dve', xt[:, o, :], ps[:, :], modT[:, 16 + o:17 + o], xt[:, o, :], ALU.mult, ALU.add,
                              R=[ps, modT, xt], W=[xt])
                    k.dma('sp', xdst[:, ts_].rearrange('(kc p) t -> p kc t', p=128), xt[:], R=[xt], W=[Dxdst])
                k.barrier()

        def phase_ffn(l, xsrc, Dxsrc, xdst, Dxdst):
            moe = (l % 2 == 1)
            li = l // 2
            TT = min(T, 1024)
            NS = TT // 512
            HCMAX = 14
            with ExitStack() as ps_:
                h2 = sb("h2", [128, 8, TT], BF16, multi=True, stack=ps_)
                xacc = sb("xacc", [128, 8, TT], F32, multi=True, stack=ps_)
                sq = sb("sq2", [128, 8, 512], F32, stack=ps_)
                hid = sb("hid", [128, HCMAX, TT], BF16, multi=True, stack=ps_)
                wgr = Ring([sb("wg%d" % i, [128, 8, 256], BF16, stack=ps_) for i in range(2)])
                wur = Ring([sb("wu%d" % i, [128, 8, 256], BF16, stack=ps_) for i in range(2)])
                wdr = Ring([sb("wd%d" % i, [128, HCMAX, 512], BF16, stack=ps_) for i in range(1)])
                sgr = Ring([sb("sg%d" % i, [128, 512], BF16, stack=ps_) for i in range(3)])
                tmp = Ring([sb("ft%d" % i, [128, 512], F32, stack=ps_) for i in range(2)])
                if moe:
                    hf = sb("hf2", [128, 8, 512], F32, stack=ps_)
                    rt = sb("rt", [128, 8, NEXP], F32, stack=ps_)
                    k.dma('sp', rt[:], moe_router[li].rearrange('(kc p) n -> p kc n', p=128), R=[Dw], W=[rt])
                    wrow = sb("wrow", [128, NEXP, TT], BF16, multi=True, stack=ps_)
                    sm = [sb("rs%d" % i, [128, 8], F32, stack=ps_) for i in range(6)]
                    sc = [sb("rc%d" % i, [128, 1], F32, stack=ps_) for i in range(4)]
                    dg = sb("dg", [128, NEXP, 128], F32, stack=ps_)
                for st_ in range(T // TT):
                    t0 = st_ * TT
                    for s in range(NS):
                        sl = slice(s * 512, (s + 1) * 512)
                        k.dma('sp', xacc[:, :, sl],
                              xsrc[:, t0 + s * 512:t0 + (s + 1) * 512].rearrange('(kc p) t -> p kc t', p=128),
                              R=[Dxsrc], W=[xacc])
                        norm_tile(xacc, xacc[:, :, sl], sq, lambda kc: h2[:, kc, sl], h2, 32, 24,
                                  hf=([hf] if moe else None))
                        if moe:
                            for q in range(4):
                                lg, m1, m2, e1, e2, wt8 = sm
                                ps = psum.get()
                                for kc in range(8):
                                    k.mm(ps, ps[:, 0:8], hf[:, kc, q * 128:(q + 1) * 128], rt[:, kc, :], R=[hf, rt],
                                         start=(kc == 0), stop=(kc == 7))
                                k.copy('dve', lg[:], ps[:, 0:8], R=[ps], W=[lg])
                                k.op('dve', lambda e: e.reduce_max(sc[0][:], lg[:], AX.X), R=[lg], W=[sc[0]])
                                k.ts('dve', e1[:], lg[:], sc[0][:, 0:1], None, ALU.is_equal, R=[lg, sc[0]], W=[e1])
                                k.stt('dve', m1[:], e1[:], -1e30, lg[:], ALU.mult, ALU.add, R=[e1, lg], W=[m1])
                                k.op('dve', lambda e: e.reduce_max(sc[1][:], m1[:], AX.X), R=[m1], W=[sc[1]])
                                k.ts('dve', e2[:], m1[:], sc[1][:, 0:1], None, ALU.is_equal, R=[m1, sc[1]], W=[e2])
                                k.tt('dve', sc[2][:], sc[0][:], sc[1][:], ALU.subtract, R=[sc[0], sc[1]], W=[sc[2]])
                                k.act(sc[3][:], sc[2][:], AF.Sigmoid, R=[sc[2]], W=[sc[3]])
                                k.act(sc[2][:], sc[2][:], AF.Sigmoid, R=[sc[2]], W=[sc[2]], scale=-1.0)
                                k.ts('dve', wt8[:], e1[:], sc[3][:, 0:1], None, ALU.mult, R=[e1, sc[3]], W=[wt8])
                                k.stt('dve', wt8[:], e2[:], sc[2][:, 0:1], wt8[:], ALU.mult, ALU.add,
                                      R=[e2, sc[2], wt8], W=[wt8])
                                k.tt('dve', dg[:], cst[:, 0:1, :].to_broadcast([128, NEXP, 128]),
                                     wt8[:, :].unsqueeze(2).to_broadcast([128, NEXP, 128]), ALU.mult,
                                     R=[cst, wt8], W=[dg])
                                for hh in range(2):
                                    ps2 = psum.get()
                                    k.mm(ps2, ps2[:, :], ones_f, dg[:, hh * 4:(hh + 1) * 4, :].rearrange('p e t -> p (e t)'), R=[cst, dg])
                                    c0 = s * 512 + q * 128
                                    k.copy('act', wrow[:, hh * 4:(hh + 1) * 4, c0:c0 + 128],
                                           ps2[:, :].rearrange('p (e t) -> p e t', e=4), R=[ps2], W=[wrow])
                    if moe:
                        passes = []
                        for e_ in range(NEXP):
                            for hh in range(2):
                                passes.append((moe_wg[li, e_], moe_wu[li, e_], moe_wd[li, e_], hh * 1792, 1792, e_))
                    else:
                        passes = [(ffn_wg[li], ffn_wu[li], ffn_wd[li], hh * 1408, 1408, None) for hh in range(2)]
                    for (wg_, wu_, wd_, h0, hn, ex) in passes:
                        HC = hn // 128
                        for cb in range(0, hn, 256):
                            cw = min(256, hn - cb)
                            wg = wgr.get()
                            wu = wur.get()
                            k.dma('pool', wg[:, :, :cw],
                                  wg_[:, h0 + cb:h0 + cb + cw].rearrange('(kc p) n -> p kc n', p=128), R=[Dw], W=[wg])
                            k.dma('pool', wu[:, :, :cw],
                                  wu_[:, h0 + cb:h0 + cb + cw].rearrange('(kc p) n -> p kc n', p=128), R=[Dw], W=[wu])
                            for jj in range(cw // 128):
                                j = cb // 128 + jj
                                for s in range(NS):
                                    sl = slice(s * 512, (s + 1) * 512)
                                    psg = psum.get()
                                    for kc in range(8):
                                        k.mm(psg, psg[:, :], wg[:, kc, jj * 128:(jj + 1) * 128], h2[:, kc, sl],
                                             R=[wg, h2], start=(kc == 0), stop=(kc == 7))
                                    psu = psum.get()
                                    for kc in range(8):
                                        k.mm(psu, psu[:, :], wu[:, kc, jj * 128:(jj + 1) * 128], h2[:, kc, sl],
                                             R=[wu, h2], start=(kc == 0), stop=(kc == 7))
                                    sg = sgr.get()
                                    k.act(sg[:], psg[:, :], AF.Silu, R=[psg], W=[sg])
                                    k.tt('dve', hid[:, j, sl], psu[:, :], sg[:], ALU.mult, R=[psu, sg], W=[hid])
                        for oh in range(2):
                            wd = wdr.get()
                            for c4 in range(0, HC, 7):
                                cn = min(7, HC - c4)
                                k.dma('pool', wd[:, c4:c4 + cn, :],
                                      wd_[h0 + c4 * 128:h0 + (c4 + cn) * 128, oh * 512:(oh + 1) * 512].rearrange(
                                          '(kc p) n -> p kc n', p=128), R=[Dw], W=[wd])
                            for oo in range(4):
                                o = oh * 4 + oo
                                for s in range(NS):
                                    sl = slice(s * 512, (s + 1) * 512)
                                    ps = psum.get()
                                    for j in range(HC):
                                        k.mm(ps, ps[:, :], wd[:, j, oo * 128:(oo + 1) * 128], hid[:, j, sl],
                                             R=[wd, hid], start=(j == 0), stop=(j == HC - 1))
                                    if ex is None:
                                        k.stt('dve', xacc[:, o, sl], ps[:, :], modT[:, 40 + o:41 + o], xacc[:, o, sl],
                                              ALU.mult, ALU.add, R=[ps, modT, xacc], W=[xacc])
                                    else:
                                        t_ = tmp.get()
                                        k.stt('dve', t_[:], ps[:, :], modT[:, 40 + o:41 + o], wrow[:, ex, sl],
                                              ALU.mult, ALU.mult, R=[ps, modT, wrow], W=[t_])
                                        k.tt('pool', xacc[:, o, sl], xacc[:, o, sl], t_[:], ALU.add,
                                             R=[t_, xacc], W=[xacc])
                    k.dma('sp', xdst[:, t0:t0 + TT].rearrange('(kc p) t -> p kc t', p=128), xacc[:], R=[xacc],
                          W=[Dxdst])
                k.barrier()

        def phase_final(xsrc, Dxsrc):
            with ExitStack() as ps_:
                xr = Ring([sb("xf%d" % i, [128, 8, 512], F32, stack=ps_) for i in range(2)])
                orr = Ring([sb("of%d" % i, [128, 8, 512], F32, multi=True, stack=ps_) for i in range(2)])
                sq = sb("sqf", [128, 8, 512], F32, stack=ps_)
                for tt in range(NT):
                    ts_ = slice(tt * 512, (tt + 1) * 512)
                    xt = xr.get()
                    ot = orr.get()
                    k.dma('sp', xt[:], xsrc[:, ts_].rearrange('(kc p) t -> p kc t', p=128), R=[Dxsrc], W=[xt])
                    norm_tile(xt, xt[:], sq, lambda kc: ot[:, kc, :], ot, None, None)
                    k.dma('sp', outT[:, ts_].rearrange('(kc p) t -> p kc t', p=128), ot[:], R=[ot], W=[Dout])
                k.barrier()

        cur, Dcur = xT_in, Dxin
        for l in layers:
            phase_ada(l)
            if 'mix' in stages:
                phase_inproj(l, cur, Dcur)
                phase_mixers(l)
                phase_merge(l, cur, Dcur, x_b, Dx_b)
                cur, Dcur = x_b, Dx_b
            if 'ffn' in stages:
                phase_ffn(l, cur, Dcur, x_a, Dx_a)
                cur, Dcur = x_a, Dx_a
        phase_final(cur, Dcur)
        k.barrier()
        print("instructions:", k.ninst, {kk: v for kk, v in k.cnt.items()})
    return nc


def _consts():
    c = np.zeros((128, 8, 128), np.float32)
    i = np.arange(128)
    c[:, 0, :] = np.eye(128)
    c[:, 1, :] = 1.0
    c[:, 2, :] = (i[:, None] <= i[None, :])
    c[:, 3, :] = np.where(i[None, :] > i[:, None], 1e9, 0.0)
    c[:, 4, :] = (i[None, :] < i[:, None])
    rot = np.zeros((128, 128), np.float32)
    for o in (0, 64):
        for m in range(32):
            rot[o + m + 32, o + m] = -1.0
            rot[o + m, o + m + 32] = 1.0
    c[:, 5, :] = rot
    c[:, 6, :] = np.where(i[None, :] < i[:, None], 1e9, 0.0)
    return c


def _fm(v, nchunk):
    return np.ascontiguousarray(np.asarray(v, np.float32).reshape(nchunk, 128).T)


def prep_inputs(inp, b, T):
    f = lambda a: np.ascontiguousarray(np.asarray(a, np.float32))
    L = DEPTH
    m = {}
    m["xT"] = np.ascontiguousarray(np.asarray(inp["x"][b, :T], np.float32).T)
    m["cT"] = _fm(inp["c"][b], 8)
    m["pos"] = np.ascontiguousarray(np.asarray(inp["positions"][b, :T], np.int32).reshape(1, T))
    m["w_ada"] = f(inp["w_ada"])
    m["b_adaT"] = np.stack([_fm(inp["b_ada"][l], 48) for l in range(L)])
    m["w_in"] = f(inp["w_in"])
    gc = np.asarray(inp["gdn_conv_w"], np.float32)
    m["gdn_convT"] = np.ascontiguousarray(gc.reshape(L, 4, 12, 128).transpose(0, 3, 2, 1))
    rep = lambda a: np.ascontiguousarray(np.broadcast_to(np.asarray(a, np.float32)[:, None, :], (L, 128, a.shape[-1])))
    m["gdn_alog"] = rep(inp["gdn_a_log"])
    m["gdn_dtb"] = rep(inp["gdn_dt_bias"])
    m["gdn_nw"] = np.ascontiguousarray(np.asarray(inp["gdn_norm_w"], np.float32).reshape(L, 128, 1))
    sc = np.asarray(inp["ssm_conv_w"], np.float32)
    m["ssm_convT"] = np.ascontiguousarray(sc.reshape(L, 4, 8, 128).transpose(0, 3, 2, 1))
    m["ssm_convb"] = np.stack([_fm(inp["ssm_conv_b"][l], 8) for l in range(L)])
    m["ssm_alog"] = rep(inp["ssm_a_log"])
    m["ssm_dtb"] = rep(inp["ssm_dt_bias"])
    dexp = np.repeat(np.asarray(inp["ssm_d"], np.float32), 64, axis=1)
    m["ssm_dexp"] = np.stack([_fm(dexp[l], 4) for l in range(L)])
    m["ssm_nw"] = np.stack([_fm(inp["ssm_norm_w"][l], 4) for l in range(L)])
    m["mla_qnw"] = np.stack([_fm(inp["mla_q_norm_w"][l], 4) for l in range(L)])
    m["mla_wuq"] = f(inp["mla_w_uq"])
    m["mla_kvnw"] = np.stack([_fm(inp["mla_kv_norm_w"][l], 2) for l in range(L)])
    m["mla_wuk"] = f(inp["mla_w_uk"])
    m["mla_wuv"] = f(inp["mla_w_uv"])
    for n in ("w_branch_a", "w_branch_b", "w_branch_c", "w_out", "ffn_w_gate", "ffn_w_up", "ffn_w_down",
              "moe_router", "moe_w_gate", "moe_w_up", "moe_w_down"):
        m[n] = f(inp[n])
    m["fnwT"] = _fm(inp["final_norm_w"], 8)
    m["consts"] = _consts()
    invf = (10000.0 ** (-np.arange(0, 64, 2, dtype=np.float32) / 64)).astype(np.float32)
    m["invf"] = np.concatenate([invf] * 4).reshape(128, 1).astype(np.float32)
    return m


def kernel(**inputs):
    B, T = inputs["x"].shape[0], inputs["x"].shape[1]
    nc = build_program(T, list(range(DEPTH)))
    in_maps = [prep_inputs(inputs, b, T) for b in range(B)]
    res = run_bass_kernel_spmd(nc, in_maps, core_ids=list(range(B)))
    out = np.stack([np.asarray(res.results[b]["outT"], np.float32).T for b in range(B)])
    return np.ascontiguousarray(out)
```

```python
import numpy as np
from contextlib import ExitStack
import concourse.bass as bass
import concourse.mybir as mybir
from concourse.bass_utils import run_bass_kernel_spmd

F32, BF16, I32 = mybir.dt.float32, mybir.dt.bfloat16, mybir.dt.int32
AF = mybir.ActivationFunctionType
ALU = mybir.AluOpType
AX = mybir.AxisListType

D = 1024
DEPTH = 4
EPS = 1e-6
IN_DIM = 7504
FFN_DIM = 2816
EXPERT_DIM = 3584
NEXP = 8
SAME_SYNC = True

C_QKV, C_GZ, C_A, C_B, C_SZ, C_XBC, C_DT, C_CQ, C_CKV, C_KR, C_GATES = (
    0, 1536, 2048, 2052, 2056, 2568, 3592, 3600, 4112, 4368, 4432)


class Buf:
    def __init__(self, t, multi=False):
        self.t = t
        self.multi = multi
        self.w = {}
        self.r = {}

    def __getitem__(self, key):
        return self.t[key]


def _merge(d, tok):
    k_, v = tok
    if d.get(k_, 0) < v:
        d[k_] = v


class Ring:
    def __init__(self, bufs):
        self.bufs = bufs
        self.i = 0

    def get(self):
        b = self.bufs[self.i % len(self.bufs)]
        self.i += 1
        return b


class KB:
    ENG = ('pe', 'act', 'dve', 'pool', 'sp')

    def __init__(self, nc, es):
        self.nc = nc
        self.es = es
        self.e = {'pe': nc.tensor, 'act': nc.scalar, 'dve': nc.vector, 'pool': nc.gpsimd, 'sp': nc.sync}
        self.sem = {}
        self.cnt = {}
        for e in self.ENG:
            self.sem[('e', e)] = es.enter_context(nc.semaphore('se_' + e))
            self.cnt[('e', e)] = 0
        self.NS = 8
        self.dma_i = {}
        for q in ('sp', 'pool'):
            self.dma_i[q] = 0
            for j in range(self.NS):
                self.sem[('d', q, j)] = es.enter_context(nc.semaphore('sd_%s%d' % (q, j)))
                self.cnt[('d', q, j)] = 0
        self.seen = {e: {} for e in self.ENG}
        self.ninst = 0

    def _wait(self, eng, deps):
        for key, v in deps.items():
            if key == ('e', eng) and (eng == 'pe' or eng == 'sp' or not SAME_SYNC):
                continue
            if self.seen[eng].get(key, 0) >= v:
                continue
            self.e[eng].wait_ge(self.sem[key], v)
            self.seen[eng][key] = v
            self.ninst += 1

    def _deps(self, R, W):
        deps = {}
        for b in R:
            for t in b.w.items():
                _merge(deps, t)
        for b in W:
            for t in b.r.items():
                _merge(deps, t)
            if not b.multi:
                for t in b.w.items():
                    _merge(deps, t)
        return deps

    def _post(self, tok, R, W):
        for b in R:
            _merge(b.r, tok)
        for b in W:
            if b.multi:
                _merge(b.w, tok)
            else:
                b.w = {tok[0]: tok[1]}
                b.r = {}

    def op(self, eng, fn, R=(), W=()):
        self._wait(eng, self._deps(R, W))
        ins = fn(self.e[eng])
        key = ('e', eng)
        self.cnt[key] += 1
        ins.then_inc(self.sem[key], 1)
        self.ninst += 1
        self._post((key, self.cnt[key]), R, W)

    def dma(self, q, out, in_, R=(), W=()):
        self._wait(q, self._deps(R, W))
        key = ('d', q, self.dma_i[q] % self.NS)
        self.dma_i[q] += 1
        if self.cnt[key] > 0:
            self._wait(q, {key: self.cnt[key]})
        ins = self.e[q].dma_start(out=out, in_=in_)
        self.cnt[key] += 16
        ins.then_inc(self.sem[key], 16)
        self.ninst += 1
        self._post((key, self.cnt[key]), R, W)

    def barrier(self):
        for e in self.ENG:
            deps = {key: v for key, v in self.cnt.items() if v > 0 and key != ('e', e)}
            self._wait(e, deps)

    def mm(self, ps, out, lhsT, rhs, R, start=True, stop=True):
        self.op('pe', lambda e: e.matmul(out, lhsT, rhs, start=start, stop=stop), R=R, W=[ps])

    def tr(self, ps, out, in_, ident, R):
        self.op('pe', lambda e: e.transpose(out, in_, ident), R=R, W=[ps])

    def act(self, out, in_, func, R, W, bias=None, scale=None, accum_out=None, eng='act'):
        kw = {}
        if bias is not None:
            kw['bias'] = bias
        if scale is not None:
            kw['scale'] = scale
        if accum_out is not None:
            kw['accum_out'] = accum_out
        self.op('act', lambda e: e.activation(out=out, in_=in_, func=func, **kw), R=R, W=W)

    def tt(self, eng, out, in0, in1, op, R, W):
        self.op(eng, lambda e: e.tensor_tensor(out, in0, in1, op), R=R, W=W)

    def ts(self, eng, out, in0, s1, s2, op0, op1=None, R=(), W=()):
        if op1 is None:
            self.op(eng, lambda e: e.tensor_scalar(out, in0, s1, None, op0), R=R, W=W)
        else:
            self.op(eng, lambda e: e.tensor_scalar(out, in0, s1, s2, op0, op1), R=R, W=W)

    def stt(self, eng, out, in0, scalar, in1, op0, op1, R, W):
        self.op(eng, lambda e: e.scalar_tensor_tensor(out, in0, scalar, in1, op0, op1), R=R, W=W)

    def copy(self, eng, out, in_, R, W):
        if eng == 'act':
            self.op('act', lambda e: e.activation(out=out, in_=in_, func=AF.Copy), R=R, W=W)
        else:
            self.op(eng, lambda e: e.tensor_copy(out, in_), R=R, W=W)


def build_program(T, layers, debug=False, stages=('mix', 'ffn'), mixers=('mla', 'ssd', 'gdn')):
    nc = bass.Bass("TRN2", target_bir_lowering=False)
    L = DEPTH
    NT = T // 512
    NQ = T // 128
    NCH = T // 64

    def din(name, shape, dt=F32):
        return nc.dram_tensor(name, list(shape), dt, kind="ExternalInput").ap()

    def dscr(name, shape, dt, out=False):
        kind = "ExternalOutput" if out else "Internal"
        return nc.dram_tensor(name, list(shape), dt, kind=kind).ap()

    xT_in = din("xT", [D, T])
    cT_in = din("cT", [128, 8])
    pos_in = din("pos", [1, T], I32)
    w_ada = din("w_ada", [L, D, 6 * D])
    b_adaT = din("b_adaT", [L, 128, 48])
    w_in = din("w_in", [L, D, IN_DIM])
    gdn_convT = din("gdn_convT", [L, 128, 12, 4])
    gdn_alog = din("gdn_alog", [L, 128, 4])
    gdn_dtb = din("gdn_dtb", [L, 128, 4])
    gdn_nw = din("gdn_nw", [L, 128, 1])
    ssm_convT = din("ssm_convT", [L, 128, 8, 4])
    ssm_convb = din("ssm_convb", [L, 128, 8])
    ssm_alog = din("ssm_alog", [L, 128, 8])
    ssm_dtb = din("ssm_dtb", [L, 128, 8])
    ssm_dexp = din("ssm_dexp", [L, 128, 4])
    ssm_nw = din("ssm_nw", [L, 128, 4])
    mla_qnw = din("mla_qnw", [L, 128, 4])
    mla_wuq = din("mla_wuq", [L, 512, 768])
    mla_kvnw = din("mla_kvnw", [L, 128, 2])
    mla_wuk = din("mla_wuk", [L, 256, 512])
    mla_wuv = din("mla_wuv", [L, 256, 512])
    w_br = [din("w_branch_a", [L, 512, D]), din("w_branch_b", [L, 512, D]), din("w_branch_c", [L, 512, D])]
    w_out = din("w_out", [L, D, D])
    ffn_wg = din("ffn_w_gate", [2, D, FFN_DIM])
    ffn_wu = din("ffn_w_up", [2, D, FFN_DIM])
    ffn_wd = din("ffn_w_down", [2, FFN_DIM, D])
    moe_router = din("moe_router", [2, D, NEXP])
    moe_wg = din("moe_w_gate", [2, NEXP, D, EXPERT_DIM])
    moe_wu = din("moe_w_up", [2, NEXP, D, EXPERT_DIM])
    moe_wd = din("moe_w_down", [2, NEXP, EXPERT_DIM, D])
    fnwT = din("fnwT", [128, 8])
    consts = din("consts", [128, 8, 128])
    invf_in = din("invf", [128, 1])

    outT = dscr("outT", [D, T], F32, out=True)
    x_a = dscr("x_a", [D, T], F32, out=debug)
    x_b = dscr("x_b", [D, T], F32, out=debug)
    proj = dscr("proj", [IN_DIM, T], BF16, out=debug)
    abdt = dscr("abdt", [T, 16], F32, out=debug)
    y_d = [dscr("y_a", [512, T], BF16, out=debug), dscr("y_b", [512, T], BF16, out=debug),
           dscr("y_c", [512, T], BF16, out=debug)]

    es = ExitStack()
    with es:
        k = KB(nc, es)

        uid = [0]

        def sb(name, shape, dt, multi=False, stack=es):
            uid[0] += 1
            return Buf(stack.enter_context(nc.sbuf_tensor("%s_%d" % (name, uid[0]), list(shape), dt)), multi=multi)

        Dx_a, Dx_b, Dproj, Dabdt = Buf(x_a, True), Buf(x_b, True), Buf(proj, True), Buf(abdt, True)
        Dy = [Buf(y, True) for y in y_d]
        Dout = Buf(outT, True)
        Dw = Buf(None)
        Dxin = Buf(xT_in)

        psum = Ring([Buf(es.enter_context(nc.psum_tensor("ps%d" % i, [128, 512], F32))) for i in range(6)])
        psacc = Ring([Buf(es.enter_context(nc.psum_tensor("pa%d" % i, [128, 512], F32))) for i in range(2)])

        cst = sb("cst", [128, 8, 128], F32)
        k.dma('sp', cst[:], consts, R=[Dw], W=[cst])
        ident_f = cst[:, 0, :]
        ones_f = cst[:, 1, :]
        cst_b = sb("cst_b", [128, 2, 128], BF16)
        k.copy('dve', cst_b[:], cst[:, 0:2, :], R=[cst], W=[cst_b])
        ident_b = cst_b[:, 0, :]
        condT = sb("condT", [128, 8], F32)
        k.dma('sp', condT[:], cT_in, R=[Dw], W=[condT])
        k.act(condT[:], condT[:], AF.Silu, R=[condT], W=[condT])
        modT = sb("modT", [128, 48], F32)
        fnw = sb("fnw", [128, 8], F32)
        k.dma('sp', fnw[:], fnwT, R=[Dw], W=[fnw])

        def phase_ada(l):
            with ExitStack() as ps_:
                wr = Ring([sb("wada%d" % i, [128, 8, 768], F32, stack=ps_) for i in range(2)])
                bt = sb("badat", [128, 48], F32, stack=ps_)
                k.dma('sp', bt[:], b_adaT[l], R=[Dw], W=[bt])
                ps = psum.get()
                for g in range(8):
                    wb = wr.get()
                    k.dma('sp', wb[:], w_ada[l][:, g * 768:(g + 1) * 768].rearrange('(kc p) n -> p kc n', p=128),
                          R=[Dw], W=[wb])
                    for jj in range(6):
                        j = g * 6 + jj
                        for kc in range(8):
                            k.mm(ps, ps[:, j:j + 1], wb[:, kc, jj * 128:(jj + 1) * 128], condT[:, kc:kc + 1],
                                 R=[wb, condT], start=(kc == 0), stop=(kc == 7))
                k.tt('dve', modT[:], ps[:, 0:48], bt[:], ALU.add, R=[ps, bt], W=[modT])
                k.ts('dve', modT[:, 8:16], modT[:, 8:16], 1.0, None, ALU.add, R=[modT], W=[modT])
                k.ts('dve', modT[:, 32:40], modT[:, 32:40], 1.0, None, ALU.add, R=[modT], W=[modT])
                k.barrier()

        def norm_tile(xt, xap, sq, hdst, hbuf, sc_off, sh_off, hf=None):
            k.act(sq[:], xap, AF.Square, R=[xt], W=[sq])
            ps = psum.get()
            for kc in range(8):
                k.mm(ps, ps[:, :], ones_f, sq[:, kc, :], R=[sq, cst], start=(kc == 0), stop=(kc == 7))
            rstd = rstd_ring.get()
            rsqrt(rstd, rstd[:], ps, ps[:, :], 1.0 / D)
            for kc in range(8):
                k.tt('pool' if kc % 2 else 'dve', sq[:, kc, :], xap[:, kc, :], rstd[:], ALU.mult,
                     R=[xt, rstd], W=[sq])
            for kc in range(8):
                if sc_off is None:
                    k.ts('dve', hdst(kc), sq[:, kc, :], fnw[:, kc:kc + 1], None, ALU.mult, R=[sq, fnw], W=[hbuf])
                else:
                    k.ts('dve', hdst(kc), sq[:, kc, :], modT[:, sc_off + kc:sc_off + kc + 1],
                         modT[:, sh_off + kc:sh_off + kc + 1], ALU.mult, ALU.add, R=[sq, modT], W=[hbuf])
                    if hf is not None:
                        k.ts('pool', hf[0][:, kc, :], sq[:, kc, :], modT[:, sc_off + kc:sc_off + kc + 1],
                             modT[:, sh_off + kc:sh_off + kc + 1], ALU.mult, ALU.add, R=[sq, modT], W=[hf[0]])

        epsT = sb("epsT", [128, 1], F32)
        k.op('dve', lambda e: e.memset(epsT[:], EPS), W=[epsT])

        def rsqrt(ob, out, ib, in_, scale):
            k.act(out, in_, AF.Sqrt, R=[ib, epsT], W=[ob], bias=epsT[:out.shape[0], 0:1], scale=scale)
            k.op('dve', lambda e: e.reciprocal(out, out), R=[ob], W=[ob])

        rstd_ring = Ring([sb("rstd%d" % i, [128, 512], F32) for i in range(2)])

        def phase_inproj(l, xsrc, Dxsrc):
            with ExitStack() as ps_:
                h1 = sb("h1", [128, 8, T], BF16, multi=True, stack=ps_)
                xr = Ring([sb("xt%d" % i, [128, 8, 512], F32, stack=ps_) for i in range(2)])
                sq = sb("sq", [128, 8, 512], F32, stack=ps_)
                hf = sb("hf", [128, 8, 512], F32, stack=ps_)
                wsm = sb("wsm", [128, 8, 16], F32, stack=ps_)
                sm_st = Ring([sb("smst%d" % i, [128, 16], F32, stack=ps_) for i in range(2)])
                k.dma('sp', wsm[:, :, 0:8], w_in[l][:, C_A:C_A + 8].rearrange('(kc p) n -> p kc n', p=128),
                      R=[Dw], W=[wsm])
                k.dma('sp', wsm[:, :, 8:16], w_in[l][:, C_DT:C_DT + 8].rearrange('(kc p) n -> p kc n', p=128),
                      R=[Dw], W=[wsm])
                for tt in range(NT):
                    xt = xr.get()
                    k.dma('sp', xt[:], xsrc[:, tt * 512:(tt + 1) * 512].rearrange('(kc p) t -> p kc t', p=128),
                          R=[Dxsrc], W=[xt])
                    norm_tile(xt, xt[:], sq, lambda kc: h1[:, kc, tt * 512:(tt + 1) * 512], h1, 8, 0, hf=[hf])
                    for q in range(4):
                        ps = psum.get()
                        for kc in range(8):
                            k.mm(ps, ps[:, 0:16], hf[:, kc, q * 128:(q + 1) * 128], wsm[:, kc, :], R=[hf, wsm],
                                 start=(kc == 0), stop=(kc == 7))
                        st = sm_st.get()
                        k.copy('act', st[:], ps[:, 0:16], R=[ps], W=[st])
                        t0 = tt * 512 + q * 128
                        k.dma('sp', abdt[t0:t0 + 128, :], st[:], R=[st], W=[Dabdt])
                groups = [(C_QKV, 1536, 'copy'), (C_GZ, 512, 'silu'), (C_SZ, 512, 'silu'), (C_XBC, 1024, 'copy'),
                          (C_CQ, 512, 'copy'), (C_CKV, 256, 'copy'), (C_KR, 64, 'copy'), (C_GATES, 3072, 'sig')]
                wr = Ring([sb("win%d" % i, [128, 8, 512], BF16, stack=ps_) for i in range(2)])
                stg = Ring([sb("stg%d" % i, [128, 512], BF16, stack=ps_) for i in range(4)])
                ecnt = 0
                for (c0, ncols, post) in groups:
                    for blk in range(0, ncols, 512):
                        bw = min(512, ncols - blk)
                        wt = wr.get()
                        k.dma('pool', wt[:, :, :bw],
                              w_in[l][:, c0 + blk:c0 + blk + bw].rearrange('(kc p) n -> p kc n', p=128),
                              R=[Dw], W=[wt])
                        for tt in range(NT):
                            for ct in range(0, bw, 128):
                                m = min(128, bw - ct)
                                ps = psum.get()
                                for kc in range(8):
                                    k.mm(ps, ps[:m, :], wt[:, kc, ct:ct + m], h1[:, kc, tt * 512:(tt + 1) * 512],
                                         R=[wt, h1], start=(kc == 0), stop=(kc == 7))
                                st = stg.get()
                                if post == 'silu':
                                    k.act(st[:m, :], ps[:m, :], AF.Silu, R=[ps], W=[st])
                                elif post == 'sig':
                                    k.act(st[:m, :], ps[:m, :], AF.Sigmoid, R=[ps], W=[st])
                                else:
                                    ecnt += 1
                                    k.copy('dve' if ecnt % 2 else 'act', st[:m, :], ps[:m, :], R=[ps], W=[st])
                                r0 = c0 + blk + ct
                                k.dma('sp', proj[r0:r0 + m, tt * 512:(tt + 1) * 512], st[:m, :], R=[st], W=[Dproj])
                k.barrier()


        def dump(name, b, ap=None, dt=None):
            if not debug:
                return
            ap = b[:] if ap is None else ap
            uid[0] += 1
            t = nc.dram_tensor("dbg_%s_%d" % (name, uid[0]), list(ap.shape), dt or ap.dtype, kind="ExternalOutput").ap()
            k.dma('sp', t, ap, R=[b], W=[Buf(None, True)])

        cols = sb("cols", [128, 4], F32)
        k.op('dve', lambda e: e.memset(cols[:, 0:1], 1.0), W=[cols])
        k.op('dve', lambda e: e.memset(cols[:, 1:2], -np.pi), W=[cols])
        invf = sb("invf", [128, 1], F32)
        k.dma('sp', invf[:], invf_in, R=[Dw], W=[invf])
        ATT_SCALE = 192.0 ** -0.5

        def phase_mla(l):
            with ExitStack() as ps_:
                wuq = sb("wuq", [128, 4, 768], BF16, stack=ps_)
                wuqr = sb("wuqr", [128, 4, 2, 128], BF16, stack=ps_)
                wuk = sb("wuk", [128, 2, 512], BF16, stack=ps_)
                wuv = sb("wuv", [128, 2, 512], BF16, stack=ps_)
                qnw = sb("qnw", [128, 4], F32, stack=ps_)
                kvnw = sb("kvnw", [128, 2], F32, stack=ps_)
                k.dma('pool', wuq[:], mla_wuq[l].rearrange('(kc p) n -> p kc n', p=128), R=[Dw], W=[wuq])
                for h in range(4):
                    k.dma('pool', wuqr[:, :, h // 2, (h % 2) * 64:(h % 2) * 64 + 64],
                          mla_wuq[l][:, h * 192 + 128:h * 192 + 192].rearrange('(kc p) n -> p kc n', p=128),
                          R=[Dw], W=[wuqr])
                k.dma('pool', wuk[:], mla_wuk[l].rearrange('(kc p) n -> p kc n', p=128), R=[Dw], W=[wuk])
                k.dma('pool', wuv[:], mla_wuv[l].rearrange('(kc p) n -> p kc n', p=128), R=[Dw], W=[wuv])
                k.dma('sp', qnw[:], mla_qnw[l], R=[Dw], W=[qnw])
                k.dma('sp', kvnw[:], mla_kvnw[l], R=[Dw], W=[kvnw])
                qn = sb("qn", [128, 4, T], BF16, multi=True, stack=ps_)
                qr = sb("qr", [128, 2, T], BF16, multi=True, stack=ps_)
                kn = sb("kn", [128, 4, T], BF16, multi=True, stack=ps_)
                krp = sb("krp", [128, T], BF16, multi=True, stack=ps_)
                vtm = sb("vtm", [128, NQ, 512], BF16, multi=True, stack=ps_)
                with ExitStack() as p1:
                    cin = Ring([sb("mcin%d" % i, [128, 7, 512], BF16, stack=p1) for i in range(2)])
                    sqm = sb("msq", [128, 4, 512], F32, stack=p1)
                    cqn = sb("cqn", [128, 4, 512], BF16, stack=p1)
                    ckvn = sb("ckvn", [128, 2, 512], BF16, stack=p1)
                    posi = sb("posi", [128, 512], I32, stack=p1)
                    posf = sb("posf", [128, 512], F32, stack=p1)
                    frac = sb("frac", [128, 512], F32, stack=p1)
                    fint = sb("fint", [128, 512], I32, stack=p1)
                    ftmp = sb("ftmp", [128, 512], F32, stack=p1)
                    sinT = sb("sinT", [128, 512], F32, stack=p1)
                    cosT = sb("cosT", [128, 512], F32, stack=p1)
                    rf = sb("rf", [128, 512], F32, stack=p1)
                    r1 = sb("r1", [128, 512], F32, stack=p1)
                    r2 = sb("r2", [128, 512], F32, stack=p1)
                    rstd = sb("mrstd", [128, 512], F32, stack=p1)
                    rot2 = cst[:, 5, :]

                    def rope(src_b, src_ap, dst_b, dst_ap, scale):
                        k.copy('act', rf[:], src_ap, R=[src_b], W=[rf])
                        ps = psum.get()
                        k.mm(ps, ps[:, :], rot2, rf[:], R=[cst, rf])
                        k.stt('dve', r1[:], rf[:], scale, cosT[:], ALU.mult, ALU.mult, R=[rf, cosT], W=[r1])
                        k.stt('dve', r2[:], ps[:, :], scale, sinT[:], ALU.mult, ALU.mult, R=[ps, sinT], W=[r2])
                        k.tt('dve', dst_ap, r1[:], r2[:], ALU.add, R=[r1, r2], W=[dst_b])

                    for tt in range(NT):
                        ts_ = slice(tt * 512, (tt + 1) * 512)
                        ci = cin.get()
                        k.dma('sp', ci[:, 0:6, :], proj[C_CQ:C_CQ + 768, ts_].rearrange('(kc p) t -> p kc t', p=128),
                              R=[Dproj], W=[ci])
                        k.dma('sp', ci[0:64, 6, :], proj[C_KR:C_KR + 64, ts_], R=[Dproj], W=[ci])
                        k.dma('sp', ci[64:128, 6, :], proj[C_KR:C_KR + 64, ts_], R=[Dproj], W=[ci])
                        k.dma('sp', posi[:], pos_in[0:1, ts_].partition_broadcast(128), R=[Dw], W=[posi])
                        k.copy('dve', posf[:], posi[:], R=[posi], W=[posf])
                        for (off, dst) in ((0.5, sinT), (0.75, cosT)):
                            k.ts('dve', frac[:], posf[:], invf[:, 0:1], 1.0 / (2 * np.pi), ALU.mult, ALU.mult,
                                 R=[posf, invf], W=[frac])
                            k.ts('dve', frac[:], frac[:], off, None, ALU.add, R=[frac], W=[frac])
                            k.copy('dve', fint[:], frac[:], R=[frac], W=[fint])
                            k.copy('dve', ftmp[:], fint[:], R=[fint], W=[ftmp])
                            k.tt('dve', frac[:], frac[:], ftmp[:], ALU.subtract, R=[frac, ftmp], W=[frac])
                            k.ts('dve', ftmp[:], frac[:], 0.0, None, ALU.is_lt, R=[frac], W=[ftmp])
                            k.tt('dve', frac[:], frac[:], ftmp[:], ALU.add, R=[frac, ftmp], W=[frac])
                            k.act(dst[:], frac[:], AF.Sin, R=[frac, cols], W=[dst], bias=cols[:, 1:2],
                                  scale=2 * np.pi)
                        for (c0, nk_, wv, dstb) in ((0, 4, qnw, cqn), (4, 2, kvnw, ckvn)):
                            k.act(sqm[:, 0:nk_, :], ci[:, c0:c0 + nk_, :], AF.Square, R=[ci], W=[sqm])
                            ps = psum.get()
                            for kc in range(nk_):
                                k.mm(ps, ps[:, :], ones_f, sqm[:, kc, :], R=[cst, sqm], start=(kc == 0),
                                     stop=(kc == nk_ - 1))
                            rsqrt(rstd, rstd[:], ps, ps[:, :], 1.0 / (nk_ * 128))
                            for kc in range(nk_):
                                k.stt('dve', dstb[:, kc, :], ci[:, c0 + kc, :], wv[:, kc:kc + 1], rstd[:], ALU.mult,
                                      ALU.mult, R=[ci, wv, rstd], W=[dstb])
                        for h in range(4):
                            ps = psum.get()
                            for kc in range(4):
                                k.mm(ps, ps[:, :], wuq[:, kc, h * 192:h * 192 + 128], cqn[:, kc, :], R=[wuq, cqn],
                                     start=(kc == 0), stop=(kc == 3))
                            k.act(qn[:, h, ts_], ps[:, :], AF.Copy, R=[ps], W=[qn], scale=ATT_SCALE)
                            ps = psum.get()
                            for kc in range(2):
                                k.mm(ps, ps[:, :], wuk[:, kc, h * 128:(h + 1) * 128], ckvn[:, kc, :], R=[wuk, ckvn],
                                     start=(kc == 0), stop=(kc == 1))
                            k.copy('dve', kn[:, h, ts_], ps[:, :], R=[ps], W=[kn])
                        for hp in range(2):
                            ps = psum.get()
                            for kc in range(4):
                                k.mm(ps, ps[:, :], wuqr[:, kc, hp, :], cqn[:, kc, :], R=[wuqr, cqn],
                                     start=(kc == 0), stop=(kc == 3))
                            rope(ps, ps[:, :], qr, qr[:, hp, ts_], ATT_SCALE)
                        rope(ci, ci[:, 6, :], krp, krp[:, ts_], 1.0)
                        for q in range(4):
                            ps = psum.get()
                            for kc in range(2):
                                k.mm(ps, ps[:, :], ckvn[:, kc, q * 128:(q + 1) * 128], wuv[:, kc, :], R=[ckvn, wuv],
                                     start=(kc == 0), stop=(kc == 1))
                            k.copy('act', vtm[:, tt * 4 + q, :], ps[:, :], R=[ps], W=[vtm])
                dump("qn", qn); dump("qr", qr); dump("kn", kn); dump("krp", krp); dump("vtm", vtm)
                with ExitStack() as p2:
                    WM = 2
                    slots = []
                    for i in range(WM):
                        slots.append(dict(
                            Ssb=sb("Ssb%d" % i, [128, T], F32, stack=p2), Psb=sb("Psb%d" % i, [128, T], BF16, stack=p2),
                            ptr=Ring([sb("pt%d_%d" % (i, j), [128, 4, 128], BF16, stack=p2) for j in range(2)]),
                            sc=sb("asc%d" % i, [128, 4], F32, stack=p2), dg=sb("adg%d" % i, [128, 128], BF16, stack=p2),
                            po=psacc.bufs[i]))
                    ost = [sb("aost%d" % i, [128, 4, 128], BF16, multi=True, stack=p2) for i in range(2)]
                    done = {}

                    def att(key):
                        qi, h = key
                        B = slots[(qi * 4 + h) % WM]
                        Ssb, Psb, ptr, s_, dg, po = B['Ssb'], B['Psb'], B['ptr'], B['sc'], B['dg'], B['po']
                        ot = ost[qi % 2]
                        nk = (qi + 1) * 128
                        qs = slice(qi * 128, (qi + 1) * 128)
                        hp, ho = h // 2, (h % 2) * 64
                        for kb in range(0, nk, 512):
                            w = min(512, nk - kb)
                            ps = psum.get()
                            k.mm(ps, ps[:, :w], qn[:, h, qs], kn[:, h, kb:kb + w], R=[qn, kn], start=True,
                                 stop=False)
                            k.mm(ps, ps[:, :w], qr[ho:ho + 64, hp, qs], krp[ho:ho + 64, kb:kb + w], R=[qr, krp],
                                 start=False, stop=True)
                            if kb + w == nk:
                                if w > 128:
                                    k.copy('act', Ssb[:, kb:nk - 128], ps[:, :w - 128], R=[ps], W=[Ssb])
                                k.tt('dve', Ssb[:, nk - 128:nk], ps[:, w - 128:w], cst[:, 3, :], ALU.subtract,
                                     R=[ps, cst], W=[Ssb])
                            else:
                                k.copy('act', Ssb[:, kb:kb + w], ps[:, :w], R=[ps], W=[Ssb])
                            yield
                        k.op('dve', lambda e: e.reduce_max(s_[:, 0:1], Ssb[:, :nk], AX.X), R=[Ssb], W=[s_])
                        k.ts('dve', s_[:, 1:2], s_[:, 0:1], -1.0, None, ALU.mult, R=[s_], W=[s_])
                        yield
                        k.act(Psb[:, :nk], Ssb[:, :nk], AF.Exp, R=[Ssb, s_], W=[Psb, s_], bias=s_[:, 1:2],
                              scale=1.0, accum_out=s_[:, 2:3])
                        yield
                        k.op('dve', lambda e: e.reciprocal(s_[:, 3:4], s_[:, 2:3]), R=[s_], W=[s_])
                        k.ts('dve', dg[:], ident_b, s_[:, 3:4], None, ALU.mult, R=[cst_b, s_], W=[dg])
                        yield
                        nb = nk // 128
                        for b4 in range(0, nb, 4):
                            n4 = min(4, nb - b4)
                            ps = psum.get()
                            for j in range(n4):
                                kb2 = (b4 + j) * 128
                                k.mm(ps, ps[:, j * 128:(j + 1) * 128], Psb[:, kb2:kb2 + 128], dg[:], R=[Psb, dg])
                            pt = ptr.get()
                            k.copy('dve' if (b4 // 4) % 2 else 'act', pt[:, 0:n4, :],
                                   ps[:, 0:n4 * 128].rearrange('p (j t) -> p j t', j=n4), R=[ps], W=[pt])
                            for j in range(n4):
                                kblk = b4 + j
                                k.mm(po, po[:, 0:128], vtm[:, kblk, h * 128:(h + 1) * 128], pt[:, j, :],
                                     R=[vtm, pt], start=(kblk == 0), stop=(kblk == nb - 1))
                            yield
                        k.copy('act', ot[:, h, :], po[:, 0:128], R=[po], W=[ot])

                    def att_done(key):
                        qi, h = key
                        done[qi] = done.get(qi, 0) + 1
                        if done[qi] == 4:
                            qs = slice(qi * 128, (qi + 1) * 128)
                            k.dma('sp', y_d[2][:, qs].rearrange('(h p) t -> p h t', p=128), ost[qi % 2][:],
                                  R=[ost[qi % 2]], W=[Dy[2]])

                    run_pipeline([(qi, h) for qi in range(NQ) for h in range(4)], WM, att, att_done)
                k.barrier()


        def bc(ap, shape):
            return ap.to_broadcast(list(shape))

        def run_pipeline(items, W, start_fn, finish_fn):
            active = []
            it = iter(items)
            pending = True
            while True:
                while len(active) < W and pending:
                    try:
                        key = next(it)
                    except StopIteration:
                        pending = False
                        break
                    active.append((key, start_fn(key)))
                if not active:
                    break
                for ent in list(active):
                    try:
                        next(ent[1])
                    except StopIteration:
                        active.remove(ent)
                        finish_fn(ent[0])

        def conv_silu(cin, cw, nchan, dst, dstb, tmpr, bias=None):
            for c in range(nchan):
                tb = tmpr.get()
                k.ts('dve', tb[:], cin[:, c, 0:512], cw[:, c, 0:1], None, ALU.mult, R=[cin, cw], W=[tb])
                for kk in range(1, 4):
                    k.stt('dve', tb[:], cin[:, c, kk:kk + 512], cw[:, c, kk:kk + 1], tb[:], ALU.mult,
                          ALU.add, R=[cin, cw, tb], W=[tb])
                if bias is None:
                    k.act(dst[:, c, :], tb[:], AF.Silu, R=[tb], W=[dstb])
                else:
                    k.act(dst[:, c, :], tb[:], AF.Silu, R=[tb, bias], W=[dstb], bias=bias[:, c:c + 1])

        def load_halo(cin, row0, nrows, tt):
            if tt == 0:
                k.op('dve', lambda e: e.memset(cin[:, :, 0:3], 0.0), W=[cin])
                k.dma('sp', cin[:, :, 3:515], proj[row0:row0 + nrows, 0:512].rearrange('(c p) t -> p c t', p=128),
                      R=[Dproj], W=[cin])
            else:
                k.dma('sp', cin[:, :, :],
                      proj[row0:row0 + nrows, tt * 512 - 3:tt * 512 + 512].rearrange('(c p) t -> p c t', p=128),
                      R=[Dproj], W=[cin])

        tri64 = cst[0:64, 2, 0:64]
        ones64 = cst[0:64, 1, 0:64]
        ones64w = cst[0:64, 1, :]
        posm64 = cst[0:64, 3, 0:64]
        strict64 = cst[0:64, 4, 0:64]
        lowm64 = cst[0:64, 6, 0:64]
        id64 = cst[0:64, 0, 0:64]

        def phase_ssd(l):
            with ExitStack() as ps_:
                def t_(name, shape, dt=F32, multi=False):
                    return sb(name, shape, dt, multi=multi, stack=ps_)
                cw = t_("scw", [128, 8, 4]); cb = t_("scb", [128, 8]); alog = t_("salog", [128, 8])
                dtb = t_("sdtb", [128, 8]); dexp = t_("sdexp", [128, 4]); nw = t_("snw", [128, 4])
                for (d_, s_) in ((cw, ssm_convT), (cb, ssm_convb), (alog, ssm_alog), (dtb, ssm_dtb), (dexp, ssm_dexp),
                                 (nw, ssm_nw)):
                    k.dma('sp', d_[:], s_[l], R=[Dw], W=[d_])
                k.act(alog[:], alog[:], AF.Exp, R=[alog], W=[alog])
                k.ts('dve', alog[:], alog[:], -1.0, None, ALU.mult, R=[alog], W=[alog])
                raw = t_("sraw", [64, NCH, 16])
                k.dma('sp', raw[:], abdt.rearrange('(c p) k -> p c k', p=64), R=[Dabdt], W=[raw])
                dt = t_("sdt", [64, NCH, 8]); ad = t_("sad", [64, NCH, 8]); acs = t_("sacs", [64, NCH, 8])
                acl = t_("sacl", [128, NCH, 8]); cd = t_("scd", [128, NCH, 8]); ds = t_("sds", [64, NCH, 8])
                eacs = t_("seacs", [64, NCH, 8])
                k.tt('dve', dt[:], raw[:, :, 8:16], bc(dtb[0:64, :].unsqueeze(1), [64, NCH, 8]), ALU.add,
                     R=[raw, dtb], W=[dt])
                k.act(dt[:], dt[:], AF.Exp, R=[dt], W=[dt])
                k.act(dt[:], dt[:], AF.Ln, R=[dt, cols], W=[dt], bias=cols[0:64, 0:1])
                k.tt('dve', ad[:], dt[:], bc(alog[0:64, :].unsqueeze(1), [64, NCH, 8]), ALU.mult, R=[dt, alog], W=[ad])
                adf = ad[:].rearrange('p c h -> p (c h)')
                ps = psum.get()
                k.mm(ps, ps[:64, :NCH * 8], tri64, adf, R=[cst, ad])
                k.copy('dve', acs[:].rearrange('p c h -> p (c h)'), ps[:64, :NCH * 8], R=[ps], W=[acs])
                ps = psum.get()
                k.mm(ps, ps[:, :NCH * 8], ones64w, adf, R=[cst, ad])
                k.copy('dve', acl[:].rearrange('p c h -> p (c h)'), ps[:, :NCH * 8], R=[ps], W=[acl])
                k.act(cd[:], acl[:], AF.Exp, R=[acl], W=[cd])
                k.tt('dve', ds[:], acl[0:64], acs[:], ALU.subtract, R=[acl, acs], W=[ds])
                k.act(ds[:], ds[:], AF.Exp, R=[ds], W=[ds])
                k.act(eacs[:], acs[:], AF.Exp, R=[acs], W=[eacs])
                state = t_("sstate", [128, 8, 64])
                k.op('dve', lambda e: e.memset(state[:], 0.0), W=[state])
                cinr = Ring([t_("scin%d" % i, [128, 8, 515], BF16) for i in range(2)])
                szr = Ring([t_("ssz%d" % i, [128, 4, 512], BF16) for i in range(2)])
                xfr = Ring([t_("sxf%d" % i, [128, 8, 512]) for i in range(2)])
                ctr = Ring([t_("sct%d" % i, [128, 512]) for i in range(2)])
                ystr = Ring([t_("syst%d" % i, [128, 4, 512], BF16, multi=True) for i in range(2)])
                WS = 2
                slots = []
                for i in range(WS):
                    slots.append(dict(
                        trig=t_("strig%d" % i, [64, 8, 64]), t1=t_("st1%d" % i, [64, 8, 64]), MT=t_("sMT%d" % i, [64, 8, 64]),
                        X=t_("sX%d" % i, [64, 8, 64]), Xds=t_("sXds%d" % i, [64, 8, 64]), Btm=t_("sBtm%d" % i, [64, 2, 128]),
                        yt=t_("syt%d" % i, [64, 8, 64]), ytm=t_("sytm%d" % i, [64, 8, 64]), yfm=t_("syfm%d" % i, [128, 4, 64]),
                        tmp=t_("stmp%d" % i, [128, 4, 64]), sq=t_("ssq%d" % i, [128, 4, 64]), rs=t_("srs%d" % i, [128, 2, 64])))
                tiles = {}
                turn = [0]
                done = {}

                def prep(tt):
                    cin = cinr.get(); szt = szr.get(); yst = ystr.get(); xf = xfr.get()
                    load_halo(cin, C_XBC, 1024, tt)
                    k.dma('sp', szt[:], proj[C_SZ:C_SZ + 512, tt * 512:(tt + 1) * 512].rearrange(
                        '(c p) t -> p c t', p=128), R=[Dproj], W=[szt])
                    conv_silu(cin, cw, 8, xf, xf, ctr, bias=cb)
                    tiles[tt] = (xf, szt, yst)

                def chunk(ch):
                    tt, cc = ch // 8, ch % 8
                    if cc == 0:
                        prep(tt)
                    xf, szt, yst = tiles[tt]
                    B = slots[ch % WS]
                    trig, t1, MT, X, Xds, Btm = B['trig'], B['t1'], B['MT'], B['X'], B['Xds'], B['Btm']
                    yt, ytm, yfm, tmp, sq, rs = B['yt'], B['ytm'], B['yfm'], B['tmp'], B['sq'], B['rs']
                    cs = slice(cc * 64, cc * 64 + 64)
                    ps_cb = psum.get()
                    for g in range(2):
                        k.mm(ps_cb, ps_cb[:64, g * 64:(g + 1) * 64], xf[:, 4 + g, cs], xf[:, 6 + g, cs], R=[xf])
                    k.tt('pool', trig[:], bc(tri64.unsqueeze(1), [64, 8, 64]),
                         bc(ad[:, ch, :].unsqueeze(2), [64, 8, 64]), ALU.mult, R=[cst, ad], W=[trig])
                    ps_r = psum.get()
                    k.mm(ps_r, ps_r[:64, :], ones64, trig[:].rearrange('p h l -> p (h l)'), R=[cst, trig])
                    k.tt('dve', t1[:], ps_r[:64, :].rearrange('p (h l) -> p h l', h=8),
                         bc(lowm64.unsqueeze(1), [64, 8, 64]), ALU.subtract, R=[ps_r, cst], W=[t1])
                    k.tt('dve', t1[:], t1[:], bc(acs[:, ch, :].unsqueeze(2), [64, 8, 64]), ALU.subtract,
                         R=[t1, acs], W=[t1])
                    k.act(t1[:], t1[:], AF.Exp, R=[t1], W=[t1])
                    k.tt('dve', MT[:].rearrange('p (g e) l -> p g e l', g=2),
                         t1[:].rearrange('p (g e) l -> p g e l', g=2),
                         bc(ps_cb[:64, 0:128].rearrange('p (g l) -> p g l', g=2).unsqueeze(2), [64, 2, 4, 64]),
                         ALU.mult, R=[t1, ps_cb], W=[MT])
                    yield
                    ps_x = psum.get()
                    for kc in range(4):
                        k.tr(ps_x, ps_x[:64, kc * 128:(kc + 1) * 128], xf[:, kc, cs], ident_f, R=[xf, cst])
                    k.tt('dve', X[:], ps_x[:64, :].rearrange('p (h q) -> p h q', h=8),
                         bc(dt[:, ch, :].unsqueeze(2), [64, 8, 64]), ALU.mult, R=[ps_x, dt], W=[X])
                    k.tt('pool', Xds[:], X[:], bc(ds[:, ch, :].unsqueeze(2), [64, 8, 64]), ALU.mult,
                         R=[X, ds], W=[Xds])
                    yield
                    ps_b = psum.get()
                    for g in range(2):
                        k.tr(ps_b, ps_b[:64, g * 128:(g + 1) * 128], xf[:, 4 + g, cs], ident_f, R=[xf, cst])
                    k.copy('act', Btm[:].rearrange('p g n -> p (g n)'), ps_b[:64, 0:256], R=[ps_b], W=[Btm])
                    yield
                    while turn[0] != ch:
                        yield
                    ps_y1 = psum.get()
                    for h in range(8):
                        k.mm(ps_y1, ps_y1[:64, h * 64:(h + 1) * 64], MT[:, h, :], X[:, h, :], R=[MT, X])
                    ps_y2 = psum.get()
                    for h in range(8):
                        k.mm(ps_y2, ps_y2[:64, h * 64:(h + 1) * 64], xf[:, 6 + h // 4, cs], state[:, h, :],
                             R=[xf, state])
                    k.tt('dve', yt[:], ps_y2[:64, :].rearrange('p (h q) -> p h q', h=8),
                         bc(eacs[:, ch, :].unsqueeze(2), [64, 8, 64]), ALU.mult, R=[ps_y2, eacs], W=[yt])
                    k.tt('dve', ytm[:], yt[:], ps_y1[:64, :].rearrange('p (h q) -> p h q', h=8), ALU.add,
                         R=[yt, ps_y1], W=[ytm])
                    ps_s = psum.get()
                    for h in range(8):
                        k.mm(ps_s, ps_s[:, h * 64:(h + 1) * 64], Btm[:, h // 4, :], Xds[:, h, :], R=[Btm, Xds])
                    k.tt('dve', state[:], state[:], bc(cd[:, ch, :].unsqueeze(2), [128, 8, 64]), ALU.mult,
                         R=[state, cd], W=[state])
                    k.tt('dve', state[:], state[:], ps_s[:, :].rearrange('p (h q) -> p h q', h=8), ALU.add,
                         R=[state, ps_s], W=[state])
                    turn[0] = ch + 1
                    yield
                    ps_t = psum.get()
                    ytf = ytm[:].rearrange('p h q -> p (h q)')
                    for kc in range(4):
                        k.tr(ps_t, ps_t[:, kc * 64:(kc + 1) * 64], ytf[:, kc * 128:(kc + 1) * 128], id64,
                             R=[ytm, cst])
                    k.tt('pool', tmp[:], xf[:, 0:4, cs], bc(dexp[:, :].unsqueeze(2), [128, 4, 64]), ALU.mult,
                         R=[xf, dexp], W=[tmp])
                    k.tt('dve', yfm[:], tmp[:], ps_t[:, 0:256].rearrange('p (c q) -> p c q', c=4), ALU.add,
                         R=[tmp, ps_t], W=[yfm])
                    k.tt('dve', yfm[:], yfm[:], szt[:, :, cs], ALU.mult, R=[yfm, szt], W=[yfm])
                    k.act(sq[:], yfm[:], AF.Square, R=[yfm], W=[sq])
                    yield
                    ps_n = psum.get()
                    for g in range(2):
                        for k2 in range(2):
                            k.mm(ps_n, ps_n[:, g * 64:(g + 1) * 64], ones_f, sq[:, g * 2 + k2, :], R=[cst, sq],
                                 start=(k2 == 0), stop=(k2 == 1))
                    rsqrt(rs, rs[:].rearrange('p g q -> p (g q)'), ps_n, ps_n[:, 0:128], 1.0 / 256)
                    for kc in range(4):
                        k.stt('dve', yst[:, kc, cs], yfm[:, kc, :], nw[:, kc:kc + 1], rs[:, kc // 2, :], ALU.mult,
                              ALU.mult, R=[yfm, nw, rs], W=[yst])

                def chunk_done(ch):
                    tt = ch // 8
                    done[tt] = done.get(tt, 0) + 1
                    if done[tt] == 8:
                        yst = tiles[tt][2]
                        k.dma('sp', y_d[1][:, tt * 512:(tt + 1) * 512].rearrange('(c p) t -> p c t', p=128), yst[:],
                              R=[yst], W=[Dy[1]])

                run_pipeline(list(range(NCH)), WS, chunk, chunk_done)
                k.barrier()

        def phase_gdn(l):
            with ExitStack() as ps_:
                def t_(name, shape, dt=F32, multi=False):
                    return sb(name, shape, dt, multi=multi, stack=ps_)
                cw = t_("gcw", [128, 12, 4]); alog = t_("galog", [128, 4]); dtb = t_("gdtb", [128, 4])
                nw = t_("gnw", [128, 1])
                for (d_, s_) in ((cw, gdn_convT), (alog, gdn_alog), (dtb, gdn_dtb), (nw, gdn_nw)):
                    k.dma('sp', d_[:], s_[l], R=[Dw], W=[d_])
                k.act(alog[:], alog[:], AF.Exp, R=[alog], W=[alog])
                k.ts('dve', alog[:], alog[:], -1.0, None, ALU.mult, R=[alog], W=[alog])
                raw = t_("graw", [64, NCH, 16])
                k.dma('sp', raw[:], abdt.rearrange('(c p) k -> p c k', p=64), R=[Dabdt], W=[raw])
                beta = t_("gbeta", [64, NCH, 4]); nbeta = t_("gnbeta", [64, NCH, 4]); g = t_("gg", [64, NCH, 4])
                G = t_("gG", [64, NCH, 4]); Glb = t_("gGlb", [128, NCH, 4]); gl = t_("ggl", [128, NCH, 4])
                eG = t_("geG", [64, NCH, 4]); kdsc = t_("gkdsc", [64, NCH, 4]); bexpG = t_("gbexpG", [64, NCH, 4])
                k.act(beta[:], raw[:, :, 4:8], AF.Sigmoid, R=[raw], W=[beta])
                k.ts('dve', nbeta[:], beta[:], -1.0, None, ALU.mult, R=[beta], W=[nbeta])
                k.tt('dve', g[:], raw[:, :, 0:4], bc(dtb[0:64, :].unsqueeze(1), [64, NCH, 4]), ALU.add,
                     R=[raw, dtb], W=[g])
                k.act(g[:], g[:], AF.Exp, R=[g], W=[g])
                k.act(g[:], g[:], AF.Ln, R=[g, cols], W=[g], bias=cols[0:64, 0:1])
                k.tt('dve', g[:], g[:], bc(alog[0:64, :].unsqueeze(1), [64, NCH, 4]), ALU.mult, R=[g, alog], W=[g])
                gf = g[:].rearrange('p c h -> p (c h)')
                ps = psum.get()
                k.mm(ps, ps[:64, :NCH * 4], tri64, gf, R=[cst, g])
                k.copy('dve', G[:].rearrange('p c h -> p (c h)'), ps[:64, :NCH * 4], R=[ps], W=[G])
                ps = psum.get()
                k.mm(ps, ps[:, :NCH * 4], ones64w, gf, R=[cst, g])
                k.copy('dve', Glb[:].rearrange('p c h -> p (c h)'), ps[:, :NCH * 4], R=[ps], W=[Glb])
                k.act(gl[:], Glb[:], AF.Exp, R=[Glb], W=[gl])
                k.act(eG[:], G[:], AF.Exp, R=[G], W=[eG])
                k.tt('dve', kdsc[:], Glb[0:64], G[:], ALU.subtract, R=[Glb, G], W=[kdsc])
                k.act(kdsc[:], kdsc[:], AF.Exp, R=[kdsc], W=[kdsc])
                k.tt('dve', bexpG[:], beta[:], eG[:], ALU.mult, R=[beta, eG], W=[bexpG])
                S = t_("gS", [128, 4, 128])
                k.op('dve', lambda e: e.memset(S[:], 0.0), W=[S])
                cinr = Ring([t_("gcin%d" % i, [128, 12, 515], BF16) for i in range(2)])
                gzr = Ring([t_("ggz%d" % i, [128, 4, 512], BF16) for i in range(2)])
                qkvr = Ring([t_("gqkv%d" % i, [128, 12, 512]) for i in range(2)])
                ctr = Ring([t_("gct%d" % i, [128, 512]) for i in range(2)])
                rsn = t_("grsn", [128, 512])
                ystr = Ring([t_("gyst%d" % i, [128, 4, 512], BF16, multi=True) for i in range(2)])
                WG = 2
                slots = []
                for i in range(WG):
                    d_ = {}
                    for n in ('trig', 't1', 'n1', 'qkd', 'qkT', 'Xa', 'Xb', 'Ya', 'Yb', 'Ra', 'Rb'):
                        d_[n] = t_("g%s%d" % (n, i), [64, 4, 64])
                    for n in ('ktm', 'kbg', 'kdec', 'vtm', 'u', 'vn', 'o', 'sq'):
                        d_[n] = t_("g%s%d" % (n, i), [64, 4, 128])
                    d_['wf'] = t_("gwf%d" % i, [128, 4, 64])
                    d_['ss'] = t_("gss%d" % i, [64, 4])
                    slots.append(d_)
                tiles = {}
                turn = [0]
                done = {}

                def v4(ps, w):
                    return ps[:64, 0:4 * w].rearrange('p (h x) -> p h x', h=4)

                def prep(tt):
                    cin = cinr.get(); gz = gzr.get(); yst = ystr.get(); qkv = qkvr.get()
                    load_halo(cin, C_QKV, 1536, tt)
                    k.dma('sp', gz[:], proj[C_GZ:C_GZ + 512, tt * 512:(tt + 1) * 512].rearrange(
                        '(c p) t -> p c t', p=128), R=[Dproj], W=[gz])
                    conv_silu(cin, cw, 12, qkv, qkv, ctr)
                    for c in range(8):
                        tb = ctr.get()
                        k.act(tb[:], qkv[:, c, :], AF.Square, R=[qkv], W=[tb])
                        ps = psum.get()
                        k.mm(ps, ps[:, :], ones_f, tb[:], R=[cst, tb])
                        rsqrt(rsn, rsn[:], ps, ps[:, :], 1.0)
                        if c < 4:
                            k.stt('dve', qkv[:, c, :], qkv[:, c, :], 128.0 ** -0.5, rsn[:], ALU.mult, ALU.mult,
                                  R=[qkv, rsn], W=[qkv])
                        else:
                            k.tt('dve', qkv[:, c, :], qkv[:, c, :], rsn[:], ALU.mult, R=[qkv, rsn], W=[qkv])
                    tiles[tt] = (qkv, gz, yst)

                def chunk(ch):
                    tt, cc = ch // 8, ch % 8
                    if cc == 0:
                        prep(tt)
                    qkv, gz, yst = tiles[tt]
                    B = slots[ch % WG]
                    cs = slice(cc * 64, cc * 64 + 64)
                    b4 = lambda t, w: bc(t[:, ch, :].unsqueeze(2), [t[:, ch, :].shape[0], 4, w])
                    ktm, vtm, trig, t1, n1, qkd, qkT = B['ktm'], B['vtm'], B['trig'], B['t1'], B['n1'], B['qkd'], B['qkT']
                    ps_k = psum.get()
                    for h in range(4):
                        k.tr(ps_k, ps_k[:64, h * 128:(h + 1) * 128], qkv[:, 4 + h, cs], ident_f, R=[qkv, cst])
                    k.copy('act', ktm[:], v4(ps_k, 128), R=[ps_k], W=[ktm])
                    yield
                    ps_v = psum.get()
                    for h in range(4):
                        k.tr(ps_v, ps_v[:64, h * 128:(h + 1) * 128], qkv[:, 8 + h, cs], ident_f, R=[qkv, cst])
                    k.copy('act', vtm[:], v4(ps_v, 128), R=[ps_v], W=[vtm])
                    yield
                    ps_kk = psum.get()
                    for h in range(4):
                        k.mm(ps_kk, ps_kk[:64, h * 64:(h + 1) * 64], qkv[:, 4 + h, cs], qkv[:, 4 + h, cs], R=[qkv])
                    ps_qk = psum.get()
                    for h in range(4):
                        k.mm(ps_qk, ps_qk[:64, h * 64:(h + 1) * 64], qkv[:, h, cs], qkv[:, 4 + h, cs], R=[qkv])
                    k.tt('pool', trig[:], bc(tri64.unsqueeze(1), [64, 4, 64]), b4(g, 64), ALU.mult,
                         R=[cst, g], W=[trig])
                    ps_g = psum.get()
                    k.mm(ps_g, ps_g[:64, 0:256], ones64, trig[:].rearrange('p h l -> p (h l)'), R=[cst, trig])
                    k.tt('dve', t1[:], v4(ps_g, 64), bc(posm64.unsqueeze(1), [64, 4, 64]), ALU.add,
                         R=[ps_g, cst], W=[t1])
                    k.tt('dve', t1[:], b4(G, 64), t1[:], ALU.subtract, R=[G, t1], W=[t1])
                    k.act(t1[:], t1[:], AF.Exp, R=[t1], W=[t1])
                    X0 = B['Xa']
                    k.tt('dve', n1[:], v4(ps_kk, 64), t1[:], ALU.mult, R=[ps_kk, t1], W=[n1])
                    k.tt('dve', n1[:], n1[:], b4(nbeta, 64), ALU.mult, R=[n1, nbeta], W=[n1])
                    k.tt('pool', X0[:], n1[:], bc(strict64.unsqueeze(1), [64, 4, 64]), ALU.mult,
                         R=[n1, cst], W=[X0])
                    k.tt('dve', qkd[:], v4(ps_qk, 64), t1[:], ALU.mult, R=[ps_qk, t1], W=[qkd])
                    yield
                    ps_y = psum.get()
                    for h in range(4):
                        k.tr(ps_y, ps_y[:64, h * 64:(h + 1) * 64], X0[:, h, :], id64, R=[X0, cst])
                    Y0 = B['Ya']
                    k.copy('act', Y0[:], v4(ps_y, 64), R=[ps_y], W=[Y0])
                    yield
                    ps_q = psum.get()
                    for h in range(4):
                        k.tr(ps_q, ps_q[:64, h * 64:(h + 1) * 64], qkd[:, h, :], id64, R=[qkd, cst])
                    k.copy('act', qkT[:], v4(ps_q, 64), R=[ps_q], W=[qkT])
                    RT = B['Ra']
                    k.tt('pool', RT[:], Y0[:], bc(id64.unsqueeze(1), [64, 4, 64]), ALU.add, R=[Y0, cst], W=[RT])
                    yield
                    Xp, Yp = X0, Y0
                    for kk in range(1, 6):
                        Xn = B['Xb'] if Xp is B['Xa'] else B['Xa']
                        Yn = B['Yb'] if Yp is B['Ya'] else B['Ya']
                        RTn = B['Rb'] if RT is B['Ra'] else B['Ra']
                        ps_a = psum.get()
                        for h in range(4):
                            k.mm(ps_a, ps_a[:64, h * 64:(h + 1) * 64], Yp[:, h, :], Xp[:, h, :], R=[Yp, Xp])
                        if kk <= 4:
                            ps_b = psum.get()
                            for h in range(4):
                                k.mm(ps_b, ps_b[:64, h * 64:(h + 1) * 64], Xp[:, h, :], Yp[:, h, :], R=[Yp, Xp])
                        k.copy('act', Xn[:], v4(ps_a, 64), R=[ps_a], W=[Xn])
                        if kk <= 4:
                            k.copy('dve', Yn[:], v4(ps_b, 64), R=[ps_b], W=[Yn])
                        yield
                        ps_c = psum.get()
                        for h in range(4):
                            k.mm(ps_c, ps_c[:64, h * 64:(h + 1) * 64], Xn[:, h, :], RT[:, h, :], R=[Xn, RT])
                        k.tt('dve', RTn[:], RT[:], v4(ps_c, 64), ALU.add, R=[RT, ps_c], W=[RTn])
                        Xp, Yp, RT = Xn, Yn, RTn
                        yield
                    kbg, kdec, u, wf, vn, o, sq, ss = B['kbg'], B['kdec'], B['u'], B['wf'], B['vn'], B['o'], B['sq'], B['ss']
                    k.tt('pool', vtm[:], vtm[:], b4(beta, 128), ALU.mult, R=[vtm, beta], W=[vtm])
                    k.tt('pool', kbg[:], ktm[:], b4(bexpG, 128), ALU.mult, R=[ktm, bexpG], W=[kbg])
                    k.tt('pool', kdec[:], ktm[:], b4(kdsc, 128), ALU.mult, R=[ktm, kdsc], W=[kdec])
                    ps_u = psum.get()
                    for h in range(4):
                        k.mm(ps_u, ps_u[:64, h * 128:(h + 1) * 128], RT[:, h, :], vtm[:, h, :], R=[RT, vtm])
                    k.copy('act', u[:], v4(ps_u, 128), R=[ps_u], W=[u])
                    yield
                    ps_w = psum.get()
                    for h in range(4):
                        k.mm(ps_w, ps_w[:, h * 64:(h + 1) * 64], kbg[:, h, :], RT[:, h, :], R=[kbg, RT])
                    k.copy('act', wf[:], ps_w[:, 0:256].rearrange('p (h x) -> p h x', h=4), R=[ps_w], W=[wf])
                    yield
                    while turn[0] != ch:
                        yield
                    ps_ws = psum.get()
                    for h in range(4):
                        k.mm(ps_ws, ps_ws[:64, h * 128:(h + 1) * 128], wf[:, h, :], S[:, h, :], R=[wf, S])
                    k.tt('dve', vn[:], u[:], v4(ps_ws, 128), ALU.subtract, R=[u, ps_ws], W=[vn])
                    ps_o1 = psum.get()
                    for h in range(4):
                        k.mm(ps_o1, ps_o1[:64, h * 128:(h + 1) * 128], qkv[:, h, cs], S[:, h, :], R=[qkv, S])
                    ps_ds = psum.get()
                    for h in range(4):
                        k.mm(ps_ds, ps_ds[:, h * 128:(h + 1) * 128], kdec[:, h, :], vn[:, h, :], R=[kdec, vn])
                    k.tt('dve', S[:], S[:], bc(gl[:, ch, :].unsqueeze(2), [128, 4, 128]), ALU.mult,
                         R=[S, gl], W=[S])
                    k.tt('dve', S[:], S[:], ps_ds[:, :].rearrange('p (h x) -> p h x', h=4), ALU.add,
                         R=[S, ps_ds], W=[S])
                    turn[0] = ch + 1
                    ps_o2 = psum.get()
                    for h in range(4):
                        k.mm(ps_o2, ps_o2[:64, h * 128:(h + 1) * 128], qkT[:, h, :], vn[:, h, :], R=[qkT, vn])
                    k.tt('dve', o[:], v4(ps_o1, 128), b4(eG, 128), ALU.mult, R=[ps_o1, eG], W=[o])
                    k.tt('dve', o[:], o[:], v4(ps_o2, 128), ALU.add, R=[o, ps_o2], W=[o])
                    yield
                    k.tt('pool', sq[:], o[:], o[:], ALU.mult, R=[o], W=[sq])
                    k.op('dve', lambda e: e.reduce_sum(ss[:], sq[:], AX.X), R=[sq], W=[ss])
                    rsqrt(ss, ss[:], ss, ss[:], 1.0 / 128)
                    k.tt('dve', sq[:], o[:], bc(ss[:, :].unsqueeze(2), [64, 4, 128]), ALU.mult, R=[o, ss], W=[sq])
                    yield
                    ps_t = psum.get()
                    for h in range(4):
                        k.tr(ps_t, ps_t[:, h * 64:(h + 1) * 64], sq[:, h, :], id64, R=[sq, cst])
                    k.stt('dve', yst[:, :, cs], ps_t[:, 0:256].rearrange('p (h x) -> p h x', h=4), nw[:, 0:1],
                          gz[:, :, cs], ALU.mult, ALU.mult, R=[ps_t, nw, gz], W=[yst])

                def chunk_done(ch):
                    tt = ch // 8
                    done[tt] = done.get(tt, 0) + 1
                    if done[tt] == 8:
                        yst = tiles[tt][2]
                        k.dma('sp', y_d[0][:, tt * 512:(tt + 1) * 512].rearrange('(c p) t -> p c t', p=128), yst[:],
                              R=[yst], W=[Dy[0]])

                run_pipeline(list(range(NCH)), WG, chunk, chunk_done)
                k.barrier()

        def phase_mixers(l):
            if 'mla' in mixers:
                phase_mla(l)
            if 'ssd' in mixers:
                phase_ssd(l)
            if 'gdn' in mixers:
                phase_gdn(l)

        def phase_merge(l, xsrc, Dxsrc, xdst, Dxdst):
            with ExitStack() as ps_:
                wb = [sb("wbr%d" % i, [128, 4, 1024], BF16, stack=ps_) for i in range(3)]
                wo = sb("wo", [128, 8, 1024], BF16, stack=ps_)
                for i in range(3):
                    k.dma('pool', wb[i][:], w_br[i][l].rearrange('(kc p) n -> p kc n', p=128), R=[Dw], W=[wb[i]])
                k.dma('pool', wo[:], w_out[l].rearrange('(kc p) n -> p kc n', p=128), R=[Dw], W=[wo])
                yr = Ring([sb("ym%d" % i, [128, 3, 4, 512], BF16, stack=ps_) for i in range(2)])
                gr = Ring([sb("gm%d" % i, [128, 24, 512], BF16, stack=ps_) for i in range(2)])
                xr = Ring([sb("xm%d" % i, [128, 8, 512], F32, stack=ps_) for i in range(2)])
                mg = Ring([sb("mg%d" % i, [128, 8, 512], BF16, stack=ps_) for i in range(2)])
                tmp = Ring([sb("mt%d" % i, [128, 512], F32, stack=ps_) for i in range(3)])
                for tt in range(NT):
                    ts_ = slice(tt * 512, (tt + 1) * 512)
                    y = yr.get()
                    g = gr.get()
                    xt = xr.get()
                    m_ = mg.get()
                    for i in range(3):
                        k.dma('sp', y[:, i], y_d[i][:, ts_].rearrange('(kc p) t -> p kc t', p=128), R=[Dy[i]], W=[y])
                    k.dma('sp', g[:], proj[C_GATES:C_GATES + 3072, ts_].rearrange('(kc p) t -> p kc t', p=128),
                          R=[Dproj], W=[g])
                    k.dma('sp', xt[:], xsrc[:, ts_].rearrange('(kc p) t -> p kc t', p=128), R=[Dxsrc], W=[xt])
                    for o in range(8):
                        tl = []
                        for i in range(3):
                            ps = psum.get()
                            for kc in range(4):
                                k.mm(ps, ps[:, :], wb[i][:, kc, o * 128:(o + 1) * 128], y[:, i, kc, :], R=[wb[i], y],
                                     start=(kc == 0), stop=(kc == 3))
                            t_ = tmp.get()
                            k.tt('dve', t_[:], ps[:, :], g[:, i * 8 + o, :], ALU.mult, R=[ps, g], W=[t_])
                            tl.append(t_)
                        k.tt('pool', tl[0][:], tl[0][:], tl[1][:], ALU.add, R=[tl[0], tl[1]], W=[tl[0]])
                        k.tt('pool', m_[:, o, :], tl[0][:], tl[2][:], ALU.add, R=[tl[0], tl[2]], W=[m_])
                    for o in range(8):
                        ps = psum.get()
                        for kc in range(8):
                            k.mm(ps, ps[:, :], wo[:, kc, o * 128:(o + 1) * 128], m_[:, kc, :], R=[wo, m_],
                                 start=(kc == 0), stop=(kc == 7))
                        k.stt('dve', xt[:, o, :], ps[:, :], modT[:, 16 + o:17 + o], xt[:, o, :], ALU.mult, ALU.add,
                              R=[ps, modT, xt], W=[xt])
                    k.dma('sp', xdst[:, ts_].rearrange('(kc p) t -> p kc t', p=128), xt[:], R=[xt], W=[Dxdst])
                k.barrier()

        def phase_ffn(l, xsrc, Dxsrc, xdst, Dxdst):
            moe = (l % 2 == 1)
            li = l // 2
            TT = min(T, 1024)
            NS = TT // 512
            HCMAX = 14
            with ExitStack() as ps_:
                h2 = sb("h2", [128, 8, TT], BF16, multi=True, stack=ps_)
                xacc = sb("xacc", [128, 8, TT], F32, multi=True, stack=ps_)
                sq = sb("sq2", [128, 8, 512], F32, stack=ps_)
                hid = sb("hid", [128, HCMAX, TT], BF16, multi=True, stack=ps_)
                wgr = Ring([sb("wg%d" % i, [128, 8, 256], BF16, stack=ps_) for i in range(2)])
                wur = Ring([sb("wu%d" % i, [128, 8, 256], BF16, stack=ps_) for i in range(2)])
                wdr = Ring([sb("wd%d" % i, [128, HCMAX, 512], BF16, stack=ps_) for i in range(1)])
                sgr = Ring([sb("sg%d" % i, [128, 512], BF16, stack=ps_) for i in range(3)])
                tmp = Ring([sb("ft%d" % i, [128, 512], F32, stack=ps_) for i in range(2)])
                if moe:
                    hf = sb("hf2", [128, 8, 512], F32, stack=ps_)
                    rt = sb("rt", [128, 8, NEXP], F32, stack=ps_)
                    k.dma('sp', rt[:], moe_router[li].rearrange('(kc p) n -> p kc n', p=128), R=[Dw], W=[rt])
                    wrow = sb("wrow", [128, NEXP, TT], BF16, multi=True, stack=ps_)
                    sm = [sb("rs%d" % i, [128, 8], F32, stack=ps_) for i in range(6)]
                    sc = [sb("rc%d" % i, [128, 1], F32, stack=ps_) for i in range(4)]
                    dg = sb("dg", [128, NEXP, 128], F32, stack=ps_)
                for st_ in range(T // TT):
                    t0 = st_ * TT
                    for s in range(NS):
                        sl = slice(s * 512, (s + 1) * 512)
                        k.dma('sp', xacc[:, :, sl],
                              xsrc[:, t0 + s * 512:t0 + (s + 1) * 512].rearrange('(kc p) t -> p kc t', p=128),
                              R=[Dxsrc], W=[xacc])
                        norm_tile(xacc, xacc[:, :, sl], sq, lambda kc: h2[:, kc, sl], h2, 32, 24,
                                  hf=([hf] if moe else None))
                        if moe:
                            for q in range(4):
                                lg, m1, m2, e1, e2, wt8 = sm
                                ps = psum.get()
                                for kc in range(8):
                                    k.mm(ps, ps[:, 0:8], hf[:, kc, q * 128:(q + 1) * 128], rt[:, kc, :], R=[hf, rt],
                                         start=(kc == 0), stop=(kc == 7))
                                k.copy('dve', lg[:], ps[:, 0:8], R=[ps], W=[lg])
                                k.op('dve', lambda e: e.reduce_max(sc[0][:], lg[:], AX.X), R=[lg], W=[sc[0]])
                                k.ts('dve', e1[:], lg[:], sc[0][:, 0:1], None, ALU.is_equal, R=[lg, sc[0]], W=[e1])
                                k.stt('dve', m1[:], e1[:], -1e30, lg[:], ALU.mult, ALU.add, R=[e1, lg], W=[m1])
                                k.op('dve', lambda e: e.reduce_max(sc[1][:], m1[:], AX.X), R=[m1], W=[sc[1]])
                                k.ts('dve', e2[:], m1[:], sc[1][:, 0:1], None, ALU.is_equal, R=[m1, sc[1]], W=[e2])
                                k.tt('dve', sc[2][:], sc[0][:], sc[1][:], ALU.subtract, R=[sc[0], sc[1]], W=[sc[2]])
                                k.act(sc[3][:], sc[2][:], AF.Sigmoid, R=[sc[2]], W=[sc[3]])
                                k.act(sc[2][:], sc[2][:], AF.Sigmoid, R=[sc[2]], W=[sc[2]], scale=-1.0)
                                k.ts('dve', wt8[:], e1[:], sc[3][:, 0:1], None, ALU.mult, R=[e1, sc[3]], W=[wt8])
                                k.stt('dve', wt8[:], e2[:], sc[2][:, 0:1], wt8[:], ALU.mult, ALU.add,
                                      R=[e2, sc[2], wt8], W=[wt8])
                                k.tt('dve', dg[:], cst[:, 0:1, :].to_broadcast([128, NEXP, 128]),
                                     wt8[:, :].unsqueeze(2).to_broadcast([128, NEXP, 128]), ALU.mult,
                                     R=[cst, wt8], W=[dg])
                                for hh in range(2):
                                    ps2 = psum.get()
                                    k.mm(ps2, ps2[:, :], ones_f, dg[:, hh * 4:(hh + 1) * 4, :].rearrange('p e t -> p (e t)'), R=[cst, dg])
                                    c0 = s * 512 + q * 128
                                    k.copy('act', wrow[:, hh * 4:(hh + 1) * 4, c0:c0 + 128],
                                           ps2[:, :].rearrange('p (e t) -> p e t', e=4), R=[ps2], W=[wrow])
                    if moe:
                        passes = []
                        for e_ in range(NEXP):
                            for hh in range(2):
                                passes.append((moe_wg[li, e_], moe_wu[li, e_], moe_wd[li, e_], hh * 1792, 1792, e_))
                    else:
                        passes = [(ffn_wg[li], ffn_wu[li], ffn_wd[li], hh * 1408, 1408, None) for hh in range(2)]
                    for (wg_, wu_, wd_, h0, hn, ex) in passes:
                        HC = hn // 128
                        for cb in range(0, hn, 256):
                            cw = min(256, hn - cb)
                            wg = wgr.get()
                            wu = wur.get()
                            k.dma('pool', wg[:, :, :cw],
                                  wg_[:, h0 + cb:h0 + cb + cw].rearrange('(kc p) n -> p kc n', p=128), R=[Dw], W=[wg])
                            k.dma('pool', wu[:, :, :cw],
                                  wu_[:, h0 + cb:h0 + cb + cw].rearrange('(kc p) n -> p kc n', p=128), R=[Dw], W=[wu])
                            for jj in range(cw // 128):
                                j = cb // 128 + jj
                                for s in range(NS):
                                    sl = slice(s * 512, (s + 1) * 512)
                                    psg = psum.get()
                                    for kc in range(8):
                                        k.mm(psg, psg[:, :], wg[:, kc, jj * 128:(jj + 1) * 128], h2[:, kc, sl],
                                             R=[wg, h2], start=(kc == 0), stop=(kc == 7))
                                    psu = psum.get()
                                    for kc in range(8):
                                        k.mm(psu, psu[:, :], wu[:, kc, jj * 128:(jj + 1) * 128], h2[:, kc, sl],
                                             R=[wu, h2], start=(kc == 0), stop=(kc == 7))
                                    sg = sgr.get()
                                    k.act(sg[:], psg[:, :], AF.Silu, R=[psg], W=[sg])
                                    k.tt('dve', hid[:, j, sl], psu[:, :], sg[:], ALU.mult, R=[psu, sg], W=[hid])
                        for oh in range(2):
                            wd = wdr.get()
                            for c4 in range(0, HC, 7):
                                cn = min(7, HC - c4)
                                k.dma('pool', wd[:, c4:c4 + cn, :],
                                      wd_[h0 + c4 * 128:h0 + (c4 + cn) * 128, oh * 512:(oh + 1) * 512].rearrange(
                                          '(kc p) n -> p kc n', p=128), R=[Dw], W=[wd])
                            for oo in range(4):
                                o = oh * 4 + oo
                                for s in range(NS):
                                    sl = slice(s * 512, (s + 1) * 512)
                                    ps = psum.get()
                                    for j in range(HC):
                                        k.mm(ps, ps[:, :], wd[:, j, oo * 128:(oo + 1) * 128], hid[:, j, sl],
                                             R=[wd, hid], start=(j == 0), stop=(j == HC - 1))
                                    if ex is None:
                                        k.stt('dve', xacc[:, o, sl], ps[:, :], modT[:, 40 + o:41 + o], xacc[:, o, sl],
                                              ALU.mult, ALU.add, R=[ps, modT, xacc], W=[xacc])
                                    else:
                                        t_ = tmp.get()
                                        k.stt('dve', t_[:], ps[:, :], modT[:, 40 + o:41 + o], wrow[:, ex, sl],
                                              ALU.mult, ALU.mult, R=[ps, modT, wrow], W=[t_])
                                        k.tt('pool', xacc[:, o, sl], xacc[:, o, sl], t_[:], ALU.add,
                                             R=[t_, xacc], W=[xacc])
                    k.dma('sp', xdst[:, t0:t0 + TT].rearrange('(kc p) t -> p kc t', p=128), xacc[:], R=[xacc],
                          W=[Dxdst])
                k.barrier()

        def phase_final(xsrc, Dxsrc):
            with ExitStack() as ps_:
                xr = Ring([sb("xf%d" % i, [128, 8, 512], F32, stack=ps_) for i in range(2)])
                orr = Ring([sb("of%d" % i, [128, 8, 512], F32, multi=True, stack=ps_) for i in range(2)])
                sq = sb("sqf", [128, 8, 512], F32, stack=ps_)
                for tt in range(NT):
                    ts_ = slice(tt * 512, (tt + 1) * 512)
                    xt = xr.get()
                    ot = orr.get()
                    k.dma('sp', xt[:], xsrc[:, ts_].rearrange('(kc p) t -> p kc t', p=128), R=[Dxsrc], W=[xt])
                    norm_tile(xt, xt[:], sq, lambda kc: ot[:, kc, :], ot, None, None)
                    k.dma('sp', outT[:, ts_].rearrange('(kc p) t -> p kc t', p=128), ot[:], R=[ot], W=[Dout])
                k.barrier()

        cur, Dcur = xT_in, Dxin
        for l in layers:
            phase_ada(l)
            if 'mix' in stages:
                phase_inproj(l, cur, Dcur)
                phase_mixers(l)
                phase_merge(l, cur, Dcur, x_b, Dx_b)
                cur, Dcur = x_b, Dx_b
            if 'ffn' in stages:
                phase_ffn(l, cur, Dcur, x_a, Dx_a)
                cur, Dcur = x_a, Dx_a
        phase_final(cur, Dcur)
        k.barrier()
        print("instructions:", k.ninst, {kk: v for kk, v in k.cnt.items()})
    return nc


def _consts():
    c = np.zeros((128, 8, 128), np.float32)
    i = np.arange(128)
    c[:, 0, :] = np.eye(128)
    c[:, 1, :] = 1.0
    c[:, 2, :] = (i[:, None] <= i[None, :])
    c[:, 3, :] = np.where(i[None, :] > i[:, None], 1e9, 0.0)
    c[:, 4, :] = (i[None, :] < i[:, None])
    rot = np.zeros((128, 128), np.float32)
    for o in (0, 64):
        for m in range(32):
            rot[o + m + 32, o + m] = -1.0
            rot[o + m, o + m + 32] = 1.0
    c[:, 5, :] = rot
    c[:, 6, :] = np.where(i[None, :] < i[:, None], 1e9, 0.0)
    return c


def _fm(v, nchunk):
    return np.ascontiguousarray(np.asarray(v, np.float32).reshape(nchunk, 128).T)


def prep_inputs(inp, b, T):
    f = lambda a: np.ascontiguousarray(np.asarray(a, np.float32))
    L = DEPTH
    m = {}
    m["xT"] = np.ascontiguousarray(np.asarray(inp["x"][b, :T], np.float32).T)
    m["cT"] = _fm(inp["c"][b], 8)
    m["pos"] = np.ascontiguousarray(np.asarray(inp["positions"][b, :T], np.int32).reshape(1, T))
    m["w_ada"] = f(inp["w_ada"])
    m["b_adaT"] = np.stack([_fm(inp["b_ada"][l], 48) for l in range(L)])
    m["w_in"] = f(inp["w_in"])
    gc = np.asarray(inp["gdn_conv_w"], np.float32)
    m["gdn_convT"] = np.ascontiguousarray(gc.reshape(L, 4, 12, 128).transpose(0, 3, 2, 1))
    rep = lambda a: np.ascontiguousarray(np.broadcast_to(np.asarray(a, np.float32)[:, None, :], (L, 128, a.shape[-1])))
    m["gdn_alog"] = rep(inp["gdn_a_log"])
    m["gdn_dtb"] = rep(inp["gdn_dt_bias"])
    m["gdn_nw"] = np.ascontiguousarray(np.asarray(inp["gdn_norm_w"], np.float32).reshape(L, 128, 1))
    sc = np.asarray(inp["ssm_conv_w"], np.float32)
    m["ssm_convT"] = np.ascontiguousarray(sc.reshape(L, 4, 8, 128).transpose(0, 3, 2, 1))
    m["ssm_convb"] = np.stack([_fm(inp["ssm_conv_b"][l], 8) for l in range(L)])
    m["ssm_alog"] = rep(inp["ssm_a_log"])
    m["ssm_dtb"] = rep(inp["ssm_dt_bias"])
    dexp = np.repeat(np.asarray(inp["ssm_d"], np.float32), 64, axis=1)
    m["ssm_dexp"] = np.stack([_fm(dexp[l], 4) for l in range(L)])
    m["ssm_nw"] = np.stack([_fm(inp["ssm_norm_w"][l], 4) for l in range(L)])
    m["mla_qnw"] = np.stack([_fm(inp["mla_q_norm_w"][l], 4) for l in range(L)])
    m["mla_wuq"] = f(inp["mla_w_uq"])
    m["mla_kvnw"] = np.stack([_fm(inp["mla_kv_norm_w"][l], 2) for l in range(L)])
    m["mla_wuk"] = f(inp["mla_w_uk"])
    m["mla_wuv"] = f(inp["mla_w_uv"])
    for n in ("w_branch_a", "w_branch_b", "w_branch_c", "w_out", "ffn_w_gate", "ffn_w_up", "ffn_w_down",
              "moe_router", "moe_w_gate", "moe_w_up", "moe_w_down"):
        m[n] = f(inp[n])
    m["fnwT"] = _fm(inp["final_norm_w"], 8)
    m["consts"] = _consts()
    invf = (10000.0 ** (-np.arange(0, 64, 2, dtype=np.float32) / 64)).astype(np.float32)
    m["invf"] = np.concatenate([invf] * 4).reshape(128, 1).astype(np.float32)
    return m


def kernel(**inputs):
    B, T = inputs["x"].shape[0], inputs["x"].shape[1]
    nc = build_program(T, list(range(DEPTH)))
    in_maps = [prep_inputs(inputs, b, T) for b in range(B)]
    res = run_bass_kernel_spmd(nc, in_maps, core_ids=list(range(B)))
    out = np.stack([np.asarray(res.results[b]["outT"], np.float32).T for b in range(B)])
    return np.ascontiguousarray(out)
```

```python
import numpy as np
from contextlib import ExitStack
import concourse.bass as bass
import concourse.mybir as mybir
from concourse.bass_utils import run_bass_kernel_spmd

F32, BF16, I32 = mybir.dt.float32, mybir.dt.bfloat16, mybir.dt.int32
AF = mybir.ActivationFunctionType
ALU = mybir.AluOpType
AX = mybir.AxisListType

D = 1024
DEPTH = 4
EPS = 1e-6
IN_DIM = 7504
FFN_DIM = 2816
EXPERT_DIM = 3584
NEXP = 8
SAME_SYNC = True

C_QKV, C_GZ, C_A, C_B, C_SZ, C_XBC, C_DT, C_CQ, C_CKV, C_KR, C_GATES = (
    0, 1536, 2048, 2052, 2056, 2568, 3592, 3600, 4112, 4368, 4432)


class Buf:
    def __init__(self, t, multi=False):
        self.t = t
        self.multi = multi
        self.w = {}
        self.r = {}

    def __getitem__(self, key):
        return self.t[key]


def _merge(d, tok):
    k_, v = tok
    if d.get(k_, 0) < v:
        d[k_] = v


class Ring:
    def __init__(self, bufs):
        self.bufs = bufs
        self.i = 0

    def get(self):
        b = self.bufs[self.i % len(self.bufs)]
        self.i += 1
        return b


class KB:
    ENG = ('pe', 'act', 'dve', 'pool', 'sp')

    def __init__(self, nc, es):
        self.nc = nc
        self.es = es
        self.e = {'pe': nc.tensor, 'act': nc.scalar, 'dve': nc.vector, 'pool': nc.gpsimd, 'sp': nc.sync}
        self.sem = {}
        self.cnt = {}
        for e in self.ENG:
            self.sem[('e', e)] = es.enter_context(nc.semaphore('se_' + e))
            self.cnt[('e', e)] = 0
        self.NS = 8
        self.dma_i = {}
        for q in ('sp', 'pool'):
            self.dma_i[q] = 0
            for j in range(self.NS):
                self.sem[('d', q, j)] = es.enter_context(nc.semaphore('sd_%s%d' % (q, j)))
                self.cnt[('d', q, j)] = 0
        self.seen = {e: {} for e in self.ENG}
        self.ninst = 0

    def _wait(self, eng, deps):
        for key, v in deps.items():
            if key == ('e', eng) and (eng == 'pe' or eng == 'sp' or not SAME_SYNC):
                continue
            if self.seen[eng].get(key, 0) >= v:
                continue
            self.e[eng].wait_ge(self.sem[key], v)
            self.seen[eng][key] = v
            self.ninst += 1

    def _deps(self, R, W):
        deps = {}
        for b in R:
            for t in b.w.items():
                _merge(deps, t)
        for b in W:
            for t in b.r.items():
                _merge(deps, t)
            if not b.multi:
                for t in b.w.items():
                    _merge(deps, t)
        return deps

    def _post(self, tok, R, W):
        for b in R:
            _merge(b.r, tok)
        for b in W:
            if b.multi:
                _merge(b.w, tok)
            else:
                b.w = {tok[0]: tok[1]}
                b.r = {}

    def op(self, eng, fn, R=(), W=()):
        self._wait(eng, self._deps(R, W))
        ins = fn(self.e[eng])
        key = ('e', eng)
        self.cnt[key] += 1
        ins.then_inc(self.sem[key], 1)
        self.ninst += 1
        self._post((key, self.cnt[key]), R, W)

    def dma(self, q, out, in_, R=(), W=()):
        self._wait(q, self._deps(R, W))
        key = ('d', q, self.dma_i[q] % self.NS)
        self.dma_i[q] += 1
        if self.cnt[key] > 0:
            self._wait(q, {key: self.cnt[key]})
        ins = self.e[q].dma_start(out=out, in_=in_)
        self.cnt[key] += 16
        ins.then_inc(self.sem[key], 16)
        self.ninst += 1
        self._post((key, self.cnt[key]), R, W)

    def barrier(self):
        for e in self.ENG:
            deps = {key: v for key, v in self.cnt.items() if v > 0 and key != ('e', e)}
            self._wait(e, deps)

    def mm(self, ps, out, lhsT, rhs, R, start=True, stop=True):
        self.op('pe', lambda e: e.matmul(out, lhsT, rhs, start=start, stop=stop), R=R, W=[ps])

    def tr(self, ps, out, in_, ident, R):
        self.op('pe', lambda e: e.transpose(out, in_, ident), R=R, W=[ps])

    def act(self, out, in_, func, R, W, bias=None, scale=None, accum_out=None, eng='act'):
        kw = {}
        if bias is not None:
            kw['bias'] = bias
        if scale is not None:
            kw['scale'] = scale
        if accum_out is not None:
            kw['accum_out'] = accum_out
        self.op('act', lambda e: e.activation(out=out, in_=in_, func=func, **kw), R=R, W=W)

    def tt(self, eng, out, in0, in1, op, R, W):
        self.op(eng, lambda e: e.tensor_tensor(out, in0, in1, op), R=R, W=W)

    def ts(self, eng, out, in0, s1, s2, op0, op1=None, R=(), W=()):
        if op1 is None:
            self.op(eng, lambda e: e.tensor_scalar(out, in0, s1, None, op0), R=R, W=W)
        else:
            self.op(eng, lambda e: e.tensor_scalar(out, in0, s1, s2, op0, op1), R=R, W=W)

    def stt(self, eng, out, in0, scalar, in1, op0, op1, R, W):
        self.op(eng, lambda e: e.scalar_tensor_tensor(out, in0, scalar, in1, op0, op1), R=R, W=W)

    def copy(self, eng, out, in_, R, W):
        if eng == 'act':
            self.op('act', lambda e: e.activation(out=out, in_=in_, func=AF.Copy), R=R, W=W)
        else:
            self.op(eng, lambda e: e.tensor_copy(out, in_), R=R, W=W)


def build_program(T, layers, debug=False, stages=('mix', 'ffn'), mixers=('mla', 'ssd', 'gdn'), pair=False, ncores=8):
    nc = bass.Bass("TRN2", target_bir_lowering=False)
    L = DEPTH
    NT = T // 512
    NQ = T // 128
    NCH = T // 64
    TH = T // 2 if pair else T

    def din(name, shape, dt=F32):
        return nc.dram_tensor(name, list(shape), dt, kind="ExternalInput").ap()

    def dscr(name, shape, dt, out=False):
        kind = "ExternalOutput" if out else "Internal"
        return nc.dram_tensor(name, list(shape), dt, kind=kind).ap()

    xT_in = din("xT", [D, T])
    cT_in = din("cT", [128, 8])
    pos_in = din("pos", [1, T], I32)
    w_ada = din("w_ada", [L, D, 6 * D])
    b_adaT = din("b_adaT", [L, 128, 48])
    w_in = din("w_in", [L, D, IN_DIM])
    gdn_convT = din("gdn_convT", [L, 128, 12, 4])
    gdn_alog = din("gdn_alog", [L, 128, 4])
    gdn_dtb = din("gdn_dtb", [L, 128, 4])
    gdn_nw = din("gdn_nw", [L, 128, 1])
    ssm_convT = din("ssm_convT", [L, 128, 8, 4])
    ssm_convb = din("ssm_convb", [L, 128, 8])
    ssm_alog = din("ssm_alog", [L, 128, 8])
    ssm_dtb = din("ssm_dtb", [L, 128, 8])
    ssm_dexp = din("ssm_dexp", [L, 128, 4])
    ssm_nw = din("ssm_nw", [L, 128, 4])
    mla_qnw = din("mla_qnw", [L, 128, 4])
    mla_wuq = din("mla_wuq", [L, 512, 768])
    mla_kvnw = din("mla_kvnw", [L, 128, 2])
    mla_wuk = din("mla_wuk", [L, 256, 512])
    mla_wuv = din("mla_wuv", [L, 256, 512])
    w_br = [din("w_branch_a", [L, 512, D]), din("w_branch_b", [L, 512, D]), din("w_branch_c", [L, 512, D])]
    w_out = din("w_out", [L, D, D])
    ffn_wg = din("ffn_w_gate", [2, D, FFN_DIM])
    ffn_wu = din("ffn_w_up", [2, D, FFN_DIM])
    ffn_wd = din("ffn_w_down", [2, FFN_DIM, D])
    moe_router = din("moe_router", [2, D, NEXP])
    moe_wg = din("moe_w_gate", [2, NEXP, D, EXPERT_DIM])
    moe_wu = din("moe_w_up", [2, NEXP, D, EXPERT_DIM])
    moe_wd = din("moe_w_down", [2, NEXP, EXPERT_DIM, D])
    fnwT = din("fnwT", [128, 8])
    consts = din("consts", [128, 8, 128])
    invf_in = din("invf", [128, 1])

    outT = dscr("outT", [D, TH], F32, out=True)
    rsel_in = din("rsel", [128, 2])
    xh = dscr("xh", [D, TH], F32)
    xg = dscr("xg", [2 * D, TH], F32)
    x_a = dscr("x_a", [D, T], F32, out=debug)
    x_b = dscr("x_b", [D, T], F32, out=debug)
    proj = dscr("proj", [IN_DIM, T], BF16, out=debug)
    abdt = dscr("abdt", [T, 16], F32, out=debug)
    y_d = [dscr("y_a", [512, T], BF16, out=debug), dscr("y_b", [512, T], BF16, out=debug),
           dscr("y_c", [512, T], BF16, out=debug)]

    es = ExitStack()
    with es:
        k = KB(nc, es)

        uid = [0]

        def sb(name, shape, dt, multi=False, stack=es):
            uid[0] += 1
            return Buf(stack.enter_context(nc.sbuf_tensor("%s_%d" % (name, uid[0]), list(shape), dt)), multi=multi)

        Dx_a, Dx_b, Dproj, Dabdt = Buf(x_a, True), Buf(x_b, True), Buf(proj, True), Buf(abdt, True)
        Dy = [Buf(y, True) for y in y_d]
        Dout = Buf(outT, True)
        Dxh, Dxg = Buf(xh, True), Buf(xg, True)
        rsel = sb("rsel", [128, 2], F32)
        k.dma('sp', rsel[:], rsel_in, R=[Buf(None)], W=[rsel])
        Dw = Buf(None)
        Dxin = Buf(xT_in)

        psum = Ring([Buf(es.enter_context(nc.psum_tensor("ps%d" % i, [128, 512], F32))) for i in range(6)])
        psacc = Ring([Buf(es.enter_context(nc.psum_tensor("pa%d" % i, [128, 512], F32))) for i in range(2)])

        cst = sb("cst", [128, 8, 128], F32)
        k.dma('sp', cst[:], consts, R=[Dw], W=[cst])
        ident_f = cst[:, 0, :]
        ones_f = cst[:, 1, :]
        cst_b = sb("cst_b", [128, 2, 128], BF16)
        k.copy('dve', cst_b[:], cst[:, 0:2, :], R=[cst], W=[cst_b])
        ident_b = cst_b[:, 0, :]
        condT = sb("condT", [128, 8], F32)
        k.dma('sp', condT[:], cT_in, R=[Dw], W=[condT])
        k.act(condT[:], condT[:], AF.Silu, R=[condT], W=[condT])
        modT = sb("modT", [128, 48], F32)
        fnw = sb("fnw", [128, 8], F32)
        k.dma('sp', fnw[:], fnwT, R=[Dw], W=[fnw])

        def phase_ada(l):
            with ExitStack() as ps_:
                wr = Ring([sb("wada%d" % i, [128, 8, 768], F32, stack=ps_) for i in range(2)])
                bt = sb("badat", [128, 48], F32, stack=ps_)
                k.dma('sp', bt[:], b_adaT[l], R=[Dw], W=[bt])
                ps = psum.get()
                for g in range(8):
                    wb = wr.get()
                    k.dma('sp', wb[:], w_ada[l][:, g * 768:(g + 1) * 768].rearrange('(kc p) n -> p kc n', p=128),
                          R=[Dw], W=[wb])
                    for jj in range(6):
                        j = g * 6 + jj
                        for kc in range(8):
                            k.mm(ps, ps[:, j:j + 1], wb[:, kc, jj * 128:(jj + 1) * 128], condT[:, kc:kc + 1],
                                 R=[wb, condT], start=(kc == 0), stop=(kc == 7))
                k.tt('dve', modT[:], ps[:, 0:48], bt[:], ALU.add, R=[ps, bt], W=[modT])
                k.ts('dve', modT[:, 8:16], modT[:, 8:16], 1.0, None, ALU.add, R=[modT], W=[modT])
                k.ts('dve', modT[:, 32:40], modT[:, 32:40], 1.0, None, ALU.add, R=[modT], W=[modT])
                k.barrier()

        def norm_tile(xt, xap, sq, hdst, hbuf, sc_off, sh_off, hf=None):
            k.act(sq[:], xap, AF.Square, R=[xt], W=[sq])
            ps = psum.get()
            for kc in range(8):
                k.mm(ps, ps[:, :], ones_f, sq[:, kc, :], R=[sq, cst], start=(kc == 0), stop=(kc == 7))
            rstd = rstd_ring.get()
            rsqrt(rstd, rstd[:], ps, ps[:, :], 1.0 / D)
            for kc in range(8):
                k.tt('pool' if kc % 2 else 'dve', sq[:, kc, :], xap[:, kc, :], rstd[:], ALU.mult,
                     R=[xt, rstd], W=[sq])
            for kc in range(8):
                if sc_off is None:
                    k.ts('dve', hdst(kc), sq[:, kc, :], fnw[:, kc:kc + 1], None, ALU.mult, R=[sq, fnw], W=[hbuf])
                else:
                    k.ts('dve', hdst(kc), sq[:, kc, :], modT[:, sc_off + kc:sc_off + kc + 1],
                         modT[:, sh_off + kc:sh_off + kc + 1], ALU.mult, ALU.add, R=[sq, modT], W=[hbuf])
                    if hf is not None:
                        k.ts('pool', hf[0][:, kc, :], sq[:, kc, :], modT[:, sc_off + kc:sc_off + kc + 1],
                             modT[:, sh_off + kc:sh_off + kc + 1], ALU.mult, ALU.add, R=[sq, modT], W=[hf[0]])

        epsT = sb("epsT", [128, 1], F32)
        k.op('dve', lambda e: e.memset(epsT[:], EPS), W=[epsT])

        def rsqrt(ob, out, ib, in_, scale):
            k.act(out, in_, AF.Sqrt, R=[ib, epsT], W=[ob], bias=epsT[:out.shape[0], 0:1], scale=scale)
            k.op('dve', lambda e: e.reciprocal(out, out), R=[ob], W=[ob])

        rstd_ring = Ring([sb("rstd%d" % i, [128, 512], F32) for i in range(2)])

        def phase_inproj(l, xsrc, Dxsrc):
            with ExitStack() as ps_:
                h1 = sb("h1", [128, 8, T], BF16, multi=True, stack=ps_)
                xr = Ring([sb("xt%d" % i, [128, 8, 512], F32, stack=ps_) for i in range(2)])
                sq = sb("sq", [128, 8, 512], F32, stack=ps_)
                hf = sb("hf", [128, 8, 512], F32, stack=ps_)
                wsm = sb("wsm", [128, 8, 16], F32, stack=ps_)
                sm_st = Ring([sb("smst%d" % i, [128, 16], F32, stack=ps_) for i in range(2)])
                k.dma('sp', wsm[:, :, 0:8], w_in[l][:, C_A:C_A + 8].rearrange('(kc p) n -> p kc n', p=128),
                      R=[Dw], W=[wsm])
                k.dma('sp', wsm[:, :, 8:16], w_in[l][:, C_DT:C_DT + 8].rearrange('(kc p) n -> p kc n', p=128),
                      R=[Dw], W=[wsm])
                for tt in range(NT):
                    xt = xr.get()
                    for (a_, b_, sap) in xsrc(tt):
                        k.dma('sp', xt[:, a_:b_, :], sap, R=[Dxsrc], W=[xt])
                    norm_tile(xt, xt[:], sq, lambda kc: h1[:, kc, tt * 512:(tt + 1) * 512], h1, 8, 0, hf=[hf])
                    for q in range(4):
                        ps = psum.get()
                        for kc in range(8):
                            k.mm(ps, ps[:, 0:16], hf[:, kc, q * 128:(q + 1) * 128], wsm[:, kc, :], R=[hf, wsm],
                                 start=(kc == 0), stop=(kc == 7))
                        st = sm_st.get()
                        k.copy('act', st[:], ps[:, 0:16], R=[ps], W=[st])
                        t0 = tt * 512 + q * 128
                        k.dma('sp', abdt[t0:t0 + 128, :], st[:], R=[st], W=[Dabdt])
                groups = [(C_QKV, 1536, 'copy'), (C_GZ, 512, 'silu'), (C_SZ, 512, 'silu'), (C_XBC, 1024, 'copy'),
                          (C_CQ, 512, 'copy'), (C_CKV, 256, 'copy'), (C_KR, 64, 'copy'), (C_GATES, 3072, 'sig')]
                wr = Ring([sb("win%d" % i, [128, 8, 512], BF16, stack=ps_) for i in range(2)])
                stg = Ring([sb("stg%d" % i, [128, 512], BF16, stack=ps_) for i in range(4)])
                ecnt = 0
                for (c0, ncols, post) in groups:
                    for blk in range(0, ncols, 512):
                        bw = min(512, ncols - blk)
                        wt = wr.get()
                        k.dma('pool', wt[:, :, :bw],
                              w_in[l][:, c0 + blk:c0 + blk + bw].rearrange('(kc p) n -> p kc n', p=128),
                              R=[Dw], W=[wt])
                        for tt in range(NT):
                            for ct in range(0, bw, 128):
                                m = min(128, bw - ct)
                                ps = psum.get()
                                for kc in range(8):
                                    k.mm(ps, ps[:m, :], wt[:, kc, ct:ct + m], h1[:, kc, tt * 512:(tt + 1) * 512],
                                         R=[wt, h1], start=(kc == 0), stop=(kc == 7))
                                st = stg.get()
                                if post == 'silu':
                                    k.act(st[:m, :], ps[:m, :], AF.Silu, R=[ps], W=[st])
                                elif post == 'sig':
                                    k.act(st[:m, :], ps[:m, :], AF.Sigmoid, R=[ps], W=[st])
                                else:
                                    ecnt += 1
                                    k.copy('dve' if ecnt % 2 else 'act', st[:m, :], ps[:m, :], R=[ps], W=[st])
                                r0 = c0 + blk + ct
                                k.dma('sp', proj[r0:r0 + m, tt * 512:(tt + 1) * 512], st[:m, :], R=[st], W=[Dproj])
                k.barrier()


        def dump(name, b, ap=None, dt=None):
            if not debug:
                return
            ap = b[:] if ap is None else ap
            uid[0] += 1
            t = nc.dram_tensor("dbg_%s_%d" % (name, uid[0]), list(ap.shape), dt or ap.dtype, kind="ExternalOutput").ap()
            k.dma('sp', t, ap, R=[b], W=[Buf(None, True)])

        cols = sb("cols", [128, 4], F32)
        k.op('dve', lambda e: e.memset(cols[:, 0:1], 1.0), W=[cols])
        k.op('dve', lambda e: e.memset(cols[:, 1:2], -np.pi), W=[cols])
        invf = sb("invf", [128, 1], F32)
        k.dma('sp', invf[:], invf_in, R=[Dw], W=[invf])
        ATT_SCALE = 192.0 ** -0.5

        def phase_mla(l):
            with ExitStack() as ps_:
                wuq = sb("wuq", [128, 4, 768], BF16, stack=ps_)
                wuqr = sb("wuqr", [128, 4, 2, 128], BF16, stack=ps_)
                wuk = sb("wuk", [128, 2, 512], BF16, stack=ps_)
                wuv = sb("wuv", [128, 2, 512], BF16, stack=ps_)
                qnw = sb("qnw", [128, 4], F32, stack=ps_)
                kvnw = sb("kvnw", [128, 2], F32, stack=ps_)
                k.dma('pool', wuq[:], mla_wuq[l].rearrange('(kc p) n -> p kc n', p=128), R=[Dw], W=[wuq])
                for h in range(4):
                    k.dma('pool', wuqr[:, :, h // 2, (h % 2) * 64:(h % 2) * 64 + 64],
                          mla_wuq[l][:, h * 192 + 128:h * 192 + 192].rearrange('(kc p) n -> p kc n', p=128),
                          R=[Dw], W=[wuqr])
                k.dma('pool', wuk[:], mla_wuk[l].rearrange('(kc p) n -> p kc n', p=128), R=[Dw], W=[wuk])
                k.dma('pool', wuv[:], mla_wuv[l].rearrange('(kc p) n -> p kc n', p=128), R=[Dw], W=[wuv])
                k.dma('sp', qnw[:], mla_qnw[l], R=[Dw], W=[qnw])
                k.dma('sp', kvnw[:], mla_kvnw[l], R=[Dw], W=[kvnw])
                qn = sb("qn", [128, 4, T], BF16, multi=True, stack=ps_)
                qr = sb("qr", [128, 2, T], BF16, multi=True, stack=ps_)
                kn = sb("kn", [128, 4, T], BF16, multi=True, stack=ps_)
                krp = sb("krp", [128, T], BF16, multi=True, stack=ps_)
                vtm = sb("vtm", [128, NQ, 512], BF16, multi=True, stack=ps_)
                with ExitStack() as p1:
                    cin = Ring([sb("mcin%d" % i, [128, 7, 512], BF16, stack=p1) for i in range(2)])
                    sqm = sb("msq", [128, 4, 512], F32, stack=p1)
                    cqn = sb("cqn", [128, 4, 512], BF16, stack=p1)
                    ckvn = sb("ckvn", [128, 2, 512], BF16, stack=p1)
                    posi = sb("posi", [128, 512], I32, stack=p1)
                    posf = sb("posf", [128, 512], F32, stack=p1)
                    frac = sb("frac", [128, 512], F32, stack=p1)
                    fint = sb("fint", [128, 512], I32, stack=p1)
                    ftmp = sb("ftmp", [128, 512], F32, stack=p1)
                    sinT = sb("sinT", [128, 512], F32, stack=p1)
                    cosT = sb("cosT", [128, 512], F32, stack=p1)
                    rf = sb("rf", [128, 512], F32, stack=p1)
                    r1 = sb("r1", [128, 512], F32, stack=p1)
                    r2 = sb("r2", [128, 512], F32, stack=p1)
                    rstd = sb("mrstd", [128, 512], F32, stack=p1)
                    rot2 = cst[:, 5, :]

                    def rope(src_b, src_ap, dst_b, dst_ap, scale):
                        k.copy('act', rf[:], src_ap, R=[src_b], W=[rf])
                        ps = psum.get()
                        k.mm(ps, ps[:, :], rot2, rf[:], R=[cst, rf])
                        k.stt('dve', r1[:], rf[:], scale, cosT[:], ALU.mult, ALU.mult, R=[rf, cosT], W=[r1])
                        k.stt('dve', r2[:], ps[:, :], scale, sinT[:], ALU.mult, ALU.mult, R=[ps, sinT], W=[r2])
                        k.tt('dve', dst_ap, r1[:], r2[:], ALU.add, R=[r1, r2], W=[dst_b])

                    for tt in range(NT):
                        ts_ = slice(tt * 512, (tt + 1) * 512)
                        ci = cin.get()
                        k.dma('sp', ci[:, 0:6, :], proj[C_CQ:C_CQ + 768, ts_].rearrange('(kc p) t -> p kc t', p=128),
                              R=[Dproj], W=[ci])
                        k.dma('sp', ci[0:64, 6, :], proj[C_KR:C_KR + 64, ts_], R=[Dproj], W=[ci])
                        k.dma('sp', ci[64:128, 6, :], proj[C_KR:C_KR + 64, ts_], R=[Dproj], W=[ci])
                        k.dma('sp', posi[:], pos_in[0:1, ts_].partition_broadcast(128), R=[Dw], W=[posi])
                        k.copy('dve', posf[:], posi[:], R=[posi], W=[posf])
                        for (off, dst) in ((0.5, sinT), (0.75, cosT)):
                            k.ts('dve', frac[:], posf[:], invf[:, 0:1], 1.0 / (2 * np.pi), ALU.mult, ALU.mult,
                                 R=[posf, invf], W=[frac])
                            k.ts('dve', frac[:], frac[:], off, None, ALU.add, R=[frac], W=[frac])
                            k.copy('dve', fint[:], frac[:], R=[frac], W=[fint])
                            k.copy('dve', ftmp[:], fint[:], R=[fint], W=[ftmp])
                            k.tt('dve', frac[:], frac[:], ftmp[:], ALU.subtract, R=[frac, ftmp], W=[frac])
                            k.ts('dve', ftmp[:], frac[:], 0.0, None, ALU.is_lt, R=[frac], W=[ftmp])
                            k.tt('dve', frac[:], frac[:], ftmp[:], ALU.add, R=[frac, ftmp], W=[frac])
                            k.act(dst[:], frac[:], AF.Sin, R=[frac, cols], W=[dst], bias=cols[:, 1:2],
                                  scale=2 * np.pi)
                        for (c0, nk_, wv, dstb) in ((0, 4, qnw, cqn), (4, 2, kvnw, ckvn)):
                            k.act(sqm[:, 0:nk_, :], ci[:, c0:c0 + nk_, :], AF.Square, R=[ci], W=[sqm])
                            ps = psum.get()
                            for kc in range(nk_):
                                k.mm(ps, ps[:, :], ones_f, sqm[:, kc, :], R=[cst, sqm], start=(kc == 0),
                                     stop=(kc == nk_ - 1))
                            rsqrt(rstd, rstd[:], ps, ps[:, :], 1.0 / (nk_ * 128))
                            for kc in range(nk_):
                                k.stt('dve', dstb[:, kc, :], ci[:, c0 + kc, :], wv[:, kc:kc + 1], rstd[:], ALU.mult,
                                      ALU.mult, R=[ci, wv, rstd], W=[dstb])
                        for h in range(4):
                            ps = psum.get()
                            for kc in range(4):
                                k.mm(ps, ps[:, :], wuq[:, kc, h * 192:h * 192 + 128], cqn[:, kc, :], R=[wuq, cqn],
                                     start=(kc == 0), stop=(kc == 3))
                            k.act(qn[:, h, ts_], ps[:, :], AF.Copy, R=[ps], W=[qn], scale=ATT_SCALE)
                            ps = psum.get()
                            for kc in range(2):
                                k.mm(ps, ps[:, :], wuk[:, kc, h * 128:(h + 1) * 128], ckvn[:, kc, :], R=[wuk, ckvn],
                                     start=(kc == 0), stop=(kc == 1))
                            k.copy('dve', kn[:, h, ts_], ps[:, :], R=[ps], W=[kn])
                        for hp in range(2):
                            ps = psum.get()
                            for kc in range(4):
                                k.mm(ps, ps[:, :], wuqr[:, kc, hp, :], cqn[:, kc, :], R=[wuqr, cqn],
                                     start=(kc == 0), stop=(kc == 3))
                            rope(ps, ps[:, :], qr, qr[:, hp, ts_], ATT_SCALE)
                        rope(ci, ci[:, 6, :], krp, krp[:, ts_], 1.0)
                        for q in range(4):
                            ps = psum.get()
                            for kc in range(2):
                                k.mm(ps, ps[:, :], ckvn[:, kc, q * 128:(q + 1) * 128], wuv[:, kc, :], R=[ckvn, wuv],
                                     start=(kc == 0), stop=(kc == 1))
                            k.copy('act', vtm[:, tt * 4 + q, :], ps[:, :], R=[ps], W=[vtm])
                dump("qn", qn); dump("qr", qr); dump("kn", kn); dump("krp", krp); dump("vtm", vtm)
                with ExitStack() as p2:
                    WM = 2
                    slots = []
                    for i in range(WM):
                        slots.append(dict(
                            Ssb=sb("Ssb%d" % i, [128, T], F32, stack=p2), Psb=sb("Psb%d" % i, [128, T], BF16, stack=p2),
                            ptr=Ring([sb("pt%d_%d" % (i, j), [128, 4, 128], BF16, stack=p2) for j in range(2)]),
                            sc=sb("asc%d" % i, [128, 4], F32, stack=p2), dg=sb("adg%d" % i, [128, 128], BF16, stack=p2),
                            po=psacc.bufs[i]))
                    ost = [sb("aost%d" % i, [128, 4, 128], BF16, multi=True, stack=p2) for i in range(2)]
                    done = {}

                    def att(key):
                        qi, h = key
                        B = slots[(qi * 4 + h) % WM]
                        Ssb, Psb, ptr, s_, dg, po = B['Ssb'], B['Psb'], B['ptr'], B['sc'], B['dg'], B['po']
                        ot = ost[qi % 2]
                        nk = (qi + 1) * 128
                        qs = slice(qi * 128, (qi + 1) * 128)
                        hp, ho = h // 2, (h % 2) * 64
                        for kb in range(0, nk, 512):
                            w = min(512, nk - kb)
                            ps = psum.get()
                            k.mm(ps, ps[:, :w], qn[:, h, qs], kn[:, h, kb:kb + w], R=[qn, kn], start=True,
                                 stop=False)
                            k.mm(ps, ps[:, :w], qr[ho:ho + 64, hp, qs], krp[ho:ho + 64, kb:kb + w], R=[qr, krp],
                                 start=False, stop=True)
                            if kb + w == nk:
                                if w > 128:
                                    k.copy('act', Ssb[:, kb:nk - 128], ps[:, :w - 128], R=[ps], W=[Ssb])
                                k.tt('dve', Ssb[:, nk - 128:nk], ps[:, w - 128:w], cst[:, 3, :], ALU.subtract,
                                     R=[ps, cst], W=[Ssb])
                            else:
                                k.copy('act', Ssb[:, kb:kb + w], ps[:, :w], R=[ps], W=[Ssb])
                            yield
                        k.op('dve', lambda e: e.reduce_max(s_[:, 0:1], Ssb[:, :nk], AX.X), R=[Ssb], W=[s_])
                        k.ts('dve', s_[:, 1:2], s_[:, 0:1], -1.0, None, ALU.mult, R=[s_], W=[s_])
                        yield
                        k.act(Psb[:, :nk], Ssb[:, :nk], AF.Exp, R=[Ssb, s_], W=[Psb, s_], bias=s_[:, 1:2],
                              scale=1.0, accum_out=s_[:, 2:3])
                        yield
                        k.op('dve', lambda e: e.reciprocal(s_[:, 3:4], s_[:, 2:3]), R=[s_], W=[s_])
                        k.ts('dve', dg[:], ident_b, s_[:, 3:4], None, ALU.mult, R=[cst_b, s_], W=[dg])
                        yield
                        nb = nk // 128
                        for b4 in range(0, nb, 4):
                            n4 = min(4, nb - b4)
                            ps = psum.get()
                            for j in range(n4):
                                kb2 = (b4 + j) * 128
                                k.mm(ps, ps[:, j * 128:(j + 1) * 128], Psb[:, kb2:kb2 + 128], dg[:], R=[Psb, dg])
                            pt = ptr.get()
                            k.copy('dve' if (b4 // 4) % 2 else 'act', pt[:, 0:n4, :],
                                   ps[:, 0:n4 * 128].rearrange('p (j t) -> p j t', j=n4), R=[ps], W=[pt])
                            for j in range(n4):
                                kblk = b4 + j
                                k.mm(po, po[:, 0:128], vtm[:, kblk, h * 128:(h + 1) * 128], pt[:, j, :],
                                     R=[vtm, pt], start=(kblk == 0), stop=(kblk == nb - 1))
                            yield
                        k.copy('act', ot[:, h, :], po[:, 0:128], R=[po], W=[ot])

                    def att_done(key):
                        qi, h = key
                        done[qi] = done.get(qi, 0) + 1
                        if done[qi] == 4:
                            qs = slice(qi * 128, (qi + 1) * 128)
                            k.dma('sp', y_d[2][:, qs].rearrange('(h p) t -> p h t', p=128), ost[qi % 2][:],
                                  R=[ost[qi % 2]], W=[Dy[2]])

                    run_pipeline([(qi, h) for qi in range(NQ) for h in range(4)], WM, att, att_done)
                k.barrier()


        def bc(ap, shape):
            return ap.to_broadcast(list(shape))

        def run_pipeline(items, W, start_fn, finish_fn):
            active = []
            it = iter(items)
            pending = True
            while True:
                while len(active) < W and pending:
                    try:
                        key = next(it)
                    except StopIteration:
                        pending = False
                        break
                    active.append((key, start_fn(key)))
                if not active:
                    break
                for ent in list(active):
                    try:
                        next(ent[1])
                    except StopIteration:
                        active.remove(ent)
                        finish_fn(ent[0])

        def conv_silu(cin, cw, nchan, dst, dstb, tmpr, bias=None):
            for c in range(nchan):
                tb = tmpr.get()
                k.ts('dve', tb[:], cin[:, c, 0:512], cw[:, c, 0:1], None, ALU.mult, R=[cin, cw], W=[tb])
                for kk in range(1, 4):
                    k.stt('dve', tb[:], cin[:, c, kk:kk + 512], cw[:, c, kk:kk + 1], tb[:], ALU.mult,
                          ALU.add, R=[cin, cw, tb], W=[tb])
                if bias is None:
                    k.act(dst[:, c, :], tb[:], AF.Silu, R=[tb], W=[dstb])
                else:
                    k.act(dst[:, c, :], tb[:], AF.Silu, R=[tb, bias], W=[dstb], bias=bias[:, c:c + 1])

        def load_halo(cin, row0, nrows, tt):
            if tt == 0:
                k.op('dve', lambda e: e.memset(cin[:, :, 0:3], 0.0), W=[cin])
                k.dma('sp', cin[:, :, 3:515], proj[row0:row0 + nrows, 0:512].rearrange('(c p) t -> p c t', p=128),
                      R=[Dproj], W=[cin])
            else:
                k.dma('sp', cin[:, :, :],
                      proj[row0:row0 + nrows, tt * 512 - 3:tt * 512 + 512].rearrange('(c p) t -> p c t', p=128),
                      R=[Dproj], W=[cin])

        tri64 = cst[0:64, 2, 0:64]
        ones64 = cst[0:64, 1, 0:64]
        ones64w = cst[0:64, 1, :]
        posm64 = cst[0:64, 3, 0:64]
        strict64 = cst[0:64, 4, 0:64]
        lowm64 = cst[0:64, 6, 0:64]
        id64 = cst[0:64, 0, 0:64]

        def phase_ssd(l):
            with ExitStack() as ps_:
                def t_(name, shape, dt=F32, multi=False):
                    return sb(name, shape, dt, multi=multi, stack=ps_)
                cw = t_("scw", [128, 8, 4]); cb = t_("scb", [128, 8]); alog = t_("salog", [128, 8])
                dtb = t_("sdtb", [128, 8]); dexp = t_("sdexp", [128, 4]); nw = t_("snw", [128, 4])
                for (d_, s_) in ((cw, ssm_convT), (cb, ssm_convb), (alog, ssm_alog), (dtb, ssm_dtb), (dexp, ssm_dexp),
                                 (nw, ssm_nw)):
                    k.dma('sp', d_[:], s_[l], R=[Dw], W=[d_])
                k.act(alog[:], alog[:], AF.Exp, R=[alog], W=[alog])
                k.ts('dve', alog[:], alog[:], -1.0, None, ALU.mult, R=[alog], W=[alog])
                raw = t_("sraw", [64, NCH, 16])
                k.dma('sp', raw[:], abdt.rearrange('(c p) k -> p c k', p=64), R=[Dabdt], W=[raw])
                dt = t_("sdt", [64, NCH, 8]); ad = t_("sad", [64, NCH, 8]); acs = t_("sacs", [64, NCH, 8])
                acl = t_("sacl", [128, NCH, 8]); cd = t_("scd", [128, NCH, 8]); ds = t_("sds", [64, NCH, 8])
                eacs = t_("seacs", [64, NCH, 8])
                k.tt('dve', dt[:], raw[:, :, 8:16], bc(dtb[0:64, :].unsqueeze(1), [64, NCH, 8]), ALU.add,
                     R=[raw, dtb], W=[dt])
                k.act(dt[:], dt[:], AF.Exp, R=[dt], W=[dt])
                k.act(dt[:], dt[:], AF.Ln, R=[dt, cols], W=[dt], bias=cols[0:64, 0:1])
                k.tt('dve', ad[:], dt[:], bc(alog[0:64, :].unsqueeze(1), [64, NCH, 8]), ALU.mult, R=[dt, alog], W=[ad])
                adf = ad[:].rearrange('p c h -> p (c h)')
                ps = psum.get()
                k.mm(ps, ps[:64, :NCH * 8], tri64, adf, R=[cst, ad])
                k.copy('dve', acs[:].rearrange('p c h -> p (c h)'), ps[:64, :NCH * 8], R=[ps], W=[acs])
                ps = psum.get()
                k.mm(ps, ps[:, :NCH * 8], ones64w, adf, R=[cst, ad])
                k.copy('dve', acl[:].rearrange('p c h -> p (c h)'), ps[:, :NCH * 8], R=[ps], W=[acl])
                k.act(cd[:], acl[:], AF.Exp, R=[acl], W=[cd])
                k.tt('dve', ds[:], acl[0:64], acs[:], ALU.subtract, R=[acl, acs], W=[ds])
                k.act(ds[:], ds[:], AF.Exp, R=[ds], W=[ds])
                k.act(eacs[:], acs[:], AF.Exp, R=[acs], W=[eacs])
                state = t_("sstate", [128, 8, 64])
                k.op('dve', lambda e: e.memset(state[:], 0.0), W=[state])
                cinr = Ring([t_("scin%d" % i, [128, 8, 515], BF16) for i in range(2)])
                szr = Ring([t_("ssz%d" % i, [128, 4, 512], BF16) for i in range(2)])
                xfr = Ring([t_("sxf%d" % i, [128, 8, 512]) for i in range(2)])
                ctr = Ring([t_("sct%d" % i, [128, 512]) for i in range(2)])
                ystr = Ring([t_("syst%d" % i, [128, 4, 512], BF16, multi=True) for i in range(2)])
                WS = 2
                slots = []
                for i in range(WS):
                    slots.append(dict(
                        trig=t_("strig%d" % i, [64, 8, 64]), t1=t_("st1%d" % i, [64, 8, 64]), MT=t_("sMT%d" % i, [64, 8, 64]),
                        X=t_("sX%d" % i, [64, 8, 64]), Xds=t_("sXds%d" % i, [64, 8, 64]), Btm=t_("sBtm%d" % i, [64, 2, 128]),
                        yt=t_("syt%d" % i, [64, 8, 64]), ytm=t_("sytm%d" % i, [64, 8, 64]), yfm=t_("syfm%d" % i, [128, 4, 64]),
                        tmp=t_("stmp%d" % i, [128, 4, 64]), sq=t_("ssq%d" % i, [128, 4, 64]), rs=t_("srs%d" % i, [128, 2, 64])))
                tiles = {}
                turn = [0]
                done = {}

                def prep(tt):
                    cin = cinr.get(); szt = szr.get(); yst = ystr.get(); xf = xfr.get()
                    load_halo(cin, C_XBC, 1024, tt)
                    k.dma('sp', szt[:], proj[C_SZ:C_SZ + 512, tt * 512:(tt + 1) * 512].rearrange(
                        '(c p) t -> p c t', p=128), R=[Dproj], W=[szt])
                    conv_silu(cin, cw, 8, xf, xf, ctr, bias=cb)
                    tiles[tt] = (xf, szt, yst)

                def chunk(ch):
                    tt, cc = ch // 8, ch % 8
                    if cc == 0:
                        prep(tt)
                    xf, szt, yst = tiles[tt]
                    B = slots[ch % WS]
                    trig, t1, MT, X, Xds, Btm = B['trig'], B['t1'], B['MT'], B['X'], B['Xds'], B['Btm']
                    yt, ytm, yfm, tmp, sq, rs = B['yt'], B['ytm'], B['yfm'], B['tmp'], B['sq'], B['rs']
                    cs = slice(cc * 64, cc * 64 + 64)
                    ps_cb = psum.get()
                    for g in range(2):
                        k.mm(ps_cb, ps_cb[:64, g * 64:(g + 1) * 64], xf[:, 4 + g, cs], xf[:, 6 + g, cs], R=[xf])
                    k.tt('pool', trig[:], bc(tri64.unsqueeze(1), [64, 8, 64]),
                         bc(ad[:, ch, :].unsqueeze(2), [64, 8, 64]), ALU.mult, R=[cst, ad], W=[trig])
                    ps_r = psum.get()
                    k.mm(ps_r, ps_r[:64, :], ones64, trig[:].rearrange('p h l -> p (h l)'), R=[cst, trig])
                    k.tt('dve', t1[:], ps_r[:64, :].rearrange('p (h l) -> p h l', h=8),
                         bc(lowm64.unsqueeze(1), [64, 8, 64]), ALU.subtract, R=[ps_r, cst], W=[t1])
                    k.tt('dve', t1[:], t1[:], bc(acs[:, ch, :].unsqueeze(2), [64, 8, 64]), ALU.subtract,
                         R=[t1, acs], W=[t1])
                    k.act(t1[:], t1[:], AF.Exp, R=[t1], W=[t1])
                    k.tt('dve', MT[:].rearrange('p (g e) l -> p g e l', g=2),
                         t1[:].rearrange('p (g e) l -> p g e l', g=2),
                         bc(ps_cb[:64, 0:128].rearrange('p (g l) -> p g l', g=2).unsqueeze(2), [64, 2, 4, 64]),
                         ALU.mult, R=[t1, ps_cb], W=[MT])
                    yield
                    ps_x = psum.get()
                    for kc in range(4):
                        k.tr(ps_x, ps_x[:64, kc * 128:(kc + 1) * 128], xf[:, kc, cs], ident_f, R=[xf, cst])
                    k.tt('dve', X[:], ps_x[:64, :].rearrange('p (h q) -> p h q', h=8),
                         bc(dt[:, ch, :].unsqueeze(2), [64, 8, 64]), ALU.mult, R=[ps_x, dt], W=[X])
                    k.tt('pool', Xds[:], X[:], bc(ds[:, ch, :].unsqueeze(2), [64, 8, 64]), ALU.mult,
                         R=[X, ds], W=[Xds])
                    yield
                    ps_b = psum.get()
                    for g in range(2):
                        k.tr(ps_b, ps_b[:64, g * 128:(g + 1) * 128], xf[:, 4 + g, cs], ident_f, R=[xf, cst])
                    k.copy('act', Btm[:].rearrange('p g n -> p (g n)'), ps_b[:64, 0:256], R=[ps_b], W=[Btm])
                    yield
                    while turn[0] != ch:
                        yield
                    ps_y1 = psum.get()
                    for h in range(8):
                        k.mm(ps_y1, ps_y1[:64, h * 64:(h + 1) * 64], MT[:, h, :], X[:, h, :], R=[MT, X])
                    ps_y2 = psum.get()
                    for h in range(8):
                        k.mm(ps_y2, ps_y2[:64, h * 64:(h + 1) * 64], xf[:, 6 + h // 4, cs], state[:, h, :],
                             R=[xf, state])
                    k.tt('dve', yt[:], ps_y2[:64, :].rearrange('p (h q) -> p h q', h=8),
                         bc(eacs[:, ch, :].unsqueeze(2), [64, 8, 64]), ALU.mult, R=[ps_y2, eacs], W=[yt])
                    k.tt('dve', ytm[:], yt[:], ps_y1[:64, :].rearrange('p (h q) -> p h q', h=8), ALU.add,
                         R=[yt, ps_y1], W=[ytm])
                    ps_s = psum.get()
                    for h in range(8):
                        k.mm(ps_s, ps_s[:, h * 64:(h + 1) * 64], Btm[:, h // 4, :], Xds[:, h, :], R=[Btm, Xds])
                    k.tt('dve', state[:], state[:], bc(cd[:, ch, :].unsqueeze(2), [128, 8, 64]), ALU.mult,
                         R=[state, cd], W=[state])
                    k.tt('dve', state[:], state[:], ps_s[:, :].rearrange('p (h q) -> p h q', h=8), ALU.add,
                         R=[state, ps_s], W=[state])
                    turn[0] = ch + 1
                    yield
                    ps_t = psum.get()
                    ytf = ytm[:].rearrange('p h q -> p (h q)')
                    for kc in range(4):
                        k.tr(ps_t, ps_t[:, kc * 64:(kc + 1) * 64], ytf[:, kc * 128:(kc + 1) * 128], id64,
                             R=[ytm, cst])
                    k.tt('pool', tmp[:], xf[:, 0:4, cs], bc(dexp[:, :].unsqueeze(2), [128, 4, 64]), ALU.mult,
                         R=[xf, dexp], W=[tmp])
                    k.tt('dve', yfm[:], tmp[:], ps_t[:, 0:256].rearrange('p (c q) -> p c q', c=4), ALU.add,
                         R=[tmp, ps_t], W=[yfm])
                    k.tt('dve', yfm[:], yfm[:], szt[:, :, cs], ALU.mult, R=[yfm, szt], W=[yfm])
                    k.act(sq[:], yfm[:], AF.Square, R=[yfm], W=[sq])
                    yield
                    ps_n = psum.get()
                    for g in range(2):
                        for k2 in range(2):
                            k.mm(ps_n, ps_n[:, g * 64:(g + 1) * 64], ones_f, sq[:, g * 2 + k2, :], R=[cst, sq],
                                 start=(k2 == 0), stop=(k2 == 1))
                    rsqrt(rs, rs[:].rearrange('p g q -> p (g q)'), ps_n, ps_n[:, 0:128], 1.0 / 256)
                    for kc in range(4):
                        k.stt('dve', yst[:, kc, cs], yfm[:, kc, :], nw[:, kc:kc + 1], rs[:, kc // 2, :], ALU.mult,
                              ALU.mult, R=[yfm, nw, rs], W=[yst])

                def chunk_done(ch):
                    tt = ch // 8
                    done[tt] = done.get(tt, 0) + 1
                    if done[tt] == 8:
                        yst = tiles[tt][2]
                        k.dma('sp', y_d[1][:, tt * 512:(tt + 1) * 512].rearrange('(c p) t -> p c t', p=128), yst[:],
                              R=[yst], W=[Dy[1]])

                run_pipeline(list(range(NCH)), WS, chunk, chunk_done)
                k.barrier()

        def phase_gdn(l):
            with ExitStack() as ps_:
                def t_(name, shape, dt=F32, multi=False):
                    return sb(name, shape, dt, multi=multi, stack=ps_)
                cw = t_("gcw", [128, 12, 4]); alog = t_("galog", [128, 4]); dtb = t_("gdtb", [128, 4])
                nw = t_("gnw", [128, 1])
                for (d_, s_) in ((cw, gdn_convT), (alog, gdn_alog), (dtb, gdn_dtb), (nw, gdn_nw)):
                    k.dma('sp', d_[:], s_[l], R=[Dw], W=[d_])
                k.act(alog[:], alog[:], AF.Exp, R=[alog], W=[alog])
                k.ts('dve', alog[:], alog[:], -1.0, None, ALU.mult, R=[alog], W=[alog])
                raw = t_("graw", [64, NCH, 16])
                k.dma('sp', raw[:], abdt.rearrange('(c p) k -> p c k', p=64), R=[Dabdt], W=[raw])
                beta = t_("gbeta", [64, NCH, 4]); nbeta = t_("gnbeta", [64, NCH, 4]); g = t_("gg", [64, NCH, 4])
                G = t_("gG", [64, NCH, 4]); Glb = t_("gGlb", [128, NCH, 4]); gl = t_("ggl", [128, NCH, 4])
                eG = t_("geG", [64, NCH, 4]); kdsc = t_("gkdsc", [64, NCH, 4]); bexpG = t_("gbexpG", [64, NCH, 4])
                k.act(beta[:], raw[:, :, 4:8], AF.Sigmoid, R=[raw], W=[beta])
                k.ts('dve', nbeta[:], beta[:], -1.0, None, ALU.mult, R=[beta], W=[nbeta])
                k.tt('dve', g[:], raw[:, :, 0:4], bc(dtb[0:64, :].unsqueeze(1), [64, NCH, 4]), ALU.add,
                     R=[raw, dtb], W=[g])
                k.act(g[:], g[:], AF.Exp, R=[g], W=[g])
                k.act(g[:], g[:], AF.Ln, R=[g, cols], W=[g], bias=cols[0:64, 0:1])
                k.tt('dve', g[:], g[:], bc(alog[0:64, :].unsqueeze(1), [64, NCH, 4]), ALU.mult, R=[g, alog], W=[g])
                gf = g[:].rearrange('p c h -> p (c h)')
                ps = psum.get()
                k.mm(ps, ps[:64, :NCH * 4], tri64, gf, R=[cst, g])
                k.copy('dve', G[:].rearrange('p c h -> p (c h)'), ps[:64, :NCH * 4], R=[ps], W=[G])
                ps = psum.get()
                k.mm(ps, ps[:, :NCH * 4], ones64w, gf, R=[cst, g])
                k.copy('dve', Glb[:].rearrange('p c h -> p (c h)'), ps[:, :NCH * 4], R=[ps], W=[Glb])
                k.act(gl[:], Glb[:], AF.Exp, R=[Glb], W=[gl])
                k.act(eG[:], G[:], AF.Exp, R=[G], W=[eG])
                k.tt('dve', kdsc[:], Glb[0:64], G[:], ALU.subtract, R=[Glb, G], W=[kdsc])
                k.act(kdsc[:], kdsc[:], AF.Exp, R=[kdsc], W=[kdsc])
                k.tt('dve', bexpG[:], beta[:], eG[:], ALU.mult, R=[beta, eG], W=[bexpG])
                S = t_("gS", [128, 4, 128])
                k.op('dve', lambda e: e.memset(S[:], 0.0), W=[S])
                cinr = Ring([t_("gcin%d" % i, [128, 12, 515], BF16) for i in range(2)])
                gzr = Ring([t_("ggz%d" % i, [128, 4, 512], BF16) for i in range(2)])
                qkvr = Ring([t_("gqkv%d" % i, [128, 12, 512]) for i in range(2)])
                ctr = Ring([t_("gct%d" % i, [128, 512]) for i in range(2)])
                rsn = t_("grsn", [128, 512])
                ystr = Ring([t_("gyst%d" % i, [128, 4, 512], BF16, multi=True) for i in range(2)])
                WG = 2
                slots = []
                for i in range(WG):
                    d_ = {}
                    for n in ('trig', 't1', 'n1', 'qkd', 'qkT', 'Xa', 'Xb', 'Ya', 'Yb', 'Ra', 'Rb'):
                        d_[n] = t_("g%s%d" % (n, i), [64, 4, 64])
                    for n in ('ktm', 'kbg', 'kdec', 'vtm', 'u', 'vn', 'o', 'sq'):
                        d_[n] = t_("g%s%d" % (n, i), [64, 4, 128])
                    d_['wf'] = t_("gwf%d" % i, [128, 4, 64])
                    d_['ss'] = t_("gss%d" % i, [64, 4])
                    slots.append(d_)
                tiles = {}
                turn = [0]
                done = {}

                def v4(ps, w):
                    return ps[:64, 0:4 * w].rearrange('p (h x) -> p h x', h=4)

                def prep(tt):
                    cin = cinr.get(); gz = gzr.get(); yst = ystr.get(); qkv = qkvr.get()
                    load_halo(cin, C_QKV, 1536, tt)
                    k.dma('sp', gz[:], proj[C_GZ:C_GZ + 512, tt * 512:(tt + 1) * 512].rearrange(
                        '(c p) t -> p c t', p=128), R=[Dproj], W=[gz])
                    conv_silu(cin, cw, 12, qkv, qkv, ctr)
                    for c in range(8):
                        tb = ctr.get()
                        k.act(tb[:], qkv[:, c, :], AF.Square, R=[qkv], W=[tb])
                        ps = psum.get()
                        k.mm(ps, ps[:, :], ones_f, tb[:], R=[cst, tb])
                        rsqrt(rsn, rsn[:], ps, ps[:, :], 1.0)
                        if c < 4:
                            k.stt('dve', qkv[:, c, :], qkv[:, c, :], 128.0 ** -0.5, rsn[:], ALU.mult, ALU.mult,
                                  R=[qkv, rsn], W=[qkv])
                        else:
                            k.tt('dve', qkv[:, c, :], qkv[:, c, :], rsn[:], ALU.mult, R=[qkv, rsn], W=[qkv])
                    tiles[tt] = (qkv, gz, yst)

                def chunk(ch):
                    tt, cc = ch // 8, ch % 8
                    if cc == 0:
                        prep(tt)
                    qkv, gz, yst = tiles[tt]
                    B = slots[ch % WG]
                    cs = slice(cc * 64, cc * 64 + 64)
                    b4 = lambda t, w: bc(t[:, ch, :].unsqueeze(2), [t[:, ch, :].shape[0], 4, w])
                    ktm, vtm, trig, t1, n1, qkd, qkT = B['ktm'], B['vtm'], B['trig'], B['t1'], B['n1'], B['qkd'], B['qkT']
                    ps_k = psum.get()
                    for h in range(4):
                        k.tr(ps_k, ps_k[:64, h * 128:(h + 1) * 128], qkv[:, 4 + h, cs], ident_f, R=[qkv, cst])
                    k.copy('act', ktm[:], v4(ps_k, 128), R=[ps_k], W=[ktm])
                    yield
                    ps_v = psum.get()
                    for h in range(4):
                        k.tr(ps_v, ps_v[:64, h * 128:(h + 1) * 128], qkv[:, 8 + h, cs], ident_f, R=[qkv, cst])
                    k.copy('act', vtm[:], v4(ps_v, 128), R=[ps_v], W=[vtm])
                    yield
                    ps_kk = psum.get()
                    for h in range(4):
                        k.mm(ps_kk, ps_kk[:64, h * 64:(h + 1) * 64], qkv[:, 4 + h, cs], qkv[:, 4 + h, cs], R=[qkv])
                    ps_qk = psum.get()
                    for h in range(4):
                        k.mm(ps_qk, ps_qk[:64, h * 64:(h + 1) * 64], qkv[:, h, cs], qkv[:, 4 + h, cs], R=[qkv])
                    k.tt('pool', trig[:], bc(tri64.unsqueeze(1), [64, 4, 64]), b4(g, 64), ALU.mult,
                         R=[cst, g], W=[trig])
                    ps_g = psum.get()
                    k.mm(ps_g, ps_g[:64, 0:256], ones64, trig[:].rearrange('p h l -> p (h l)'), R=[cst, trig])
                    k.tt('dve', t1[:], v4(ps_g, 64), bc(posm64.unsqueeze(1), [64, 4, 64]), ALU.add,
                         R=[ps_g, cst], W=[t1])
                    k.tt('dve', t1[:], b4(G, 64), t1[:], ALU.subtract, R=[G, t1], W=[t1])
                    k.act(t1[:], t1[:], AF.Exp, R=[t1], W=[t1])
                    X0 = B['Xa']
                    k.tt('dve', n1[:], v4(ps_kk, 64), t1[:], ALU.mult, R=[ps_kk, t1], W=[n1])
                    k.tt('dve', n1[:], n1[:], b4(nbeta, 64), ALU.mult, R=[n1, nbeta], W=[n1])
                    k.tt('pool', X0[:], n1[:], bc(strict64.unsqueeze(1), [64, 4, 64]), ALU.mult,
                         R=[n1, cst], W=[X0])
                    k.tt('dve', qkd[:], v4(ps_qk, 64), t1[:], ALU.mult, R=[ps_qk, t1], W=[qkd])
                    yield
                    ps_y = psum.get()
                    for h in range(4):
                        k.tr(ps_y, ps_y[:64, h * 64:(h + 1) * 64], X0[:, h, :], id64, R=[X0, cst])
                    Y0 = B['Ya']
                    k.copy('act', Y0[:], v4(ps_y, 64), R=[ps_y], W=[Y0])
                    yield
                    ps_q = psum.get()
                    for h in range(4):
                        k.tr(ps_q, ps_q[:64, h * 64:(h + 1) * 64], qkd[:, h, :], id64, R=[qkd, cst])
                    k.copy('act', qkT[:], v4(ps_q, 64), R=[ps_q], W=[qkT])
                    RT = B['Ra']
                    k.tt('pool', RT[:], Y0[:], bc(id64.unsqueeze(1), [64, 4, 64]), ALU.add, R=[Y0, cst], W=[RT])
                    yield
                    Xp, Yp = X0, Y0
                    for kk in range(1, 6):
                        Xn = B['Xb'] if Xp is B['Xa'] else B['Xa']
                        Yn = B['Yb'] if Yp is B['Ya'] else B['Ya']
                        RTn = B['Rb'] if RT is B['Ra'] else B['Ra']
                        ps_a = psum.get()
                        for h in range(4):
                            k.mm(ps_a, ps_a[:64, h * 64:(h + 1) * 64], Yp[:, h, :], Xp[:, h, :], R=[Yp, Xp])
                        if kk <= 4:
                            ps_b = psum.get()
                            for h in range(4):
                                k.mm(ps_b, ps_b[:64, h * 64:(h + 1) * 64], Xp[:, h, :], Yp[:, h, :], R=[Yp, Xp])
                        k.copy('act', Xn[:], v4(ps_a, 64), R=[ps_a], W=[Xn])
                        if kk <= 4:
                            k.copy('dve', Yn[:], v4(ps_b, 64), R=[ps_b], W=[Yn])
                        yield
                        ps_c = psum.get()
                        for h in range(4):
                            k.mm(ps_c, ps_c[:64, h * 64:(h + 1) * 64], Xn[:, h, :], RT[:, h, :], R=[Xn, RT])
                        k.tt('dve', RTn[:], RT[:], v4(ps_c, 64), ALU.add, R=[RT, ps_c], W=[RTn])
                        Xp, Yp, RT = Xn, Yn, RTn
                        yield
                    kbg, kdec, u, wf, vn, o, sq, ss = B['kbg'], B['kdec'], B['u'], B['wf'], B['vn'], B['o'], B['sq'], B['ss']
                    k.tt('pool', vtm[:], vtm[:], b4(beta, 128), ALU.mult, R=[vtm, beta], W=[vtm])
                    k.tt('pool', kbg[:], ktm[:], b4(bexpG, 128), ALU.mult, R=[ktm, bexpG], W=[kbg])
                    k.tt('pool', kdec[:], ktm[:], b4(kdsc, 128), ALU.mult, R=[ktm, kdsc], W=[kdec])
                    ps_u = psum.get()
                    for h in range(4):
                        k.mm(ps_u, ps_u[:64, h * 128:(h + 1) * 128], RT[:, h, :], vtm[:, h, :], R=[RT, vtm])
                    k.copy('act', u[:], v4(ps_u, 128), R=[ps_u], W=[u])
                    yield
                    ps_w = psum.get()
                    for h in range(4):
                        k.mm(ps_w, ps_w[:, h * 64:(h + 1) * 64], kbg[:, h, :], RT[:, h, :], R=[kbg, RT])
                    k.copy('act', wf[:], ps_w[:, 0:256].rearrange('p (h x) -> p h x', h=4), R=[ps_w], W=[wf])
                    yield
                    while turn[0] != ch:
                        yield
                    ps_ws = psum.get()
                    for h in range(4):
                        k.mm(ps_ws, ps_ws[:64, h * 128:(h + 1) * 128], wf[:, h, :], S[:, h, :], R=[wf, S])
                    k.tt('dve', vn[:], u[:], v4(ps_ws, 128), ALU.subtract, R=[u, ps_ws], W=[vn])
                    ps_o1 = psum.get()
                    for h in range(4):
                        k.mm(ps_o1, ps_o1[:64, h * 128:(h + 1) * 128], qkv[:, h, cs], S[:, h, :], R=[qkv, S])
                    ps_ds = psum.get()
                    for h in range(4):
                        k.mm(ps_ds, ps_ds[:, h * 128:(h + 1) * 128], kdec[:, h, :], vn[:, h, :], R=[kdec, vn])
                    k.tt('dve', S[:], S[:], bc(gl[:, ch, :].unsqueeze(2), [128, 4, 128]), ALU.mult,
                         R=[S, gl], W=[S])
                    k.tt('dve', S[:], S[:], ps_ds[:, :].rearrange('p (h x) -> p h x', h=4), ALU.add,
                         R=[S, ps_ds], W=[S])
                    turn[0] = ch + 1
                    ps_o2 = psum.get()
                    for h in range(4):
                        k.mm(ps_o2, ps_o2[:64, h * 128:(h + 1) * 128], qkT[:, h, :], vn[:, h, :], R=[qkT, vn])
                    k.tt('dve', o[:], v4(ps_o1, 128), b4(eG, 128), ALU.mult, R=[ps_o1, eG], W=[o])
                    k.tt('dve', o[:], o[:], v4(ps_o2, 128), ALU.add, R=[o, ps_o2], W=[o])
                    yield
                    k.tt('pool', sq[:], o[:], o[:], ALU.mult, R=[o], W=[sq])
                    k.op('dve', lambda e: e.reduce_sum(ss[:], sq[:], AX.X), R=[sq], W=[ss])
                    rsqrt(ss, ss[:], ss, ss[:], 1.0 / 128)
                    k.tt('dve', sq[:], o[:], bc(ss[:, :].unsqueeze(2), [64, 4, 128]), ALU.mult, R=[o, ss], W=[sq])
                    yield
                    ps_t = psum.get()
                    for h in range(4):
                        k.tr(ps_t, ps_t[:, h * 64:(h + 1) * 64], sq[:, h, :], id64, R=[sq, cst])
                    k.stt('dve', yst[:, :, cs], ps_t[:, 0:256].rearrange('p (h x) -> p h x', h=4), nw[:, 0:1],
                          gz[:, :, cs], ALU.mult, ALU.mult, R=[ps_t, nw, gz], W=[yst])

                def chunk_done(ch):
                    tt = ch // 8
                    done[tt] = done.get(tt, 0) + 1
                    if done[tt] == 8:
                        yst = tiles[tt][2]
                        k.dma('sp', y_d[0][:, tt * 512:(tt + 1) * 512].rearrange('(c p) t -> p c t', p=128), yst[:],
                              R=[yst], W=[Dy[0]])

                run_pipeline(list(range(NCH)), WG, chunk, chunk_done)
                k.barrier()

        def phase_mixers(l):
            if 'mla' in mixers:
                phase_mla(l)
            if 'ssd' in mixers:
                phase_ssd(l)
            if 'gdn' in mixers:
                phase_gdn(l)

        def phase_merge(l, xsrc, Dxsrc, xdst, Dxdst):
            with ExitStack() as ps_:
                wb = [sb("wbr%d" % i, [128, 4, 1024], BF16, stack=ps_) for i in range(3)]
                wo = sb("wo", [128, 8, 1024], BF16, stack=ps_)
                for i in range(3):
                    k.dma('pool', wb[i][:], w_br[i][l].rearrange('(kc p) n -> p kc n', p=128), R=[Dw], W=[wb[i]])
                k.dma('pool', wo[:], w_out[l].rearrange('(kc p) n -> p kc n', p=128), R=[Dw], W=[wo])
                yr = Ring([sb("ym%d" % i, [128, 3, 4, 512], BF16, stack=ps_) for i in range(2)])
                gr = Ring([sb("gm%d" % i, [128, 24, 512], BF16, stack=ps_) for i in range(2)])
                xr = Ring([sb("xm%d" % i, [128, 8, 512], F32, stack=ps_) for i in range(2)])
                mg = Ring([sb("mg%d" % i, [128, 8, 512], BF16, stack=ps_) for i in range(2)])
                tmp = Ring([sb("mt%d" % i, [128, 512], F32, stack=ps_) for i in range(3)])
                for tt in range(NT):
                    ts_ = slice(tt * 512, (tt + 1) * 512)
                    y = yr.get()
                    g = gr.get()
                    xt = xr.get()
                    m_ = mg.get()
                    for i in range(3):
                        k.dma('sp', y[:, i], y_d[i][:, ts_].rearrange('(kc p) t -> p kc t', p=128), R=[Dy[i]], W=[y])
                    k.dma('sp', g[:], proj[C_GATES:C_GATES + 3072, ts_].rearrange('(kc p) t -> p kc t', p=128),
                          R=[Dproj], W=[g])
                    for (a_, b_, sap) in xsrc(tt):
                        k.dma('sp', xt[:, a_:b_, :], sap, R=[Dxsrc], W=[xt])
                    for o in range(8):
                        tl = []
                        for i in range(3):
                            ps = psum.get()
                            for kc in range(4):
                                k.mm(ps, ps[:, :], wb[i][:, kc, o * 128:(o + 1) * 128], y[:, i, kc, :], R=[wb[i], y],
                                     start=(kc == 0), stop=(kc == 3))
                            t_ = tmp.get()
                            k.tt('dve', t_[:], ps[:, :], g[:, i * 8 + o, :], ALU.mult, R=[ps, g], W=[t_])
                            tl.append(t_)
                        k.tt('pool', tl[0][:], tl[0][:], tl[1][:], ALU.add, R=[tl[0], tl[1]], W=[tl[0]])
                        k.tt('pool', m_[:, o, :], tl[0][:], tl[2][:], ALU.add, R=[tl[0], tl[2]], W=[m_])
                    for o in range(8):
                        ps = psum.get()
                        for kc in range(8):
                            k.mm(ps, ps[:, :], wo[:, kc, o * 128:(o + 1) * 128], m_[:, kc, :], R=[wo, m_],
                                 start=(kc == 0), stop=(kc == 7))
                        k.stt('dve', xt[:, o, :], ps[:, :], modT[:, 16 + o:17 + o], xt[:, o, :], ALU.mult, ALU.add,
                              R=[ps, modT, xt], W=[xt])
                    k.dma('sp', xdst[:, ts_].rearrange('(kc p) t -> p kc t', p=128), xt[:], R=[xt], W=[Dxdst])
                k.barrier()

        def phase_ffn(l, xsrc, Dxsrc, xdst, Dxdst):
            moe = (l % 2 == 1)
            li = l // 2
            TT = min(TH, 1024)
            NS = TT // 512
            HCMAX = 14
            with ExitStack() as ps_:
                h2 = sb("h2", [128, 8, TT], BF16, multi=True, stack=ps_)
                xacc = sb("xacc", [128, 8, TT], F32, multi=True, stack=ps_)
                sq = sb("sq2", [128, 8, 512], F32, stack=ps_)
                hid = sb("hid", [128, HCMAX, TT], BF16, multi=True, stack=ps_)
                wgr = Ring([sb("wg%d" % i, [128, 8, 256], BF16, stack=ps_) for i in range(2)])
                wur = Ring([sb("wu%d" % i, [128, 8, 256], BF16, stack=ps_) for i in range(2)])
                wdr = Ring([sb("wd%d" % i, [128, HCMAX, 512], BF16, stack=ps_) for i in range(1)])
                sgr = Ring([sb("sg%d" % i, [128, 512], BF16, stack=ps_) for i in range(3)])
                tmp = Ring([sb("ft%d" % i, [128, 512], F32, stack=ps_) for i in range(2)])
                if moe:
                    hf = sb("hf2", [128, 8, 512], F32, stack=ps_)
                    rt = sb("rt", [128, 8, NEXP], F32, stack=ps_)
                    k.dma('sp', rt[:], moe_router[li].rearrange('(kc p) n -> p kc n', p=128), R=[Dw], W=[rt])
                    wrow = sb("wrow", [128, NEXP, TT], BF16, multi=True, stack=ps_)
                    sm = [sb("rs%d" % i, [128, 8], F32, stack=ps_) for i in range(6)]
                    sc = [sb("rc%d" % i, [128, 1], F32, stack=ps_) for i in range(4)]
                    dg = sb("dg", [128, NEXP, 128], F32, stack=ps_)
                for st_ in range(TH // TT):
                    t0 = st_ * TT
                    for s in range(NS):
                        sl = slice(s * 512, (s + 1) * 512)
                        k.dma('sp', xacc[:, :, sl],
                              xsrc[:, t0 + s * 512:t0 + (s + 1) * 512].rearrange('(kc p) t -> p kc t', p=128),
                              R=[Dxsrc], W=[xacc])
                        if pair:
                            k.dma('sp', sq[:], xsrc[:, TH + t0 + s * 512:TH + t0 + (s + 1) * 512].rearrange(
                                '(kc p) t -> p kc t', p=128), R=[Dxsrc], W=[sq])
                            k.ts('dve', xacc[:, :, sl], xacc[:, :, sl], rsel[:, 0:1], None, ALU.mult,
                                 R=[xacc, rsel], W=[xacc])
                            k.stt('dve', xacc[:, :, sl], sq[:], rsel[:, 1:2], xacc[:, :, sl], ALU.mult, ALU.add,
                                  R=[sq, rsel, xacc], W=[xacc])
                        norm_tile(xacc, xacc[:, :, sl], sq, lambda kc: h2[:, kc, sl], h2, 32, 24,
                                  hf=([hf] if moe else None))
                        if moe:
                            for q in range(4):
                                lg, m1, m2, e1, e2, wt8 = sm
                                ps = psum.get()
                                for kc in range(8):
                                    k.mm(ps, ps[:, 0:8], hf[:, kc, q * 128:(q + 1) * 128], rt[:, kc, :], R=[hf, rt],
                                         start=(kc == 0), stop=(kc == 7))
                                k.copy('dve', lg[:], ps[:, 0:8], R=[ps], W=[lg])
                                k.op('dve', lambda e: e.reduce_max(sc[0][:], lg[:], AX.X), R=[lg], W=[sc[0]])
                                k.ts('dve', e1[:], lg[:], sc[0][:, 0:1], None, ALU.is_equal, R=[lg, sc[0]], W=[e1])
                                k.stt('dve', m1[:], e1[:], -1e30, lg[:], ALU.mult, ALU.add, R=[e1, lg], W=[m1])
                                k.op('dve', lambda e: e.reduce_max(sc[1][:], m1[:], AX.X), R=[m1], W=[sc[1]])
                                k.ts('dve', e2[:], m1[:], sc[1][:, 0:1], None, ALU.is_equal, R=[m1, sc[1]], W=[e2])
                                k.tt('dve', sc[2][:], sc[0][:], sc[1][:], ALU.subtract, R=[sc[0], sc[1]], W=[sc[2]])
                                k.act(sc[3][:], sc[2][:], AF.Sigmoid, R=[sc[2]], W=[sc[3]])
                                k.act(sc[2][:], sc[2][:], AF.Sigmoid, R=[sc[2]], W=[sc[2]], scale=-1.0)
                                k.ts('dve', wt8[:], e1[:], sc[3][:, 0:1], None, ALU.mult, R=[e1, sc[3]], W=[wt8])
                                k.stt('dve', wt8[:], e2[:], sc[2][:, 0:1], wt8[:], ALU.mult, ALU.add,
                                      R=[e2, sc[2], wt8], W=[wt8])
                                k.tt('dve', dg[:], cst[:, 0:1, :].to_broadcast([128, NEXP, 128]),
                                     wt8[:, :].unsqueeze(2).to_broadcast([128, NEXP, 128]), ALU.mult,
                                     R=[cst, wt8], W=[dg])
                                for hh in range(2):
                                    ps2 = psum.get()
                                    k.mm(ps2, ps2[:, :], ones_f, dg[:, hh * 4:(hh + 1) * 4, :].rearrange('p e t -> p (e t)'), R=[cst, dg])
                                    c0 = s * 512 + q * 128
                                    k.copy('act', wrow[:, hh * 4:(hh + 1) * 4, c0:c0 + 128],
                                           ps2[:, :].rearrange('p (e t) -> p e t', e=4), R=[ps2], W=[wrow])
                    if moe:
                        passes = []
                        for e_ in range(NEXP):
                            for hh in range(2):
                                passes.append((moe_wg[li, e_], moe_wu[li, e_], moe_wd[li, e_], hh * 1792, 1792, e_))
                    else:
                        passes = [(ffn_wg[li], ffn_wu[li], ffn_wd[li], hh * 1408, 1408, None) for hh in range(2)]
                    for (wg_, wu_, wd_, h0, hn, ex) in passes:
                        HC = hn // 128
                        for cb in range(0, hn, 256):
                            cw = min(256, hn - cb)
                            wg = wgr.get()
                            wu = wur.get()
                            k.dma('pool', wg[:, :, :cw],
                                  wg_[:, h0 + cb:h0 + cb + cw].rearrange('(kc p) n -> p kc n', p=128), R=[Dw], W=[wg])
                            k.dma('pool', wu[:, :, :cw],
                                  wu_[:, h0 + cb:h0 + cb + cw].rearrange('(kc p) n -> p kc n', p=128), R=[Dw], W=[wu])
                            for jj in range(cw // 128):
                                j = cb // 128 + jj
                                for s in range(NS):
                                    sl = slice(s * 512, (s + 1) * 512)
                                    psg = psum.get()
                                    for kc in range(8):
                                        k.mm(psg, psg[:, :], wg[:, kc, jj * 128:(jj + 1) * 128], h2[:, kc, sl],
                                             R=[wg, h2], start=(kc == 0), stop=(kc == 7))
                                    psu = psum.get()
                                    for kc in range(8):
                                        k.mm(psu, psu[:, :], wu[:, kc, jj * 128:(jj + 1) * 128], h2[:, kc, sl],
                                             R=[wu, h2], start=(kc == 0), stop=(kc == 7))
                                    sg = sgr.get()
                                    k.act(sg[:], psg[:, :], AF.Silu, R=[psg], W=[sg])
                                    k.tt('dve', hid[:, j, sl], psu[:, :], sg[:], ALU.mult, R=[psu, sg], W=[hid])
                        for oh in range(2):
                            wd = wdr.get()
                            for c4 in range(0, HC, 7):
                                cn = min(7, HC - c4)
                                k.dma('pool', wd[:, c4:c4 + cn, :],
                                      wd_[h0 + c4 * 128:h0 + (c4 + cn) * 128, oh * 512:(oh + 1) * 512].rearrange(
                                          '(kc p) n -> p kc n', p=128), R=[Dw], W=[wd])
                            for oo in range(4):
                                o = oh * 4 + oo
                                for s in range(NS):
                                    sl = slice(s * 512, (s + 1) * 512)
                                    ps = psum.get()
                                    for j in range(HC):
                                        k.mm(ps, ps[:, :], wd[:, j, oo * 128:(oo + 1) * 128], hid[:, j, sl],
                                             R=[wd, hid], start=(j == 0), stop=(j == HC - 1))
                                    if ex is None:
                                        k.stt('dve', xacc[:, o, sl], ps[:, :], modT[:, 40 + o:41 + o], xacc[:, o, sl],
                                              ALU.mult, ALU.add, R=[ps, modT, xacc], W=[xacc])
                                    else:
                                        t_ = tmp.get()
                                        k.stt('dve', t_[:], ps[:, :], modT[:, 40 + o:41 + o], wrow[:, ex, sl],
                                              ALU.mult, ALU.mult, R=[ps, modT, wrow], W=[t_])
                                        k.tt('pool', xacc[:, o, sl], xacc[:, o, sl], t_[:], ALU.add,
                                             R=[t_, xacc], W=[xacc])
                    k.dma('sp', xdst[:, t0:t0 + TT].rearrange('(kc p) t -> p kc t', p=128), xacc[:], R=[xacc],
                          W=[Dxdst])
                k.barrier()

        def phase_final(xsrc, Dxsrc, ntiles):
            with ExitStack() as ps_:
                xr = Ring([sb("xf%d" % i, [128, 8, 512], F32, stack=ps_) for i in range(2)])
                orr = Ring([sb("of%d" % i, [128, 8, 512], F32, multi=True, stack=ps_) for i in range(2)])
                sq = sb("sqf", [128, 8, 512], F32, stack=ps_)
                for tt in range(ntiles):
                    ts_ = slice(tt * 512, (tt + 1) * 512)
                    xt = xr.get()
                    ot = orr.get()
                    for (a_, b_, sap) in xsrc(tt):
                        k.dma('sp', xt[:, a_:b_, :], sap, R=[Dxsrc], W=[xt])
                    norm_tile(xt, xt[:], sq, lambda kc: ot[:, kc, :], ot, None, None)
                    k.dma('sp', outT[:, ts_].rearrange('(kc p) t -> p kc t', p=128), ot[:], R=[ot], W=[Dout])
                k.barrier()

        def tiles_of(ap):
            return lambda tt: [(0, 8, ap[:, tt * 512:(tt + 1) * 512].rearrange('(kc p) t -> p kc t', p=128))]

        def tiles_of_xg(tt):
            r_, off = (tt * 512) // TH, (tt * 512) % TH
            return [(2 * kb, 2 * kb + 2,
                     xg[kb * 512 + r_ * 256:kb * 512 + r_ * 256 + 256, off:off + 512].rearrange(
                         '(kl p) t -> p kl t', p=128)) for kb in range(4)]

        k.csem = es.enter_context(nc.semaphore('s_cc'))
        k.sem[('c',)] = k.csem
        k.cnt[('c',)] = 0
        cur, Dcur = tiles_of(xT_in), Dxin
        fin, Dfin, nfin = cur, Dcur, NT
        for li_, l in enumerate(layers):
            phase_ada(l)
            full, Dfull = None, None
            if 'mix' in stages:
                phase_inproj(l, cur, Dcur)
                phase_mixers(l)
                phase_merge(l, cur, Dcur, x_b, Dx_b)
                cur, Dcur = tiles_of(x_b), Dx_b
                full, Dfull = x_b, Dx_b
                fin, Dfin, nfin = cur, Dcur, NT
            if 'ffn' in stages:
                if full is None:
                    assert not pair
                    full, Dfull = (xT_in, Dxin) if li_ == 0 else (x_a, Dx_a)
                if pair:
                    phase_ffn(l, full, Dfull, xh, Dxh)
                    fin, Dfin, nfin = tiles_of(xh), Dxh, TH // 512
                    if li_ != len(layers) - 1:
                        k._wait('pool', k._deps([Dxh], [Dxg]))
                        for kb in range(4):
                            ins = nc.gpsimd.collective_compute(
                                "AllGather", ALU.bypass,
                                replica_groups=[[2 * i_, 2 * i_ + 1] for i_ in range(ncores // 2)],
                                ins=[xh[kb * 256:(kb + 1) * 256, :].opt()],
                                outs=[xg[kb * 512:(kb + 1) * 512, :].opt()])
                            k.cnt[('c',)] += 1
                            ins.then_inc(k.csem, 1)
                        k._post((('c',), k.cnt[('c',)]), [Dxh], [Dxg])
                        k.barrier()
                        cur, Dcur = tiles_of_xg, Dxg
                else:
                    phase_ffn(l, full, Dfull, x_a, Dx_a)
                    cur, Dcur = tiles_of(x_a), Dx_a
                    fin, Dfin, nfin = cur, Dcur, NT
        phase_final(fin, Dfin, nfin)
        k.barrier()
        print("instructions:", k.ninst, {kk: v for kk, v in k.cnt.items()})
    return nc


def _consts():
    c = np.zeros((128, 8, 128), np.float32)
    i = np.arange(128)
    c[:, 0, :] = np.eye(128)
    c[:, 1, :] = 1.0
    c[:, 2, :] = (i[:, None] <= i[None, :])
    c[:, 3, :] = np.where(i[None, :] > i[:, None], 1e9, 0.0)
    c[:, 4, :] = (i[None, :] < i[:, None])
    rot = np.zeros((128, 128), np.float32)
    for o in (0, 64):
        for m in range(32):
            rot[o + m + 32, o + m] = -1.0
            rot[o + m, o + m + 32] = 1.0
    c[:, 5, :] = rot
    c[:, 6, :] = np.where(i[None, :] < i[:, None], 1e9, 0.0)
    return c


def _fm(v, nchunk):
    return np.ascontiguousarray(np.asarray(v, np.float32).reshape(nchunk, 128).T)


def prep_inputs(inp, b, T):
    f = lambda a: np.ascontiguousarray(np.asarray(a, np.float32))
    L = DEPTH
    m = {}
    m["xT"] = np.ascontiguousarray(np.asarray(inp["x"][b, :T], np.float32).T)
    m["cT"] = _fm(inp["c"][b], 8)
    m["pos"] = np.ascontiguousarray(np.asarray(inp["positions"][b, :T], np.int32).reshape(1, T))
    m["w_ada"] = f(inp["w_ada"])
    m["b_adaT"] = np.stack([_fm(inp["b_ada"][l], 48) for l in range(L)])
    m["w_in"] = f(inp["w_in"])
    gc = np.asarray(inp["gdn_conv_w"], np.float32)
    m["gdn_convT"] = np.ascontiguousarray(gc.reshape(L, 4, 12, 128).transpose(0, 3, 2, 1))
    rep = lambda a: np.ascontiguousarray(np.broadcast_to(np.asarray(a, np.float32)[:, None, :], (L, 128, a.shape[-1])))
    m["gdn_alog"] = rep(inp["gdn_a_log"])
    m["gdn_dtb"] = rep(inp["gdn_dt_bias"])
    m["gdn_nw"] = np.ascontiguousarray(np.asarray(inp["gdn_norm_w"], np.float32).reshape(L, 128, 1))
    sc = np.asarray(inp["ssm_conv_w"], np.float32)
    m["ssm_convT"] = np.ascontiguousarray(sc.reshape(L, 4, 8, 128).transpose(0, 3, 2, 1))
    m["ssm_convb"] = np.stack([_fm(inp["ssm_conv_b"][l], 8) for l in range(L)])
    m["ssm_alog"] = rep(inp["ssm_a_log"])
    m["ssm_dtb"] = rep(inp["ssm_dt_bias"])
    dexp = np.repeat(np.asarray(inp["ssm_d"], np.float32), 64, axis=1)
    m["ssm_dexp"] = np.stack([_fm(dexp[l], 4) for l in range(L)])
    m["ssm_nw"] = np.stack([_fm(inp["ssm_norm_w"][l], 4) for l in range(L)])
    m["mla_qnw"] = np.stack([_fm(inp["mla_q_norm_w"][l], 4) for l in range(L)])
    m["mla_wuq"] = f(inp["mla_w_uq"])
    m["mla_kvnw"] = np.stack([_fm(inp["mla_kv_norm_w"][l], 2) for l in range(L)])
    m["mla_wuk"] = f(inp["mla_w_uk"])
    m["mla_wuv"] = f(inp["mla_w_uv"])
    for n in ("w_branch_a", "w_branch_b", "w_branch_c", "w_out", "ffn_w_gate", "ffn_w_up", "ffn_w_down",
              "moe_router", "moe_w_gate", "moe_w_up", "moe_w_down"):
        m[n] = f(inp[n])
    m["fnwT"] = _fm(inp["final_norm_w"], 8)
    m["consts"] = _consts()
    m["rsel"] = np.stack([np.ones(128, np.float32), np.zeros(128, np.float32)], 1)
    invf = (10000.0 ** (-np.arange(0, 64, 2, dtype=np.float32) / 64)).astype(np.float32)
    m["invf"] = np.concatenate([invf] * 4).reshape(128, 1).astype(np.float32)
    return m


def kernel(**inputs):
    B, T = inputs["x"].shape[0], inputs["x"].shape[1]
    nc = build_program(T, list(range(DEPTH)), pair=True)
    base = [prep_inputs(inputs, b, T) for b in range(B)]
    in_maps = []
    for c in range(2 * B):
        m = dict(base[c // 2])
        rs = np.zeros((128, 2), np.float32)
        rs[:, c % 2] = 1.0
        m["rsel"] = rs
        in_maps.append(m)
    res = run_bass_kernel_spmd(nc, in_maps, core_ids=list(range(2 * B)))
    TH = T // 2
    out = np.zeros((B, T, D), np.float32)
    for c in range(2 * B):
        out[c // 2, (c % 2) * TH:(c % 2 + 1) * TH, :] = np.asarray(res.results[c]["outT"], np.float32).T
    return out
```

```python
import numpy as np
from contextlib import ExitStack
import concourse.bass as bass
import concourse.mybir as mybir
from concourse.bass_utils import run_bass_kernel_spmd

F32, BF16, I32 = mybir.dt.float32, mybir.dt.bfloat16, mybir.dt.int32
AF = mybir.ActivationFunctionType
ALU = mybir.AluOpType
AX = mybir.AxisListType

D = 1024
DEPTH = 4
EPS = 1e-6
IN_DIM = 7504
FFN_DIM = 2816
EXPERT_DIM = 3584
NEXP = 8
SAME_SYNC = True

C_QKV, C_GZ, C_A, C_B, C_SZ, C_XBC, C_DT, C_CQ, C_CKV, C_KR, C_GATES = (
    0, 1536, 2048, 2052, 2056, 2568, 3592, 3600, 4112, 4368, 4432)


class Buf:
    def __init__(self, t, multi=False):
        self.t = t
        self.multi = multi
        self.w = {}
        self.r = {}

    def __getitem__(self, key):
        return self.t[key]


def _merge(d, tok):
    k_, v = tok
    if d.get(k_, 0) < v:
        d[k_] = v


class Ring:
    def __init__(self, bufs):
        self.bufs = bufs
        self.i = 0

    def get(self):
        b = self.bufs[self.i % len(self.bufs)]
        self.i += 1
        return b


class KB:
    ENG = ('pe', 'act', 'dve', 'pool', 'sp')

    def __init__(self, nc, es):
        self.nc = nc
        self.es = es
        self.e = {'pe': nc.tensor, 'act': nc.scalar, 'dve': nc.vector, 'pool': nc.gpsimd, 'sp': nc.sync}
        self.sem = {}
        self.cnt = {}
        for e in self.ENG:
            self.sem[('e', e)] = es.enter_context(nc.semaphore('se_' + e))
            self.cnt[('e', e)] = 0
        self.NS = 8
        self.dma_i = {}
        for q in ('sp', 'pool'):
            self.dma_i[q] = 0
            for j in range(self.NS):
                self.sem[('d', q, j)] = es.enter_context(nc.semaphore('sd_%s%d' % (q, j)))
                self.cnt[('d', q, j)] = 0
        self.seen = {e: {} for e in self.ENG}
        self.ninst = 0

    def _wait(self, eng, deps):
        for key, v in deps.items():
            if key == ('e', eng) and (eng == 'pe' or eng == 'sp' or not SAME_SYNC):
                continue
            if self.seen[eng].get(key, 0) >= v:
                continue
            self.e[eng].wait_ge(self.sem[key], v)
            self.seen[eng][key] = v
            self.ninst += 1

    def _deps(self, R, W):
        deps = {}
        for b in R:
            for t in b.w.items():
                _merge(deps, t)
        for b in W:
            for t in b.r.items():
                _merge(deps, t)
            if not b.multi:
                for t in b.w.items():
                    _merge(deps, t)
        return deps

    def _post(self, tok, R, W):
        for b in R:
            _merge(b.r, tok)
        for b in W:
            if b.multi:
                _merge(b.w, tok)
            else:
                b.w = {tok[0]: tok[1]}
                b.r = {}

    def op(self, eng, fn, R=(), W=()):
        self._wait(eng, self._deps(R, W))
        ins = fn(self.e[eng])
        key = ('e', eng)
        self.cnt[key] += 1
        ins.then_inc(self.sem[key], 1)
        self.ninst += 1
        self._post((key, self.cnt[key]), R, W)

    def dma(self, q, out, in_, R=(), W=()):
        self._wait(q, self._deps(R, W))
        key = ('d', q, self.dma_i[q] % self.NS)
        self.dma_i[q] += 1
        if self.cnt[key] > 0:
            self._wait(q, {key: self.cnt[key]})
        ins = self.e[q].dma_start(out=out, in_=in_)
        self.cnt[key] += 16
        ins.then_inc(self.sem[key], 16)
        self.ninst += 1
        self._post((key, self.cnt[key]), R, W)

    def barrier(self):
        for e in self.ENG:
            deps = {key: v for key, v in self.cnt.items() if v > 0 and key != ('e', e)}
            self._wait(e, deps)

    def mm(self, ps, out, lhsT, rhs, R, start=True, stop=True):
        self.op('pe', lambda e: e.matmul(out, lhsT, rhs, start=start, stop=stop), R=R, W=[ps])

    def tr(self, ps, out, in_, ident, R):
        self.op('pe', lambda e: e.transpose(out, in_, ident), R=R, W=[ps])

    def act(self, out, in_, func, R, W, bias=None, scale=None, accum_out=None, eng='act'):
        kw = {}
        if bias is not None:
            kw['bias'] = bias
        if scale is not None:
            kw['scale'] = scale
        if accum_out is not None:
            kw['accum_out'] = accum_out
        self.op('act', lambda e: e.activation(out=out, in_=in_, func=func, **kw), R=R, W=W)

    def tt(self, eng, out, in0, in1, op, R, W):
        self.op(eng, lambda e: e.tensor_tensor(out, in0, in1, op), R=R, W=W)

    def ts(self, eng, out, in0, s1, s2, op0, op1=None, R=(), W=()):
        if op1 is None:
            self.op(eng, lambda e: e.tensor_scalar(out, in0, s1, None, op0), R=R, W=W)
        else:
            self.op(eng, lambda e: e.tensor_scalar(out, in0, s1, s2, op0, op1), R=R, W=W)

    def stt(self, eng, out, in0, scalar, in1, op0, op1, R, W):
        self.op(eng, lambda e: e.scalar_tensor_tensor(out, in0, scalar, in1, op0, op1), R=R, W=W)

    def copy(self, eng, out, in_, R, W):
        if eng == 'act':
            self.op('act', lambda e: e.activation(out=out, in_=in_, func=AF.Copy), R=R, W=W)
        else:
            self.op(eng, lambda e: e.tensor_copy(out, in_), R=R, W=W)


def build_program(T, layers, debug=False, stages=('mix', 'ffn'), mixers=('mla', 'ssd', 'gdn'), pair=False, ncores=8):
    nc = bass.Bass("TRN2", target_bir_lowering=False)
    L = DEPTH
    NT = T // 512
    NQ = T // 128
    NCH = T // 64
    TH = T // 2 if pair else T
    HG = 2 if pair else 4
    HM = 2 if pair else 4

    def din(name, shape, dt=F32):
        return nc.dram_tensor(name, list(shape), dt, kind="ExternalInput").ap()

    def dscr(name, shape, dt, out=False):
        kind = "ExternalOutput" if out else "Internal"
        return nc.dram_tensor(name, list(shape), dt, kind=kind).ap()

    xT_in = din("xT", [D, T])
    cT_in = din("cT", [128, 8])
    pos_in = din("pos", [1, T], I32)
    w_ada = din("w_ada", [L, D, 6 * D])
    b_adaT = din("b_adaT", [L, 128, 48])
    w_in = din("w_in", [L, D, IN_DIM])
    gdn_convT = din("gdn_convT", [L, 128, 12, 4])
    gdn_alog = din("gdn_alog", [L, 128, 4])
    gdn_dtb = din("gdn_dtb", [L, 128, 4])
    gdn_nw = din("gdn_nw", [L, 128, 1])
    ssm_convT = din("ssm_convT", [L, 128, 8, 4])
    ssm_convb = din("ssm_convb", [L, 128, 8])
    ssm_alog = din("ssm_alog", [L, 128, 8])
    ssm_dtb = din("ssm_dtb", [L, 128, 8])
    ssm_dexp = din("ssm_dexp", [L, 128, 4])
    ssm_nw = din("ssm_nw", [L, 128, 4])
    mla_qnw = din("mla_qnw", [L, 128, 4])
    mla_wuq = din("mla_wuq", [L, 512, 768])
    mla_kvnw = din("mla_kvnw", [L, 128, 2])
    mla_wuk = din("mla_wuk", [L, 256, 512])
    mla_wuv = din("mla_wuv", [L, 256, 512])
    w_br = [din("w_branch_a", [L, 512, D]), din("w_branch_b", [L, 512, D]), din("w_branch_c", [L, 512, D])]
    w_out = din("w_out", [L, D, D])
    ffn_wg = din("ffn_w_gate", [2, D, FFN_DIM])
    ffn_wu = din("ffn_w_up", [2, D, FFN_DIM])
    ffn_wd = din("ffn_w_down", [2, FFN_DIM, D])
    moe_router = din("moe_router", [2, D, NEXP])
    moe_wg = din("moe_w_gate", [2, NEXP, D, EXPERT_DIM])
    moe_wu = din("moe_w_up", [2, NEXP, D, EXPERT_DIM])
    moe_wd = din("moe_w_down", [2, NEXP, EXPERT_DIM, D])
    fnwT = din("fnwT", [128, 8])
    consts = din("consts", [128, 8, 128])
    invf_in = din("invf", [128, 1])

    outT = dscr("outT", [D, TH], F32, out=True)
    rsel_in = din("rsel", [128, 2])
    xh = dscr("xh", [D, TH], F32)
    xg = dscr("xg", [2 * D, TH], F32)
    yam = dscr("yam", [HG * 128, T], BF16)
    ycm = dscr("ycm", [HM * 128, T], BF16)
    x_a = dscr("x_a", [D, T], F32, out=debug)
    x_b = dscr("x_b", [D, T], F32, out=debug)
    proj = dscr("proj", [IN_DIM, T], BF16, out=debug)
    abdt = dscr("abdt", [T, 16], F32, out=debug)
    y_d = [dscr("y_a", [512, T], BF16, out=debug), dscr("y_b", [512, T], BF16, out=debug),
           dscr("y_c", [512, T], BF16, out=debug)]

    es = ExitStack()
    with es:
        k = KB(nc, es)

        uid = [0]

        def sb(name, shape, dt, multi=False, stack=es):
            uid[0] += 1
            return Buf(stack.enter_context(nc.sbuf_tensor("%s_%d" % (name, uid[0]), list(shape), dt)), multi=multi)

        Dx_a, Dx_b, Dproj, Dabdt = Buf(x_a, True), Buf(x_b, True), Buf(proj, True), Buf(abdt, True)
        Dy = [Buf(y, True) for y in y_d]
        Dout = Buf(outT, True)
        Dxh, Dxg = Buf(xh, True), Buf(xg, True)
        Dyam, Dycm = Buf(yam, True), Buf(ycm, True)

        def allgather(pieces, Dsrc, Ddst):
            k._wait('pool', k._deps([Dsrc], [Ddst]))
            for (sap, dap) in pieces:
                ins = nc.gpsimd.collective_compute(
                    "AllGather", ALU.bypass, replica_groups=[[2 * i_, 2 * i_ + 1] for i_ in range(ncores // 2)],
                    ins=[sap.opt()], outs=[dap.opt()])
                k.cnt[('c',)] += 1
                ins.then_inc(k.csem, 1)
            k._post((('c',), k.cnt[('c',)]), [Dsrc], [Ddst])
            k.barrier()

        k.csem = es.enter_context(nc.semaphore('s_cc'))
        k.sem[('c',)] = k.csem
        k.cnt[('c',)] = 0
        rsel = sb("rsel", [128, 2], F32)
        k.dma('sp', rsel[:], rsel_in, R=[Buf(None)], W=[rsel])
        Dw = Buf(None)
        Dxin = Buf(xT_in)

        psum = Ring([Buf(es.enter_context(nc.psum_tensor("ps%d" % i, [128, 512], F32))) for i in range(6)])
        psacc = Ring([Buf(es.enter_context(nc.psum_tensor("pa%d" % i, [128, 512], F32))) for i in range(2)])

        cst = sb("cst", [128, 8, 128], F32)
        k.dma('sp', cst[:], consts, R=[Dw], W=[cst])
        ident_f = cst[:, 0, :]
        ones_f = cst[:, 1, :]
        cst_b = sb("cst_b", [128, 2, 128], BF16)
        k.copy('dve', cst_b[:], cst[:, 0:2, :], R=[cst], W=[cst_b])
        ident_b = cst_b[:, 0, :]
        condT = sb("condT", [128, 8], F32)
        k.dma('sp', condT[:], cT_in, R=[Dw], W=[condT])
        k.act(condT[:], condT[:], AF.Silu, R=[condT], W=[condT])
        modT = sb("modT", [128, 48], F32)
        fnw = sb("fnw", [128, 8], F32)
        k.dma('sp', fnw[:], fnwT, R=[Dw], W=[fnw])

        def phase_ada(l):
            with ExitStack() as ps_:
                wr = Ring([sb("wada%d" % i, [128, 8, 768], F32, stack=ps_) for i in range(2)])
                bt = sb("badat", [128, 48], F32, stack=ps_)
                k.dma('sp', bt[:], b_adaT[l], R=[Dw], W=[bt])
                ps = psum.get()
                for g in range(8):
                    wb = wr.get()
                    k.dma('sp', wb[:], w_ada[l][:, g * 768:(g + 1) * 768].rearrange('(kc p) n -> p kc n', p=128),
                          R=[Dw], W=[wb])
                    for jj in range(6):
                        j = g * 6 + jj
                        for kc in range(8):
                            k.mm(ps, ps[:, j:j + 1], wb[:, kc, jj * 128:(jj + 1) * 128], condT[:, kc:kc + 1],
                                 R=[wb, condT], start=(kc == 0), stop=(kc == 7))
                k.tt('dve', modT[:], ps[:, 0:48], bt[:], ALU.add, R=[ps, bt], W=[modT])
                k.ts('dve', modT[:, 8:16], modT[:, 8:16], 1.0, None, ALU.add, R=[modT], W=[modT])
                k.ts('dve', modT[:, 32:40], modT[:, 32:40], 1.0, None, ALU.add, R=[modT], W=[modT])
                k.barrier()

        def norm_tile(xt, xap, sq, hdst, hbuf, sc_off, sh_off, hf=None):
            k.act(sq[:], xap, AF.Square, R=[xt], W=[sq])
            ps = psum.get()
            for kc in range(8):
                k.mm(ps, ps[:, :], ones_f, sq[:, kc, :], R=[sq, cst], start=(kc == 0), stop=(kc == 7))
            rstd = rstd_ring.get()
            rsqrt(rstd, rstd[:], ps, ps[:, :], 1.0 / D)
            for kc in range(8):
                k.tt('pool' if kc % 2 else 'dve', sq[:, kc, :], xap[:, kc, :], rstd[:], ALU.mult,
                     R=[xt, rstd], W=[sq])
            for kc in range(8):
                if sc_off is None:
                    k.ts('dve', hdst(kc), sq[:, kc, :], fnw[:, kc:kc + 1], None, ALU.mult, R=[sq, fnw], W=[hbuf])
                else:
                    k.ts('dve', hdst(kc), sq[:, kc, :], modT[:, sc_off + kc:sc_off + kc + 1],
                         modT[:, sh_off + kc:sh_off + kc + 1], ALU.mult, ALU.add, R=[sq, modT], W=[hbuf])
                    if hf is not None:
                        k.ts('pool', hf[0][:, kc, :], sq[:, kc, :], modT[:, sc_off + kc:sc_off + kc + 1],
                             modT[:, sh_off + kc:sh_off + kc + 1], ALU.mult, ALU.add, R=[sq, modT], W=[hf[0]])

        epsT = sb("epsT", [128, 1], F32)
        k.op('dve', lambda e: e.memset(epsT[:], EPS), W=[epsT])

        def rsqrt(ob, out, ib, in_, scale):
            k.act(out, in_, AF.Sqrt, R=[ib, epsT], W=[ob], bias=epsT[:out.shape[0], 0:1], scale=scale)
            k.op('dve', lambda e: e.reciprocal(out, out), R=[ob], W=[ob])

        rstd_ring = Ring([sb("rstd%d" % i, [128, 512], F32) for i in range(2)])

        def phase_inproj(l, xsrc, Dxsrc):
            with ExitStack() as ps_:
                h1 = sb("h1", [128, 8, T], BF16, multi=True, stack=ps_)
                xr = Ring([sb("xt%d" % i, [128, 8, 512], F32, stack=ps_) for i in range(2)])
                sq = sb("sq", [128, 8, 512], F32, stack=ps_)
                hf = sb("hf", [128, 8, 512], F32, stack=ps_)
                wsm = sb("wsm", [128, 8, 16], F32, stack=ps_)
                sm_st = Ring([sb("smst%d" % i, [128, 16], F32, stack=ps_) for i in range(2)])
                k.dma('sp', wsm[:, :, 0:8], w_in[l][:, C_A:C_A + 8].rearrange('(kc p) n -> p kc n', p=128),
                      R=[Dw], W=[wsm])
                k.dma('sp', wsm[:, :, 8:16], w_in[l][:, C_DT:C_DT + 8].rearrange('(kc p) n -> p kc n', p=128),
                      R=[Dw], W=[wsm])
                for tt in range(NT):
                    xt = xr.get()
                    for (a_, b_, sap) in xsrc(tt):
                        k.dma('sp', xt[:, a_:b_, :], sap, R=[Dxsrc], W=[xt])
                    norm_tile(xt, xt[:], sq, lambda kc: h1[:, kc, tt * 512:(tt + 1) * 512], h1, 8, 0, hf=[hf])
                    for q in range(4):
                        ps = psum.get()
                        for kc in range(8):
                            k.mm(ps, ps[:, 0:16], hf[:, kc, q * 128:(q + 1) * 128], wsm[:, kc, :], R=[hf, wsm],
                                 start=(kc == 0), stop=(kc == 7))
                        st = sm_st.get()
                        k.copy('act', st[:], ps[:, 0:16], R=[ps], W=[st])
                        t0 = tt * 512 + q * 128
                        k.dma('sp', abdt[t0:t0 + 128, :], st[:], R=[st], W=[Dabdt])
                groups = [(C_QKV, 1536, 'copy'), (C_GZ, 512, 'silu'), (C_SZ, 512, 'silu'), (C_XBC, 1024, 'copy'),
                          (C_CQ, 512, 'copy'), (C_CKV, 256, 'copy'), (C_KR, 64, 'copy'), (C_GATES, 3072, 'sig')]
                wr = Ring([sb("win%d" % i, [128, 8, 512], BF16, stack=ps_) for i in range(2)])
                stg = Ring([sb("stg%d" % i, [128, 512], BF16, stack=ps_) for i in range(4)])
                ecnt = 0
                for (c0, ncols, post) in groups:
                    for blk in range(0, ncols, 512):
                        bw = min(512, ncols - blk)
                        wt = wr.get()
                        k.dma('pool', wt[:, :, :bw],
                              w_in[l][:, c0 + blk:c0 + blk + bw].rearrange('(kc p) n -> p kc n', p=128),
                              R=[Dw], W=[wt])
                        for tt in range(NT):
                            for ct in range(0, bw, 128):
                                m = min(128, bw - ct)
                                ps = psum.get()
                                for kc in range(8):
                                    k.mm(ps, ps[:m, :], wt[:, kc, ct:ct + m], h1[:, kc, tt * 512:(tt + 1) * 512],
                                         R=[wt, h1], start=(kc == 0), stop=(kc == 7))
                                st = stg.get()
                                if post == 'silu':
                                    k.act(st[:m, :], ps[:m, :], AF.Silu, R=[ps], W=[st])
                                elif post == 'sig':
                                    k.act(st[:m, :], ps[:m, :], AF.Sigmoid, R=[ps], W=[st])
                                else:
                                    ecnt += 1
                                    k.copy('dve' if ecnt % 2 else 'act', st[:m, :], ps[:m, :], R=[ps], W=[st])
                                r0 = c0 + blk + ct
                                k.dma('sp', proj[r0:r0 + m, tt * 512:(tt + 1) * 512], st[:m, :], R=[st], W=[Dproj])
                k.barrier()


        def dump(name, b, ap=None, dt=None):
            if not debug:
                return
            ap = b[:] if ap is None else ap
            uid[0] += 1
            t = nc.dram_tensor("dbg_%s_%d" % (name, uid[0]), list(ap.shape), dt or ap.dtype, kind="ExternalOutput").ap()
            k.dma('sp', t, ap, R=[b], W=[Buf(None, True)])

        cols = sb("cols", [128, 4], F32)
        k.op('dve', lambda e: e.memset(cols[:, 0:1], 1.0), W=[cols])
        k.op('dve', lambda e: e.memset(cols[:, 1:2], -np.pi), W=[cols])
        invf = sb("invf", [128, 1], F32)
        k.dma('sp', invf[:], invf_in, R=[Dw], W=[invf])
        ATT_SCALE = 192.0 ** -0.5

        def phase_mla(l):
            with ExitStack() as ps_:
                wuq = sb("wuq", [128, 4, 768], BF16, stack=ps_)
                wuqr = sb("wuqr", [128, 4, 2, 128], BF16, stack=ps_)
                wuk = sb("wuk", [128, 2, 512], BF16, stack=ps_)
                wuv = sb("wuv", [128, 2, 512], BF16, stack=ps_)
                qnw = sb("qnw", [128, 4], F32, stack=ps_)
                kvnw = sb("kvnw", [128, 2], F32, stack=ps_)
                k.dma('pool', wuq[:], mla_wuq[l].rearrange('(kc p) n -> p kc n', p=128), R=[Dw], W=[wuq])
                for h in range(HM):
                    k.dma('pool', wuqr[:, :, h // 2, (h % 2) * 64:(h % 2) * 64 + 64],
                          mla_wuq[l][:, h * 192 + 128:h * 192 + 192].rearrange('(kc p) n -> p kc n', p=128),
                          R=[Dw], W=[wuqr])
                k.dma('pool', wuk[:], mla_wuk[l].rearrange('(kc p) n -> p kc n', p=128), R=[Dw], W=[wuk])
                k.dma('pool', wuv[:], mla_wuv[l].rearrange('(kc p) n -> p kc n', p=128), R=[Dw], W=[wuv])
                k.dma('sp', qnw[:], mla_qnw[l], R=[Dw], W=[qnw])
                k.dma('sp', kvnw[:], mla_kvnw[l], R=[Dw], W=[kvnw])
                qn = sb("qn", [128, 4, T], BF16, multi=True, stack=ps_)
                qr = sb("qr", [128, 2, T], BF16, multi=True, stack=ps_)
                kn = sb("kn", [128, 4, T], BF16, multi=True, stack=ps_)
                krp = sb("krp", [128, T], BF16, multi=True, stack=ps_)
                vtm = sb("vtm", [128, NQ, 512], BF16, multi=True, stack=ps_)
                with ExitStack() as p1:
                    cin = Ring([sb("mcin%d" % i, [128, 7, 512], BF16, stack=p1) for i in range(2)])
                    sqm = sb("msq", [128, 4, 512], F32, stack=p1)
                    cqn = sb("cqn", [128, 4, 512], BF16, stack=p1)
                    ckvn = sb("ckvn", [128, 2, 512], BF16, stack=p1)
                    posi = sb("posi", [128, 512], I32, stack=p1)
                    posf = sb("posf", [128, 512], F32, stack=p1)
                    frac = sb("frac", [128, 512], F32, stack=p1)
                    fint = sb("fint", [128, 512], I32, stack=p1)
                    ftmp = sb("ftmp", [128, 512], F32, stack=p1)
                    sinT = sb("sinT", [128, 512], F32, stack=p1)
                    cosT = sb("cosT", [128, 512], F32, stack=p1)
                    rf = sb("rf", [128, 512], F32, stack=p1)
                    r1 = sb("r1", [128, 512], F32, stack=p1)
                    r2 = sb("r2", [128, 512], F32, stack=p1)
                    rstd = sb("mrstd", [128, 512], F32, stack=p1)
                    rot2 = cst[:, 5, :]

                    def rope(src_b, src_ap, dst_b, dst_ap, scale):
                        k.copy('act', rf[:], src_ap, R=[src_b], W=[rf])
                        ps = psum.get()
                        k.mm(ps, ps[:, :], rot2, rf[:], R=[cst, rf])
                        k.stt('dve', r1[:], rf[:], scale, cosT[:], ALU.mult, ALU.mult, R=[rf, cosT], W=[r1])
                        k.stt('dve', r2[:], ps[:, :], scale, sinT[:], ALU.mult, ALU.mult, R=[ps, sinT], W=[r2])
                        k.tt('dve', dst_ap, r1[:], r2[:], ALU.add, R=[r1, r2], W=[dst_b])

                    for tt in range(NT):
                        ts_ = slice(tt * 512, (tt + 1) * 512)
                        ci = cin.get()
                        k.dma('sp', ci[:, 0:6, :], proj[C_CQ:C_CQ + 768, ts_].rearrange('(kc p) t -> p kc t', p=128),
                              R=[Dproj], W=[ci])
                        k.dma('sp', ci[0:64, 6, :], proj[C_KR:C_KR + 64, ts_], R=[Dproj], W=[ci])
                        k.dma('sp', ci[64:128, 6, :], proj[C_KR:C_KR + 64, ts_], R=[Dproj], W=[ci])
                        k.dma('sp', posi[:], pos_in[0:1, ts_].partition_broadcast(128), R=[Dw], W=[posi])
                        k.copy('dve', posf[:], posi[:], R=[posi], W=[posf])
                        for (off, dst) in ((0.5, sinT), (0.75, cosT)):
                            k.ts('dve', frac[:], posf[:], invf[:, 0:1], 1.0 / (2 * np.pi), ALU.mult, ALU.mult,
                                 R=[posf, invf], W=[frac])
                            k.ts('dve', frac[:], frac[:], off, None, ALU.add, R=[frac], W=[frac])
                            k.copy('dve', fint[:], frac[:], R=[frac], W=[fint])
                            k.copy('dve', ftmp[:], fint[:], R=[fint], W=[ftmp])
                            k.tt('dve', frac[:], frac[:], ftmp[:], ALU.subtract, R=[frac, ftmp], W=[frac])
                            k.ts('dve', ftmp[:], frac[:], 0.0, None, ALU.is_lt, R=[frac], W=[ftmp])
                            k.tt('dve', frac[:], frac[:], ftmp[:], ALU.add, R=[frac, ftmp], W=[frac])
                            k.act(dst[:], frac[:], AF.Sin, R=[frac, cols], W=[dst], bias=cols[:, 1:2],
                                  scale=2 * np.pi)
                        for (c0, nk_, wv, dstb) in ((0, 4, qnw, cqn), (4, 2, kvnw, ckvn)):
                            k.act(sqm[:, 0:nk_, :], ci[:, c0:c0 + nk_, :], AF.Square, R=[ci], W=[sqm])
                            ps = psum.get()
                            for kc in range(nk_):
                                k.mm(ps, ps[:, :], ones_f, sqm[:, kc, :], R=[cst, sqm], start=(kc == 0),
                                     stop=(kc == nk_ - 1))
                            rsqrt(rstd, rstd[:], ps, ps[:, :], 1.0 / (nk_ * 128))
                            for kc in range(nk_):
                                k.stt('dve', dstb[:, kc, :], ci[:, c0 + kc, :], wv[:, kc:kc + 1], rstd[:], ALU.mult,
                                      ALU.mult, R=[ci, wv, rstd], W=[dstb])
                        for h in range(HM):
                            ps = psum.get()
                            for kc in range(4):
                                k.mm(ps, ps[:, :], wuq[:, kc, h * 192:h * 192 + 128], cqn[:, kc, :], R=[wuq, cqn],
                                     start=(kc == 0), stop=(kc == 3))
                            k.act(qn[:, h, ts_], ps[:, :], AF.Copy, R=[ps], W=[qn], scale=ATT_SCALE)
                            ps = psum.get()
                            for kc in range(2):
                                k.mm(ps, ps[:, :], wuk[:, kc, h * 128:(h + 1) * 128], ckvn[:, kc, :], R=[wuk, ckvn],
                                     start=(kc == 0), stop=(kc == 1))
                            k.copy('dve', kn[:, h, ts_], ps[:, :], R=[ps], W=[kn])
                        for hp in range(HM // 2):
                            ps = psum.get()
                            for kc in range(4):
                                k.mm(ps, ps[:, :], wuqr[:, kc, hp, :], cqn[:, kc, :], R=[wuqr, cqn],
                                     start=(kc == 0), stop=(kc == 3))
                            rope(ps, ps[:, :], qr, qr[:, hp, ts_], ATT_SCALE)
                        rope(ci, ci[:, 6, :], krp, krp[:, ts_], 1.0)
                        for q in range(4):
                            ps = psum.get()
                            for kc in range(2):
                                k.mm(ps, ps[:, :], ckvn[:, kc, q * 128:(q + 1) * 128], wuv[:, kc, :], R=[ckvn, wuv],
                                     start=(kc == 0), stop=(kc == 1))
                            k.copy('act', vtm[:, tt * 4 + q, :], ps[:, :], R=[ps], W=[vtm])
                dump("qn", qn); dump("qr", qr); dump("kn", kn); dump("krp", krp); dump("vtm", vtm)
                with ExitStack() as p2:
                    WM = 2
                    slots = []
                    for i in range(WM):
                        slots.append(dict(
                            Ssb=sb("Ssb%d" % i, [128, T], F32, stack=p2), Psb=sb("Psb%d" % i, [128, T], BF16, stack=p2),
                            ptr=Ring([sb("pt%d_%d" % (i, j), [128, 4, 128], BF16, stack=p2) for j in range(2)]),
                            sc=sb("asc%d" % i, [128, 4], F32, stack=p2), dg=sb("adg%d" % i, [128, 128], BF16, stack=p2),
                            po=psacc.bufs[i]))
                    ost = [sb("aost%d" % i, [128, 4, 128], BF16, multi=True, stack=p2) for i in range(2)]
                    done = {}

                    def att(key):
                        qi, h = key
                        B = slots[(qi * 4 + h) % WM]
                        Ssb, Psb, ptr, s_, dg, po = B['Ssb'], B['Psb'], B['ptr'], B['sc'], B['dg'], B['po']
                        ot = ost[qi % 2]
                        nk = (qi + 1) * 128
                        qs = slice(qi * 128, (qi + 1) * 128)
                        hp, ho = h // 2, (h % 2) * 64
                        for kb in range(0, nk, 512):
                            w = min(512, nk - kb)
                            ps = psum.get()
                            k.mm(ps, ps[:, :w], qn[:, h, qs], kn[:, h, kb:kb + w], R=[qn, kn], start=True,
                                 stop=False)
                            k.mm(ps, ps[:, :w], qr[ho:ho + 64, hp, qs], krp[ho:ho + 64, kb:kb + w], R=[qr, krp],
                                 start=False, stop=True)
                            if kb + w == nk:
                                if w > 128:
                                    k.copy('act', Ssb[:, kb:nk - 128], ps[:, :w - 128], R=[ps], W=[Ssb])
                                k.tt('dve', Ssb[:, nk - 128:nk], ps[:, w - 128:w], cst[:, 3, :], ALU.subtract,
                                     R=[ps, cst], W=[Ssb])
                            else:
                                k.copy('act', Ssb[:, kb:kb + w], ps[:, :w], R=[ps], W=[Ssb])
                            yield
                        k.op('dve', lambda e: e.reduce_max(s_[:, 0:1], Ssb[:, :nk], AX.X), R=[Ssb], W=[s_])
                        k.ts('dve', s_[:, 1:2], s_[:, 0:1], -1.0, None, ALU.mult, R=[s_], W=[s_])
                        yield
                        k.act(Psb[:, :nk], Ssb[:, :nk], AF.Exp, R=[Ssb, s_], W=[Psb, s_], bias=s_[:, 1:2],
                              scale=1.0, accum_out=s_[:, 2:3])
                        yield
                        k.op('dve', lambda e: e.reciprocal(s_[:, 3:4], s_[:, 2:3]), R=[s_], W=[s_])
                        k.ts('dve', dg[:], ident_b, s_[:, 3:4], None, ALU.mult, R=[cst_b, s_], W=[dg])
                        yield
                        nb = nk // 128
                        for b4 in range(0, nb, 4):
                            n4 = min(4, nb - b4)
                            ps = psum.get()
                            for j in range(n4):
                                kb2 = (b4 + j) * 128
                                k.mm(ps, ps[:, j * 128:(j + 1) * 128], Psb[:, kb2:kb2 + 128], dg[:], R=[Psb, dg])
                            pt = ptr.get()
                            k.copy('dve' if (b4 // 4) % 2 else 'act', pt[:, 0:n4, :],
                                   ps[:, 0:n4 * 128].rearrange('p (j t) -> p j t', j=n4), R=[ps], W=[pt])
                            for j in range(n4):
                                kblk = b4 + j
                                k.mm(po, po[:, 0:128], vtm[:, kblk, h * 128:(h + 1) * 128], pt[:, j, :],
                                     R=[vtm, pt], start=(kblk == 0), stop=(kblk == nb - 1))
                            yield
                        k.copy('act', ot[:, h, :], po[:, 0:128], R=[po], W=[ot])

                    def att_done(key):
                        qi, h = key
                        done[qi] = done.get(qi, 0) + 1
                        if done[qi] == HM:
                            qs = slice(qi * 128, (qi + 1) * 128)
                            k.dma('sp', (ycm if pair else y_d[2])[:, qs].rearrange('(h p) t -> p h t', p=128),
                                  ost[qi % 2][:, 0:HM, :], R=[ost[qi % 2]], W=[Dycm if pair else Dy[2]])

                    run_pipeline([(qi, h) for qi in range(NQ) for h in range(HM)], WM, att, att_done)
                if pair:
                    k.barrier()
                    allgather([(ycm, y_d[2])], Dycm, Dy[2])
                k.barrier()


        def bc(ap, shape):
            return ap.to_broadcast(list(shape))

        def run_pipeline(items, W, start_fn, finish_fn):
            active = []
            it = iter(items)
            pending = True
            while True:
                while len(active) < W and pending:
                    try:
                        key = next(it)
                    except StopIteration:
                        pending = False
                        break
                    active.append((key, start_fn(key)))
                if not active:
                    break
                for ent in list(active):
                    try:
                        next(ent[1])
                    except StopIteration:
                        active.remove(ent)
                        finish_fn(ent[0])

        def conv_silu(cin, cw, nchan, dst, dstb, tmpr, bias=None):
            for c in (range(nchan) if isinstance(nchan, int) else nchan):
                tb = tmpr.get()
                k.ts('dve', tb[:], cin[:, c, 0:512], cw[:, c, 0:1], None, ALU.mult, R=[cin, cw], W=[tb])
                for kk in range(1, 4):
                    k.stt('dve', tb[:], cin[:, c, kk:kk + 512], cw[:, c, kk:kk + 1], tb[:], ALU.mult,
                          ALU.add, R=[cin, cw, tb], W=[tb])
                if bias is None:
                    k.act(dst[:, c, :], tb[:], AF.Silu, R=[tb], W=[dstb])
                else:
                    k.act(dst[:, c, :], tb[:], AF.Silu, R=[tb, bias], W=[dstb], bias=bias[:, c:c + 1])

        def load_halo(cin, row0, nrows, tt):
            if tt == 0:
                k.op('dve', lambda e: e.memset(cin[:, :, 0:3], 0.0), W=[cin])
                k.dma('sp', cin[:, :, 3:515], proj[row0:row0 + nrows, 0:512].rearrange('(c p) t -> p c t', p=128),
                      R=[Dproj], W=[cin])
            else:
                k.dma('sp', cin[:, :, :],
                      proj[row0:row0 + nrows, tt * 512 - 3:tt * 512 + 512].rearrange('(c p) t -> p c t', p=128),
                      R=[Dproj], W=[cin])

        tri64 = cst[0:64, 2, 0:64]
        ones64 = cst[0:64, 1, 0:64]
        ones64w = cst[0:64, 1, :]
        posm64 = cst[0:64, 3, 0:64]
        strict64 = cst[0:64, 4, 0:64]
        lowm64 = cst[0:64, 6, 0:64]
        id64 = cst[0:64, 0, 0:64]

        def phase_ssd(l):
            with ExitStack() as ps_:
                def t_(name, shape, dt=F32, multi=False):
                    return sb(name, shape, dt, multi=multi, stack=ps_)
                cw = t_("scw", [128, 8, 4]); cb = t_("scb", [128, 8]); alog = t_("salog", [128, 8])
                dtb = t_("sdtb", [128, 8]); dexp = t_("sdexp", [128, 4]); nw = t_("snw", [128, 4])
                for (d_, s_) in ((cw, ssm_convT), (cb, ssm_convb), (alog, ssm_alog), (dtb, ssm_dtb), (dexp, ssm_dexp),
                                 (nw, ssm_nw)):
                    k.dma('sp', d_[:], s_[l], R=[Dw], W=[d_])
                k.act(alog[:], alog[:], AF.Exp, R=[alog], W=[alog])
                k.ts('dve', alog[:], alog[:], -1.0, None, ALU.mult, R=[alog], W=[alog])
                raw = t_("sraw", [64, NCH, 16])
                k.dma('sp', raw[:], abdt.rearrange('(c p) k -> p c k', p=64), R=[Dabdt], W=[raw])
                dt = t_("sdt", [64, NCH, 8]); ad = t_("sad", [64, NCH, 8]); acs = t_("sacs", [64, NCH, 8])
                acl = t_("sacl", [128, NCH, 8]); cd = t_("scd", [128, NCH, 8]); ds = t_("sds", [64, NCH, 8])
                eacs = t_("seacs", [64, NCH, 8])
                k.tt('dve', dt[:], raw[:, :, 8:16], bc(dtb[0:64, :].unsqueeze(1), [64, NCH, 8]), ALU.add,
                     R=[raw, dtb], W=[dt])
                k.act(dt[:], dt[:], AF.Exp, R=[dt], W=[dt])
                k.act(dt[:], dt[:], AF.Ln, R=[dt, cols], W=[dt], bias=cols[0:64, 0:1])
                k.tt('dve', ad[:], dt[:], bc(alog[0:64, :].unsqueeze(1), [64, NCH, 8]), ALU.mult, R=[dt, alog], W=[ad])
                adf = ad[:].rearrange('p c h -> p (c h)')
                ps = psum.get()
                k.mm(ps, ps[:64, :NCH * 8], tri64, adf, R=[cst, ad])
                k.copy('dve', acs[:].rearrange('p c h -> p (c h)'), ps[:64, :NCH * 8], R=[ps], W=[acs])
                ps = psum.get()
                k.mm(ps, ps[:, :NCH * 8], ones64w, adf, R=[cst, ad])
                k.copy('dve', acl[:].rearrange('p c h -> p (c h)'), ps[:, :NCH * 8], R=[ps], W=[acl])
                k.act(cd[:], acl[:], AF.Exp, R=[acl], W=[cd])
                k.tt('dve', ds[:], acl[0:64], acs[:], ALU.subtract, R=[acl, acs], W=[ds])
                k.act(ds[:], ds[:], AF.Exp, R=[ds], W=[ds])
                k.act(eacs[:], acs[:], AF.Exp, R=[acs], W=[eacs])
                state = t_("sstate", [128, 8, 64])
                k.op('dve', lambda e: e.memset(state[:], 0.0), W=[state])
                cinr = Ring([t_("scin%d" % i, [128, 8, 515], BF16) for i in range(2)])
                szr = Ring([t_("ssz%d" % i, [128, 4, 512], BF16) for i in range(2)])
                xfr = Ring([t_("sxf%d" % i, [128, 8, 512]) for i in range(2)])
                ctr = Ring([t_("sct%d" % i, [128, 512]) for i in range(2)])
                ystr = Ring([t_("syst%d" % i, [128, 4, 512], BF16, multi=True) for i in range(2)])
                WS = 2
                slots = []
                for i in range(WS):
                    slots.append(dict(
                        trig=t_("strig%d" % i, [64, 8, 64]), t1=t_("st1%d" % i, [64, 8, 64]), MT=t_("sMT%d" % i, [64, 8, 64]),
                        X=t_("sX%d" % i, [64, 8, 64]), Xds=t_("sXds%d" % i, [64, 8, 64]), Btm=t_("sBtm%d" % i, [64, 2, 128]),
                        yt=t_("syt%d" % i, [64, 8, 64]), ytm=t_("sytm%d" % i, [64, 8, 64]), yfm=t_("syfm%d" % i, [128, 4, 64]),
                        tmp=t_("stmp%d" % i, [128, 4, 64]), sq=t_("ssq%d" % i, [128, 4, 64]), rs=t_("srs%d" % i, [128, 2, 64])))
                tiles = {}
                turn = [0]
                done = {}

                def prep(tt):
                    cin = cinr.get(); szt = szr.get(); yst = ystr.get(); xf = xfr.get()
                    load_halo(cin, C_XBC, 1024, tt)
                    k.dma('sp', szt[:], proj[C_SZ:C_SZ + 512, tt * 512:(tt + 1) * 512].rearrange(
                        '(c p) t -> p c t', p=128), R=[Dproj], W=[szt])
                    conv_silu(cin, cw, 8, xf, xf, ctr, bias=cb)
                    tiles[tt] = (xf, szt, yst)

                def chunk(ch):
                    tt, cc = ch // 8, ch % 8
                    if cc == 0:
                        prep(tt)
                    xf, szt, yst = tiles[tt]
                    B = slots[ch % WS]
                    trig, t1, MT, X, Xds, Btm = B['trig'], B['t1'], B['MT'], B['X'], B['Xds'], B['Btm']
                    yt, ytm, yfm, tmp, sq, rs = B['yt'], B['ytm'], B['yfm'], B['tmp'], B['sq'], B['rs']
                    cs = slice(cc * 64, cc * 64 + 64)
                    ps_cb = psum.get()
                    for g in range(2):
                        k.mm(ps_cb, ps_cb[:64, g * 64:(g + 1) * 64], xf[:, 4 + g, cs], xf[:, 6 + g, cs], R=[xf])
                    k.tt('pool', trig[:], bc(tri64.unsqueeze(1), [64, 8, 64]),
                         bc(ad[:, ch, :].unsqueeze(2), [64, 8, 64]), ALU.mult, R=[cst, ad], W=[trig])
                    ps_r = psum.get()
                    k.mm(ps_r, ps_r[:64, :], ones64, trig[:].rearrange('p h l -> p (h l)'), R=[cst, trig])
                    k.tt('dve', t1[:], ps_r[:64, :].rearrange('p (h l) -> p h l', h=8),
                         bc(lowm64.unsqueeze(1), [64, 8, 64]), ALU.subtract, R=[ps_r, cst], W=[t1])
                    k.tt('dve', t1[:], t1[:], bc(acs[:, ch, :].unsqueeze(2), [64, 8, 64]), ALU.subtract,
                         R=[t1, acs], W=[t1])
                    k.act(t1[:], t1[:], AF.Exp, R=[t1], W=[t1])
                    k.tt('dve', MT[:].rearrange('p (g e) l -> p g e l', g=2),
                         t1[:].rearrange('p (g e) l -> p g e l', g=2),
                         bc(ps_cb[:64, 0:128].rearrange('p (g l) -> p g l', g=2).unsqueeze(2), [64, 2, 4, 64]),
                         ALU.mult, R=[t1, ps_cb], W=[MT])
                    yield
                    ps_x = psum.get()
                    for kc in range(4):
                        k.tr(ps_x, ps_x[:64, kc * 128:(kc + 1) * 128], xf[:, kc, cs], ident_f, R=[xf, cst])
                    k.tt('dve', X[:], ps_x[:64, :].rearrange('p (h q) -> p h q', h=8),
                         bc(dt[:, ch, :].unsqueeze(2), [64, 8, 64]), ALU.mult, R=[ps_x, dt], W=[X])
                    k.tt('pool', Xds[:], X[:], bc(ds[:, ch, :].unsqueeze(2), [64, 8, 64]), ALU.mult,
                         R=[X, ds], W=[Xds])
                    yield
                    ps_b = psum.get()
                    for g in range(2):
                        k.tr(ps_b, ps_b[:64, g * 128:(g + 1) * 128], xf[:, 4 + g, cs], ident_f, R=[xf, cst])
                    k.copy('act', Btm[:].rearrange('p g n -> p (g n)'), ps_b[:64, 0:256], R=[ps_b], W=[Btm])
                    yield
                    while turn[0] != ch:
                        yield
                    ps_y1 = psum.get()
                    for h in range(8):
                        k.mm(ps_y1, ps_y1[:64, h * 64:(h + 1) * 64], MT[:, h, :], X[:, h, :], R=[MT, X])
                    ps_y2 = psum.get()
                    for h in range(8):
                        k.mm(ps_y2, ps_y2[:64, h * 64:(h + 1) * 64], xf[:, 6 + h // 4, cs], state[:, h, :],
                             R=[xf, state])
                    k.tt('dve', yt[:], ps_y2[:64, :].rearrange('p (h q) -> p h q', h=8),
                         bc(eacs[:, ch, :].unsqueeze(2), [64, 8, 64]), ALU.mult, R=[ps_y2, eacs], W=[yt])
                    k.tt('dve', ytm[:], yt[:], ps_y1[:64, :].rearrange('p (h q) -> p h q', h=8), ALU.add,
                         R=[yt, ps_y1], W=[ytm])
                    ps_s = psum.get()
                    for h in range(8):
                        k.mm(ps_s, ps_s[:, h * 64:(h + 1) * 64], Btm[:, h // 4, :], Xds[:, h, :], R=[Btm, Xds])
                    k.tt('dve', state[:], state[:], bc(cd[:, ch, :].unsqueeze(2), [128, 8, 64]), ALU.mult,
                         R=[state, cd], W=[state])
                    k.tt('dve', state[:], state[:], ps_s[:, :].rearrange('p (h q) -> p h q', h=8), ALU.add,
                         R=[state, ps_s], W=[state])
                    turn[0] = ch + 1
                    yield
                    ps_t = psum.get()
                    ytf = ytm[:].rearrange('p h q -> p (h q)')
                    for kc in range(4):
                        k.tr(ps_t, ps_t[:, kc * 64:(kc + 1) * 64], ytf[:, kc * 128:(kc + 1) * 128], id64,
                             R=[ytm, cst])
                    k.tt('pool', tmp[:], xf[:, 0:4, cs], bc(dexp[:, :].unsqueeze(2), [128, 4, 64]), ALU.mult,
                         R=[xf, dexp], W=[tmp])
                    k.tt('dve', yfm[:], tmp[:], ps_t[:, 0:256].rearrange('p (c q) -> p c q', c=4), ALU.add,
                         R=[tmp, ps_t], W=[yfm])
                    k.tt('dve', yfm[:], yfm[:], szt[:, :, cs], ALU.mult, R=[yfm, szt], W=[yfm])
                    k.act(sq[:], yfm[:], AF.Square, R=[yfm], W=[sq])
                    yield
                    ps_n = psum.get()
                    for g in range(2):
                        for k2 in range(2):
                            k.mm(ps_n, ps_n[:, g * 64:(g + 1) * 64], ones_f, sq[:, g * 2 + k2, :], R=[cst, sq],
                                 start=(k2 == 0), stop=(k2 == 1))
                    rsqrt(rs, rs[:].rearrange('p g q -> p (g q)'), ps_n, ps_n[:, 0:128], 1.0 / 256)
                    for kc in range(4):
                        k.stt('dve', yst[:, kc, cs], yfm[:, kc, :], nw[:, kc:kc + 1], rs[:, kc // 2, :], ALU.mult,
                              ALU.mult, R=[yfm, nw, rs], W=[yst])

                def chunk_done(ch):
                    tt = ch // 8
                    done[tt] = done.get(tt, 0) + 1
                    if done[tt] == 8:
                        yst = tiles[tt][2]
                        k.dma('sp', y_d[1][:, tt * 512:(tt + 1) * 512].rearrange('(c p) t -> p c t', p=128), yst[:],
                              R=[yst], W=[Dy[1]])

                run_pipeline(list(range(NCH)), WS, chunk, chunk_done)
                k.barrier()

        def phase_gdn(l):
            with ExitStack() as ps_:
                def t_(name, shape, dt=F32, multi=False):
                    return sb(name, shape, dt, multi=multi, stack=ps_)
                cw = t_("gcw", [128, 12, 4]); alog = t_("galog", [128, 4]); dtb = t_("gdtb", [128, 4])
                nw = t_("gnw", [128, 1])
                for (d_, s_) in ((cw, gdn_convT), (alog, gdn_alog), (dtb, gdn_dtb), (nw, gdn_nw)):
                    k.dma('sp', d_[:], s_[l], R=[Dw], W=[d_])
                k.act(alog[:], alog[:], AF.Exp, R=[alog], W=[alog])
                k.ts('dve', alog[:], alog[:], -1.0, None, ALU.mult, R=[alog], W=[alog])
                raw = t_("graw", [64, NCH, 16])
                k.dma('sp', raw[:], abdt.rearrange('(c p) k -> p c k', p=64), R=[Dabdt], W=[raw])
                beta = t_("gbeta", [64, NCH, HG]); nbeta = t_("gnbeta", [64, NCH, HG]); g = t_("gg", [64, NCH, HG])
                G = t_("gG", [64, NCH, HG]); Glb = t_("gGlb", [128, NCH, HG]); gl = t_("ggl", [128, NCH, HG])
                eG = t_("geG", [64, NCH, HG]); kdsc = t_("gkdsc", [64, NCH, HG]); bexpG = t_("gbexpG", [64, NCH, HG])
                k.act(beta[:], raw[:, :, 4:4 + HG], AF.Sigmoid, R=[raw], W=[beta])
                k.ts('dve', nbeta[:], beta[:], -1.0, None, ALU.mult, R=[beta], W=[nbeta])
                k.tt('dve', g[:], raw[:, :, 0:HG], bc(dtb[0:64, 0:HG].unsqueeze(1), [64, NCH, HG]), ALU.add,
                     R=[raw, dtb], W=[g])
                k.act(g[:], g[:], AF.Exp, R=[g], W=[g])
                k.act(g[:], g[:], AF.Ln, R=[g, cols], W=[g], bias=cols[0:64, 0:1])
                k.tt('dve', g[:], g[:], bc(alog[0:64, 0:HG].unsqueeze(1), [64, NCH, HG]), ALU.mult, R=[g, alog], W=[g])
                gf = g[:].rearrange('p c h -> p (c h)')
                ps = psum.get()
                k.mm(ps, ps[:64, :NCH * HG], tri64, gf, R=[cst, g])
                k.copy('dve', G[:].rearrange('p c h -> p (c h)'), ps[:64, :NCH * HG], R=[ps], W=[G])
                ps = psum.get()
                k.mm(ps, ps[:, :NCH * HG], ones64w, gf, R=[cst, g])
                k.copy('dve', Glb[:].rearrange('p c h -> p (c h)'), ps[:, :NCH * HG], R=[ps], W=[Glb])
                k.act(gl[:], Glb[:], AF.Exp, R=[Glb], W=[gl])
                k.act(eG[:], G[:], AF.Exp, R=[G], W=[eG])
                k.tt('dve', kdsc[:], Glb[0:64], G[:], ALU.subtract, R=[Glb, G], W=[kdsc])
                k.act(kdsc[:], kdsc[:], AF.Exp, R=[kdsc], W=[kdsc])
                k.tt('dve', bexpG[:], beta[:], eG[:], ALU.mult, R=[beta, eG], W=[bexpG])
                S = t_("gS", [128, HG, 128])
                k.op('dve', lambda e: e.memset(S[:], 0.0), W=[S])
                cinr = Ring([t_("gcin%d" % i, [128, 12, 515], BF16) for i in range(2)])
                gzr = Ring([t_("ggz%d" % i, [128, HG, 512], BF16) for i in range(2)])
                qkvr = Ring([t_("gqkv%d" % i, [128, 12, 512]) for i in range(2)])
                ctr = Ring([t_("gct%d" % i, [128, 512]) for i in range(2)])
                rsn = t_("grsn", [128, 512])
                ystr = Ring([t_("gyst%d" % i, [128, HG, 512], BF16, multi=True) for i in range(2)])
                WG = 2
                slots = []
                for i in range(WG):
                    d_ = {}
                    for n in ('trig', 't1', 'n1', 'qkd', 'qkT', 'Xa', 'Xb', 'Ya', 'Yb', 'Ra', 'Rb'):
                        d_[n] = t_("g%s%d" % (n, i), [64, HG, 64])
                    for n in ('ktm', 'kbg', 'kdec', 'vtm', 'u', 'vn', 'o', 'sq'):
                        d_[n] = t_("g%s%d" % (n, i), [64, HG, 128])
                    d_['wf'] = t_("gwf%d" % i, [128, HG, 64])
                    d_['ss'] = t_("gss%d" % i, [64, HG])
                    slots.append(d_)
                tiles = {}
                turn = [0]
                done = {}

                def v4(ps, w):
                    return ps[:64, 0:HG * w].rearrange('p (h x) -> p h x', h=HG)

                def prep(tt):
                    cin = cinr.get(); gz = gzr.get(); yst = ystr.get(); qkv = qkvr.get()
                    load_halo(cin, C_QKV, 1536, tt)
                    k.dma('sp', gz[:], proj[C_GZ:C_GZ + HG * 128, tt * 512:(tt + 1) * 512].rearrange(
                        '(c p) t -> p c t', p=128), R=[Dproj], W=[gz])
                    conv_silu(cin, cw, [c_ + h_ for c_ in (0, 4, 8) for h_ in range(HG)], qkv, qkv, ctr)
                    for c in [c_ + h_ for c_ in (0, 4) for h_ in range(HG)]:
                        tb = ctr.get()
                        k.act(tb[:], qkv[:, c, :], AF.Square, R=[qkv], W=[tb])
                        ps = psum.get()
                        k.mm(ps, ps[:, :], ones_f, tb[:], R=[cst, tb])
                        rsqrt(rsn, rsn[:], ps, ps[:, :], 1.0)
                        if c < 4:
                            k.stt('dve', qkv[:, c, :], qkv[:, c, :], 128.0 ** -0.5, rsn[:], ALU.mult, ALU.mult,
                                  R=[qkv, rsn], W=[qkv])
                        else:
                            k.tt('dve', qkv[:, c, :], qkv[:, c, :], rsn[:], ALU.mult, R=[qkv, rsn], W=[qkv])
                    tiles[tt] = (qkv, gz, yst)

                def chunk(ch):
                    tt, cc = ch // 8, ch % 8
                    if cc == 0:
                        prep(tt)
                    qkv, gz, yst = tiles[tt]
                    B = slots[ch % WG]
                    cs = slice(cc * 64, cc * 64 + 64)
                    b4 = lambda t, w: bc(t[:, ch, :].unsqueeze(2), [t[:, ch, :].shape[0], HG, w])
                    ktm, vtm, trig, t1, n1, qkd, qkT = B['ktm'], B['vtm'], B['trig'], B['t1'], B['n1'], B['qkd'], B['qkT']
                    ps_k = psum.get()
                    for h in range(HG):
                        k.tr(ps_k, ps_k[:64, h * 128:(h + 1) * 128], qkv[:, 4 + h, cs], ident_f, R=[qkv, cst])
                    k.copy('act', ktm[:], v4(ps_k, 128), R=[ps_k], W=[ktm])
                    yield
                    ps_v = psum.get()
                    for h in range(HG):
                        k.tr(ps_v, ps_v[:64, h * 128:(h + 1) * 128], qkv[:, 8 + h, cs], ident_f, R=[qkv, cst])
                    k.copy('act', vtm[:], v4(ps_v, 128), R=[ps_v], W=[vtm])
                    yield
                    ps_kk = psum.get()
                    for h in range(HG):
                        k.mm(ps_kk, ps_kk[:64, h * 64:(h + 1) * 64], qkv[:, 4 + h, cs], qkv[:, 4 + h, cs], R=[qkv])
                    ps_qk = psum.get()
                    for h in range(HG):
                        k.mm(ps_qk, ps_qk[:64, h * 64:(h + 1) * 64], qkv[:, h, cs], qkv[:, 4 + h, cs], R=[qkv])
                    k.tt('pool', trig[:], bc(tri64.unsqueeze(1), [64, HG, 64]), b4(g, 64), ALU.mult,
                         R=[cst, g], W=[trig])
                    ps_g = psum.get()
                    k.mm(ps_g, ps_g[:64, 0:HG * 64], ones64, trig[:].rearrange('p h l -> p (h l)'), R=[cst, trig])
                    k.tt('dve', t1[:], v4(ps_g, 64), bc(posm64.unsqueeze(1), [64, HG, 64]), ALU.add,
                         R=[ps_g, cst], W=[t1])
                    k.tt('dve', t1[:], b4(G, 64), t1[:], ALU.subtract, R=[G, t1], W=[t1])
                    k.act(t1[:], t1[:], AF.Exp, R=[t1], W=[t1])
                    X0 = B['Xa']
                    k.tt('dve', n1[:], v4(ps_kk, 64), t1[:], ALU.mult, R=[ps_kk, t1], W=[n1])
                    k.tt('dve', n1[:], n1[:], b4(nbeta, 64), ALU.mult, R=[n1, nbeta], W=[n1])
                    k.tt('pool', X0[:], n1[:], bc(strict64.unsqueeze(1), [64, HG, 64]), ALU.mult,
                         R=[n1, cst], W=[X0])
                    k.tt('dve', qkd[:], v4(ps_qk, 64), t1[:], ALU.mult, R=[ps_qk, t1], W=[qkd])
                    yield
                    ps_y = psum.get()
                    for h in range(HG):
                        k.tr(ps_y, ps_y[:64, h * 64:(h + 1) * 64], X0[:, h, :], id64, R=[X0, cst])
                    Y0 = B['Ya']
                    k.copy('act', Y0[:], v4(ps_y, 64), R=[ps_y], W=[Y0])
                    yield
                    ps_q = psum.get()
                    for h in range(HG):
                        k.tr(ps_q, ps_q[:64, h * 64:(h + 1) * 64], qkd[:, h, :], id64, R=[qkd, cst])
                    k.copy('act', qkT[:], v4(ps_q, 64), R=[ps_q], W=[qkT])
                    RT = B['Ra']
                    k.tt('pool', RT[:], Y0[:], bc(id64.unsqueeze(1), [64, HG, 64]), ALU.add, R=[Y0, cst], W=[RT])
                    yield
                    Xp, Yp = X0, Y0
                    for kk in range(1, 6):
                        Xn = B['Xb'] if Xp is B['Xa'] else B['Xa']
                        Yn = B['Yb'] if Yp is B['Ya'] else B['Ya']
                        RTn = B['Rb'] if RT is B['Ra'] else B['Ra']
                        ps_a = psum.get()
                        for h in range(HG):
                            k.mm(ps_a, ps_a[:64, h * 64:(h + 1) * 64], Yp[:, h, :], Xp[:, h, :], R=[Yp, Xp])
                        if kk <= 4:
                            ps_b = psum.get()
                            for h in range(HG):
                                k.mm(ps_b, ps_b[:64, h * 64:(h + 1) * 64], Xp[:, h, :], Yp[:, h, :], R=[Yp, Xp])
                        k.copy('act', Xn[:], v4(ps_a, 64), R=[ps_a], W=[Xn])
                        if kk <= 4:
                            k.copy('dve', Yn[:], v4(ps_b, 64), R=[ps_b], W=[Yn])
                        yield
                        ps_c = psum.get()
                        for h in range(HG):
                            k.mm(ps_c, ps_c[:64, h * 64:(h + 1) * 64], Xn[:, h, :], RT[:, h, :], R=[Xn, RT])
                        k.tt('dve', RTn[:], RT[:], v4(ps_c, 64), ALU.add, R=[RT, ps_c], W=[RTn])
                        Xp, Yp, RT = Xn, Yn, RTn
                        yield
                    kbg, kdec, u, wf, vn, o, sq, ss = B['kbg'], B['kdec'], B['u'], B['wf'], B['vn'], B['o'], B['sq'], B['ss']
                    k.tt('pool', vtm[:], vtm[:], b4(beta, 128), ALU.mult, R=[vtm, beta], W=[vtm])
                    k.tt('pool', kbg[:], ktm[:], b4(bexpG, 128), ALU.mult, R=[ktm, bexpG], W=[kbg])
                    k.tt('pool', kdec[:], ktm[:], b4(kdsc, 128), ALU.mult, R=[ktm, kdsc], W=[kdec])
                    ps_u = psum.get()
                    for h in range(HG):
                        k.mm(ps_u, ps_u[:64, h * 128:(h + 1) * 128], RT[:, h, :], vtm[:, h, :], R=[RT, vtm])
                    k.copy('act', u[:], v4(ps_u, 128), R=[ps_u], W=[u])
                    yield
                    ps_w = psum.get()
                    for h in range(HG):
                        k.mm(ps_w, ps_w[:, h * 64:(h + 1) * 64], kbg[:, h, :], RT[:, h, :], R=[kbg, RT])
                    k.copy('act', wf[:], ps_w[:, 0:HG * 64].rearrange('p (h x) -> p h x', h=HG), R=[ps_w], W=[wf])
                    yield
                    while turn[0] != ch:
                        yield
                    ps_ws = psum.get()
                    for h in range(HG):
                        k.mm(ps_ws, ps_ws[:64, h * 128:(h + 1) * 128], wf[:, h, :], S[:, h, :], R=[wf, S])
                    k.tt('dve', vn[:], u[:], v4(ps_ws, 128), ALU.subtract, R=[u, ps_ws], W=[vn])
                    ps_o1 = psum.get()
                    for h in range(HG):
                        k.mm(ps_o1, ps_o1[:64, h * 128:(h + 1) * 128], qkv[:, h, cs], S[:, h, :], R=[qkv, S])
                    ps_ds = psum.get()
                    for h in range(HG):
                        k.mm(ps_ds, ps_ds[:, h * 128:(h + 1) * 128], kdec[:, h, :], vn[:, h, :], R=[kdec, vn])
                    k.tt('dve', S[:], S[:], bc(gl[:, ch, :].unsqueeze(2), [128, HG, 128]), ALU.mult,
                         R=[S, gl], W=[S])
                    k.tt('dve', S[:], S[:], ps_ds[:, 0:HG * 128].rearrange('p (h x) -> p h x', h=HG), ALU.add,
                         R=[S, ps_ds], W=[S])
                    turn[0] = ch + 1
                    ps_o2 = psum.get()
                    for h in range(HG):
                        k.mm(ps_o2, ps_o2[:64, h * 128:(h + 1) * 128], qkT[:, h, :], vn[:, h, :], R=[qkT, vn])
                    k.tt('dve', o[:], v4(ps_o1, 128), b4(eG, 128), ALU.mult, R=[ps_o1, eG], W=[o])
                    k.tt('dve', o[:], o[:], v4(ps_o2, 128), ALU.add, R=[o, ps_o2], W=[o])
                    yield
                    k.tt('pool', sq[:], o[:], o[:], ALU.mult, R=[o], W=[sq])
                    k.op('dve', lambda e: e.reduce_sum(ss[:], sq[:], AX.X), R=[sq], W=[ss])
                    rsqrt(ss, ss[:], ss, ss[:], 1.0 / 128)
                    k.tt('dve', sq[:], o[:], bc(ss[:, :].unsqueeze(2), [64, HG, 128]), ALU.mult, R=[o, ss], W=[sq])
                    yield
                    ps_t = psum.get()
                    for h in range(HG):
                        k.tr(ps_t, ps_t[:, h * 64:(h + 1) * 64], sq[:, h, :], id64, R=[sq, cst])
                    k.stt('dve', yst[:, :, cs], ps_t[:, 0:HG * 64].rearrange('p (h x) -> p h x', h=HG), nw[:, 0:1],
                          gz[:, :, cs], ALU.mult, ALU.mult, R=[ps_t, nw, gz], W=[yst])

                def chunk_done(ch):
                    tt = ch // 8
                    done[tt] = done.get(tt, 0) + 1
                    if done[tt] == 8:
                        yst = tiles[tt][2]
                        k.dma('sp', (yam if pair else y_d[0])[:, tt * 512:(tt + 1) * 512].rearrange(
                            '(c p) t -> p c t', p=128), yst[:], R=[yst], W=[Dyam if pair else Dy[0]])

                run_pipeline(list(range(NCH)), WG, chunk, chunk_done)
                k.barrier()
                if pair:
                    allgather([(yam, y_d[0])], Dyam, Dy[0])

        def phase_mixers(l):
            if 'mla' in mixers:
                phase_mla(l)
            if 'ssd' in mixers:
                phase_ssd(l)
            if 'gdn' in mixers:
                phase_gdn(l)

        def phase_merge(l, xsrc, Dxsrc, xdst, Dxdst):
            with ExitStack() as ps_:
                wb = [sb("wbr%d" % i, [128, 4, 1024], BF16, stack=ps_) for i in range(3)]
                wo = sb("wo", [128, 8, 1024], BF16, stack=ps_)
                for i in range(3):
                    k.dma('pool', wb[i][:], w_br[i][l].rearrange('(kc p) n -> p kc n', p=128), R=[Dw], W=[wb[i]])
                k.dma('pool', wo[:], w_out[l].rearrange('(kc p) n -> p kc n', p=128), R=[Dw], W=[wo])
                yr = Ring([sb("ym%d" % i, [128, 3, 4, 512], BF16, stack=ps_) for i in range(2)])
                gr = Ring([sb("gm%d" % i, [128, 24, 512], BF16, stack=ps_) for i in range(2)])
                xr = Ring([sb("xm%d" % i, [128, 8, 512], F32, stack=ps_) for i in range(2)])
                mg = Ring([sb("mg%d" % i, [128, 8, 512], BF16, stack=ps_) for i in range(2)])
                tmp = Ring([sb("mt%d" % i, [128, 512], F32, stack=ps_) for i in range(3)])
                for tt in range(NT):
                    ts_ = slice(tt * 512, (tt + 1) * 512)
                    y = yr.get()
                    g = gr.get()
                    xt = xr.get()
                    m_ = mg.get()
                    for i in range(3):
                        k.dma('sp', y[:, i], y_d[i][:, ts_].rearrange('(kc p) t -> p kc t', p=128), R=[Dy[i]], W=[y])
                    k.dma('sp', g[:], proj[C_GATES:C_GATES + 3072, ts_].rearrange('(kc p) t -> p kc t', p=128),
                          R=[Dproj], W=[g])
                    for (a_, b_, sap) in xsrc(tt):
                        k.dma('sp', xt[:, a_:b_, :], sap, R=[Dxsrc], W=[xt])
                    for o in range(8):
                        tl = []
                        for i in range(3):
                            ps = psum.get()
                            for kc in range(4):
                                k.mm(ps, ps[:, :], wb[i][:, kc, o * 128:(o + 1) * 128], y[:, i, kc, :], R=[wb[i], y],
                                     start=(kc == 0), stop=(kc == 3))
                            t_ = tmp.get()
                            k.tt('dve', t_[:], ps[:, :], g[:, i * 8 + o, :], ALU.mult, R=[ps, g], W=[t_])
                            tl.append(t_)
                        k.tt('pool', tl[0][:], tl[0][:], tl[1][:], ALU.add, R=[tl[0], tl[1]], W=[tl[0]])
                        k.tt('pool', m_[:, o, :], tl[0][:], tl[2][:], ALU.add, R=[tl[0], tl[2]], W=[m_])
                    for o in range(8):
                        ps = psum.get()
                        for kc in range(8):
                            k.mm(ps, ps[:, :], wo[:, kc, o * 128:(o + 1) * 128], m_[:, kc, :], R=[wo, m_],
                                 start=(kc == 0), stop=(kc == 7))
                        k.stt('dve', xt[:, o, :], ps[:, :], modT[:, 16 + o:17 + o], xt[:, o, :], ALU.mult, ALU.add,
                              R=[ps, modT, xt], W=[xt])
                    k.dma('sp', xdst[:, ts_].rearrange('(kc p) t -> p kc t', p=128), xt[:], R=[xt], W=[Dxdst])
                k.barrier()

        def phase_ffn(l, xsrc, Dxsrc, xdst, Dxdst):
            moe = (l % 2 == 1)
            li = l // 2
            TT = min(TH, 1024)
            NS = TT // 512
            HCMAX = 14
            with ExitStack() as ps_:
                h2 = sb("h2", [128, 8, TT], BF16, multi=True, stack=ps_)
                xacc = sb("xacc", [128, 8, TT], F32, multi=True, stack=ps_)
                sq = sb("sq2", [128, 8, 512], F32, stack=ps_)
                hid = sb("hid", [128, HCMAX, TT], BF16, multi=True, stack=ps_)
                wgr = Ring([sb("wg%d" % i, [128, 8, 256], BF16, stack=ps_) for i in range(2)])
                wur = Ring([sb("wu%d" % i, [128, 8, 256], BF16, stack=ps_) for i in range(2)])
                wdr = Ring([sb("wd%d" % i, [128, HCMAX, 512], BF16, stack=ps_) for i in range(1)])
                sgr = Ring([sb("sg%d" % i, [128, 512], BF16, stack=ps_) for i in range(3)])
                tmp = Ring([sb("ft%d" % i, [128, 512], F32, stack=ps_) for i in range(2)])
                if moe:
                    hf = sb("hf2", [128, 8, 512], F32, stack=ps_)
                    rt = sb("rt", [128, 8, NEXP], F32, stack=ps_)
                    k.dma('sp', rt[:], moe_router[li].rearrange('(kc p) n -> p kc n', p=128), R=[Dw], W=[rt])
                    wrow = sb("wrow", [128, NEXP, TT], BF16, multi=True, stack=ps_)
                    sm = [sb("rs%d" % i, [128, 8], F32, stack=ps_) for i in range(6)]
                    sc = [sb("rc%d" % i, [128, 1], F32, stack=ps_) for i in range(4)]
                    dg = sb("dg", [128, NEXP, 128], F32, stack=ps_)
                for st_ in range(TH // TT):
                    t0 = st_ * TT
                    for s in range(NS):
                        sl = slice(s * 512, (s + 1) * 512)
                        k.dma('sp', xacc[:, :, sl],
                              xsrc[:, t0 + s * 512:t0 + (s + 1) * 512].rearrange('(kc p) t -> p kc t', p=128),
                              R=[Dxsrc], W=[xacc])
                        if pair:
                            k.dma('sp', sq[:], xsrc[:, TH + t0 + s * 512:TH + t0 + (s + 1) * 512].rearrange(
                                '(kc p) t -> p kc t', p=128), R=[Dxsrc], W=[sq])
                            k.ts('dve', xacc[:, :, sl], xacc[:, :, sl], rsel[:, 0:1], None, ALU.mult,
                                 R=[xacc, rsel], W=[xacc])
                            k.stt('dve', xacc[:, :, sl], sq[:], rsel[:, 1:2], xacc[:, :, sl], ALU.mult, ALU.add,
                                  R=[sq, rsel, xacc], W=[xacc])
                        norm_tile(xacc, xacc[:, :, sl], sq, lambda kc: h2[:, kc, sl], h2, 32, 24,
                                  hf=([hf] if moe else None))
                        if moe:
                            for q in range(4):
                                lg, m1, m2, e1, e2, wt8 = sm
                                ps = psum.get()
                                for kc in range(8):
                                    k.mm(ps, ps[:, 0:8], hf[:, kc, q * 128:(q + 1) * 128], rt[:, kc, :], R=[hf, rt],
                                         start=(kc == 0), stop=(kc == 7))
                                k.copy('dve', lg[:], ps[:, 0:8], R=[ps], W=[lg])
                                k.op('dve', lambda e: e.reduce_max(sc[0][:], lg[:], AX.X), R=[lg], W=[sc[0]])
                                k.ts('dve', e1[:], lg[:], sc[0][:, 0:1], None, ALU.is_equal, R=[lg, sc[0]], W=[e1])
                                k.stt('dve', m1[:], e1[:], -1e30, lg[:], ALU.mult, ALU.add, R=[e1, lg], W=[m1])
                                k.op('dve', lambda e: e.reduce_max(sc[1][:], m1[:], AX.X), R=[m1], W=[sc[1]])
                                k.ts('dve', e2[:], m1[:], sc[1][:, 0:1], None, ALU.is_equal, R=[m1, sc[1]], W=[e2])
                                k.tt('dve', sc[2][:], sc[0][:], sc[1][:], ALU.subtract, R=[sc[0], sc[1]], W=[sc[2]])
                                k.act(sc[3][:], sc[2][:], AF.Sigmoid, R=[sc[2]], W=[sc[3]])
                                k.act(sc[2][:], sc[2][:], AF.Sigmoid, R=[sc[2]], W=[sc[2]], scale=-1.0)
                                k.ts('dve', wt8[:], e1[:], sc[3][:, 0:1], None, ALU.mult, R=[e1, sc[3]], W=[wt8])
                                k.stt('dve', wt8[:], e2[:], sc[2][:, 0:1], wt8[:], ALU.mult, ALU.add,
                                      R=[e2, sc[2], wt8], W=[wt8])
                                k.tt('dve', dg[:], cst[:, 0:1, :].to_broadcast([128, NEXP, 128]),
                                     wt8[:, :].unsqueeze(2).to_broadcast([128, NEXP, 128]), ALU.mult,
                                     R=[cst, wt8], W=[dg])
                                for hh in range(2):
                                    ps2 = psum.get()
                                    k.mm(ps2, ps2[:, :], ones_f, dg[:, hh * 4:(hh + 1) * 4, :].rearrange('p e t -> p (e t)'), R=[cst, dg])
                                    c0 = s * 512 + q * 128
                                    k.copy('act', wrow[:, hh * 4:(hh + 1) * 4, c0:c0 + 128],
                                           ps2[:, :].rearrange('p (e t) -> p e t', e=4), R=[ps2], W=[wrow])
                    if moe:
                        passes = []
                        for e_ in range(NEXP):
                            for hh in range(2):
                                passes.append((moe_wg[li, e_], moe_wu[li, e_], moe_wd[li, e_], hh * 1792, 1792, e_))
                    else:
                        passes = [(ffn_wg[li], ffn_wu[li], ffn_wd[li], hh * 1408, 1408, None) for hh in range(2)]
                    for (wg_, wu_, wd_, h0, hn, ex) in passes:
                        HC = hn // 128
                        for cb in range(0, hn, 256):
                            cw = min(256, hn - cb)
                            wg = wgr.get()
                            wu = wur.get()
                            k.dma('pool', wg[:, :, :cw],
                                  wg_[:, h0 + cb:h0 + cb + cw].rearrange('(kc p) n -> p kc n', p=128), R=[Dw], W=[wg])
                            k.dma('pool', wu[:, :, :cw],
                                  wu_[:, h0 + cb:h0 + cb + cw].rearrange('(kc p) n -> p kc n', p=128), R=[Dw], W=[wu])
                            for jj in range(cw // 128):
                                j = cb // 128 + jj
                                for s in range(NS):
                                    sl = slice(s * 512, (s + 1) * 512)
                                    psg = psum.get()
                                    for kc in range(8):
                                        k.mm(psg, psg[:, :], wg[:, kc, jj * 128:(jj + 1) * 128], h2[:, kc, sl],
                                             R=[wg, h2], start=(kc == 0), stop=(kc == 7))
                                    psu = psum.get()
                                    for kc in range(8):
                                        k.mm(psu, psu[:, :], wu[:, kc, jj * 128:(jj + 1) * 128], h2[:, kc, sl],
                                             R=[wu, h2], start=(kc == 0), stop=(kc == 7))
                                    sg = sgr.get()
                                    k.act(sg[:], psg[:, :], AF.Silu, R=[psg], W=[sg])
                                    k.tt('dve', hid[:, j, sl], psu[:, :], sg[:], ALU.mult, R=[psu, sg], W=[hid])
                        for oh in range(2):
                            wd = wdr.get()
                            for c4 in range(0, HC, 7):
                                cn = min(7, HC - c4)
                                k.dma('pool', wd[:, c4:c4 + cn, :],
                                      wd_[h0 + c4 * 128:h0 + (c4 + cn) * 128, oh * 512:(oh + 1) * 512].rearrange(
                                          '(kc p) n -> p kc n', p=128), R=[Dw], W=[wd])
                            for oo in range(4):
                                o = oh * 4 + oo
                                for s in range(NS):
                                    sl = slice(s * 512, (s + 1) * 512)
                                    ps = psum.get()
                                    for j in range(HC):
                                        k.mm(ps, ps[:, :], wd[:, j, oo * 128:(oo + 1) * 128], hid[:, j, sl],
                                             R=[wd, hid], start=(j == 0), stop=(j == HC - 1))
                                    if ex is None:
                                        k.stt('dve', xacc[:, o, sl], ps[:, :], modT[:, 40 + o:41 + o], xacc[:, o, sl],
                                              ALU.mult, ALU.add, R=[ps, modT, xacc], W=[xacc])
                                    else:
                                        t_ = tmp.get()
                                        k.stt('dve', t_[:], ps[:, :], modT[:, 40 + o:41 + o], wrow[:, ex, sl],
                                              ALU.mult, ALU.mult, R=[ps, modT, wrow], W=[t_])
                                        k.tt('pool', xacc[:, o, sl], xacc[:, o, sl], t_[:], ALU.add,
                                             R=[t_, xacc], W=[xacc])
                    k.dma('sp', xdst[:, t0:t0 + TT].rearrange('(kc p) t -> p kc t', p=128), xacc[:], R=[xacc],
                          W=[Dxdst])
                k.barrier()

        def phase_final(xsrc, Dxsrc, ntiles):
            with ExitStack() as ps_:
                xr = Ring([sb("xf%d" % i, [128, 8, 512], F32, stack=ps_) for i in range(2)])
                orr = Ring([sb("of%d" % i, [128, 8, 512], F32, multi=True, stack=ps_) for i in range(2)])
                sq = sb("sqf", [128, 8, 512], F32, stack=ps_)
                for tt in range(ntiles):
                    ts_ = slice(tt * 512, (tt + 1) * 512)
                    xt = xr.get()
                    ot = orr.get()
                    for (a_, b_, sap) in xsrc(tt):
                        k.dma('sp', xt[:, a_:b_, :], sap, R=[Dxsrc], W=[xt])
                    norm_tile(xt, xt[:], sq, lambda kc: ot[:, kc, :], ot, None, None)
                    k.dma('sp', outT[:, ts_].rearrange('(kc p) t -> p kc t', p=128), ot[:], R=[ot], W=[Dout])
                k.barrier()

        def tiles_of(ap):
            return lambda tt: [(0, 8, ap[:, tt * 512:(tt + 1) * 512].rearrange('(kc p) t -> p kc t', p=128))]

        def tiles_of_xg(tt):
            r_, off = (tt * 512) // TH, (tt * 512) % TH
            return [(2 * kb, 2 * kb + 2,
                     xg[kb * 512 + r_ * 256:kb * 512 + r_ * 256 + 256, off:off + 512].rearrange(
                         '(kl p) t -> p kl t', p=128)) for kb in range(4)]

        cur, Dcur = tiles_of(xT_in), Dxin
        fin, Dfin, nfin = cur, Dcur, NT
        for li_, l in enumerate(layers):
            phase_ada(l)
            full, Dfull = None, None
            if 'mix' in stages:
                phase_inproj(l, cur, Dcur)
                phase_mixers(l)
                phase_merge(l, cur, Dcur, x_b, Dx_b)
                cur, Dcur = tiles_of(x_b), Dx_b
                full, Dfull = x_b, Dx_b
                fin, Dfin, nfin = cur, Dcur, NT
            if 'ffn' in stages:
                if full is None:
                    assert not pair
                    full, Dfull = (xT_in, Dxin) if li_ == 0 else (x_a, Dx_a)
                if pair:
                    phase_ffn(l, full, Dfull, xh, Dxh)
                    fin, Dfin, nfin = tiles_of(xh), Dxh, TH // 512
                    if li_ != len(layers) - 1:
                        allgather([(xh[kb * 256:(kb + 1) * 256, :], xg[kb * 512:(kb + 1) * 512, :])
                                   for kb in range(4)], Dxh, Dxg)
                        cur, Dcur = tiles_of_xg, Dxg
                else:
                    phase_ffn(l, full, Dfull, x_a, Dx_a)
                    cur, Dcur = tiles_of(x_a), Dx_a
                    fin, Dfin, nfin = cur, Dcur, NT
        phase_final(fin, Dfin, nfin)
        k.barrier()
        print("instructions:", k.ninst, {kk: v for kk, v in k.cnt.items()})
    return nc


def _consts():
    c = np.zeros((128, 8, 128), np.float32)
    i = np.arange(128)
    c[:, 0, :] = np.eye(128)
    c[:, 1, :] = 1.0
    c[:, 2, :] = (i[:, None] <= i[None, :])
    c[:, 3, :] = np.where(i[None, :] > i[:, None], 1e9, 0.0)
    c[:, 4, :] = (i[None, :] < i[:, None])
    rot = np.zeros((128, 128), np.float32)
    for o in (0, 64):
        for m in range(32):
            rot[o + m + 32, o + m] = -1.0
            rot[o + m, o + m + 32] = 1.0
    c[:, 5, :] = rot
    c[:, 6, :] = np.where(i[None, :] < i[:, None], 1e9, 0.0)
    return c


def _fm(v, nchunk):
    return np.ascontiguousarray(np.asarray(v, np.float32).reshape(nchunk, 128).T)


_PERM_CACHE = {}


def _perm_weights(inp, r):
    if r in _PERM_CACHE:
        return _PERM_CACHE[r]
    perm = [2 * r, 2 * r + 1] + [h for h in range(4) if h not in (2 * r, 2 * r + 1)]
    out = {}
    cols = np.arange(IN_DIM)
    for base in (0, 512, 1024, C_GZ):
        for i, h in enumerate(perm):
            cols[base + i * 128:base + (i + 1) * 128] = np.arange(base + h * 128, base + (h + 1) * 128)
    for base in (C_A, C_B):
        for i, h in enumerate(perm):
            cols[base + i] = base + h
    out["w_in"] = np.ascontiguousarray(np.asarray(inp["w_in"], np.float32)[:, :, cols])
    cc = np.arange(1536)
    for base in (0, 512, 1024):
        for i, h in enumerate(perm):
            cc[base + i * 128:base + (i + 1) * 128] = np.arange(base + h * 128, base + (h + 1) * 128)
    out["gdn_conv_w"] = np.ascontiguousarray(np.asarray(inp["gdn_conv_w"], np.float32)[:, :, cc])
    out["gdn_a_log"] = np.ascontiguousarray(np.asarray(inp["gdn_a_log"], np.float32)[:, perm])
    out["gdn_dt_bias"] = np.ascontiguousarray(np.asarray(inp["gdn_dt_bias"], np.float32)[:, perm])
    cq = np.concatenate([np.arange(h * 192, (h + 1) * 192) for h in perm])
    ck = np.concatenate([np.arange(h * 128, (h + 1) * 128) for h in perm])
    out["mla_w_uq"] = np.ascontiguousarray(np.asarray(inp["mla_w_uq"], np.float32)[:, :, cq])
    out["mla_w_uk"] = np.ascontiguousarray(np.asarray(inp["mla_w_uk"], np.float32)[:, :, ck])
    out["mla_w_uv"] = np.ascontiguousarray(np.asarray(inp["mla_w_uv"], np.float32)[:, :, ck])
    _PERM_CACHE[r] = out
    return out


def prep_inputs(inp, b, T, r=None):
    if r is not None and r != 0:
        inp = dict(inp)
        inp.update(_perm_weights(inp, r))
    f = lambda a: np.ascontiguousarray(np.asarray(a, np.float32))
    L = DEPTH
    m = {}
    m["xT"] = np.ascontiguousarray(np.asarray(inp["x"][b, :T], np.float32).T)
    m["cT"] = _fm(inp["c"][b], 8)
    m["pos"] = np.ascontiguousarray(np.asarray(inp["positions"][b, :T], np.int32).reshape(1, T))
    m["w_ada"] = f(inp["w_ada"])
    m["b_adaT"] = np.stack([_fm(inp["b_ada"][l], 48) for l in range(L)])
    m["w_in"] = f(inp["w_in"])
    gc = np.asarray(inp["gdn_conv_w"], np.float32)
    m["gdn_convT"] = np.ascontiguousarray(gc.reshape(L, 4, 12, 128).transpose(0, 3, 2, 1))
    rep = lambda a: np.ascontiguousarray(np.broadcast_to(np.asarray(a, np.float32)[:, None, :], (L, 128, a.shape[-1])))
    m["gdn_alog"] = rep(inp["gdn_a_log"])
    m["gdn_dtb"] = rep(inp["gdn_dt_bias"])
    m["gdn_nw"] = np.ascontiguousarray(np.asarray(inp["gdn_norm_w"], np.float32).reshape(L, 128, 1))
    sc = np.asarray(inp["ssm_conv_w"], np.float32)
    m["ssm_convT"] = np.ascontiguousarray(sc.reshape(L, 4, 8, 128).transpose(0, 3, 2, 1))
    m["ssm_convb"] = np.stack([_fm(inp["ssm_conv_b"][l], 8) for l in range(L)])
    m["ssm_alog"] = rep(inp["ssm_a_log"])
    m["ssm_dtb"] = rep(inp["ssm_dt_bias"])
    dexp = np.repeat(np.asarray(inp["ssm_d"], np.float32), 64, axis=1)
    m["ssm_dexp"] = np.stack([_fm(dexp[l], 4) for l in range(L)])
    m["ssm_nw"] = np.stack([_fm(inp["ssm_norm_w"][l], 4) for l in range(L)])
    m["mla_qnw"] = np.stack([_fm(inp["mla_q_norm_w"][l], 4) for l in range(L)])
    m["mla_wuq"] = f(inp["mla_w_uq"])
    m["mla_kvnw"] = np.stack([_fm(inp["mla_kv_norm_w"][l], 2) for l in range(L)])
    m["mla_wuk"] = f(inp["mla_w_uk"])
    m["mla_wuv"] = f(inp["mla_w_uv"])
    for n in ("w_branch_a", "w_branch_b", "w_branch_c", "w_out", "ffn_w_gate", "ffn_w_up", "ffn_w_down",
              "moe_router", "moe_w_gate", "moe_w_up", "moe_w_down"):
        m[n] = f(inp[n])
    m["fnwT"] = _fm(inp["final_norm_w"], 8)
    m["consts"] = _consts()
    m["rsel"] = np.stack([np.ones(128, np.float32), np.zeros(128, np.float32)], 1)
    invf = (10000.0 ** (-np.arange(0, 64, 2, dtype=np.float32) / 64)).astype(np.float32)
    m["invf"] = np.concatenate([invf] * 4).reshape(128, 1).astype(np.float32)
    return m


def kernel(**inputs):
    B, T = inputs["x"].shape[0], inputs["x"].shape[1]
    nc = build_program(T, list(range(DEPTH)), pair=True)
    base = [prep_inputs(inputs, b, T) for b in range(B)]
    in_maps = []
    _PERM_CACHE.clear()
    base1 = [prep_inputs(inputs, b, T, r=1) for b in range(B)]
    for c in range(2 * B):
        m = dict((base, base1)[c % 2][c // 2])
        rs = np.zeros((128, 2), np.float32)
        rs[:, c % 2] = 1.0
        m["rsel"] = rs
        in_maps.append(m)
    res = run_bass_kernel_spmd(nc, in_maps, core_ids=list(range(2 * B)))
    TH = T // 2
    out = np.zeros((B, T, D), np.float32)
    for c in range(2 * B):
        out[c // 2, (c % 2) * TH:(c % 2 + 1) * TH, :] = np.asarray(res.results[c]["outT"], np.float32).T
    return out
```

```python
import numpy as np
from contextlib import ExitStack
import concourse.bass as bass
import concourse.mybir as mybir
from concourse.bass_utils import run_bass_kernel_spmd

F32, BF16, I32 = mybir.dt.float32, mybir.dt.bfloat16, mybir.dt.int32
AF = mybir.ActivationFunctionType
ALU = mybir.AluOpType
AX = mybir.AxisListType

D = 1024
DEPTH = 4
EPS = 1e-6
IN_DIM = 7504
FFN_DIM = 2816
EXPERT_DIM = 3584
NEXP = 8
SAME_SYNC = True

C_QKV, C_GZ, C_A, C_B, C_SZ, C_XBC, C_DT, C_CQ, C_CKV, C_KR, C_GATES = (
    0, 1536, 2048, 2052, 2056, 2568, 3592, 3600, 4112, 4368, 4432)


class Buf:
    def __init__(self, t, multi=False):
        self.t = t
        self.multi = multi
        self.w = {}
        self.r = {}

    def __getitem__(self, key):
        return self.t[key]


def _merge(d, tok):
    k_, v = tok
    if d.get(k_, 0) < v:
        d[k_] = v


class Ring:
    def __init__(self, bufs):
        self.bufs = bufs
        self.i = 0

    def get(self):
        b = self.bufs[self.i % len(self.bufs)]
        self.i += 1
        return b


class KB:
    ENG = ('pe', 'act', 'dve', 'pool', 'sp')

    def __init__(self, nc, es):
        self.nc = nc
        self.es = es
        self.e = {'pe': nc.tensor, 'act': nc.scalar, 'dve': nc.vector, 'pool': nc.gpsimd, 'sp': nc.sync}
        self.sem = {}
        self.cnt = {}
        for e in self.ENG:
            self.sem[('e', e)] = es.enter_context(nc.semaphore('se_' + e))
            self.cnt[('e', e)] = 0
        self.NS = 8
        self.dma_i = {}
        for q in ('sp', 'pool'):
            self.dma_i[q] = 0
            for j in range(self.NS):
                self.sem[('d', q, j)] = es.enter_context(nc.semaphore('sd_%s%d' % (q, j)))
                self.cnt[('d', q, j)] = 0
        self.seen = {e: {} for e in self.ENG}
        self.ninst = 0

    def _wait(self, eng, deps):
        for key, v in deps.items():
            if key == ('e', eng) and (eng == 'pe' or eng == 'sp' or not SAME_SYNC):
                continue
            if self.seen[eng].get(key, 0) >= v:
                continue
            self.e[eng].wait_ge(self.sem[key], v)
            self.seen[eng][key] = v
            self.ninst += 1

    def _deps(self, R, W):
        deps = {}
        for b in R:
            for t in b.w.items():
                _merge(deps, t)
        for b in W:
            for t in b.r.items():
                _merge(deps, t)
            if not b.multi:
                for t in b.w.items():
                    _merge(deps, t)
        return deps

    def _post(self, tok, R, W):
        for b in R:
            _merge(b.r, tok)
        for b in W:
            if b.multi:
                _merge(b.w, tok)
            else:
                b.w = {tok[0]: tok[1]}
                b.r = {}

    def op(self, eng, fn, R=(), W=()):
        self._wait(eng, self._deps(R, W))
        ins = fn(self.e[eng])
        key = ('e', eng)
        self.cnt[key] += 1
        ins.then_inc(self.sem[key], 1)
        self.ninst += 1
        self._post((key, self.cnt[key]), R, W)

    def dma(self, q, out, in_, R=(), W=()):
        self._wait(q, self._deps(R, W))
        key = ('d', q, self.dma_i[q] % self.NS)
        self.dma_i[q] += 1
        if self.cnt[key] > 0:
            self._wait(q, {key: self.cnt[key]})
        ins = self.e[q].dma_start(out=out, in_=in_)
        self.cnt[key] += 16
        ins.then_inc(self.sem[key], 16)
        self.ninst += 1
        self._post((key, self.cnt[key]), R, W)

    def barrier(self):
        for e in self.ENG:
            deps = {key: v for key, v in self.cnt.items() if v > 0 and key != ('e', e)}
            self._wait(e, deps)

    def mm(self, ps, out, lhsT, rhs, R, start=True, stop=True):
        self.op('pe', lambda e: e.matmul(out, lhsT, rhs, start=start, stop=stop), R=R, W=[ps])

    def tr(self, ps, out, in_, ident, R):
        self.op('pe', lambda e: e.transpose(out, in_, ident), R=R, W=[ps])

    def act(self, out, in_, func, R, W, bias=None, scale=None, accum_out=None, eng='act'):
        kw = {}
        if bias is not None:
            kw['bias'] = bias
        if scale is not None:
            kw['scale'] = scale
        if accum_out is not None:
            kw['accum_out'] = accum_out
        self.op('act', lambda e: e.activation(out=out, in_=in_, func=func, **kw), R=R, W=W)

    def tt(self, eng, out, in0, in1, op, R, W):
        self.op(eng, lambda e: e.tensor_tensor(out, in0, in1, op), R=R, W=W)

    def ts(self, eng, out, in0, s1, s2, op0, op1=None, R=(), W=()):
        if op1 is None:
            self.op(eng, lambda e: e.tensor_scalar(out, in0, s1, None, op0), R=R, W=W)
        else:
            self.op(eng, lambda e: e.tensor_scalar(out, in0, s1, s2, op0, op1), R=R, W=W)

    def stt(self, eng, out, in0, scalar, in1, op0, op1, R, W):
        self.op(eng, lambda e: e.scalar_tensor_tensor(out, in0, scalar, in1, op0, op1), R=R, W=W)

    def copy(self, eng, out, in_, R, W):
        if eng == 'act':
            self.op('act', lambda e: e.activation(out=out, in_=in_, func=AF.Copy), R=R, W=W)
        else:
            self.op(eng, lambda e: e.tensor_copy(out, in_), R=R, W=W)


def build_program(T, layers, debug=False, stages=('mix', 'ffn'), mixers=('mla', 'ssd', 'gdn'), pair=False, ncores=8):
    nc = bass.Bass("TRN2", target_bir_lowering=False)
    L = DEPTH
    NT = T // 512
    NQ = T // 128
    NCH = T // 64
    TH = T // 2 if pair else T
    HG = 2 if pair else 4
    HM = 2 if pair else 4
    GS = 1 if pair else 2
    HS = 4 * GS
    XC = 2 * GS

    def din(name, shape, dt=F32):
        return nc.dram_tensor(name, list(shape), dt, kind="ExternalInput").ap()

    def dscr(name, shape, dt, out=False):
        kind = "ExternalOutput" if out else "Internal"
        return nc.dram_tensor(name, list(shape), dt, kind=kind).ap()

    xT_in = din("xT", [D, T])
    cT_in = din("cT", [128, 8])
    pos_in = din("pos", [1, T], I32)
    w_ada = din("w_ada", [L, D, 6 * D])
    b_adaT = din("b_adaT", [L, 128, 48])
    w_in = din("w_in", [L, D, IN_DIM])
    gdn_convT = din("gdn_convT", [L, 128, 12, 4])
    gdn_alog = din("gdn_alog", [L, 128, 4])
    gdn_dtb = din("gdn_dtb", [L, 128, 4])
    gdn_nw = din("gdn_nw", [L, 128, 1])
    ssm_convT = din("ssm_convT", [L, 128, 8, 4])
    ssm_convb = din("ssm_convb", [L, 128, 8])
    ssm_alog = din("ssm_alog", [L, 128, 8])
    ssm_dtb = din("ssm_dtb", [L, 128, 8])
    ssm_dexp = din("ssm_dexp", [L, 128, 4])
    ssm_nw = din("ssm_nw", [L, 128, 4])
    mla_qnw = din("mla_qnw", [L, 128, 4])
    mla_wuq = din("mla_wuq", [L, 512, 768])
    mla_kvnw = din("mla_kvnw", [L, 128, 2])
    mla_wuk = din("mla_wuk", [L, 256, 512])
    mla_wuv = din("mla_wuv", [L, 256, 512])
    w_br = [din("w_branch_a", [L, 512, D]), din("w_branch_b", [L, 512, D]), din("w_branch_c", [L, 512, D])]
    w_out = din("w_out", [L, D, D])
    ffn_wg = din("ffn_w_gate", [2, D, FFN_DIM])
    ffn_wu = din("ffn_w_up", [2, D, FFN_DIM])
    ffn_wd = din("ffn_w_down", [2, FFN_DIM, D])
    moe_router = din("moe_router", [2, D, NEXP])
    moe_wg = din("moe_w_gate", [2, NEXP, D, EXPERT_DIM])
    moe_wu = din("moe_w_up", [2, NEXP, D, EXPERT_DIM])
    moe_wd = din("moe_w_down", [2, NEXP, EXPERT_DIM, D])
    fnwT = din("fnwT", [128, 8])
    consts = din("consts", [128, 8, 128])
    invf_in = din("invf", [128, 1])

    outT = dscr("outT", [D, TH], F32, out=True)
    rsel_in = din("rsel", [128, 2])
    xh = dscr("xh", [D, TH], F32)
    xg = dscr("xg", [2 * D, TH], F32)
    yam = dscr("yam", [HG * 128, T], BF16)
    ycm = dscr("ycm", [HM * 128, T], BF16)
    ybm = dscr("ybm", [XC * 128, T], BF16)
    x_a = dscr("x_a", [D, T], F32, out=debug)
    x_b = dscr("x_b", [D, T], F32, out=debug)
    proj = dscr("proj", [IN_DIM, T], BF16, out=debug)
    abdt = dscr("abdt", [T, 16], F32, out=debug)
    y_d = [dscr("y_a", [512, T], BF16, out=debug), dscr("y_b", [512, T], BF16, out=debug),
           dscr("y_c", [512, T], BF16, out=debug)]

    es = ExitStack()
    with es:
        k = KB(nc, es)

        uid = [0]

        def sb(name, shape, dt, multi=False, stack=es):
            uid[0] += 1
            return Buf(stack.enter_context(nc.sbuf_tensor("%s_%d" % (name, uid[0]), list(shape), dt)), multi=multi)

        Dx_a, Dx_b, Dproj, Dabdt = Buf(x_a, True), Buf(x_b, True), Buf(proj, True), Buf(abdt, True)
        Dy = [Buf(y, True) for y in y_d]
        Dout = Buf(outT, True)
        Dxh, Dxg = Buf(xh, True), Buf(xg, True)
        Dyam, Dycm, Dybm = Buf(yam, True), Buf(ycm, True), Buf(ybm, True)

        def allgather(pieces, Dsrc, Ddst):
            k._wait('pool', k._deps([Dsrc], [Ddst]))
            for (sap, dap) in pieces:
                ins = nc.gpsimd.collective_compute(
                    "AllGather", ALU.bypass, replica_groups=[[2 * i_, 2 * i_ + 1] for i_ in range(ncores // 2)],
                    ins=[sap.opt()], outs=[dap.opt()])
                k.cnt[('c',)] += 1
                ins.then_inc(k.csem, 1)
            k._post((('c',), k.cnt[('c',)]), [Dsrc], [Ddst])
            k.barrier()

        k.csem = es.enter_context(nc.semaphore('s_cc'))
        k.sem[('c',)] = k.csem
        k.cnt[('c',)] = 0
        rsel = sb("rsel", [128, 2], F32)
        k.dma('sp', rsel[:], rsel_in, R=[Buf(None)], W=[rsel])
        Dw = Buf(None)
        Dxin = Buf(xT_in)

        psum = Ring([Buf(es.enter_context(nc.psum_tensor("ps%d" % i, [128, 512], F32))) for i in range(6)])
        psacc = Ring([Buf(es.enter_context(nc.psum_tensor("pa%d" % i, [128, 512], F32))) for i in range(2)])

        cst = sb("cst", [128, 8, 128], F32)
        k.dma('sp', cst[:], consts, R=[Dw], W=[cst])
        ident_f = cst[:, 0, :]
        ones_f = cst[:, 1, :]
        cst_b = sb("cst_b", [128, 2, 128], BF16)
        k.copy('dve', cst_b[:], cst[:, 0:2, :], R=[cst], W=[cst_b])
        ident_b = cst_b[:, 0, :]
        condT = sb("condT", [128, 8], F32)
        k.dma('sp', condT[:], cT_in, R=[Dw], W=[condT])
        k.act(condT[:], condT[:], AF.Silu, R=[condT], W=[condT])
        modT = sb("modT", [128, 48], F32)
        fnw = sb("fnw", [128, 8], F32)
        k.dma('sp', fnw[:], fnwT, R=[Dw], W=[fnw])

        def phase_ada(l):
            with ExitStack() as ps_:
                wr = Ring([sb("wada%d" % i, [128, 8, 768], F32, stack=ps_) for i in range(2)])
                bt = sb("badat", [128, 48], F32, stack=ps_)
                k.dma('sp', bt[:], b_adaT[l], R=[Dw], W=[bt])
                ps = psum.get()
                for g in range(8):
                    wb = wr.get()
                    k.dma('sp', wb[:], w_ada[l][:, g * 768:(g + 1) * 768].rearrange('(kc p) n -> p kc n', p=128),
                          R=[Dw], W=[wb])
                    for jj in range(6):
                        j = g * 6 + jj
                        for kc in range(8):
                            k.mm(ps, ps[:, j:j + 1], wb[:, kc, jj * 128:(jj + 1) * 128], condT[:, kc:kc + 1],
                                 R=[wb, condT], start=(kc == 0), stop=(kc == 7))
                k.tt('dve', modT[:], ps[:, 0:48], bt[:], ALU.add, R=[ps, bt], W=[modT])
                k.ts('dve', modT[:, 8:16], modT[:, 8:16], 1.0, None, ALU.add, R=[modT], W=[modT])
                k.ts('dve', modT[:, 32:40], modT[:, 32:40], 1.0, None, ALU.add, R=[modT], W=[modT])
                k.barrier()

        def norm_tile(xt, xap, sq, hdst, hbuf, sc_off, sh_off, hf=None):
            k.act(sq[:], xap, AF.Square, R=[xt], W=[sq])
            ps = psum.get()
            for kc in range(8):
                k.mm(ps, ps[:, :], ones_f, sq[:, kc, :], R=[sq, cst], start=(kc == 0), stop=(kc == 7))
            rstd = rstd_ring.get()
            rsqrt(rstd, rstd[:], ps, ps[:, :], 1.0 / D)
            for kc in range(8):
                k.tt('pool' if kc % 2 else 'dve', sq[:, kc, :], xap[:, kc, :], rstd[:], ALU.mult,
                     R=[xt, rstd], W=[sq])
            for kc in range(8):
                if sc_off is None:
                    k.ts('dve', hdst(kc), sq[:, kc, :], fnw[:, kc:kc + 1], None, ALU.mult, R=[sq, fnw], W=[hbuf])
                else:
                    k.ts('dve', hdst(kc), sq[:, kc, :], modT[:, sc_off + kc:sc_off + kc + 1],
                         modT[:, sh_off + kc:sh_off + kc + 1], ALU.mult, ALU.add, R=[sq, modT], W=[hbuf])
                    if hf is not None:
                        k.ts('pool', hf[0][:, kc, :], sq[:, kc, :], modT[:, sc_off + kc:sc_off + kc + 1],
                             modT[:, sh_off + kc:sh_off + kc + 1], ALU.mult, ALU.add, R=[sq, modT], W=[hf[0]])

        epsT = sb("epsT", [128, 1], F32)
        k.op('dve', lambda e: e.memset(epsT[:], EPS), W=[epsT])

        def rsqrt(ob, out, ib, in_, scale):
            k.act(out, in_, AF.Sqrt, R=[ib, epsT], W=[ob], bias=epsT[:out.shape[0], 0:1], scale=scale)
            k.op('dve', lambda e: e.reciprocal(out, out), R=[ob], W=[ob])

        rstd_ring = Ring([sb("rstd%d" % i, [128, 512], F32) for i in range(2)])

        def phase_inproj(l, xsrc, Dxsrc):
            with ExitStack() as ps_:
                h1 = sb("h1", [128, 8, T], BF16, multi=True, stack=ps_)
                xr = Ring([sb("xt%d" % i, [128, 8, 512], F32, stack=ps_) for i in range(2)])
                sq = sb("sq", [128, 8, 512], F32, stack=ps_)
                hf = sb("hf", [128, 8, 512], F32, stack=ps_)
                wsm = sb("wsm", [128, 8, 16], F32, stack=ps_)
                sm_st = Ring([sb("smst%d" % i, [128, 16], F32, stack=ps_) for i in range(2)])
                k.dma('sp', wsm[:, :, 0:8], w_in[l][:, C_A:C_A + 8].rearrange('(kc p) n -> p kc n', p=128),
                      R=[Dw], W=[wsm])
                k.dma('sp', wsm[:, :, 8:16], w_in[l][:, C_DT:C_DT + 8].rearrange('(kc p) n -> p kc n', p=128),
                      R=[Dw], W=[wsm])
                for tt in range(NT):
                    xt = xr.get()
                    for (a_, b_, sap) in xsrc(tt):
                        k.dma('sp', xt[:, a_:b_, :], sap, R=[Dxsrc], W=[xt])
                    norm_tile(xt, xt[:], sq, lambda kc: h1[:, kc, tt * 512:(tt + 1) * 512], h1, 8, 0, hf=[hf])
                    for q in range(4):
                        ps = psum.get()
                        for kc in range(8):
                            k.mm(ps, ps[:, 0:16], hf[:, kc, q * 128:(q + 1) * 128], wsm[:, kc, :], R=[hf, wsm],
                                 start=(kc == 0), stop=(kc == 7))
                        st = sm_st.get()
                        k.copy('act', st[:], ps[:, 0:16], R=[ps], W=[st])
                        t0 = tt * 512 + q * 128
                        k.dma('sp', abdt[t0:t0 + 128, :], st[:], R=[st], W=[Dabdt])
                groups = [(C_QKV, HG * 128, 'copy'), (C_QKV + 512, HG * 128, 'copy'), (C_QKV + 1024, HG * 128, 'copy'),
                          (C_GZ, HG * 128, 'silu'), (C_SZ, XC * 128, 'silu'), (C_XBC, XC * 128, 'copy'),
                          (C_XBC + 512, GS * 128, 'copy'), (C_XBC + 768, GS * 128, 'copy'),
                          (C_CQ, 512, 'copy'), (C_CKV, 256, 'copy'), (C_KR, 64, 'copy'), (C_GATES, 3072, 'sig')]
                wr = Ring([sb("win%d" % i, [128, 8, 512], BF16, stack=ps_) for i in range(2)])
                stg = Ring([sb("stg%d" % i, [128, 512], BF16, stack=ps_) for i in range(4)])
                ecnt = 0
                for (c0, ncols, post) in groups:
                    for blk in range(0, ncols, 512):
                        bw = min(512, ncols - blk)
                        wt = wr.get()
                        k.dma('pool', wt[:, :, :bw],
                              w_in[l][:, c0 + blk:c0 + blk + bw].rearrange('(kc p) n -> p kc n', p=128),
                              R=[Dw], W=[wt])
                        for tt in range(NT):
                            for ct in range(0, bw, 128):
                                m = min(128, bw - ct)
                                ps = psum.get()
                                for kc in range(8):
                                    k.mm(ps, ps[:m, :], wt[:, kc, ct:ct + m], h1[:, kc, tt * 512:(tt + 1) * 512],
                                         R=[wt, h1], start=(kc == 0), stop=(kc == 7))
                                st = stg.get()
                                if post == 'silu':
                                    k.act(st[:m, :], ps[:m, :], AF.Silu, R=[ps], W=[st])
                                elif post == 'sig':
                                    k.act(st[:m, :], ps[:m, :], AF.Sigmoid, R=[ps], W=[st])
                                else:
                                    ecnt += 1
                                    k.copy('dve' if ecnt % 2 else 'act', st[:m, :], ps[:m, :], R=[ps], W=[st])
                                r0 = c0 + blk + ct
                                k.dma('sp', proj[r0:r0 + m, tt * 512:(tt + 1) * 512], st[:m, :], R=[st], W=[Dproj])
                k.barrier()


        def dump(name, b, ap=None, dt=None):
            if not debug:
                return
            ap = b[:] if ap is None else ap
            uid[0] += 1
            t = nc.dram_tensor("dbg_%s_%d" % (name, uid[0]), list(ap.shape), dt or ap.dtype, kind="ExternalOutput").ap()
            k.dma('sp', t, ap, R=[b], W=[Buf(None, True)])

        cols = sb("cols", [128, 4], F32)
        k.op('dve', lambda e: e.memset(cols[:, 0:1], 1.0), W=[cols])
        k.op('dve', lambda e: e.memset(cols[:, 1:2], -np.pi), W=[cols])
        invf = sb("invf", [128, 1], F32)
        k.dma('sp', invf[:], invf_in, R=[Dw], W=[invf])
        ATT_SCALE = 192.0 ** -0.5

        def phase_mla(l):
            with ExitStack() as ps_:
                wuq = sb("wuq", [128, 4, 768], BF16, stack=ps_)
                wuqr = sb("wuqr", [128, 4, 2, 128], BF16, stack=ps_)
                wuk = sb("wuk", [128, 2, 512], BF16, stack=ps_)
                wuv = sb("wuv", [128, 2, 512], BF16, stack=ps_)
                qnw = sb("qnw", [128, 4], F32, stack=ps_)
                kvnw = sb("kvnw", [128, 2], F32, stack=ps_)
                k.dma('pool', wuq[:], mla_wuq[l].rearrange('(kc p) n -> p kc n', p=128), R=[Dw], W=[wuq])
                for h in range(HM):
                    k.dma('pool', wuqr[:, :, h // 2, (h % 2) * 64:(h % 2) * 64 + 64],
                          mla_wuq[l][:, h * 192 + 128:h * 192 + 192].rearrange('(kc p) n -> p kc n', p=128),
                          R=[Dw], W=[wuqr])
                k.dma('pool', wuk[:], mla_wuk[l].rearrange('(kc p) n -> p kc n', p=128), R=[Dw], W=[wuk])
                k.dma('pool', wuv[:], mla_wuv[l].rearrange('(kc p) n -> p kc n', p=128), R=[Dw], W=[wuv])
                k.dma('sp', qnw[:], mla_qnw[l], R=[Dw], W=[qnw])
                k.dma('sp', kvnw[:], mla_kvnw[l], R=[Dw], W=[kvnw])
                qn = sb("qn", [128, 4, T], BF16, multi=True, stack=ps_)
                qr = sb("qr", [128, 2, T], BF16, multi=True, stack=ps_)
                kn = sb("kn", [128, 4, T], BF16, multi=True, stack=ps_)
                krp = sb("krp", [128, T], BF16, multi=True, stack=ps_)
                vtm = sb("vtm", [128, NQ, 512], BF16, multi=True, stack=ps_)
                with ExitStack() as p1:
                    cin = Ring([sb("mcin%d" % i, [128, 7, 512], BF16, stack=p1) for i in range(2)])
                    sqm = sb("msq", [128, 4, 512], F32, stack=p1)
                    cqn = sb("cqn", [128, 4, 512], BF16, stack=p1)
                    ckvn = sb("ckvn", [128, 2, 512], BF16, stack=p1)
                    posi = sb("posi", [128, 512], I32, stack=p1)
                    posf = sb("posf", [128, 512], F32, stack=p1)
                    frac = sb("frac", [128, 512], F32, stack=p1)
                    fint = sb("fint", [128, 512], I32, stack=p1)
                    ftmp = sb("ftmp", [128, 512], F32, stack=p1)
                    sinT = sb("sinT", [128, 512], F32, stack=p1)
                    cosT = sb("cosT", [128, 512], F32, stack=p1)
                    rf = sb("rf", [128, 512], F32, stack=p1)
                    r1 = sb("r1", [128, 512], F32, stack=p1)
                    r2 = sb("r2", [128, 512], F32, stack=p1)
                    rstd = sb("mrstd", [128, 512], F32, stack=p1)
                    rot2 = cst[:, 5, :]

                    def rope(src_b, src_ap, dst_b, dst_ap, scale):
                        k.copy('act', rf[:], src_ap, R=[src_b], W=[rf])
                        ps = psum.get()
                        k.mm(ps, ps[:, :], rot2, rf[:], R=[cst, rf])
                        k.stt('dve', r1[:], rf[:], scale, cosT[:], ALU.mult, ALU.mult, R=[rf, cosT], W=[r1])
                        k.stt('dve', r2[:], ps[:, :], scale, sinT[:], ALU.mult, ALU.mult, R=[ps, sinT], W=[r2])
                        k.tt('dve', dst_ap, r1[:], r2[:], ALU.add, R=[r1, r2], W=[dst_b])

                    for tt in range(NT):
                        ts_ = slice(tt * 512, (tt + 1) * 512)
                        ci = cin.get()
                        k.dma('sp', ci[:, 0:6, :], proj[C_CQ:C_CQ + 768, ts_].rearrange('(kc p) t -> p kc t', p=128),
                              R=[Dproj], W=[ci])
                        k.dma('sp', ci[0:64, 6, :], proj[C_KR:C_KR + 64, ts_], R=[Dproj], W=[ci])
                        k.dma('sp', ci[64:128, 6, :], proj[C_KR:C_KR + 64, ts_], R=[Dproj], W=[ci])
                        k.dma('sp', posi[:], pos_in[0:1, ts_].partition_broadcast(128), R=[Dw], W=[posi])
                        k.copy('dve', posf[:], posi[:], R=[posi], W=[posf])
                        for (off, dst) in ((0.5, sinT), (0.75, cosT)):
                            k.ts('dve', frac[:], posf[:], invf[:, 0:1], 1.0 / (2 * np.pi), ALU.mult, ALU.mult,
                                 R=[posf, invf], W=[frac])
                            k.ts('dve', frac[:], frac[:], off, None, ALU.add, R=[frac], W=[frac])
                            k.copy('dve', fint[:], frac[:], R=[frac], W=[fint])
                            k.copy('dve', ftmp[:], fint[:], R=[fint], W=[ftmp])
                            k.tt('dve', frac[:], frac[:], ftmp[:], ALU.subtract, R=[frac, ftmp], W=[frac])
                            k.ts('dve', ftmp[:], frac[:], 0.0, None, ALU.is_lt, R=[frac], W=[ftmp])
                            k.tt('dve', frac[:], frac[:], ftmp[:], ALU.add, R=[frac, ftmp], W=[frac])
                            k.act(dst[:], frac[:], AF.Sin, R=[frac, cols], W=[dst], bias=cols[:, 1:2],
                                  scale=2 * np.pi)
                        for (c0, nk_, wv, dstb) in ((0, 4, qnw, cqn), (4, 2, kvnw, ckvn)):
                            k.act(sqm[:, 0:nk_, :], ci[:, c0:c0 + nk_, :], AF.Square, R=[ci], W=[sqm])
                            ps = psum.get()
                            for kc in range(nk_):
                                k.mm(ps, ps[:, :], ones_f, sqm[:, kc, :], R=[cst, sqm], start=(kc == 0),
                                     stop=(kc == nk_ - 1))
                            rsqrt(rstd, rstd[:], ps, ps[:, :], 1.0 / (nk_ * 128))
                            for kc in range(nk_):
                                k.stt('dve', dstb[:, kc, :], ci[:, c0 + kc, :], wv[:, kc:kc + 1], rstd[:], ALU.mult,
                                      ALU.mult, R=[ci, wv, rstd], W=[dstb])
                        for h in range(HM):
                            ps = psum.get()
                            for kc in range(4):
                                k.mm(ps, ps[:, :], wuq[:, kc, h * 192:h * 192 + 128], cqn[:, kc, :], R=[wuq, cqn],
                                     start=(kc == 0), stop=(kc == 3))
                            k.act(qn[:, h, ts_], ps[:, :], AF.Copy, R=[ps], W=[qn], scale=ATT_SCALE)
                            ps = psum.get()
                            for kc in range(2):
                                k.mm(ps, ps[:, :], wuk[:, kc, h * 128:(h + 1) * 128], ckvn[:, kc, :], R=[wuk, ckvn],
                                     start=(kc == 0), stop=(kc == 1))
                            k.copy('dve', kn[:, h, ts_], ps[:, :], R=[ps], W=[kn])
                        for hp in range(HM // 2):
                            ps = psum.get()
                            for kc in range(4):
                                k.mm(ps, ps[:, :], wuqr[:, kc, hp, :], cqn[:, kc, :], R=[wuqr, cqn],
                                     start=(kc == 0), stop=(kc == 3))
                            rope(ps, ps[:, :], qr, qr[:, hp, ts_], ATT_SCALE)
                        rope(ci, ci[:, 6, :], krp, krp[:, ts_], 1.0)
                        for q in range(4):
                            ps = psum.get()
                            for kc in range(2):
                                k.mm(ps, ps[:, :], ckvn[:, kc, q * 128:(q + 1) * 128], wuv[:, kc, :], R=[ckvn, wuv],
                                     start=(kc == 0), stop=(kc == 1))
                            k.copy('act', vtm[:, tt * 4 + q, :], ps[:, :], R=[ps], W=[vtm])
                dump("qn", qn); dump("qr", qr); dump("kn", kn); dump("krp", krp); dump("vtm", vtm)
                with ExitStack() as p2:
                    WM = 2
                    slots = []
                    for i in range(WM):
                        slots.append(dict(
                            Ssb=sb("Ssb%d" % i, [128, T], F32, stack=p2), Psb=sb("Psb%d" % i, [128, T], BF16, stack=p2),
                            ptr=Ring([sb("pt%d_%d" % (i, j), [128, 4, 128], BF16, stack=p2) for j in range(2)]),
                            sc=sb("asc%d" % i, [128, 4], F32, stack=p2), dg=sb("adg%d" % i, [128, 128], BF16, stack=p2),
                            po=psacc.bufs[i]))
                    ost = [sb("aost%d" % i, [128, 4, 128], BF16, multi=True, stack=p2) for i in range(2)]
                    done = {}

                    def att(key):
                        qi, h = key
                        B = slots[(qi * 4 + h) % WM]
                        Ssb, Psb, ptr, s_, dg, po = B['Ssb'], B['Psb'], B['ptr'], B['sc'], B['dg'], B['po']
                        ot = ost[qi % 2]
                        nk = (qi + 1) * 128
                        qs = slice(qi * 128, (qi + 1) * 128)
                        hp, ho = h // 2, (h % 2) * 64
                        for kb in range(0, nk, 512):
                            w = min(512, nk - kb)
                            ps = psum.get()
                            k.mm(ps, ps[:, :w], qn[:, h, qs], kn[:, h, kb:kb + w], R=[qn, kn], start=True,
                                 stop=False)
                            k.mm(ps, ps[:, :w], qr[ho:ho + 64, hp, qs], krp[ho:ho + 64, kb:kb + w], R=[qr, krp],
                                 start=False, stop=True)
                            if kb + w == nk:
                                if w > 128:
                                    k.copy('act', Ssb[:, kb:nk - 128], ps[:, :w - 128], R=[ps], W=[Ssb])
                                k.tt('dve', Ssb[:, nk - 128:nk], ps[:, w - 128:w], cst[:, 3, :], ALU.subtract,
                                     R=[ps, cst], W=[Ssb])
                            else:
                                k.copy('act', Ssb[:, kb:kb + w], ps[:, :w], R=[ps], W=[Ssb])
                            yield
                        k.op('dve', lambda e: e.reduce_max(s_[:, 0:1], Ssb[:, :nk], AX.X), R=[Ssb], W=[s_])
                        k.ts('dve', s_[:, 1:2], s_[:, 0:1], -1.0, None, ALU.mult, R=[s_], W=[s_])
                        yield
                        k.act(Psb[:, :nk], Ssb[:, :nk], AF.Exp, R=[Ssb, s_], W=[Psb, s_], bias=s_[:, 1:2],
                              scale=1.0, accum_out=s_[:, 2:3])
                        yield
                        k.op('dve', lambda e: e.reciprocal(s_[:, 3:4], s_[:, 2:3]), R=[s_], W=[s_])
                        k.ts('dve', dg[:], ident_b, s_[:, 3:4], None, ALU.mult, R=[cst_b, s_], W=[dg])
                        yield
                        nb = nk // 128
                        for b4 in range(0, nb, 4):
                            n4 = min(4, nb - b4)
                            ps = psum.get()
                            for j in range(n4):
                                kb2 = (b4 + j) * 128
                                k.mm(ps, ps[:, j * 128:(j + 1) * 128], Psb[:, kb2:kb2 + 128], dg[:], R=[Psb, dg])
                            pt = ptr.get()
                            k.copy('dve' if (b4 // 4) % 2 else 'act', pt[:, 0:n4, :],
                                   ps[:, 0:n4 * 128].rearrange('p (j t) -> p j t', j=n4), R=[ps], W=[pt])
                            for j in range(n4):
                                kblk = b4 + j
                                k.mm(po, po[:, 0:128], vtm[:, kblk, h * 128:(h + 1) * 128], pt[:, j, :],
                                     R=[vtm, pt], start=(kblk == 0), stop=(kblk == nb - 1))
                            yield
                        k.copy('act', ot[:, h, :], po[:, 0:128], R=[po], W=[ot])

                    def att_done(key):
                        qi, h = key
                        done[qi] = done.get(qi, 0) + 1
                        if done[qi] == HM:
                            qs = slice(qi * 128, (qi + 1) * 128)
                            k.dma('sp', (ycm if pair else y_d[2])[:, qs].rearrange('(h p) t -> p h t', p=128),
                                  ost[qi % 2][:, 0:HM, :], R=[ost[qi % 2]], W=[Dycm if pair else Dy[2]])

                    run_pipeline([(qi, h) for qi in range(NQ) for h in range(HM)], WM, att, att_done)
                if pair:
                    k.barrier()
                    allgather([(ycm, y_d[2])], Dycm, Dy[2])
                k.barrier()


        def bc(ap, shape):
            return ap.to_broadcast(list(shape))

        def run_pipeline(items, W, start_fn, finish_fn):
            active = []
            it = iter(items)
            pending = True
            while True:
                while len(active) < W and pending:
                    try:
                        key = next(it)
                    except StopIteration:
                        pending = False
                        break
                    active.append((key, start_fn(key)))
                if not active:
                    break
                for ent in list(active):
                    try:
                        next(ent[1])
                    except StopIteration:
                        active.remove(ent)
                        finish_fn(ent[0])

        def conv_silu(cin, cw, nchan, dst, dstb, tmpr, bias=None):
            for c in (range(nchan) if isinstance(nchan, int) else nchan):
                tb = tmpr.get()
                k.ts('dve', tb[:], cin[:, c, 0:512], cw[:, c, 0:1], None, ALU.mult, R=[cin, cw], W=[tb])
                for kk in range(1, 4):
                    k.stt('dve', tb[:], cin[:, c, kk:kk + 512], cw[:, c, kk:kk + 1], tb[:], ALU.mult,
                          ALU.add, R=[cin, cw, tb], W=[tb])
                if bias is None:
                    k.act(dst[:, c, :], tb[:], AF.Silu, R=[tb], W=[dstb])
                else:
                    k.act(dst[:, c, :], tb[:], AF.Silu, R=[tb, bias], W=[dstb], bias=bias[:, c:c + 1])

        def load_halo(cin, row0, nrows, tt):
            if tt == 0:
                k.op('dve', lambda e: e.memset(cin[:, :, 0:3], 0.0), W=[cin])
                k.dma('sp', cin[:, :, 3:515], proj[row0:row0 + nrows, 0:512].rearrange('(c p) t -> p c t', p=128),
                      R=[Dproj], W=[cin])
            else:
                k.dma('sp', cin[:, :, :],
                      proj[row0:row0 + nrows, tt * 512 - 3:tt * 512 + 512].rearrange('(c p) t -> p c t', p=128),
                      R=[Dproj], W=[cin])

        tri64 = cst[0:64, 2, 0:64]
        ones64 = cst[0:64, 1, 0:64]
        ones64w = cst[0:64, 1, :]
        posm64 = cst[0:64, 3, 0:64]
        strict64 = cst[0:64, 4, 0:64]
        lowm64 = cst[0:64, 6, 0:64]
        id64 = cst[0:64, 0, 0:64]

        def phase_ssd(l):
            with ExitStack() as ps_:
                def t_(name, shape, dt=F32, multi=False):
                    return sb(name, shape, dt, multi=multi, stack=ps_)
                cw = t_("scw", [128, 8, 4]); cb = t_("scb", [128, 8]); alog = t_("salog", [128, 8])
                dtb = t_("sdtb", [128, 8]); dexp = t_("sdexp", [128, 4]); nw = t_("snw", [128, 4])
                for (d_, s_) in ((cw, ssm_convT), (cb, ssm_convb), (alog, ssm_alog), (dtb, ssm_dtb), (dexp, ssm_dexp),
                                 (nw, ssm_nw)):
                    k.dma('sp', d_[:], s_[l], R=[Dw], W=[d_])
                k.act(alog[:], alog[:], AF.Exp, R=[alog], W=[alog])
                k.ts('dve', alog[:], alog[:], -1.0, None, ALU.mult, R=[alog], W=[alog])
                raw = t_("sraw", [64, NCH, 16])
                k.dma('sp', raw[:], abdt.rearrange('(c p) k -> p c k', p=64), R=[Dabdt], W=[raw])
                dt = t_("sdt", [64, NCH, HS]); ad = t_("sad", [64, NCH, HS]); acs = t_("sacs", [64, NCH, HS])
                acl = t_("sacl", [128, NCH, HS]); cd = t_("scd", [128, NCH, HS]); ds = t_("sds", [64, NCH, HS])
                eacs = t_("seacs", [64, NCH, HS])
                k.tt('dve', dt[:], raw[:, :, 8:8 + HS], bc(dtb[0:64, 0:HS].unsqueeze(1), [64, NCH, HS]), ALU.add,
                     R=[raw, dtb], W=[dt])
                k.act(dt[:], dt[:], AF.Exp, R=[dt], W=[dt])
                k.act(dt[:], dt[:], AF.Ln, R=[dt, cols], W=[dt], bias=cols[0:64, 0:1])
                k.tt('dve', ad[:], dt[:], bc(alog[0:64, 0:HS].unsqueeze(1), [64, NCH, HS]), ALU.mult, R=[dt, alog], W=[ad])
                adf = ad[:].rearrange('p c h -> p (c h)')
                ps = psum.get()
                k.mm(ps, ps[:64, :NCH * HS], tri64, adf, R=[cst, ad])
                k.copy('dve', acs[:].rearrange('p c h -> p (c h)'), ps[:64, :NCH * HS], R=[ps], W=[acs])
                ps = psum.get()
                k.mm(ps, ps[:, :NCH * HS], ones64w, adf, R=[cst, ad])
                k.copy('dve', acl[:].rearrange('p c h -> p (c h)'), ps[:, :NCH * HS], R=[ps], W=[acl])
                k.act(cd[:], acl[:], AF.Exp, R=[acl], W=[cd])
                k.tt('dve', ds[:], acl[0:64], acs[:], ALU.subtract, R=[acl, acs], W=[ds])
                k.act(ds[:], ds[:], AF.Exp, R=[ds], W=[ds])
                k.act(eacs[:], acs[:], AF.Exp, R=[acs], W=[eacs])
                state = t_("sstate", [128, HS, 64])
                k.op('dve', lambda e: e.memset(state[:], 0.0), W=[state])
                cinr = Ring([t_("scin%d" % i, [128, 8, 515], BF16) for i in range(2)])
                szr = Ring([t_("ssz%d" % i, [128, XC, 512], BF16) for i in range(2)])
                xfr = Ring([t_("sxf%d" % i, [128, 8, 512]) for i in range(2)])
                ctr = Ring([t_("sct%d" % i, [128, 512]) for i in range(2)])
                ystr = Ring([t_("syst%d" % i, [128, XC, 512], BF16, multi=True) for i in range(2)])
                WS = 2
                slots = []
                for i in range(WS):
                    slots.append(dict(
                        trig=t_("strig%d" % i, [64, HS, 64]), t1=t_("st1%d" % i, [64, HS, 64]), MT=t_("sMT%d" % i, [64, HS, 64]),
                        X=t_("sX%d" % i, [64, HS, 64]), Xds=t_("sXds%d" % i, [64, HS, 64]), Btm=t_("sBtm%d" % i, [64, GS, 128]),
                        yt=t_("syt%d" % i, [64, HS, 64]), ytm=t_("sytm%d" % i, [64, HS, 64]), yfm=t_("syfm%d" % i, [128, XC, 64]),
                        tmp=t_("stmp%d" % i, [128, XC, 64]), sq=t_("ssq%d" % i, [128, XC, 64]), rs=t_("srs%d" % i, [128, GS, 64])))
                tiles = {}
                turn = [0]
                done = {}

                def prep(tt):
                    cin = cinr.get(); szt = szr.get(); yst = ystr.get(); xf = xfr.get()
                    load_halo(cin, C_XBC, 1024, tt)
                    k.dma('sp', szt[:], proj[C_SZ:C_SZ + XC * 128, tt * 512:(tt + 1) * 512].rearrange(
                        '(c p) t -> p c t', p=128), R=[Dproj], W=[szt])
                    conv_silu(cin, cw, list(range(XC)) + [4 + g_ for g_ in range(GS)] + [6 + g_ for g_ in range(GS)], xf, xf, ctr, bias=cb)
                    tiles[tt] = (xf, szt, yst)

                def chunk(ch):
                    tt, cc = ch // 8, ch % 8
                    if cc == 0:
                        prep(tt)
                    xf, szt, yst = tiles[tt]
                    B = slots[ch % WS]
                    trig, t1, MT, X, Xds, Btm = B['trig'], B['t1'], B['MT'], B['X'], B['Xds'], B['Btm']
                    yt, ytm, yfm, tmp, sq, rs = B['yt'], B['ytm'], B['yfm'], B['tmp'], B['sq'], B['rs']
                    cs = slice(cc * 64, cc * 64 + 64)
                    ps_cb = psum.get()
                    for g in range(GS):
                        k.mm(ps_cb, ps_cb[:64, g * 64:(g + 1) * 64], xf[:, 4 + g, cs], xf[:, 6 + g, cs], R=[xf])
                    k.tt('pool', trig[:], bc(tri64.unsqueeze(1), [64, HS, 64]),
                         bc(ad[:, ch, :].unsqueeze(2), [64, HS, 64]), ALU.mult, R=[cst, ad], W=[trig])
                    ps_r = psum.get()
                    k.mm(ps_r, ps_r[:64, 0:HS * 64], ones64, trig[:].rearrange('p h l -> p (h l)'), R=[cst, trig])
                    k.tt('dve', t1[:], ps_r[:64, 0:HS * 64].rearrange('p (h l) -> p h l', h=HS),
                         bc(lowm64.unsqueeze(1), [64, HS, 64]), ALU.subtract, R=[ps_r, cst], W=[t1])
                    k.tt('dve', t1[:], t1[:], bc(acs[:, ch, :].unsqueeze(2), [64, HS, 64]), ALU.subtract,
                         R=[t1, acs], W=[t1])
                    k.act(t1[:], t1[:], AF.Exp, R=[t1], W=[t1])
                    k.tt('dve', MT[:].rearrange('p (g e) l -> p g e l', g=GS),
                         t1[:].rearrange('p (g e) l -> p g e l', g=GS),
                         bc(ps_cb[:64, 0:GS * 64].rearrange('p (g l) -> p g l', g=GS).unsqueeze(2), [64, GS, 4, 64]),
                         ALU.mult, R=[t1, ps_cb], W=[MT])
                    yield
                    ps_x = psum.get()
                    for kc in range(XC):
                        k.tr(ps_x, ps_x[:64, kc * 128:(kc + 1) * 128], xf[:, kc, cs], ident_f, R=[xf, cst])
                    k.tt('dve', X[:], ps_x[:64, 0:HS * 64].rearrange('p (h q) -> p h q', h=HS),
                         bc(dt[:, ch, :].unsqueeze(2), [64, HS, 64]), ALU.mult, R=[ps_x, dt], W=[X])
                    k.tt('pool', Xds[:], X[:], bc(ds[:, ch, :].unsqueeze(2), [64, HS, 64]), ALU.mult,
                         R=[X, ds], W=[Xds])
                    yield
                    ps_b = psum.get()
                    for g in range(GS):
                        k.tr(ps_b, ps_b[:64, g * 128:(g + 1) * 128], xf[:, 4 + g, cs], ident_f, R=[xf, cst])
                    k.copy('act', Btm[:].rearrange('p g n -> p (g n)'), ps_b[:64, 0:GS * 128], R=[ps_b], W=[Btm])
                    yield
                    while turn[0] != ch:
                        yield
                    ps_y1 = psum.get()
                    for h in range(HS):
                        k.mm(ps_y1, ps_y1[:64, h * 64:(h + 1) * 64], MT[:, h, :], X[:, h, :], R=[MT, X])
                    ps_y2 = psum.get()
                    for h in range(HS):
                        k.mm(ps_y2, ps_y2[:64, h * 64:(h + 1) * 64], xf[:, 6 + h // 4, cs], state[:, h, :],
                             R=[xf, state])
                    k.tt('dve', yt[:], ps_y2[:64, 0:HS * 64].rearrange('p (h q) -> p h q', h=HS),
                         bc(eacs[:, ch, :].unsqueeze(2), [64, HS, 64]), ALU.mult, R=[ps_y2, eacs], W=[yt])
                    k.tt('dve', ytm[:], yt[:], ps_y1[:64, 0:HS * 64].rearrange('p (h q) -> p h q', h=HS), ALU.add,
                         R=[yt, ps_y1], W=[ytm])
                    ps_s = psum.get()
                    for h in range(HS):
                        k.mm(ps_s, ps_s[:, h * 64:(h + 1) * 64], Btm[:, h // 4, :], Xds[:, h, :], R=[Btm, Xds])
                    k.tt('dve', state[:], state[:], bc(cd[:, ch, :].unsqueeze(2), [128, HS, 64]), ALU.mult,
                         R=[state, cd], W=[state])
                    k.tt('dve', state[:], state[:], ps_s[:, 0:HS * 64].rearrange('p (h q) -> p h q', h=HS), ALU.add,
                         R=[state, ps_s], W=[state])
                    turn[0] = ch + 1
                    yield
                    ps_t = psum.get()
                    ytf = ytm[:].rearrange('p h q -> p (h q)')
                    for kc in range(XC):
                        k.tr(ps_t, ps_t[:, kc * 64:(kc + 1) * 64], ytf[:, kc * 128:(kc + 1) * 128], id64,
                             R=[ytm, cst])
                    k.tt('pool', tmp[:], xf[:, 0:XC, cs], bc(dexp[:, 0:XC].unsqueeze(2), [128, XC, 64]), ALU.mult,
                         R=[xf, dexp], W=[tmp])
                    k.tt('dve', yfm[:], tmp[:], ps_t[:, 0:XC * 64].rearrange('p (c q) -> p c q', c=XC), ALU.add,
                         R=[tmp, ps_t], W=[yfm])
                    k.tt('dve', yfm[:], yfm[:], szt[:, :, cs], ALU.mult, R=[yfm, szt], W=[yfm])
                    k.act(sq[:], yfm[:], AF.Square, R=[yfm], W=[sq])
                    yield
                    ps_n = psum.get()
                    for g in range(GS):
                        for k2 in range(2):
                            k.mm(ps_n, ps_n[:, g * 64:(g + 1) * 64], ones_f, sq[:, g * 2 + k2, :], R=[cst, sq],
                                 start=(k2 == 0), stop=(k2 == 1))
                    rsqrt(rs, rs[:].rearrange('p g q -> p (g q)'), ps_n, ps_n[:, 0:GS * 64], 1.0 / 256)
                    for kc in range(XC):
                        k.stt('dve', yst[:, kc, cs], yfm[:, kc, :], nw[:, kc:kc + 1], rs[:, kc // 2, :], ALU.mult,
                              ALU.mult, R=[yfm, nw, rs], W=[yst])

                def chunk_done(ch):
                    tt = ch // 8
                    done[tt] = done.get(tt, 0) + 1
                    if done[tt] == 8:
                        yst = tiles[tt][2]
                        k.dma('sp', (ybm if pair else y_d[1])[:, tt * 512:(tt + 1) * 512].rearrange(
                            '(c p) t -> p c t', p=128), yst[:], R=[yst], W=[Dybm if pair else Dy[1]])

                run_pipeline(list(range(NCH)), WS, chunk, chunk_done)
                k.barrier()
                if pair:
                    allgather([(ybm, y_d[1])], Dybm, Dy[1])

        def phase_gdn(l):
            with ExitStack() as ps_:
                def t_(name, shape, dt=F32, multi=False):
                    return sb(name, shape, dt, multi=multi, stack=ps_)
                cw = t_("gcw", [128, 12, 4]); alog = t_("galog", [128, 4]); dtb = t_("gdtb", [128, 4])
                nw = t_("gnw", [128, 1])
                for (d_, s_) in ((cw, gdn_convT), (alog, gdn_alog), (dtb, gdn_dtb), (nw, gdn_nw)):
                    k.dma('sp', d_[:], s_[l], R=[Dw], W=[d_])
                k.act(alog[:], alog[:], AF.Exp, R=[alog], W=[alog])
                k.ts('dve', alog[:], alog[:], -1.0, None, ALU.mult, R=[alog], W=[alog])
                raw = t_("graw", [64, NCH, 16])
                k.dma('sp', raw[:], abdt.rearrange('(c p) k -> p c k', p=64), R=[Dabdt], W=[raw])
                beta = t_("gbeta", [64, NCH, HG]); nbeta = t_("gnbeta", [64, NCH, HG]); g = t_("gg", [64, NCH, HG])
                G = t_("gG", [64, NCH, HG]); Glb = t_("gGlb", [128, NCH, HG]); gl = t_("ggl", [128, NCH, HG])
                eG = t_("geG", [64, NCH, HG]); kdsc = t_("gkdsc", [64, NCH, HG]); bexpG = t_("gbexpG", [64, NCH, HG])
                k.act(beta[:], raw[:, :, 4:4 + HG], AF.Sigmoid, R=[raw], W=[beta])
                k.ts('dve', nbeta[:], beta[:], -1.0, None, ALU.mult, R=[beta], W=[nbeta])
                k.tt('dve', g[:], raw[:, :, 0:HG], bc(dtb[0:64, 0:HG].unsqueeze(1), [64, NCH, HG]), ALU.add,
                     R=[raw, dtb], W=[g])
                k.act(g[:], g[:], AF.Exp, R=[g], W=[g])
                k.act(g[:], g[:], AF.Ln, R=[g, cols], W=[g], bias=cols[0:64, 0:1])
                k.tt('dve', g[:], g[:], bc(alog[0:64, 0:HG].unsqueeze(1), [64, NCH, HG]), ALU.mult, R=[g, alog], W=[g])
                gf = g[:].rearrange('p c h -> p (c h)')
                ps = psum.get()
                k.mm(ps, ps[:64, :NCH * HG], tri64, gf, R=[cst, g])
                k.copy('dve', G[:].rearrange('p c h -> p (c h)'), ps[:64, :NCH * HG], R=[ps], W=[G])
                ps = psum.get()
                k.mm(ps, ps[:, :NCH * HG], ones64w, gf, R=[cst, g])
                k.copy('dve', Glb[:].rearrange('p c h -> p (c h)'), ps[:, :NCH * HG], R=[ps], W=[Glb])
                k.act(gl[:], Glb[:], AF.Exp, R=[Glb], W=[gl])
                k.act(eG[:], G[:], AF.Exp, R=[G], W=[eG])
                k.tt('dve', kdsc[:], Glb[0:64], G[:], ALU.subtract, R=[Glb, G], W=[kdsc])
                k.act(kdsc[:], kdsc[:], AF.Exp, R=[kdsc], W=[kdsc])
                k.tt('dve', bexpG[:], beta[:], eG[:], ALU.mult, R=[beta, eG], W=[bexpG])
                S = t_("gS", [128, HG, 128])
                k.op('dve', lambda e: e.memset(S[:], 0.0), W=[S])
                cinr = Ring([t_("gcin%d" % i, [128, 12, 515], BF16) for i in range(2)])
                gzr = Ring([t_("ggz%d" % i, [128, HG, 512], BF16) for i in range(2)])
                qkvr = Ring([t_("gqkv%d" % i, [128, 12, 512]) for i in range(2)])
                ctr = Ring([t_("gct%d" % i, [128, 512]) for i in range(2)])
                rsn = t_("grsn", [128, 512])
                ystr = Ring([t_("gyst%d" % i, [128, HG, 512], BF16, multi=True) for i in range(2)])
                WG = 2
                slots = []
                for i in range(WG):
                    d_ = {}
                    for n in ('trig', 't1', 'n1', 'qkd', 'qkT', 'Xa', 'Xb', 'Ya', 'Yb', 'Ra', 'Rb'):
                        d_[n] = t_("g%s%d" % (n, i), [64, HG, 64])
                    for n in ('ktm', 'kbg', 'kdec', 'vtm', 'u', 'vn', 'o', 'sq'):
                        d_[n] = t_("g%s%d" % (n, i), [64, HG, 128])
                    d_['wf'] = t_("gwf%d" % i, [128, HG, 64])
                    d_['ss'] = t_("gss%d" % i, [64, HG])
                    slots.append(d_)
                tiles = {}
                turn = [0]
                done = {}

                def v4(ps, w):
                    return ps[:64, 0:HG * w].rearrange('p (h x) -> p h x', h=HG)

                def prep(tt):
                    cin = cinr.get(); gz = gzr.get(); yst = ystr.get(); qkv = qkvr.get()
                    load_halo(cin, C_QKV, 1536, tt)
                    k.dma('sp', gz[:], proj[C_GZ:C_GZ + HG * 128, tt * 512:(tt + 1) * 512].rearrange(
                        '(c p) t -> p c t', p=128), R=[Dproj], W=[gz])
                    conv_silu(cin, cw, [c_ + h_ for c_ in (0, 4, 8) for h_ in range(HG)], qkv, qkv, ctr)
                    for c in [c_ + h_ for c_ in (0, 4) for h_ in range(HG)]:
                        tb = ctr.get()
                        k.act(tb[:], qkv[:, c, :], AF.Square, R=[qkv], W=[tb])
                        ps = psum.get()
                        k.mm(ps, ps[:, :], ones_f, tb[:], R=[cst, tb])
                        rsqrt(rsn, rsn[:], ps, ps[:, :], 1.0)
                        if c < 4:
                            k.stt('dve', qkv[:, c, :], qkv[:, c, :], 128.0 ** -0.5, rsn[:], ALU.mult, ALU.mult,
                                  R=[qkv, rsn], W=[qkv])
                        else:
                            k.tt('dve', qkv[:, c, :], qkv[:, c, :], rsn[:], ALU.mult, R=[qkv, rsn], W=[qkv])
                    tiles[tt] = (qkv, gz, yst)

                def chunk(ch):
                    tt, cc = ch // 8, ch % 8
                    if cc == 0:
                        prep(tt)
                    qkv, gz, yst = tiles[tt]
                    B = slots[ch % WG]
                    cs = slice(cc * 64, cc * 64 + 64)
                    b4 = lambda t, w: bc(t[:, ch, :].unsqueeze(2), [t[:, ch, :].shape[0], HG, w])
                    ktm, vtm, trig, t1, n1, qkd, qkT = B['ktm'], B['vtm'], B['trig'], B['t1'], B['n1'], B['qkd'], B['qkT']
                    ps_k = psum.get()
                    for h in range(HG):
                        k.tr(ps_k, ps_k[:64, h * 128:(h + 1) * 128], qkv[:, 4 + h, cs], ident_f, R=[qkv, cst])
                    k.copy('act', ktm[:], v4(ps_k, 128), R=[ps_k], W=[ktm])
                    yield
                    ps_v = psum.get()
                    for h in range(HG):
                        k.tr(ps_v, ps_v[:64, h * 128:(h + 1) * 128], qkv[:, 8 + h, cs], ident_f, R=[qkv, cst])
                    k.copy('act', vtm[:], v4(ps_v, 128), R=[ps_v], W=[vtm])
                    yield
                    ps_kk = psum.get()
                    for h in range(HG):
                        k.mm(ps_kk, ps_kk[:64, h * 64:(h + 1) * 64], qkv[:, 4 + h, cs], qkv[:, 4 + h, cs], R=[qkv])
                    ps_qk = psum.get()
                    for h in range(HG):
                        k.mm(ps_qk, ps_qk[:64, h * 64:(h + 1) * 64], qkv[:, h, cs], qkv[:, 4 + h, cs], R=[qkv])
                    k.tt('pool', trig[:], bc(tri64.unsqueeze(1), [64, HG, 64]), b4(g, 64), ALU.mult,
                         R=[cst, g], W=[trig])
                    ps_g = psum.get()
                    k.mm(ps_g, ps_g[:64, 0:HG * 64], ones64, trig[:].rearrange('p h l -> p (h l)'), R=[cst, trig])
                    k.tt('dve', t1[:], v4(ps_g, 64), bc(posm64.unsqueeze(1), [64, HG, 64]), ALU.add,
                         R=[ps_g, cst], W=[t1])
                    k.tt('dve', t1[:], b4(G, 64), t1[:], ALU.subtract, R=[G, t1], W=[t1])
                    k.act(t1[:], t1[:], AF.Exp, R=[t1], W=[t1])
                    X0 = B['Xa']
                    k.tt('dve', n1[:], v4(ps_kk, 64), t1[:], ALU.mult, R=[ps_kk, t1], W=[n1])
                    k.tt('dve', n1[:], n1[:], b4(nbeta, 64), ALU.mult, R=[n1, nbeta], W=[n1])
                    k.tt('pool', X0[:], n1[:], bc(strict64.unsqueeze(1), [64, HG, 64]), ALU.mult,
                         R=[n1, cst], W=[X0])
                    k.tt('dve', qkd[:], v4(ps_qk, 64), t1[:], ALU.mult, R=[ps_qk, t1], W=[qkd])
                    yield
                    ps_y = psum.get()
                    for h in range(HG):
                        k.tr(ps_y, ps_y[:64, h * 64:(h + 1) * 64], X0[:, h, :], id64, R=[X0, cst])
                    Y0 = B['Ya']
                    k.copy('act', Y0[:], v4(ps_y, 64), R=[ps_y], W=[Y0])
                    yield
                    ps_q = psum.get()
                    for h in range(HG):
                        k.tr(ps_q, ps_q[:64, h * 64:(h + 1) * 64], qkd[:, h, :], id64, R=[qkd, cst])
                    k.copy('act', qkT[:], v4(ps_q, 64), R=[ps_q], W=[qkT])
                    RT = B['Ra']
                    k.tt('pool', RT[:], Y0[:], bc(id64.unsqueeze(1), [64, HG, 64]), ALU.add, R=[Y0, cst], W=[RT])
                    yield
                    Xp, Yp = X0, Y0
                    for kk in range(1, 6):
                        Xn = B['Xb'] if Xp is B['Xa'] else B['Xa']
                        Yn = B['Yb'] if Yp is B['Ya'] else B['Ya']
                        RTn = B['Rb'] if RT is B['Ra'] else B['Ra']
                        ps_a = psum.get()
                        for h in range(HG):
                            k.mm(ps_a, ps_a[:64, h * 64:(h + 1) * 64], Yp[:, h, :], Xp[:, h, :], R=[Yp, Xp])
                        if kk <= 4:
                            ps_b = psum.get()
                            for h in range(HG):
                                k.mm(ps_b, ps_b[:64, h * 64:(h + 1) * 64], Xp[:, h, :], Yp[:, h, :], R=[Yp, Xp])
                        k.copy('act', Xn[:], v4(ps_a, 64), R=[ps_a], W=[Xn])
                        if kk <= 4:
                            k.copy('dve', Yn[:], v4(ps_b, 64), R=[ps_b], W=[Yn])
                        yield
                        ps_c = psum.get()
                        for h in range(HG):
                            k.mm(ps_c, ps_c[:64, h * 64:(h + 1) * 64], Xn[:, h, :], RT[:, h, :], R=[Xn, RT])
                        k.tt('dve', RTn[:], RT[:], v4(ps_c, 64), ALU.add, R=[RT, ps_c], W=[RTn])
                        Xp, Yp, RT = Xn, Yn, RTn
                        yield
                    kbg, kdec, u, wf, vn, o, sq, ss = B['kbg'], B['kdec'], B['u'], B['wf'], B['vn'], B['o'], B['sq'], B['ss']
                    k.tt('pool', vtm[:], vtm[:], b4(beta, 128), ALU.mult, R=[vtm, beta], W=[vtm])
                    k.tt('pool', kbg[:], ktm[:], b4(bexpG, 128), ALU.mult, R=[ktm, bexpG], W=[kbg])
                    k.tt('pool', kdec[:], ktm[:], b4(kdsc, 128), ALU.mult, R=[ktm, kdsc], W=[kdec])
                    ps_u = psum.get()
                    for h in range(HG):
                        k.mm(ps_u, ps_u[:64, h * 128:(h + 1) * 128], RT[:, h, :], vtm[:, h, :], R=[RT, vtm])
                    k.copy('act', u[:], v4(ps_u, 128), R=[ps_u], W=[u])
                    yield
                    ps_w = psum.get()
                    for h in range(HG):
                        k.mm(ps_w, ps_w[:, h * 64:(h + 1) * 64], kbg[:, h, :], RT[:, h, :], R=[kbg, RT])
                    k.copy('act', wf[:], ps_w[:, 0:HG * 64].rearrange('p (h x) -> p h x', h=HG), R=[ps_w], W=[wf])
                    yield
                    while turn[0] != ch:
                        yield
                    ps_ws = psum.get()
                    for h in range(HG):
                        k.mm(ps_ws, ps_ws[:64, h * 128:(h + 1) * 128], wf[:, h, :], S[:, h, :], R=[wf, S])
                    k.tt('dve', vn[:], u[:], v4(ps_ws, 128), ALU.subtract, R=[u, ps_ws], W=[vn])
                    ps_o1 = psum.get()
                    for h in range(HG):
                        k.mm(ps_o1, ps_o1[:64, h * 128:(h + 1) * 128], qkv[:, h, cs], S[:, h, :], R=[qkv, S])
                    ps_ds = psum.get()
                    for h in range(HG):
                        k.mm(ps_ds, ps_ds[:, h * 128:(h + 1) * 128], kdec[:, h, :], vn[:, h, :], R=[kdec, vn])
                    k.tt('dve', S[:], S[:], bc(gl[:, ch, :].unsqueeze(2), [128, HG, 128]), ALU.mult,
                         R=[S, gl], W=[S])
                    k.tt('dve', S[:], S[:], ps_ds[:, 0:HG * 128].rearrange('p (h x) -> p h x', h=HG), ALU.add,
                         R=[S, ps_ds], W=[S])
                    turn[0] = ch + 1
                    ps_o2 = psum.get()
                    for h in range(HG):
                        k.mm(ps_o2, ps_o2[:64, h * 128:(h + 1) * 128], qkT[:, h, :], vn[:, h, :], R=[qkT, vn])
                    k.tt('dve', o[:], v4(ps_o1, 128), b4(eG, 128), ALU.mult, R=[ps_o1, eG], W=[o])
                    k.tt('dve', o[:], o[:], v4(ps_o2, 128), ALU.add, R=[o, ps_o2], W=[o])
                    yield
                    k.tt('pool', sq[:], o[:], o[:], ALU.mult, R=[o], W=[sq])
                    k.op('dve', lambda e: e.reduce_sum(ss[:], sq[:], AX.X), R=[sq], W=[ss])
                    rsqrt(ss, ss[:], ss, ss[:], 1.0 / 128)
                    k.tt('dve', sq[:], o[:], bc(ss[:, :].unsqueeze(2), [64, HG, 128]), ALU.mult, R=[o, ss], W=[sq])
                    yield
                    ps_t = psum.get()
                    for h in range(HG):
                        k.tr(ps_t, ps_t[:, h * 64:(h + 1) * 64], sq[:, h, :], id64, R=[sq, cst])
                    k.stt('dve', yst[:, :, cs], ps_t[:, 0:HG * 64].rearrange('p (h x) -> p h x', h=HG), nw[:, 0:1],
                          gz[:, :, cs], ALU.mult, ALU.mult, R=[ps_t, nw, gz], W=[yst])

                def chunk_done(ch):
                    tt = ch // 8
                    done[tt] = done.get(tt, 0) + 1
                    if done[tt] == 8:
                        yst = tiles[tt][2]
                        k.dma('sp', (yam if pair else y_d[0])[:, tt * 512:(tt + 1) * 512].rearrange(
                            '(c p) t -> p c t', p=128), yst[:], R=[yst], W=[Dyam if pair else Dy[0]])

                run_pipeline(list(range(NCH)), WG, chunk, chunk_done)
                k.barrier()
                if pair:
                    allgather([(yam, y_d[0])], Dyam, Dy[0])

        def phase_mixers(l):
            if 'mla' in mixers:
                phase_mla(l)
            if 'ssd' in mixers:
                phase_ssd(l)
            if 'gdn' in mixers:
                phase_gdn(l)

        def phase_merge(l, xsrc, Dxsrc, xdst, Dxdst):
            with ExitStack() as ps_:
                wb = [sb("wbr%d" % i, [128, 4, 1024], BF16, stack=ps_) for i in range(3)]
                wo = sb("wo", [128, 8, 1024], BF16, stack=ps_)
                for i in range(3):
                    k.dma('pool', wb[i][:], w_br[i][l].rearrange('(kc p) n -> p kc n', p=128), R=[Dw], W=[wb[i]])
                k.dma('pool', wo[:], w_out[l].rearrange('(kc p) n -> p kc n', p=128), R=[Dw], W=[wo])
                yr = Ring([sb("ym%d" % i, [128, 3, 4, 512], BF16, stack=ps_) for i in range(2)])
                gr = Ring([sb("gm%d" % i, [128, 24, 512], BF16, stack=ps_) for i in range(2)])
                xr = Ring([sb("xm%d" % i, [128, 8, 512], F32, stack=ps_) for i in range(2)])
                mg = Ring([sb("mg%d" % i, [128, 8, 512], BF16, stack=ps_) for i in range(2)])
                tmp = Ring([sb("mt%d" % i, [128, 512], F32, stack=ps_) for i in range(3)])
                for tt in range(NT):
                    ts_ = slice(tt * 512, (tt + 1) * 512)
                    y = yr.get()
                    g = gr.get()
                    xt = xr.get()
                    m_ = mg.get()
                    for i in range(3):
                        k.dma('sp', y[:, i], y_d[i][:, ts_].rearrange('(kc p) t -> p kc t', p=128), R=[Dy[i]], W=[y])
                    k.dma('sp', g[:], proj[C_GATES:C_GATES + 3072, ts_].rearrange('(kc p) t -> p kc t', p=128),
                          R=[Dproj], W=[g])
                    for (a_, b_, sap) in xsrc(tt):
                        k.dma('sp', xt[:, a_:b_, :], sap, R=[Dxsrc], W=[xt])
                    for o in range(8):
                        tl = []
                        for i in range(3):
                            ps = psum.get()
                            for kc in range(4):
                                k.mm(ps, ps[:, :], wb[i][:, kc, o * 128:(o + 1) * 128], y[:, i, kc, :], R=[wb[i], y],
                                     start=(kc == 0), stop=(kc == 3))
                            t_ = tmp.get()
                            k.tt('dve', t_[:], ps[:, :], g[:, i * 8 + o, :], ALU.mult, R=[ps, g], W=[t_])
                            tl.append(t_)
                        k.tt('pool', tl[0][:], tl[0][:], tl[1][:], ALU.add, R=[tl[0], tl[1]], W=[tl[0]])
                        k.tt('pool', m_[:, o, :], tl[0][:], tl[2][:], ALU.add, R=[tl[0], tl[2]], W=[m_])
                    for o in range(8):
                        ps = psum.get()
                        for kc in range(8):
                            k.mm(ps, ps[:, :], wo[:, kc, o * 128:(o + 1) * 128], m_[:, kc, :], R=[wo, m_],
                                 start=(kc == 0), stop=(kc == 7))
                        k.stt('dve', xt[:, o, :], ps[:, :], modT[:, 16 + o:17 + o], xt[:, o, :], ALU.mult, ALU.add,
                              R=[ps, modT, xt], W=[xt])
                    k.dma('sp', xdst[:, ts_].rearrange('(kc p) t -> p kc t', p=128), xt[:], R=[xt], W=[Dxdst])
                k.barrier()

        def phase_ffn(l, xsrc, Dxsrc, xdst, Dxdst):
            moe = (l % 2 == 1)
            li = l // 2
            TT = min(TH, 1024)
            NS = TT // 512
            HCMAX = 14
            with ExitStack() as ps_:
                h2 = sb("h2", [128, 8, TT], BF16, multi=True, stack=ps_)
                xacc = sb("xacc", [128, 8, TT], F32, multi=True, stack=ps_)
                sq = sb("sq2", [128, 8, 512], F32, stack=ps_)
                hid = sb("hid", [128, HCMAX, TT], BF16, multi=True, stack=ps_)
                wgr = Ring([sb("wg%d" % i, [128, 8, 256], BF16, stack=ps_) for i in range(2)])
                wur = Ring([sb("wu%d" % i, [128, 8, 256], BF16, stack=ps_) for i in range(2)])
                wdr = Ring([sb("wd%d" % i, [128, HCMAX, 512], BF16, stack=ps_) for i in range(1)])
                sgr = Ring([sb("sg%d" % i, [128, 512], BF16, stack=ps_) for i in range(3)])
                tmp = Ring([sb("ft%d" % i, [128, 512], F32, stack=ps_) for i in range(2)])
                if moe:
                    hf = sb("hf2", [128, 8, 512], F32, stack=ps_)
                    rt = sb("rt", [128, 8, NEXP], F32, stack=ps_)
                    k.dma('sp', rt[:], moe_router[li].rearrange('(kc p) n -> p kc n', p=128), R=[Dw], W=[rt])
                    wrow = sb("wrow", [128, NEXP, TT], BF16, multi=True, stack=ps_)
                    sm = [sb("rs%d" % i, [128, 8], F32, stack=ps_) for i in range(6)]
                    sc = [sb("rc%d" % i, [128, 1], F32, stack=ps_) for i in range(4)]
                    dg = sb("dg", [128, NEXP, 128], F32, stack=ps_)
                for st_ in range(TH // TT):
                    t0 = st_ * TT
                    for s in range(NS):
                        sl = slice(s * 512, (s + 1) * 512)
                        k.dma('sp', xacc[:, :, sl],
                              xsrc[:, t0 + s * 512:t0 + (s + 1) * 512].rearrange('(kc p) t -> p kc t', p=128),
                              R=[Dxsrc], W=[xacc])
                        if pair:
                            k.dma('sp', sq[:], xsrc[:, TH + t0 + s * 512:TH + t0 + (s + 1) * 512].rearrange(
                                '(kc p) t -> p kc t', p=128), R=[Dxsrc], W=[sq])
                            k.ts('dve', xacc[:, :, sl], xacc[:, :, sl], rsel[:, 0:1], None, ALU.mult,
                                 R=[xacc, rsel], W=[xacc])
                            k.stt('dve', xacc[:, :, sl], sq[:], rsel[:, 1:2], xacc[:, :, sl], ALU.mult, ALU.add,
                                  R=[sq, rsel, xacc], W=[xacc])
                        norm_tile(xacc, xacc[:, :, sl], sq, lambda kc: h2[:, kc, sl], h2, 32, 24,
                                  hf=([hf] if moe else None))
                        if moe:
                            for q in range(4):
                                lg, m1, m2, e1, e2, wt8 = sm
                                ps = psum.get()
                                for kc in range(8):
                                    k.mm(ps, ps[:, 0:8], hf[:, kc, q * 128:(q + 1) * 128], rt[:, kc, :], R=[hf, rt],
                                         start=(kc == 0), stop=(kc == 7))
                                k.copy('dve', lg[:], ps[:, 0:8], R=[ps], W=[lg])
                                k.op('dve', lambda e: e.reduce_max(sc[0][:], lg[:], AX.X), R=[lg], W=[sc[0]])
                                k.ts('dve', e1[:], lg[:], sc[0][:, 0:1], None, ALU.is_equal, R=[lg, sc[0]], W=[e1])
                                k.stt('dve', m1[:], e1[:], -1e30, lg[:], ALU.mult, ALU.add, R=[e1, lg], W=[m1])
                                k.op('dve', lambda e: e.reduce_max(sc[1][:], m1[:], AX.X), R=[m1], W=[sc[1]])
                                k.ts('dve', e2[:], m1[:], sc[1][:, 0:1], None, ALU.is_equal, R=[m1, sc[1]], W=[e2])
                                k.tt('dve', sc[2][:], sc[0][:], sc[1][:], ALU.subtract, R=[sc[0], sc[1]], W=[sc[2]])
                                k.act(sc[3][:], sc[2][:], AF.Sigmoid, R=[sc[2]], W=[sc[3]])
                                k.act(sc[2][:], sc[2][:], AF.Sigmoid, R=[sc[2]], W=[sc[2]], scale=-1.0)
                                k.ts('dve', wt8[:], e1[:], sc[3][:, 0:1], None, ALU.mult, R=[e1, sc[3]], W=[wt8])
                                k.stt('dve', wt8[:], e2[:], sc[2][:, 0:1], wt8[:], ALU.mult, ALU.add,
                                      R=[e2, sc[2], wt8], W=[wt8])
                                k.tt('dve', dg[:], cst[:, 0:1, :].to_broadcast([128, NEXP, 128]),
                                     wt8[:, :].unsqueeze(2).to_broadcast([128, NEXP, 128]), ALU.mult,
                                     R=[cst, wt8], W=[dg])
                                for hh in range(2):
                                    ps2 = psum.get()
                                    k.mm(ps2, ps2[:, :], ones_f, dg[:, hh * 4:(hh + 1) * 4, :].rearrange('p e t -> p (e t)'), R=[cst, dg])
                                    c0 = s * 512 + q * 128
                                    k.copy('act', wrow[:, hh * 4:(hh + 1) * 4, c0:c0 + 128],
                                           ps2[:, :].rearrange('p (e t) -> p e t', e=4), R=[ps2], W=[wrow])
                    if moe:
                        passes = []
                        for e_ in range(NEXP):
                            for hh in range(2):
                                passes.append((moe_wg[li, e_], moe_wu[li, e_], moe_wd[li, e_], hh * 1792, 1792, e_))
                    else:
                        passes = [(ffn_wg[li], ffn_wu[li], ffn_wd[li], hh * 1408, 1408, None) for hh in range(2)]
                    for (wg_, wu_, wd_, h0, hn, ex) in passes:
                        HC = hn // 128
                        for cb in range(0, hn, 256):
                            cw = min(256, hn - cb)
                            wg = wgr.get()
                            wu = wur.get()
                            k.dma('pool', wg[:, :, :cw],
                                  wg_[:, h0 + cb:h0 + cb + cw].rearrange('(kc p) n -> p kc n', p=128), R=[Dw], W=[wg])
                            k.dma('pool', wu[:, :, :cw],
                                  wu_[:, h0 + cb:h0 + cb + cw].rearrange('(kc p) n -> p kc n', p=128), R=[Dw], W=[wu])
                            for jj in range(cw // 128):
                                j = cb // 128 + jj
                                for s in range(NS):
                                    sl = slice(s * 512, (s + 1) * 512)
                                    psg = psum.get()
                                    for kc in range(8):
                                        k.mm(psg, psg[:, :], wg[:, kc, jj * 128:(jj + 1) * 128], h2[:, kc, sl],
                                             R=[wg, h2], start=(kc == 0), stop=(kc == 7))
                                    psu = psum.get()
                                    for kc in range(8):
                                        k.mm(psu, psu[:, :], wu[:, kc, jj * 128:(jj + 1) * 128], h2[:, kc, sl],
                                             R=[wu, h2], start=(kc == 0), stop=(kc == 7))
                                    sg = sgr.get()
                                    k.act(sg[:], psg[:, :], AF.Silu, R=[psg], W=[sg])
                                    k.tt('dve', hid[:, j, sl], psu[:, :], sg[:], ALU.mult, R=[psu, sg], W=[hid])
                        for oh in range(2):
                            wd = wdr.get()
                            for c4 in range(0, HC, 7):
                                cn = min(7, HC - c4)
                                k.dma('pool', wd[:, c4:c4 + cn, :],
                                      wd_[h0 + c4 * 128:h0 + (c4 + cn) * 128, oh * 512:(oh + 1) * 512].rearrange(
                                          '(kc p) n -> p kc n', p=128), R=[Dw], W=[wd])
                            for oo in range(4):
                                o = oh * 4 + oo
                                for s in range(NS):
                                    sl = slice(s * 512, (s + 1) * 512)
                                    ps = psum.get()
                                    for j in range(HC):
                                        k.mm(ps, ps[:, :], wd[:, j, oo * 128:(oo + 1) * 128], hid[:, j, sl],
                                             R=[wd, hid], start=(j == 0), stop=(j == HC - 1))
                                    if ex is None:
                                        k.stt('dve', xacc[:, o, sl], ps[:, :], modT[:, 40 + o:41 + o], xacc[:, o, sl],
                                              ALU.mult, ALU.add, R=[ps, modT, xacc], W=[xacc])
                                    else:
                                        t_ = tmp.get()
                                        k.stt('dve', t_[:], ps[:, :], modT[:, 40 + o:41 + o], wrow[:, ex, sl],
                                              ALU.mult, ALU.mult, R=[ps, modT, wrow], W=[t_])
                                        k.tt('pool', xacc[:, o, sl], xacc[:, o, sl], t_[:], ALU.add,
                                             R=[t_, xacc], W=[xacc])
                    k.dma('sp', xdst[:, t0:t0 + TT].rearrange('(kc p) t -> p kc t', p=128), xacc[:], R=[xacc],
                          W=[Dxdst])
                k.barrier()

        def phase_final(xsrc, Dxsrc, ntiles):
            with ExitStack() as ps_:
                xr = Ring([sb("xf%d" % i, [128, 8, 512], F32, stack=ps_) for i in range(2)])
                orr = Ring([sb("of%d" % i, [128, 8, 512], F32, multi=True, stack=ps_) for i in range(2)])
                sq = sb("sqf", [128, 8, 512], F32, stack=ps_)
                for tt in range(ntiles):
                    ts_ = slice(tt * 512, (tt + 1) * 512)
                    xt = xr.get()
                    ot = orr.get()
                    for (a_, b_, sap) in xsrc(tt):
                        k.dma('sp', xt[:, a_:b_, :], sap, R=[Dxsrc], W=[xt])
                    norm_tile(xt, xt[:], sq, lambda kc: ot[:, kc, :], ot, None, None)
                    k.dma('sp', outT[:, ts_].rearrange('(kc p) t -> p kc t', p=128), ot[:], R=[ot], W=[Dout])
                k.barrier()

        def tiles_of(ap):
            return lambda tt: [(0, 8, ap[:, tt * 512:(tt + 1) * 512].rearrange('(kc p) t -> p kc t', p=128))]

        def tiles_of_xg(tt):
            r_, off = (tt * 512) // TH, (tt * 512) % TH
            return [(2 * kb, 2 * kb + 2,
                     xg[kb * 512 + r_ * 256:kb * 512 + r_ * 256 + 256, off:off + 512].rearrange(
                         '(kl p) t -> p kl t', p=128)) for kb in range(4)]

        cur, Dcur = tiles_of(xT_in), Dxin
        fin, Dfin, nfin = cur, Dcur, NT
        for li_, l in enumerate(layers):
            phase_ada(l)
            full, Dfull = None, None
            if 'mix' in stages:
                phase_inproj(l, cur, Dcur)
                phase_mixers(l)
                phase_merge(l, cur, Dcur, x_b, Dx_b)
                cur, Dcur = tiles_of(x_b), Dx_b
                full, Dfull = x_b, Dx_b
                fin, Dfin, nfin = cur, Dcur, NT
            if 'ffn' in stages:
                if full is None:
                    assert not pair
                    full, Dfull = (xT_in, Dxin) if li_ == 0 else (x_a, Dx_a)
                if pair:
                    phase_ffn(l, full, Dfull, xh, Dxh)
                    fin, Dfin, nfin = tiles_of(xh), Dxh, TH // 512
                    if li_ != len(layers) - 1:
                        allgather([(xh[kb * 256:(kb + 1) * 256, :], xg[kb * 512:(kb + 1) * 512, :])
                                   for kb in range(4)], Dxh, Dxg)
                        cur, Dcur = tiles_of_xg, Dxg
                else:
                    phase_ffn(l, full, Dfull, x_a, Dx_a)
                    cur, Dcur = tiles_of(x_a), Dx_a
                    fin, Dfin, nfin = cur, Dcur, NT
        phase_final(fin, Dfin, nfin)
        k.barrier()
        print("instructions:", k.ninst, {kk: v for kk, v in k.cnt.items()})
    return nc


def _consts():
    c = np.zeros((128, 8, 128), np.float32)
    i = np.arange(128)
    c[:, 0, :] = np.eye(128)
    c[:, 1, :] = 1.0
    c[:, 2, :] = (i[:, None] <= i[None, :])
    c[:, 3, :] = np.where(i[None, :] > i[:, None], 1e9, 0.0)
    c[:, 4, :] = (i[None, :] < i[:, None])
    rot = np.zeros((128, 128), np.float32)
    for o in (0, 64):
        for m in range(32):
            rot[o + m + 32, o + m] = -1.0
            rot[o + m, o + m + 32] = 1.0
    c[:, 5, :] = rot
    c[:, 6, :] = np.where(i[None, :] < i[:, None], 1e9, 0.0)
    return c


def _fm(v, nchunk):
    return np.ascontiguousarray(np.asarray(v, np.float32).reshape(nchunk, 128).T)


_PERM_CACHE = {}


def _perm_weights(inp, r):
    if r in _PERM_CACHE:
        return _PERM_CACHE[r]
    perm = [2 * r, 2 * r + 1] + [h for h in range(4) if h not in (2 * r, 2 * r + 1)]
    out = {}
    cols = np.arange(IN_DIM)
    for base in (0, 512, 1024, C_GZ):
        for i, h in enumerate(perm):
            cols[base + i * 128:base + (i + 1) * 128] = np.arange(base + h * 128, base + (h + 1) * 128)
    for base in (C_A, C_B):
        for i, h in enumerate(perm):
            cols[base + i] = base + h
    gp = [r, 1 - r]

    def blocks(arr, base, bs):
        src = arr.copy()
        for i, g_ in enumerate(gp):
            arr[base + i * bs:base + (i + 1) * bs] = src[base + g_ * bs:base + (g_ + 1) * bs]

    for (base, bs) in ((C_SZ, 256), (C_XBC, 256), (C_XBC + 512, 128), (C_XBC + 768, 128), (C_DT, 4)):
        blocks(cols, base, bs)
    out["w_in"] = np.ascontiguousarray(np.asarray(inp["w_in"], np.float32)[:, :, cols])
    sc_ = np.arange(1024)
    for (base, bs) in ((0, 256), (512, 128), (768, 128)):
        blocks(sc_, base, bs)
    out["ssm_conv_w"] = np.ascontiguousarray(np.asarray(inp["ssm_conv_w"], np.float32)[:, :, sc_])
    out["ssm_conv_b"] = np.ascontiguousarray(np.asarray(inp["ssm_conv_b"], np.float32)[:, sc_])
    h8 = np.arange(8)
    blocks(h8, 0, 4)
    for n_ in ("ssm_a_log", "ssm_dt_bias", "ssm_d"):
        out[n_] = np.ascontiguousarray(np.asarray(inp[n_], np.float32)[:, h8])
    n512 = np.arange(512)
    blocks(n512, 0, 256)
    out["ssm_norm_w"] = np.ascontiguousarray(np.asarray(inp["ssm_norm_w"], np.float32)[:, n512])
    cc = np.arange(1536)
    for base in (0, 512, 1024):
        for i, h in enumerate(perm):
            cc[base + i * 128:base + (i + 1) * 128] = np.arange(base + h * 128, base + (h + 1) * 128)
    out["gdn_conv_w"] = np.ascontiguousarray(np.asarray(inp["gdn_conv_w"], np.float32)[:, :, cc])
    out["gdn_a_log"] = np.ascontiguousarray(np.asarray(inp["gdn_a_log"], np.float32)[:, perm])
    out["gdn_dt_bias"] = np.ascontiguousarray(np.asarray(inp["gdn_dt_bias"], np.float32)[:, perm])
    cq = np.concatenate([np.arange(h * 192, (h + 1) * 192) for h in perm])
    ck = np.concatenate([np.arange(h * 128, (h + 1) * 128) for h in perm])
    out["mla_w_uq"] = np.ascontiguousarray(np.asarray(inp["mla_w_uq"], np.float32)[:, :, cq])
    out["mla_w_uk"] = np.ascontiguousarray(np.asarray(inp["mla_w_uk"], np.float32)[:, :, ck])
    out["mla_w_uv"] = np.ascontiguousarray(np.asarray(inp["mla_w_uv"], np.float32)[:, :, ck])
    _PERM_CACHE[r] = out
    return out


def prep_inputs(inp, b, T, r=None):
    if r is not None and r != 0:
        inp = dict(inp)
        inp.update(_perm_weights(inp, r))
    f = lambda a: np.ascontiguousarray(np.asarray(a, np.float32))
    L = DEPTH
    m = {}
    m["xT"] = np.ascontiguousarray(np.asarray(inp["x"][b, :T], np.float32).T)
    m["cT"] = _fm(inp["c"][b], 8)
    m["pos"] = np.ascontiguousarray(np.asarray(inp["positions"][b, :T], np.int32).reshape(1, T))
    m["w_ada"] = f(inp["w_ada"])
    m["b_adaT"] = np.stack([_fm(inp["b_ada"][l], 48) for l in range(L)])
    m["w_in"] = f(inp["w_in"])
    gc = np.asarray(inp["gdn_conv_w"], np.float32)
    m["gdn_convT"] = np.ascontiguousarray(gc.reshape(L, 4, 12, 128).transpose(0, 3, 2, 1))
    rep = lambda a: np.ascontiguousarray(np.broadcast_to(np.asarray(a, np.float32)[:, None, :], (L, 128, a.shape[-1])))
    m["gdn_alog"] = rep(inp["gdn_a_log"])
    m["gdn_dtb"] = rep(inp["gdn_dt_bias"])
    m["gdn_nw"] = np.ascontiguousarray(np.asarray(inp["gdn_norm_w"], np.float32).reshape(L, 128, 1))
    sc = np.asarray(inp["ssm_conv_w"], np.float32)
    m["ssm_convT"] = np.ascontiguousarray(sc.reshape(L, 4, 8, 128).transpose(0, 3, 2, 1))
    m["ssm_convb"] = np.stack([_fm(inp["ssm_conv_b"][l], 8) for l in range(L)])
    m["ssm_alog"] = rep(inp["ssm_a_log"])
    m["ssm_dtb"] = rep(inp["ssm_dt_bias"])
    dexp = np.repeat(np.asarray(inp["ssm_d"], np.float32), 64, axis=1)
    m["ssm_dexp"] = np.stack([_fm(dexp[l], 4) for l in range(L)])
    m["ssm_nw"] = np.stack([_fm(inp["ssm_norm_w"][l], 4) for l in range(L)])
    m["mla_qnw"] = np.stack([_fm(inp["mla_q_norm_w"][l], 4) for l in range(L)])
    m["mla_wuq"] = f(inp["mla_w_uq"])
    m["mla_kvnw"] = np.stack([_fm(inp["mla_kv_norm_w"][l], 2) for l in range(L)])
    m["mla_wuk"] = f(inp["mla_w_uk"])
    m["mla_wuv"] = f(inp["mla_w_uv"])
    for n in ("w_branch_a", "w_branch_b", "w_branch_c", "w_out", "ffn_w_gate", "ffn_w_up", "ffn_w_down",
              "moe_router", "moe_w_gate", "moe_w_up", "moe_w_down"):
        m[n] = f(inp[n])
    m["fnwT"] = _fm(inp["final_norm_w"], 8)
    m["consts"] = _consts()
    m["rsel"] = np.stack([np.ones(128, np.float32), np.zeros(128, np.float32)], 1)
    invf = (10000.0 ** (-np.arange(0, 64, 2, dtype=np.float32) / 64)).astype(np.float32)
    m["invf"] = np.concatenate([invf] * 4).reshape(128, 1).astype(np.float32)
    return m


def kernel(**inputs):
    B, T = inputs["x"].shape[0], inputs["x"].shape[1]
    nc = build_program(T, list(range(DEPTH)), pair=True)
    base = [prep_inputs(inputs, b, T) for b in range(B)]
    in_maps = []
    _PERM_CACHE.clear()
    base1 = [prep_inputs(inputs, b, T, r=1) for b in range(B)]
    for c in range(2 * B):
        m = dict((base, base1)[c % 2][c // 2])
        rs = np.zeros((128, 2), np.float32)
        rs[:, c % 2] = 1.0
        m["rsel"] = rs
        in_maps.append(m)
    res = run_bass_kernel_spmd(nc, in_maps, core_ids=list(range(2 * B)))
    TH = T // 2
    out = np.zeros((B, T, D), np.float32)
    for c in range(2 * B):
        out[c // 2, (c % 2) * TH:(c % 2 + 1) * TH, :] = np.asarray(res.results[c]["outT"], np.float32).T
    return out
```

```python
import numpy as np
from contextlib import ExitStack
import concourse.bass as bass
import concourse.mybir as mybir
from concourse.bass_utils import run_bass_kernel_spmd

F32, BF16, I32 = mybir.dt.float32, mybir.dt.bfloat16, mybir.dt.int32
AF = mybir.ActivationFunctionType
ALU = mybir.AluOpType
AX = mybir.AxisListType

D = 1024
DEPTH = 4
EPS = 1e-6
IN_DIM = 7504
FFN_DIM = 2816
EXPERT_DIM = 3584
NEXP = 8
SAME_SYNC = True

C_QKV, C_GZ, C_A, C_B, C_SZ, C_XBC, C_DT, C_CQ, C_CKV, C_KR, C_GATES = (
    0, 1536, 2048, 2052, 2056, 2568, 3592, 3600, 4112, 4368, 4432)


class Buf:
    def __init__(self, t, multi=False):
        self.t = t
        self.multi = multi
        self.w = {}
        self.r = {}

    def __getitem__(self, key):
        return self.t[key]


def _merge(d, tok):
    k_, v = tok
    if d.get(k_, 0) < v:
        d[k_] = v


class Ring:
    def __init__(self, bufs):
        self.bufs = bufs
        self.i = 0

    def get(self):
        b = self.bufs[self.i % len(self.bufs)]
        self.i += 1
        return b


class KB:
    ENG = ('pe', 'act', 'dve', 'pool', 'sp')

    def __init__(self, nc, es):
        self.nc = nc
        self.es = es
        self.e = {'pe': nc.tensor, 'act': nc.scalar, 'dve': nc.vector, 'pool': nc.gpsimd, 'sp': nc.sync}
        self.sem = {}
        self.cnt = {}
        for e in self.ENG:
            self.sem[('e', e)] = es.enter_context(nc.semaphore('se_' + e))
            self.cnt[('e', e)] = 0
        self.NS = 8
        self.dma_i = {}
        for q in ('sp', 'pool'):
            self.dma_i[q] = 0
            for j in range(self.NS):
                self.sem[('d', q, j)] = es.enter_context(nc.semaphore('sd_%s%d' % (q, j)))
                self.cnt[('d', q, j)] = 0
        self.seen = {e: {} for e in self.ENG}
        self.ninst = 0

    def _wait(self, eng, deps):
        for key, v in deps.items():
            if key == ('e', eng) and (eng == 'pe' or eng == 'sp' or not SAME_SYNC):
                continue
            if self.seen[eng].get(key, 0) >= v:
                continue
            self.e[eng].wait_ge(self.sem[key], v)
            self.seen[eng][key] = v
            self.ninst += 1

    def _deps(self, R, W):
        deps = {}
        for b in R:
            for t in b.w.items():
                _merge(deps, t)
        for b in W:
            for t in b.r.items():
                _merge(deps, t)
            if not b.multi:
                for t in b.w.items():
                    _merge(deps, t)
        return deps

    def _post(self, tok, R, W):
        for b in R:
            _merge(b.r, tok)
        for b in W:
            if b.multi:
                _merge(b.w, tok)
            else:
                b.w = {tok[0]: tok[1]}
                b.r = {}

    def op(self, eng, fn, R=(), W=()):
        self._wait(eng, self._deps(R, W))
        ins = fn(self.e[eng])
        key = ('e', eng)
        self.cnt[key] += 1
        ins.then_inc(self.sem[key], 1)
        self.ninst += 1
        self._post((key, self.cnt[key]), R, W)

    def dma(self, q, out, in_, R=(), W=()):
        self._wait(q, self._deps(R, W))
        key = ('d', q, self.dma_i[q] % self.NS)
        self.dma_i[q] += 1
        if self.cnt[key] > 0:
            self._wait(q, {key: self.cnt[key]})
        ins = self.e[q].dma_start(out=out, in_=in_)
        self.cnt[key] += 16
        ins.then_inc(self.sem[key], 16)
        self.ninst += 1
        self._post((key, self.cnt[key]), R, W)

    def barrier(self):
        for e in self.ENG:
            deps = {key: v for key, v in self.cnt.items() if v > 0 and key != ('e', e)}
            self._wait(e, deps)

    def mm(self, ps, out, lhsT, rhs, R, start=True, stop=True):
        self.op('pe', lambda e: e.matmul(out, lhsT, rhs, start=start, stop=stop), R=R, W=[ps])

    def tr(self, ps, out, in_, ident, R):
        self.op('pe', lambda e: e.transpose(out, in_, ident), R=R, W=[ps])

    def act(self, out, in_, func, R, W, bias=None, scale=None, accum_out=None, eng='act'):
        kw = {}
        if bias is not None:
            kw['bias'] = bias
        if scale is not None:
            kw['scale'] = scale
        if accum_out is not None:
            kw['accum_out'] = accum_out
        self.op('act', lambda e: e.activation(out=out, in_=in_, func=func, **kw), R=R, W=W)

    def tt(self, eng, out, in0, in1, op, R, W):
        self.op(eng, lambda e: e.tensor_tensor(out, in0, in1, op), R=R, W=W)

    def ts(self, eng, out, in0, s1, s2, op0, op1=None, R=(), W=()):
        if op1 is None:
            self.op(eng, lambda e: e.tensor_scalar(out, in0, s1, None, op0), R=R, W=W)
        else:
            self.op(eng, lambda e: e.tensor_scalar(out, in0, s1, s2, op0, op1), R=R, W=W)

    def stt(self, eng, out, in0, scalar, in1, op0, op1, R, W):
        self.op(eng, lambda e: e.scalar_tensor_tensor(out, in0, scalar, in1, op0, op1), R=R, W=W)

    def copy(self, eng, out, in_, R, W):
        if eng == 'act':
            self.op('act', lambda e: e.activation(out=out, in_=in_, func=AF.Copy), R=R, W=W)
        else:
            self.op(eng, lambda e: e.tensor_copy(out, in_), R=R, W=W)


def build_program(T, layers, debug=False, stages=('mix', 'ffn'), mixers=('mla', 'ssd', 'gdn'), pair=False, ncores=8):
    nc = bass.Bass("TRN2", target_bir_lowering=False)
    L = DEPTH
    NT = T // 512
    NQ = T // 128
    NCH = T // 64
    TH = T // 2 if pair else T
    HG = 2 if pair else 4
    HM = 2 if pair else 4
    GS = 1 if pair else 2
    HS = 4 * GS
    XC = 2 * GS

    def din(name, shape, dt=F32):
        return nc.dram_tensor(name, list(shape), dt, kind="ExternalInput").ap()

    def dscr(name, shape, dt, out=False):
        kind = "ExternalOutput" if out else "Internal"
        return nc.dram_tensor(name, list(shape), dt, kind=kind).ap()

    xT_in = din("xT", [D, T])
    cT_in = din("cT", [128, 8])
    pos_in = din("pos", [1, T], I32)
    w_ada = din("w_ada", [L, D, 6 * D])
    b_adaT = din("b_adaT", [L, 128, 48])
    w_in = din("w_in", [L, D, IN_DIM])
    gdn_convT = din("gdn_convT", [L, 128, 12, 4])
    gdn_alog = din("gdn_alog", [L, 128, 4])
    gdn_dtb = din("gdn_dtb", [L, 128, 4])
    gdn_nw = din("gdn_nw", [L, 128, 1])
    ssm_convT = din("ssm_convT", [L, 128, 8, 4])
    ssm_convb = din("ssm_convb", [L, 128, 8])
    ssm_alog = din("ssm_alog", [L, 128, 8])
    ssm_dtb = din("ssm_dtb", [L, 128, 8])
    ssm_dexp = din("ssm_dexp", [L, 128, 4])
    ssm_nw = din("ssm_nw", [L, 128, 4])
    mla_qnw = din("mla_qnw", [L, 128, 4])
    mla_wuq = din("mla_wuq", [L, 512, 768])
    mla_kvnw = din("mla_kvnw", [L, 128, 2])
    mla_wuk = din("mla_wuk", [L, 256, 512])
    mla_wuv = din("mla_wuv", [L, 256, 512])
    w_br = [din("w_branch_a", [L, 512, D]), din("w_branch_b", [L, 512, D]), din("w_branch_c", [L, 512, D])]
    w_out = din("w_out", [L, D, D])
    ffn_wg = din("ffn_w_gate", [2, D, FFN_DIM])
    ffn_wu = din("ffn_w_up", [2, D, FFN_DIM])
    ffn_wd = din("ffn_w_down", [2, FFN_DIM, D])
    moe_router = din("moe_router", [2, D, NEXP])
    moe_wg = din("moe_w_gate", [2, NEXP, D, EXPERT_DIM])
    moe_wu = din("moe_w_up", [2, NEXP, D, EXPERT_DIM])
    moe_wd = din("moe_w_down", [2, NEXP, EXPERT_DIM, D])
    fnwT = din("fnwT", [128, 8])
    consts = din("consts", [128, 8, 128])
    invf_in = din("invf", [128, 1])

    outT = dscr("outT", [D, TH], F32, out=True)
    rsel_in = din("rsel", [128, 2])
    xh = dscr("xh", [D, TH], F32)
    xg = dscr("xg", [2 * D, TH], F32)
    yam = dscr("yam", [HG * 128, T], BF16)
    ycm = dscr("ycm", [HM * 128, T], BF16)
    ybm = dscr("ybm", [XC * 128, T], BF16)
    x_a = dscr("x_a", [D, T], F32, out=debug)
    x_b = dscr("x_b", [D, T], F32, out=debug)
    proj = dscr("proj", [IN_DIM, T], BF16, out=debug)
    abdt = dscr("abdt", [T, 16], F32, out=debug)
    y_d = [dscr("y_a", [512, T], BF16, out=debug), dscr("y_b", [512, T], BF16, out=debug),
           dscr("y_c", [512, T], BF16, out=debug)]

    es = ExitStack()
    with es:
        k = KB(nc, es)

        uid = [0]

        def sb(name, shape, dt, multi=False, stack=es):
            uid[0] += 1
            return Buf(stack.enter_context(nc.sbuf_tensor("%s_%d" % (name, uid[0]), list(shape), dt)), multi=multi)

        Dx_a, Dx_b, Dproj, Dabdt = Buf(x_a, True), Buf(x_b, True), Buf(proj, True), Buf(abdt, True)
        Dy = [Buf(y, True) for y in y_d]
        Dout = Buf(outT, True)
        Dxh, Dxg = Buf(xh, True), Buf(xg, True)
        Dyam, Dycm, Dybm = Buf(yam, True), Buf(ycm, True), Buf(ybm, True)

        def allgather(pieces, Dsrc, Ddst):
            k._wait('pool', k._deps([Dsrc], [Ddst]))
            for (sap, dap) in pieces:
                ins = nc.gpsimd.collective_compute(
                    "AllGather", ALU.bypass, replica_groups=[[2 * i_, 2 * i_ + 1] for i_ in range(ncores // 2)],
                    ins=[sap.opt()], outs=[dap.opt()])
                k.cnt[('c',)] += 1
                ins.then_inc(k.csem, 1)
            k._post((('c',), k.cnt[('c',)]), [Dsrc], [Ddst])
            k.barrier()

        k.csem = es.enter_context(nc.semaphore('s_cc'))
        k.sem[('c',)] = k.csem
        k.cnt[('c',)] = 0
        rsel = sb("rsel", [128, 2], F32)
        k.dma('sp', rsel[:], rsel_in, R=[Buf(None)], W=[rsel])
        Dw = Buf(None)
        Dxin = Buf(xT_in)

        psum = Ring([Buf(es.enter_context(nc.psum_tensor("ps%d" % i, [128, 512], F32))) for i in range(6)])
        psacc = Ring([Buf(es.enter_context(nc.psum_tensor("pa%d" % i, [128, 512], F32))) for i in range(2)])

        cst = sb("cst", [128, 8, 128], F32)
        k.dma('sp', cst[:], consts, R=[Dw], W=[cst])
        ident_f = cst[:, 0, :]
        ones_f = cst[:, 1, :]
        cst_b = sb("cst_b", [128, 2, 128], BF16)
        k.copy('dve', cst_b[:], cst[:, 0:2, :], R=[cst], W=[cst_b])
        ident_b = cst_b[:, 0, :]
        condT = sb("condT", [128, 8], F32)
        k.dma('sp', condT[:], cT_in, R=[Dw], W=[condT])
        k.act(condT[:], condT[:], AF.Silu, R=[condT], W=[condT])
        modT = sb("modT", [128, 48], F32)
        fnw = sb("fnw", [128, 8], F32)
        k.dma('sp', fnw[:], fnwT, R=[Dw], W=[fnw])

        def phase_ada(l):
            with ExitStack() as ps_:
                wr = Ring([sb("wada%d" % i, [128, 8, 768], F32, stack=ps_) for i in range(2)])
                bt = sb("badat", [128, 48], F32, stack=ps_)
                k.dma('sp', bt[:], b_adaT[l], R=[Dw], W=[bt])
                ps = psum.get()
                for g in range(8):
                    wb = wr.get()
                    k.dma('sp', wb[:], w_ada[l][:, g * 768:(g + 1) * 768].rearrange('(kc p) n -> p kc n', p=128),
                          R=[Dw], W=[wb])
                    for jj in range(6):
                        j = g * 6 + jj
                        for kc in range(8):
                            k.mm(ps, ps[:, j:j + 1], wb[:, kc, jj * 128:(jj + 1) * 128], condT[:, kc:kc + 1],
                                 R=[wb, condT], start=(kc == 0), stop=(kc == 7))
                k.tt('dve', modT[:], ps[:, 0:48], bt[:], ALU.add, R=[ps, bt], W=[modT])
                k.ts('dve', modT[:, 8:16], modT[:, 8:16], 1.0, None, ALU.add, R=[modT], W=[modT])
                k.ts('dve', modT[:, 32:40], modT[:, 32:40], 1.0, None, ALU.add, R=[modT], W=[modT])
                k.barrier()

        def norm_tile(xt, xap, sq, hdst, hbuf, sc_off, sh_off, hf=None):
            k.act(sq[:], xap, AF.Square, R=[xt], W=[sq])
            ps = psum.get()
            for kc in range(8):
                k.mm(ps, ps[:, :], ones_f, sq[:, kc, :], R=[sq, cst], start=(kc == 0), stop=(kc == 7))
            rstd = rstd_ring.get()
            rsqrt(rstd, rstd[:], ps, ps[:, :], 1.0 / D)
            for kc in range(8):
                k.tt('pool' if kc % 2 else 'dve', sq[:, kc, :], xap[:, kc, :], rstd[:], ALU.mult,
                     R=[xt, rstd], W=[sq])
            for kc in range(8):
                if sc_off is None:
                    k.ts('dve', hdst(kc), sq[:, kc, :], fnw[:, kc:kc + 1], None, ALU.mult, R=[sq, fnw], W=[hbuf])
                else:
                    k.ts('dve', hdst(kc), sq[:, kc, :], modT[:, sc_off + kc:sc_off + kc + 1],
                         modT[:, sh_off + kc:sh_off + kc + 1], ALU.mult, ALU.add, R=[sq, modT], W=[hbuf])
                    if hf is not None:
                        k.ts('pool', hf[0][:, kc, :], sq[:, kc, :], modT[:, sc_off + kc:sc_off + kc + 1],
                             modT[:, sh_off + kc:sh_off + kc + 1], ALU.mult, ALU.add, R=[sq, modT], W=[hf[0]])

        epsT = sb("epsT", [128, 1], F32)
        k.op('dve', lambda e: e.memset(epsT[:], EPS), W=[epsT])

        def rsqrt(ob, out, ib, in_, scale):
            k.act(out, in_, AF.Sqrt, R=[ib, epsT], W=[ob], bias=epsT[:out.shape[0], 0:1], scale=scale)
            k.op('dve', lambda e: e.reciprocal(out, out), R=[ob], W=[ob])

        rstd_ring = Ring([sb("rstd%d" % i, [128, 512], F32) for i in range(2)])

        def phase_inproj(l, xsrc, Dxsrc):
            with ExitStack() as ps_:
                h1 = sb("h1", [128, 8, T], BF16, multi=True, stack=ps_)
                xr = Ring([sb("xt%d" % i, [128, 8, 512], F32, stack=ps_) for i in range(2)])
                sq = sb("sq", [128, 8, 512], F32, stack=ps_)
                hf = sb("hf", [128, 8, 512], F32, stack=ps_)
                wsm = sb("wsm", [128, 8, 16], F32, stack=ps_)
                sm_st = Ring([sb("smst%d" % i, [128, 16], F32, stack=ps_) for i in range(2)])
                k.dma('sp', wsm[:, :, 0:8], w_in[l][:, C_A:C_A + 8].rearrange('(kc p) n -> p kc n', p=128),
                      R=[Dw], W=[wsm])
                k.dma('sp', wsm[:, :, 8:16], w_in[l][:, C_DT:C_DT + 8].rearrange('(kc p) n -> p kc n', p=128),
                      R=[Dw], W=[wsm])
                for tt in range(NT):
                    xt = xr.get()
                    for (a_, b_, sap) in xsrc(tt):
                        k.dma('sp', xt[:, a_:b_, :], sap, R=[Dxsrc], W=[xt])
                    norm_tile(xt, xt[:], sq, lambda kc: h1[:, kc, tt * 512:(tt + 1) * 512], h1, 8, 0, hf=[hf])
                    for q in range(4):
                        ps = psum.get()
                        for kc in range(8):
                            k.mm(ps, ps[:, 0:16], hf[:, kc, q * 128:(q + 1) * 128], wsm[:, kc, :], R=[hf, wsm],
                                 start=(kc == 0), stop=(kc == 7))
                        st = sm_st.get()
                        k.copy('act', st[:], ps[:, 0:16], R=[ps], W=[st])
                        t0 = tt * 512 + q * 128
                        k.dma('sp', abdt[t0:t0 + 128, :], st[:], R=[st], W=[Dabdt])
                groups = [(C_QKV, HG * 128, 'copy'), (C_QKV + 512, HG * 128, 'copy'), (C_QKV + 1024, HG * 128, 'copy'),
                          (C_GZ, HG * 128, 'silu'), (C_SZ, XC * 128, 'silu'), (C_XBC, XC * 128, 'copy'),
                          (C_XBC + 512, GS * 128, 'copy'), (C_XBC + 768, GS * 128, 'copy'),
                          (C_CQ, 512, 'copy'), (C_CKV, 256, 'copy'), (C_KR, 64, 'copy'), (C_GATES, 3072, 'sig')]
                wr = Ring([sb("win%d" % i, [128, 8, 512], BF16, stack=ps_) for i in range(2)])
                stg = Ring([sb("stg%d" % i, [128, 512], BF16, stack=ps_) for i in range(4)])
                ecnt = 0
                for (c0, ncols, post) in groups:
                    for blk in range(0, ncols, 512):
                        bw = min(512, ncols - blk)
                        wt = wr.get()
                        k.dma('pool', wt[:, :, :bw],
                              w_in[l][:, c0 + blk:c0 + blk + bw].rearrange('(kc p) n -> p kc n', p=128),
                              R=[Dw], W=[wt])
                        for tt in range(NT):
                            for ct in range(0, bw, 128):
                                m = min(128, bw - ct)
                                ps = psum.get()
                                for kc in range(8):
                                    k.mm(ps, ps[:m, :], wt[:, kc, ct:ct + m], h1[:, kc, tt * 512:(tt + 1) * 512],
                                         R=[wt, h1], start=(kc == 0), stop=(kc == 7))
                                st = stg.get()
                                if post == 'silu':
                                    k.act(st[:m, :], ps[:m, :], AF.Silu, R=[ps], W=[st])
                                elif post == 'sig':
                                    k.act(st[:m, :], ps[:m, :], AF.Sigmoid, R=[ps], W=[st])
                                else:
                                    ecnt += 1
                                    k.copy('dve' if ecnt % 2 else 'act', st[:m, :], ps[:m, :], R=[ps], W=[st])
                                r0 = c0 + blk + ct
                                k.dma('sp', proj[r0:r0 + m, tt * 512:(tt + 1) * 512], st[:m, :], R=[st], W=[Dproj])
                k.barrier()


        def dump(name, b, ap=None, dt=None):
            if not debug:
                return
            ap = b[:] if ap is None else ap
            uid[0] += 1
            t = nc.dram_tensor("dbg_%s_%d" % (name, uid[0]), list(ap.shape), dt or ap.dtype, kind="ExternalOutput").ap()
            k.dma('sp', t, ap, R=[b], W=[Buf(None, True)])

        cols = sb("cols", [128, 4], F32)
        k.op('dve', lambda e: e.memset(cols[:, 0:1], 1.0), W=[cols])
        k.op('dve', lambda e: e.memset(cols[:, 1:2], -np.pi), W=[cols])
        invf = sb("invf", [128, 1], F32)
        k.dma('sp', invf[:], invf_in, R=[Dw], W=[invf])
        ATT_SCALE = 192.0 ** -0.5

        def phase_mla(l):
            with ExitStack() as ps_:
                wuq = sb("wuq", [128, 4, 768], BF16, stack=ps_)
                wuqr = sb("wuqr", [128, 4, 2, 128], BF16, stack=ps_)
                wuk = sb("wuk", [128, 2, 512], BF16, stack=ps_)
                wuv = sb("wuv", [128, 2, 512], BF16, stack=ps_)
                qnw = sb("qnw", [128, 4], F32, stack=ps_)
                kvnw = sb("kvnw", [128, 2], F32, stack=ps_)
                k.dma('pool', wuq[:], mla_wuq[l].rearrange('(kc p) n -> p kc n', p=128), R=[Dw], W=[wuq])
                for h in range(HM):
                    k.dma('pool', wuqr[:, :, h // 2, (h % 2) * 64:(h % 2) * 64 + 64],
                          mla_wuq[l][:, h * 192 + 128:h * 192 + 192].rearrange('(kc p) n -> p kc n', p=128),
                          R=[Dw], W=[wuqr])
                k.dma('pool', wuk[:], mla_wuk[l].rearrange('(kc p) n -> p kc n', p=128), R=[Dw], W=[wuk])
                k.dma('pool', wuv[:], mla_wuv[l].rearrange('(kc p) n -> p kc n', p=128), R=[Dw], W=[wuv])
                k.dma('sp', qnw[:], mla_qnw[l], R=[Dw], W=[qnw])
                k.dma('sp', kvnw[:], mla_kvnw[l], R=[Dw], W=[kvnw])
                qn = sb("qn", [128, 4, T], BF16, multi=True, stack=ps_)
                qr = sb("qr", [128, 2, T], BF16, multi=True, stack=ps_)
                kn = sb("kn", [128, 4, T], BF16, multi=True, stack=ps_)
                krp = sb("krp", [128, T], BF16, multi=True, stack=ps_)
                vtm = sb("vtm", [128, NQ, 512], BF16, multi=True, stack=ps_)
                with ExitStack() as p1:
                    cin = Ring([sb("mcin%d" % i, [128, 7, 512], BF16, stack=p1) for i in range(2)])
                    sqm = sb("msq", [128, 4, 512], F32, stack=p1)
                    cqn = sb("cqn", [128, 4, 512], BF16, stack=p1)
                    ckvn = sb("ckvn", [128, 2, 512], BF16, stack=p1)
                    posi = sb("posi", [128, 512], I32, stack=p1)
                    posf = sb("posf", [128, 512], F32, stack=p1)
                    frac = sb("frac", [128, 512], F32, stack=p1)
                    fint = sb("fint", [128, 512], I32, stack=p1)
                    ftmp = sb("ftmp", [128, 512], F32, stack=p1)
                    sinT = sb("sinT", [128, 512], F32, stack=p1)
                    cosT = sb("cosT", [128, 512], F32, stack=p1)
                    rf = sb("rf", [128, 512], F32, stack=p1)
                    r1 = sb("r1", [128, 512], F32, stack=p1)
                    r2 = sb("r2", [128, 512], F32, stack=p1)
                    rstd = sb("mrstd", [128, 512], F32, stack=p1)
                    rot2 = cst[:, 5, :]

                    def rope(src_b, src_ap, dst_b, dst_ap, scale):
                        k.copy('act', rf[:], src_ap, R=[src_b], W=[rf])
                        ps = psum.get()
                        k.mm(ps, ps[:, :], rot2, rf[:], R=[cst, rf])
                        k.stt('dve', r1[:], rf[:], scale, cosT[:], ALU.mult, ALU.mult, R=[rf, cosT], W=[r1])
                        k.stt('dve', r2[:], ps[:, :], scale, sinT[:], ALU.mult, ALU.mult, R=[ps, sinT], W=[r2])
                        k.tt('dve', dst_ap, r1[:], r2[:], ALU.add, R=[r1, r2], W=[dst_b])

                    for tt in range(NT):
                        ts_ = slice(tt * 512, (tt + 1) * 512)
                        ci = cin.get()
                        k.dma('sp', ci[:, 0:6, :], proj[C_CQ:C_CQ + 768, ts_].rearrange('(kc p) t -> p kc t', p=128),
                              R=[Dproj], W=[ci])
                        k.dma('sp', ci[0:64, 6, :], proj[C_KR:C_KR + 64, ts_], R=[Dproj], W=[ci])
                        k.dma('sp', ci[64:128, 6, :], proj[C_KR:C_KR + 64, ts_], R=[Dproj], W=[ci])
                        k.dma('sp', posi[:], pos_in[0:1, ts_].partition_broadcast(128), R=[Dw], W=[posi])
                        k.copy('dve', posf[:], posi[:], R=[posi], W=[posf])
                        for (off, dst) in ((0.5, sinT), (0.75, cosT)):
                            k.ts('dve', frac[:], posf[:], invf[:, 0:1], 1.0 / (2 * np.pi), ALU.mult, ALU.mult,
                                 R=[posf, invf], W=[frac])
                            k.ts('dve', frac[:], frac[:], off, None, ALU.add, R=[frac], W=[frac])
                            k.copy('dve', fint[:], frac[:], R=[frac], W=[fint])
                            k.copy('dve', ftmp[:], fint[:], R=[fint], W=[ftmp])
                            k.tt('dve', frac[:], frac[:], ftmp[:], ALU.subtract, R=[frac, ftmp], W=[frac])
                            k.ts('dve', ftmp[:], frac[:], 0.0, None, ALU.is_lt, R=[frac], W=[ftmp])
                            k.tt('dve', frac[:], frac[:], ftmp[:], ALU.add, R=[frac, ftmp], W=[frac])
                            k.act(dst[:], frac[:], AF.Sin, R=[frac, cols], W=[dst], bias=cols[:, 1:2],
                                  scale=2 * np.pi)
                        for (c0, nk_, wv, dstb) in ((0, 4, qnw, cqn), (4, 2, kvnw, ckvn)):
                            k.act(sqm[:, 0:nk_, :], ci[:, c0:c0 + nk_, :], AF.Square, R=[ci], W=[sqm])
                            ps = psum.get()
                            for kc in range(nk_):
                                k.mm(ps, ps[:, :], ones_f, sqm[:, kc, :], R=[cst, sqm], start=(kc == 0),
                                     stop=(kc == nk_ - 1))
                            rsqrt(rstd, rstd[:], ps, ps[:, :], 1.0 / (nk_ * 128))
                            for kc in range(nk_):
                                k.stt('dve', dstb[:, kc, :], ci[:, c0 + kc, :], wv[:, kc:kc + 1], rstd[:], ALU.mult,
                                      ALU.mult, R=[ci, wv, rstd], W=[dstb])
                        for h in range(HM):
                            ps = psum.get()
                            for kc in range(4):
                                k.mm(ps, ps[:, :], wuq[:, kc, h * 192:h * 192 + 128], cqn[:, kc, :], R=[wuq, cqn],
                                     start=(kc == 0), stop=(kc == 3))
                            k.act(qn[:, h, ts_], ps[:, :], AF.Copy, R=[ps], W=[qn], scale=ATT_SCALE)
                            ps = psum.get()
                            for kc in range(2):
                                k.mm(ps, ps[:, :], wuk[:, kc, h * 128:(h + 1) * 128], ckvn[:, kc, :], R=[wuk, ckvn],
                                     start=(kc == 0), stop=(kc == 1))
                            k.copy('dve', kn[:, h, ts_], ps[:, :], R=[ps], W=[kn])
                        for hp in range(HM // 2):
                            ps = psum.get()
                            for kc in range(4):
                                k.mm(ps, ps[:, :], wuqr[:, kc, hp, :], cqn[:, kc, :], R=[wuqr, cqn],
                                     start=(kc == 0), stop=(kc == 3))
                            rope(ps, ps[:, :], qr, qr[:, hp, ts_], ATT_SCALE)
                        rope(ci, ci[:, 6, :], krp, krp[:, ts_], 1.0)
                        for q in range(4):
                            ps = psum.get()
                            for kc in range(2):
                                k.mm(ps, ps[:, :], ckvn[:, kc, q * 128:(q + 1) * 128], wuv[:, kc, :], R=[ckvn, wuv],
                                     start=(kc == 0), stop=(kc == 1))
                            k.copy('act', vtm[:, tt * 4 + q, :], ps[:, :], R=[ps], W=[vtm])
                dump("qn", qn); dump("qr", qr); dump("kn", kn); dump("krp", krp); dump("vtm", vtm)
                with ExitStack() as p2:
                    WM = 2
                    slots = []
                    for i in range(WM):
                        slots.append(dict(
                            Ssb=sb("Ssb%d" % i, [128, T], F32, stack=p2), Psb=sb("Psb%d" % i, [128, T], BF16, stack=p2),
                            ptr=Ring([sb("pt%d_%d" % (i, j), [128, 4, 128], BF16, stack=p2) for j in range(2)]),
                            sc=sb("asc%d" % i, [128, 4], F32, stack=p2), dg=sb("adg%d" % i, [128, 128], BF16, stack=p2),
                            po=psacc.bufs[i]))
                    ost = [sb("aost%d" % i, [128, 4, 128], BF16, multi=True, stack=p2) for i in range(2)]
                    done = {}

                    def att(key):
                        qi, h = key
                        B = slots[(qi * 4 + h) % WM]
                        Ssb, Psb, ptr, s_, dg, po = B['Ssb'], B['Psb'], B['ptr'], B['sc'], B['dg'], B['po']
                        ot = ost[qi % 2]
                        nk = (qi + 1) * 128
                        qs = slice(qi * 128, (qi + 1) * 128)
                        hp, ho = h // 2, (h % 2) * 64
                        for kb in range(0, nk, 512):
                            w = min(512, nk - kb)
                            ps = psum.get()
                            k.mm(ps, ps[:, :w], qn[:, h, qs], kn[:, h, kb:kb + w], R=[qn, kn], start=True,
                                 stop=False)
                            k.mm(ps, ps[:, :w], qr[ho:ho + 64, hp, qs], krp[ho:ho + 64, kb:kb + w], R=[qr, krp],
                                 start=False, stop=True)
                            if kb + w == nk:
                                if w > 128:
                                    k.copy('act', Ssb[:, kb:nk - 128], ps[:, :w - 128], R=[ps], W=[Ssb])
                                k.tt('dve', Ssb[:, nk - 128:nk], ps[:, w - 128:w], cst[:, 3, :], ALU.subtract,
                                     R=[ps, cst], W=[Ssb])
                            else:
                                k.copy('act', Ssb[:, kb:kb + w], ps[:, :w], R=[ps], W=[Ssb])
                            yield
                        k.op('dve', lambda e: e.reduce_max(s_[:, 0:1], Ssb[:, :nk], AX.X), R=[Ssb], W=[s_])
                        k.ts('dve', s_[:, 1:2], s_[:, 0:1], -1.0, None, ALU.mult, R=[s_], W=[s_])
                        yield
                        k.act(Psb[:, :nk], Ssb[:, :nk], AF.Exp, R=[Ssb, s_], W=[Psb, s_], bias=s_[:, 1:2],
                              scale=1.0, accum_out=s_[:, 2:3])
                        yield
                        k.op('dve', lambda e: e.reciprocal(s_[:, 3:4], s_[:, 2:3]), R=[s_], W=[s_])
                        k.ts('dve', dg[:], ident_b, s_[:, 3:4], None, ALU.mult, R=[cst_b, s_], W=[dg])
                        yield
                        nb = nk // 128
                        for b4 in range(0, nb, 4):
                            n4 = min(4, nb - b4)
                            ps = psum.get()
                            for j in range(n4):
                                kb2 = (b4 + j) * 128
                                k.mm(ps, ps[:, j * 128:(j + 1) * 128], Psb[:, kb2:kb2 + 128], dg[:], R=[Psb, dg])
                            pt = ptr.get()
                            k.copy('dve' if (b4 // 4) % 2 else 'act', pt[:, 0:n4, :],
                                   ps[:, 0:n4 * 128].rearrange('p (j t) -> p j t', j=n4), R=[ps], W=[pt])
                            for j in range(n4):
                                kblk = b4 + j
                                k.mm(po, po[:, 0:128], vtm[:, kblk, h * 128:(h + 1) * 128], pt[:, j, :],
                                     R=[vtm, pt], start=(kblk == 0), stop=(kblk == nb - 1))
                            yield
                        k.copy('act', ot[:, h, :], po[:, 0:128], R=[po], W=[ot])

                    def att_done(key):
                        qi, h = key
                        done[qi] = done.get(qi, 0) + 1
                        if done[qi] == HM:
                            qs = slice(qi * 128, (qi + 1) * 128)
                            k.dma('sp', (ycm if pair else y_d[2])[:, qs].rearrange('(h p) t -> p h t', p=128),
                                  ost[qi % 2][:, 0:HM, :], R=[ost[qi % 2]], W=[Dycm if pair else Dy[2]])

                    run_pipeline([(qi, h) for qi in range(NQ) for h in range(HM)], WM, att, att_done)
                if pair:
                    k.barrier()
                    allgather([(ycm, y_d[2])], Dycm, Dy[2])
                k.barrier()


        def bc(ap, shape):
            return ap.to_broadcast(list(shape))

        def run_pipeline(items, W, start_fn, finish_fn):
            active = []
            it = iter(items)
            pending = True
            while True:
                while len(active) < W and pending:
                    try:
                        key = next(it)
                    except StopIteration:
                        pending = False
                        break
                    active.append((key, start_fn(key)))
                if not active:
                    break
                for ent in list(active):
                    try:
                        next(ent[1])
                    except StopIteration:
                        active.remove(ent)
                        finish_fn(ent[0])

        def conv_silu(cin, cw, nchan, dst, dstb, tmpr, bias=None):
            for c in (range(nchan) if isinstance(nchan, int) else nchan):
                tb = tmpr.get()
                k.ts('dve', tb[:], cin[:, c, 0:512], cw[:, c, 0:1], None, ALU.mult, R=[cin, cw], W=[tb])
                for kk in range(1, 4):
                    k.stt('dve', tb[:], cin[:, c, kk:kk + 512], cw[:, c, kk:kk + 1], tb[:], ALU.mult,
                          ALU.add, R=[cin, cw, tb], W=[tb])
                if bias is None:
                    k.act(dst[:, c, :], tb[:], AF.Silu, R=[tb], W=[dstb])
                else:
                    k.act(dst[:, c, :], tb[:], AF.Silu, R=[tb, bias], W=[dstb], bias=bias[:, c:c + 1])

        def load_halo(cin, row0, nrows, tt):
            if tt == 0:
                k.op('dve', lambda e: e.memset(cin[:, :, 0:3], 0.0), W=[cin])
                k.dma('sp', cin[:, :, 3:515], proj[row0:row0 + nrows, 0:512].rearrange('(c p) t -> p c t', p=128),
                      R=[Dproj], W=[cin])
            else:
                k.dma('sp', cin[:, :, :],
                      proj[row0:row0 + nrows, tt * 512 - 3:tt * 512 + 512].rearrange('(c p) t -> p c t', p=128),
                      R=[Dproj], W=[cin])

        tri64 = cst[0:64, 2, 0:64]
        ones64 = cst[0:64, 1, 0:64]
        ones64w = cst[0:64, 1, :]
        posm64 = cst[0:64, 3, 0:64]
        strict64 = cst[0:64, 4, 0:64]
        lowm64 = cst[0:64, 6, 0:64]
        id64 = cst[0:64, 0, 0:64]

        def phase_ssd(l):
            with ExitStack() as ps_:
                def t_(name, shape, dt=F32, multi=False):
                    return sb(name, shape, dt, multi=multi, stack=ps_)
                cw = t_("scw", [128, 8, 4]); cb = t_("scb", [128, 8]); alog = t_("salog", [128, 8])
                dtb = t_("sdtb", [128, 8]); dexp = t_("sdexp", [128, 4]); nw = t_("snw", [128, 4])
                for (d_, s_) in ((cw, ssm_convT), (cb, ssm_convb), (alog, ssm_alog), (dtb, ssm_dtb), (dexp, ssm_dexp),
                                 (nw, ssm_nw)):
                    k.dma('sp', d_[:], s_[l], R=[Dw], W=[d_])
                k.act(alog[:], alog[:], AF.Exp, R=[alog], W=[alog])
                k.ts('dve', alog[:], alog[:], -1.0, None, ALU.mult, R=[alog], W=[alog])
                raw = t_("sraw", [64, NCH, 16])
                k.dma('sp', raw[:], abdt.rearrange('(c p) k -> p c k', p=64), R=[Dabdt], W=[raw])
                dt = t_("sdt", [64, NCH, HS]); ad = t_("sad", [64, NCH, HS]); acs = t_("sacs", [64, NCH, HS])
                acl = t_("sacl", [128, NCH, HS]); cd = t_("scd", [128, NCH, HS]); ds = t_("sds", [64, NCH, HS])
                eacs = t_("seacs", [64, NCH, HS])
                k.tt('dve', dt[:], raw[:, :, 8:8 + HS], bc(dtb[0:64, 0:HS].unsqueeze(1), [64, NCH, HS]), ALU.add,
                     R=[raw, dtb], W=[dt])
                k.act(dt[:], dt[:], AF.Exp, R=[dt], W=[dt])
                k.act(dt[:], dt[:], AF.Ln, R=[dt, cols], W=[dt], bias=cols[0:64, 0:1])
                k.tt('dve', ad[:], dt[:], bc(alog[0:64, 0:HS].unsqueeze(1), [64, NCH, HS]), ALU.mult, R=[dt, alog], W=[ad])
                adf = ad[:].rearrange('p c h -> p (c h)')
                ps = psum.get()
                k.mm(ps, ps[:64, :NCH * HS], tri64, adf, R=[cst, ad])
                k.copy('dve', acs[:].rearrange('p c h -> p (c h)'), ps[:64, :NCH * HS], R=[ps], W=[acs])
                ps = psum.get()
                k.mm(ps, ps[:, :NCH * HS], ones64w, adf, R=[cst, ad])
                k.copy('dve', acl[:].rearrange('p c h -> p (c h)'), ps[:, :NCH * HS], R=[ps], W=[acl])
                k.act(cd[:], acl[:], AF.Exp, R=[acl], W=[cd])
                k.tt('dve', ds[:], acl[0:64], acs[:], ALU.subtract, R=[acl, acs], W=[ds])
                k.act(ds[:], ds[:], AF.Exp, R=[ds], W=[ds])
                k.act(eacs[:], acs[:], AF.Exp, R=[acs], W=[eacs])
                state = t_("sstate", [128, HS, 64])
                k.op('dve', lambda e: e.memset(state[:], 0.0), W=[state])
                cinr = Ring([t_("scin%d" % i, [128, 8, 515], BF16) for i in range(2)])
                szr = Ring([t_("ssz%d" % i, [128, XC, 512], BF16) for i in range(2)])
                xfr = Ring([t_("sxf%d" % i, [128, 8, 512]) for i in range(2)])
                ctr = Ring([t_("sct%d" % i, [128, 512]) for i in range(2)])
                ystr = Ring([t_("syst%d" % i, [128, XC, 512], BF16, multi=True) for i in range(2)])
                WS = 4
                slots = []
                for i in range(WS):
                    slots.append(dict(
                        trig=t_("strig%d" % i, [64, HS, 64]), t1=t_("st1%d" % i, [64, HS, 64]), MT=t_("sMT%d" % i, [64, HS, 64]),
                        X=t_("sX%d" % i, [64, HS, 64]), Xds=t_("sXds%d" % i, [64, HS, 64]), Btm=t_("sBtm%d" % i, [64, GS, 128]),
                        yt=t_("syt%d" % i, [64, HS, 64]), ytm=t_("sytm%d" % i, [64, HS, 64]), yfm=t_("syfm%d" % i, [128, XC, 64]),
                        tmp=t_("stmp%d" % i, [128, XC, 64]), sq=t_("ssq%d" % i, [128, XC, 64]), rs=t_("srs%d" % i, [128, GS, 64])))
                tiles = {}
                turn = [0]
                done = {}

                def prep(tt):
                    cin = cinr.get(); szt = szr.get(); yst = ystr.get(); xf = xfr.get()
                    load_halo(cin, C_XBC, 1024, tt)
                    k.dma('sp', szt[:], proj[C_SZ:C_SZ + XC * 128, tt * 512:(tt + 1) * 512].rearrange(
                        '(c p) t -> p c t', p=128), R=[Dproj], W=[szt])
                    conv_silu(cin, cw, list(range(XC)) + [4 + g_ for g_ in range(GS)] + [6 + g_ for g_ in range(GS)], xf, xf, ctr, bias=cb)
                    tiles[tt] = (xf, szt, yst)

                def chunk(ch):
                    tt, cc = ch // 8, ch % 8
                    if cc == 0:
                        prep(tt)
                    xf, szt, yst = tiles[tt]
                    B = slots[ch % WS]
                    trig, t1, MT, X, Xds, Btm = B['trig'], B['t1'], B['MT'], B['X'], B['Xds'], B['Btm']
                    yt, ytm, yfm, tmp, sq, rs = B['yt'], B['ytm'], B['yfm'], B['tmp'], B['sq'], B['rs']
                    cs = slice(cc * 64, cc * 64 + 64)
                    ps_cb = psum.get()
                    for g in range(GS):
                        k.mm(ps_cb, ps_cb[:64, g * 64:(g + 1) * 64], xf[:, 4 + g, cs], xf[:, 6 + g, cs], R=[xf])
                    k.tt('pool', trig[:], bc(tri64.unsqueeze(1), [64, HS, 64]),
                         bc(ad[:, ch, :].unsqueeze(2), [64, HS, 64]), ALU.mult, R=[cst, ad], W=[trig])
                    ps_r = psum.get()
                    k.mm(ps_r, ps_r[:64, 0:HS * 64], ones64, trig[:].rearrange('p h l -> p (h l)'), R=[cst, trig])
                    k.tt('dve', t1[:], ps_r[:64, 0:HS * 64].rearrange('p (h l) -> p h l', h=HS),
                         bc(lowm64.unsqueeze(1), [64, HS, 64]), ALU.subtract, R=[ps_r, cst], W=[t1])
                    k.tt('dve', t1[:], t1[:], bc(acs[:, ch, :].unsqueeze(2), [64, HS, 64]), ALU.subtract,
                         R=[t1, acs], W=[t1])
                    k.act(t1[:], t1[:], AF.Exp, R=[t1], W=[t1])
                    k.tt('dve', MT[:].rearrange('p (g e) l -> p g e l', g=GS),
                         t1[:].rearrange('p (g e) l -> p g e l', g=GS),
                         bc(ps_cb[:64, 0:GS * 64].rearrange('p (g l) -> p g l', g=GS).unsqueeze(2), [64, GS, 4, 64]),
                         ALU.mult, R=[t1, ps_cb], W=[MT])
                    yield
                    ps_x = psum.get()
                    for kc in range(XC):
                        k.tr(ps_x, ps_x[:64, kc * 128:(kc + 1) * 128], xf[:, kc, cs], ident_f, R=[xf, cst])
                    k.tt('dve', X[:], ps_x[:64, 0:HS * 64].rearrange('p (h q) -> p h q', h=HS),
                         bc(dt[:, ch, :].unsqueeze(2), [64, HS, 64]), ALU.mult, R=[ps_x, dt], W=[X])
                    k.tt('pool', Xds[:], X[:], bc(ds[:, ch, :].unsqueeze(2), [64, HS, 64]), ALU.mult,
                         R=[X, ds], W=[Xds])
                    yield
                    ps_b = psum.get()
                    for g in range(GS):
                        k.tr(ps_b, ps_b[:64, g * 128:(g + 1) * 128], xf[:, 4 + g, cs], ident_f, R=[xf, cst])
                    k.copy('act', Btm[:].rearrange('p g n -> p (g n)'), ps_b[:64, 0:GS * 128], R=[ps_b], W=[Btm])
                    yield
                    while turn[0] != ch:
                        yield
                    ps_y1 = psum.get()
                    for h in range(HS):
                        k.mm(ps_y1, ps_y1[:64, h * 64:(h + 1) * 64], MT[:, h, :], X[:, h, :], R=[MT, X])
                    ps_y2 = psum.get()
                    for h in range(HS):
                        k.mm(ps_y2, ps_y2[:64, h * 64:(h + 1) * 64], xf[:, 6 + h // 4, cs], state[:, h, :],
                             R=[xf, state])
                    k.tt('dve', yt[:], ps_y2[:64, 0:HS * 64].rearrange('p (h q) -> p h q', h=HS),
                         bc(eacs[:, ch, :].unsqueeze(2), [64, HS, 64]), ALU.mult, R=[ps_y2, eacs], W=[yt])
                    k.tt('dve', ytm[:], yt[:], ps_y1[:64, 0:HS * 64].rearrange('p (h q) -> p h q', h=HS), ALU.add,
                         R=[yt, ps_y1], W=[ytm])
                    ps_s = psum.get()
                    for h in range(HS):
                        k.mm(ps_s, ps_s[:, h * 64:(h + 1) * 64], Btm[:, h // 4, :], Xds[:, h, :], R=[Btm, Xds])
                    k.tt('dve', state[:], state[:], bc(cd[:, ch, :].unsqueeze(2), [128, HS, 64]), ALU.mult,
                         R=[state, cd], W=[state])
                    k.tt('dve', state[:], state[:], ps_s[:, 0:HS * 64].rearrange('p (h q) -> p h q', h=HS), ALU.add,
                         R=[state, ps_s], W=[state])
                    turn[0] = ch + 1
                    yield
                    ps_t = psum.get()
                    ytf = ytm[:].rearrange('p h q -> p (h q)')
                    for kc in range(XC):
                        k.tr(ps_t, ps_t[:, kc * 64:(kc + 1) * 64], ytf[:, kc * 128:(kc + 1) * 128], id64,
                             R=[ytm, cst])
                    k.tt('pool', tmp[:], xf[:, 0:XC, cs], bc(dexp[:, 0:XC].unsqueeze(2), [128, XC, 64]), ALU.mult,
                         R=[xf, dexp], W=[tmp])
                    k.tt('dve', yfm[:], tmp[:], ps_t[:, 0:XC * 64].rearrange('p (c q) -> p c q', c=XC), ALU.add,
                         R=[tmp, ps_t], W=[yfm])
                    k.tt('dve', yfm[:], yfm[:], szt[:, :, cs], ALU.mult, R=[yfm, szt], W=[yfm])
                    k.act(sq[:], yfm[:], AF.Square, R=[yfm], W=[sq])
                    yield
                    ps_n = psum.get()
                    for g in range(GS):
                        for k2 in range(2):
                            k.mm(ps_n, ps_n[:, g * 64:(g + 1) * 64], ones_f, sq[:, g * 2 + k2, :], R=[cst, sq],
                                 start=(k2 == 0), stop=(k2 == 1))
                    rsqrt(rs, rs[:].rearrange('p g q -> p (g q)'), ps_n, ps_n[:, 0:GS * 64], 1.0 / 256)
                    for kc in range(XC):
                        k.stt('dve', yst[:, kc, cs], yfm[:, kc, :], nw[:, kc:kc + 1], rs[:, kc // 2, :], ALU.mult,
                              ALU.mult, R=[yfm, nw, rs], W=[yst])

                def chunk_done(ch):
                    tt = ch // 8
                    done[tt] = done.get(tt, 0) + 1
                    if done[tt] == 8:
                        yst = tiles[tt][2]
                        k.dma('sp', (ybm if pair else y_d[1])[:, tt * 512:(tt + 1) * 512].rearrange(
                            '(c p) t -> p c t', p=128), yst[:], R=[yst], W=[Dybm if pair else Dy[1]])

                run_pipeline(list(range(NCH)), WS, chunk, chunk_done)
                k.barrier()
                if pair:
                    allgather([(ybm, y_d[1])], Dybm, Dy[1])

        def phase_gdn(l):
            with ExitStack() as ps_:
                def t_(name, shape, dt=F32, multi=False):
                    return sb(name, shape, dt, multi=multi, stack=ps_)
                cw = t_("gcw", [128, 12, 4]); alog = t_("galog", [128, 4]); dtb = t_("gdtb", [128, 4])
                nw = t_("gnw", [128, 1])
                for (d_, s_) in ((cw, gdn_convT), (alog, gdn_alog), (dtb, gdn_dtb), (nw, gdn_nw)):
                    k.dma('sp', d_[:], s_[l], R=[Dw], W=[d_])
                k.act(alog[:], alog[:], AF.Exp, R=[alog], W=[alog])
                k.ts('dve', alog[:], alog[:], -1.0, None, ALU.mult, R=[alog], W=[alog])
                raw = t_("graw", [64, NCH, 16])
                k.dma('sp', raw[:], abdt.rearrange('(c p) k -> p c k', p=64), R=[Dabdt], W=[raw])
                beta = t_("gbeta", [64, NCH, HG]); nbeta = t_("gnbeta", [64, NCH, HG]); g = t_("gg", [64, NCH, HG])
                G = t_("gG", [64, NCH, HG]); Glb = t_("gGlb", [128, NCH, HG]); gl = t_("ggl", [128, NCH, HG])
                eG = t_("geG", [64, NCH, HG]); kdsc = t_("gkdsc", [64, NCH, HG]); bexpG = t_("gbexpG", [64, NCH, HG])
                k.act(beta[:], raw[:, :, 4:4 + HG], AF.Sigmoid, R=[raw], W=[beta])
                k.ts('dve', nbeta[:], beta[:], -1.0, None, ALU.mult, R=[beta], W=[nbeta])
                k.tt('dve', g[:], raw[:, :, 0:HG], bc(dtb[0:64, 0:HG].unsqueeze(1), [64, NCH, HG]), ALU.add,
                     R=[raw, dtb], W=[g])
                k.act(g[:], g[:], AF.Exp, R=[g], W=[g])
                k.act(g[:], g[:], AF.Ln, R=[g, cols], W=[g], bias=cols[0:64, 0:1])
                k.tt('dve', g[:], g[:], bc(alog[0:64, 0:HG].unsqueeze(1), [64, NCH, HG]), ALU.mult, R=[g, alog], W=[g])
                gf = g[:].rearrange('p c h -> p (c h)')
                ps = psum.get()
                k.mm(ps, ps[:64, :NCH * HG], tri64, gf, R=[cst, g])
                k.copy('dve', G[:].rearrange('p c h -> p (c h)'), ps[:64, :NCH * HG], R=[ps], W=[G])
                ps = psum.get()
                k.mm(ps, ps[:, :NCH * HG], ones64w, gf, R=[cst, g])
                k.copy('dve', Glb[:].rearrange('p c h -> p (c h)'), ps[:, :NCH * HG], R=[ps], W=[Glb])
                k.act(gl[:], Glb[:], AF.Exp, R=[Glb], W=[gl])
                k.act(eG[:], G[:], AF.Exp, R=[G], W=[eG])
                k.tt('dve', kdsc[:], Glb[0:64], G[:], ALU.subtract, R=[Glb, G], W=[kdsc])
                k.act(kdsc[:], kdsc[:], AF.Exp, R=[kdsc], W=[kdsc])
                k.tt('dve', bexpG[:], beta[:], eG[:], ALU.mult, R=[beta, eG], W=[bexpG])
                S = t_("gS", [128, HG, 128])
                k.op('dve', lambda e: e.memset(S[:], 0.0), W=[S])
                cinr = Ring([t_("gcin%d" % i, [128, 12, 515], BF16) for i in range(2)])
                gzr = Ring([t_("ggz%d" % i, [128, HG, 512], BF16) for i in range(2)])
                qkvr = Ring([t_("gqkv%d" % i, [128, 12, 512]) for i in range(2)])
                ctr = Ring([t_("gct%d" % i, [128, 512]) for i in range(2)])
                rsn = t_("grsn", [128, 512])
                ystr = Ring([t_("gyst%d" % i, [128, HG, 512], BF16, multi=True) for i in range(2)])
                WG = 4
                slots = []
                for i in range(WG):
                    d_ = {}
                    for n in ('trig', 't1', 'n1', 'qkd', 'qkT', 'Xa', 'Xb', 'Ya', 'Yb', 'Ra', 'Rb'):
                        d_[n] = t_("g%s%d" % (n, i), [64, HG, 64])
                    for n in ('ktm', 'kbg', 'kdec', 'vtm', 'u', 'vn', 'o', 'sq'):
                        d_[n] = t_("g%s%d" % (n, i), [64, HG, 128])
                    d_['wf'] = t_("gwf%d" % i, [128, HG, 64])
                    d_['ss'] = t_("gss%d" % i, [64, HG])
                    slots.append(d_)
                tiles = {}
                turn = [0]
                done = {}

                def v4(ps, w):
                    return ps[:64, 0:HG * w].rearrange('p (h x) -> p h x', h=HG)

                def prep(tt):
                    cin = cinr.get(); gz = gzr.get(); yst = ystr.get(); qkv = qkvr.get()
                    load_halo(cin, C_QKV, 1536, tt)
                    k.dma('sp', gz[:], proj[C_GZ:C_GZ + HG * 128, tt * 512:(tt + 1) * 512].rearrange(
                        '(c p) t -> p c t', p=128), R=[Dproj], W=[gz])
                    conv_silu(cin, cw, [c_ + h_ for c_ in (0, 4, 8) for h_ in range(HG)], qkv, qkv, ctr)
                    for c in [c_ + h_ for c_ in (0, 4) for h_ in range(HG)]:
                        tb = ctr.get()
                        k.act(tb[:], qkv[:, c, :], AF.Square, R=[qkv], W=[tb])
                        ps = psum.get()
                        k.mm(ps, ps[:, :], ones_f, tb[:], R=[cst, tb])
                        rsqrt(rsn, rsn[:], ps, ps[:, :], 1.0)
                        if c < 4:
                            k.stt('dve', qkv[:, c, :], qkv[:, c, :], 128.0 ** -0.5, rsn[:], ALU.mult, ALU.mult,
                                  R=[qkv, rsn], W=[qkv])
                        else:
                            k.tt('dve', qkv[:, c, :], qkv[:, c, :], rsn[:], ALU.mult, R=[qkv, rsn], W=[qkv])
                    tiles[tt] = (qkv, gz, yst)

                def chunk(ch):
                    tt, cc = ch // 8, ch % 8
                    if cc == 0:
                        prep(tt)
                    qkv, gz, yst = tiles[tt]
                    B = slots[ch % WG]
                    cs = slice(cc * 64, cc * 64 + 64)
                    b4 = lambda t, w: bc(t[:, ch, :].unsqueeze(2), [t[:, ch, :].shape[0], HG, w])
                    ktm, vtm, trig, t1, n1, qkd, qkT = B['ktm'], B['vtm'], B['trig'], B['t1'], B['n1'], B['qkd'], B['qkT']
                    ps_k = psum.get()
                    for h in range(HG):
                        k.tr(ps_k, ps_k[:64, h * 128:(h + 1) * 128], qkv[:, 4 + h, cs], ident_f, R=[qkv, cst])
                    k.copy('act', ktm[:], v4(ps_k, 128), R=[ps_k], W=[ktm])
                    yield
                    ps_v = psum.get()
                    for h in range(HG):
                        k.tr(ps_v, ps_v[:64, h * 128:(h + 1) * 128], qkv[:, 8 + h, cs], ident_f, R=[qkv, cst])
                    k.copy('act', vtm[:], v4(ps_v, 128), R=[ps_v], W=[vtm])
                    yield
                    ps_kk = psum.get()
                    for h in range(HG):
                        k.mm(ps_kk, ps_kk[:64, h * 64:(h + 1) * 64], qkv[:, 4 + h, cs], qkv[:, 4 + h, cs], R=[qkv])
                    ps_qk = psum.get()
                    for h in range(HG):
                        k.mm(ps_qk, ps_qk[:64, h * 64:(h + 1) * 64], qkv[:, h, cs], qkv[:, 4 + h, cs], R=[qkv])
                    k.tt('pool', trig[:], bc(tri64.unsqueeze(1), [64, HG, 64]), b4(g, 64), ALU.mult,
                         R=[cst, g], W=[trig])
                    ps_g = psum.get()
                    k.mm(ps_g, ps_g[:64, 0:HG * 64], ones64, trig[:].rearrange('p h l -> p (h l)'), R=[cst, trig])
                    k.tt('dve', t1[:], v4(ps_g, 64), bc(posm64.unsqueeze(1), [64, HG, 64]), ALU.add,
                         R=[ps_g, cst], W=[t1])
                    k.tt('dve', t1[:], b4(G, 64), t1[:], ALU.subtract, R=[G, t1], W=[t1])
                    k.act(t1[:], t1[:], AF.Exp, R=[t1], W=[t1])
                    X0 = B['Xa']
                    k.tt('dve', n1[:], v4(ps_kk, 64), t1[:], ALU.mult, R=[ps_kk, t1], W=[n1])
                    k.tt('dve', n1[:], n1[:], b4(nbeta, 64), ALU.mult, R=[n1, nbeta], W=[n1])
                    k.tt('pool', X0[:], n1[:], bc(strict64.unsqueeze(1), [64, HG, 64]), ALU.mult,
                         R=[n1, cst], W=[X0])
                    k.tt('dve', qkd[:], v4(ps_qk, 64), t1[:], ALU.mult, R=[ps_qk, t1], W=[qkd])
                    yield
                    ps_y = psum.get()
                    for h in range(HG):
                        k.tr(ps_y, ps_y[:64, h * 64:(h + 1) * 64], X0[:, h, :], id64, R=[X0, cst])
                    Y0 = B['Ya']
                    k.copy('act', Y0[:], v4(ps_y, 64), R=[ps_y], W=[Y0])
                    yield
                    ps_q = psum.get()
                    for h in range(HG):
                        k.tr(ps_q, ps_q[:64, h * 64:(h + 1) * 64], qkd[:, h, :], id64, R=[qkd, cst])
                    k.copy('act', qkT[:], v4(ps_q, 64), R=[ps_q], W=[qkT])
                    RT = B['Ra']
                    k.tt('pool', RT[:], Y0[:], bc(id64.unsqueeze(1), [64, HG, 64]), ALU.add, R=[Y0, cst], W=[RT])
                    yield
                    Xp, Yp = X0, Y0
                    for kk in range(1, 6):
                        Xn = B['Xb'] if Xp is B['Xa'] else B['Xa']
                        Yn = B['Yb'] if Yp is B['Ya'] else B['Ya']
                        RTn = B['Rb'] if RT is B['Ra'] else B['Ra']
                        ps_a = psum.get()
                        for h in range(HG):
                            k.mm(ps_a, ps_a[:64, h * 64:(h + 1) * 64], Yp[:, h, :], Xp[:, h, :], R=[Yp, Xp])
                        if kk <= 4:
                            ps_b = psum.get()
                            for h in range(HG):
                                k.mm(ps_b, ps_b[:64, h * 64:(h + 1) * 64], Xp[:, h, :], Yp[:, h, :], R=[Yp, Xp])
                        k.copy('act', Xn[:], v4(ps_a, 64), R=[ps_a], W=[Xn])
                        if kk <= 4:
                            k.copy('dve', Yn[:], v4(ps_b, 64), R=[ps_b], W=[Yn])
                        yield
                        ps_c = psum.get()
                        for h in range(HG):
                            k.mm(ps_c, ps_c[:64, h * 64:(h + 1) * 64], Xn[:, h, :], RT[:, h, :], R=[Xn, RT])
                        k.tt('dve', RTn[:], RT[:], v4(ps_c, 64), ALU.add, R=[RT, ps_c], W=[RTn])
                        Xp, Yp, RT = Xn, Yn, RTn
                        yield
                    kbg, kdec, u, wf, vn, o, sq, ss = B['kbg'], B['kdec'], B['u'], B['wf'], B['vn'], B['o'], B['sq'], B['ss']
                    k.tt('pool', vtm[:], vtm[:], b4(beta, 128), ALU.mult, R=[vtm, beta], W=[vtm])
                    k.tt('pool', kbg[:], ktm[:], b4(bexpG, 128), ALU.mult, R=[ktm, bexpG], W=[kbg])
                    k.tt('pool', kdec[:], ktm[:], b4(kdsc, 128), ALU.mult, R=[ktm, kdsc], W=[kdec])
                    ps_u = psum.get()
                    for h in range(HG):
                        k.mm(ps_u, ps_u[:64, h * 128:(h + 1) * 128], RT[:, h, :], vtm[:, h, :], R=[RT, vtm])
                    k.copy('act', u[:], v4(ps_u, 128), R=[ps_u], W=[u])
                    yield
                    ps_w = psum.get()
                    for h in range(HG):
                        k.mm(ps_w, ps_w[:, h * 64:(h + 1) * 64], kbg[:, h, :], RT[:, h, :], R=[kbg, RT])
                    k.copy('act', wf[:], ps_w[:, 0:HG * 64].rearrange('p (h x) -> p h x', h=HG), R=[ps_w], W=[wf])
                    yield
                    while turn[0] != ch:
                        yield
                    ps_ws = psum.get()
                    for h in range(HG):
                        k.mm(ps_ws, ps_ws[:64, h * 128:(h + 1) * 128], wf[:, h, :], S[:, h, :], R=[wf, S])
                    k.tt('dve', vn[:], u[:], v4(ps_ws, 128), ALU.subtract, R=[u, ps_ws], W=[vn])
                    ps_o1 = psum.get()
                    for h in range(HG):
                        k.mm(ps_o1, ps_o1[:64, h * 128:(h + 1) * 128], qkv[:, h, cs], S[:, h, :], R=[qkv, S])
                    ps_ds = psum.get()
                    for h in range(HG):
                        k.mm(ps_ds, ps_ds[:, h * 128:(h + 1) * 128], kdec[:, h, :], vn[:, h, :], R=[kdec, vn])
                    k.tt('dve', S[:], S[:], bc(gl[:, ch, :].unsqueeze(2), [128, HG, 128]), ALU.mult,
                         R=[S, gl], W=[S])
                    k.tt('dve', S[:], S[:], ps_ds[:, 0:HG * 128].rearrange('p (h x) -> p h x', h=HG), ALU.add,
                         R=[S, ps_ds], W=[S])
                    turn[0] = ch + 1
                    ps_o2 = psum.get()
                    for h in range(HG):
                        k.mm(ps_o2, ps_o2[:64, h * 128:(h + 1) * 128], qkT[:, h, :], vn[:, h, :], R=[qkT, vn])
                    k.tt('dve', o[:], v4(ps_o1, 128), b4(eG, 128), ALU.mult, R=[ps_o1, eG], W=[o])
                    k.tt('dve', o[:], o[:], v4(ps_o2, 128), ALU.add, R=[o, ps_o2], W=[o])
                    yield
                    k.tt('pool', sq[:], o[:], o[:], ALU.mult, R=[o], W=[sq])
                    k.op('dve', lambda e: e.reduce_sum(ss[:], sq[:], AX.X), R=[sq], W=[ss])
                    rsqrt(ss, ss[:], ss, ss[:], 1.0 / 128)
                    k.tt('dve', sq[:], o[:], bc(ss[:, :].unsqueeze(2), [64, HG, 128]), ALU.mult, R=[o, ss], W=[sq])
                    yield
                    ps_t = psum.get()
                    for h in range(HG):
                        k.tr(ps_t, ps_t[:, h * 64:(h + 1) * 64], sq[:, h, :], id64, R=[sq, cst])
                    k.stt('dve', yst[:, :, cs], ps_t[:, 0:HG * 64].rearrange('p (h x) -> p h x', h=HG), nw[:, 0:1],
                          gz[:, :, cs], ALU.mult, ALU.mult, R=[ps_t, nw, gz], W=[yst])

                def chunk_done(ch):
                    tt = ch // 8
                    done[tt] = done.get(tt, 0) + 1
                    if done[tt] == 8:
                        yst = tiles[tt][2]
                        k.dma('sp', (yam if pair else y_d[0])[:, tt * 512:(tt + 1) * 512].rearrange(
                            '(c p) t -> p c t', p=128), yst[:], R=[yst], W=[Dyam if pair else Dy[0]])

                run_pipeline(list(range(NCH)), WG, chunk, chunk_done)
                k.barrier()
                if pair:
                    allgather([(yam, y_d[0])], Dyam, Dy[0])

        def phase_mixers(l):
            if 'mla' in mixers:
                phase_mla(l)
            if 'ssd' in mixers:
                phase_ssd(l)
            if 'gdn' in mixers:
                phase_gdn(l)

        def phase_merge(l, xsrc, Dxsrc, xdst, Dxdst):
            with ExitStack() as ps_:
                wb = [sb("wbr%d" % i, [128, 4, 1024], BF16, stack=ps_) for i in range(3)]
                wo = sb("wo", [128, 8, 1024], BF16, stack=ps_)
                for i in range(3):
                    k.dma('pool', wb[i][:], w_br[i][l].rearrange('(kc p) n -> p kc n', p=128), R=[Dw], W=[wb[i]])
                k.dma('pool', wo[:], w_out[l].rearrange('(kc p) n -> p kc n', p=128), R=[Dw], W=[wo])
                yr = Ring([sb("ym%d" % i, [128, 3, 4, 512], BF16, stack=ps_) for i in range(2)])
                gr = Ring([sb("gm%d" % i, [128, 24, 512], BF16, stack=ps_) for i in range(2)])
                xr = Ring([sb("xm%d" % i, [128, 8, 512], F32, stack=ps_) for i in range(2)])
                mg = Ring([sb("mg%d" % i, [128, 8, 512], BF16, stack=ps_) for i in range(2)])
                tmp = Ring([sb("mt%d" % i, [128, 512], F32, stack=ps_) for i in range(3)])
                for tt in range(NT):
                    ts_ = slice(tt * 512, (tt + 1) * 512)
                    y = yr.get()
                    g = gr.get()
                    xt = xr.get()
                    m_ = mg.get()
                    for i in range(3):
                        k.dma('sp', y[:, i], y_d[i][:, ts_].rearrange('(kc p) t -> p kc t', p=128), R=[Dy[i]], W=[y])
                    k.dma('sp', g[:], proj[C_GATES:C_GATES + 3072, ts_].rearrange('(kc p) t -> p kc t', p=128),
                          R=[Dproj], W=[g])
                    for (a_, b_, sap) in xsrc(tt):
                        k.dma('sp', xt[:, a_:b_, :], sap, R=[Dxsrc], W=[xt])
                    for o in range(8):
                        tl = []
                        for i in range(3):
                            ps = psum.get()
                            for kc in range(4):
                                k.mm(ps, ps[:, :], wb[i][:, kc, o * 128:(o + 1) * 128], y[:, i, kc, :], R=[wb[i], y],
                                     start=(kc == 0), stop=(kc == 3))
                            t_ = tmp.get()
                            k.tt('dve', t_[:], ps[:, :], g[:, i * 8 + o, :], ALU.mult, R=[ps, g], W=[t_])
                            tl.append(t_)
                        k.tt('pool', tl[0][:], tl[0][:], tl[1][:], ALU.add, R=[tl[0], tl[1]], W=[tl[0]])
                        k.tt('pool', m_[:, o, :], tl[0][:], tl[2][:], ALU.add, R=[tl[0], tl[2]], W=[m_])
                    for o in range(8):
                        ps = psum.get()
                        for kc in range(8):
                            k.mm(ps, ps[:, :], wo[:, kc, o * 128:(o + 1) * 128], m_[:, kc, :], R=[wo, m_],
                                 start=(kc == 0), stop=(kc == 7))
                        k.stt('dve', xt[:, o, :], ps[:, :], modT[:, 16 + o:17 + o], xt[:, o, :], ALU.mult, ALU.add,
                              R=[ps, modT, xt], W=[xt])
                    k.dma('sp', xdst[:, ts_].rearrange('(kc p) t -> p kc t', p=128), xt[:], R=[xt], W=[Dxdst])
                k.barrier()

        def phase_ffn(l, xsrc, Dxsrc, xdst, Dxdst):
            moe = (l % 2 == 1)
            li = l // 2
            TT = min(TH, 1024)
            NS = TT // 512
            HCMAX = 14
            with ExitStack() as ps_:
                h2 = sb("h2", [128, 8, TT], BF16, multi=True, stack=ps_)
                xacc = sb("xacc", [128, 8, TT], F32, multi=True, stack=ps_)
                sq = sb("sq2", [128, 8, 512], F32, stack=ps_)
                hid = sb("hid", [128, HCMAX, TT], BF16, multi=True, stack=ps_)
                wgr = Ring([sb("wg%d" % i, [128, 8, 256], BF16, stack=ps_) for i in range(2)])
                wur = Ring([sb("wu%d" % i, [128, 8, 256], BF16, stack=ps_) for i in range(2)])
                wdr = Ring([sb("wd%d" % i, [128, HCMAX, 512], BF16, stack=ps_) for i in range(1)])
                sgr = Ring([sb("sg%d" % i, [128, 512], BF16, stack=ps_) for i in range(3)])
                tmp = Ring([sb("ft%d" % i, [128, 512], F32, stack=ps_) for i in range(2)])
                if moe:
                    hf = sb("hf2", [128, 8, 512], F32, stack=ps_)
                    rt = sb("rt", [128, 8, NEXP], F32, stack=ps_)
                    k.dma('sp', rt[:], moe_router[li].rearrange('(kc p) n -> p kc n', p=128), R=[Dw], W=[rt])
                    wrow = sb("wrow", [128, NEXP, TT], BF16, multi=True, stack=ps_)
                    sm = [sb("rs%d" % i, [128, 8], F32, stack=ps_) for i in range(6)]
                    sc = [sb("rc%d" % i, [128, 1], F32, stack=ps_) for i in range(4)]
                    dg = sb("dg", [128, NEXP, 128], F32, stack=ps_)
                for st_ in range(TH // TT):
                    t0 = st_ * TT
                    for s in range(NS):
                        sl = slice(s * 512, (s + 1) * 512)
                        k.dma('sp', xacc[:, :, sl],
                              xsrc[:, t0 + s * 512:t0 + (s + 1) * 512].rearrange('(kc p) t -> p kc t', p=128),
                              R=[Dxsrc], W=[xacc])
                        if pair:
                            k.dma('sp', sq[:], xsrc[:, TH + t0 + s * 512:TH + t0 + (s + 1) * 512].rearrange(
                                '(kc p) t -> p kc t', p=128), R=[Dxsrc], W=[sq])
                            k.ts('dve', xacc[:, :, sl], xacc[:, :, sl], rsel[:, 0:1], None, ALU.mult,
                                 R=[xacc, rsel], W=[xacc])
                            k.stt('dve', xacc[:, :, sl], sq[:], rsel[:, 1:2], xacc[:, :, sl], ALU.mult, ALU.add,
                                  R=[sq, rsel, xacc], W=[xacc])
                        norm_tile(xacc, xacc[:, :, sl], sq, lambda kc: h2[:, kc, sl], h2, 32, 24,
                                  hf=([hf] if moe else None))
                        if moe:
                            for q in range(4):
                                lg, m1, m2, e1, e2, wt8 = sm
                                ps = psum.get()
                                for kc in range(8):
                                    k.mm(ps, ps[:, 0:8], hf[:, kc, q * 128:(q + 1) * 128], rt[:, kc, :], R=[hf, rt],
                                         start=(kc == 0), stop=(kc == 7))
                                k.copy('dve', lg[:], ps[:, 0:8], R=[ps], W=[lg])
                                k.op('dve', lambda e: e.reduce_max(sc[0][:], lg[:], AX.X), R=[lg], W=[sc[0]])
                                k.ts('dve', e1[:], lg[:], sc[0][:, 0:1], None, ALU.is_equal, R=[lg, sc[0]], W=[e1])
                                k.stt('dve', m1[:], e1[:], -1e30, lg[:], ALU.mult, ALU.add, R=[e1, lg], W=[m1])
                                k.op('dve', lambda e: e.reduce_max(sc[1][:], m1[:], AX.X), R=[m1], W=[sc[1]])
                                k.ts('dve', e2[:], m1[:], sc[1][:, 0:1], None, ALU.is_equal, R=[m1, sc[1]], W=[e2])
                                k.tt('dve', sc[2][:], sc[0][:], sc[1][:], ALU.subtract, R=[sc[0], sc[1]], W=[sc[2]])
                                k.act(sc[3][:], sc[2][:], AF.Sigmoid, R=[sc[2]], W=[sc[3]])
                                k.act(sc[2][:], sc[2][:], AF.Sigmoid, R=[sc[2]], W=[sc[2]], scale=-1.0)
                                k.ts('dve', wt8[:], e1[:], sc[3][:, 0:1], None, ALU.mult, R=[e1, sc[3]], W=[wt8])
                                k.stt('dve', wt8[:], e2[:], sc[2][:, 0:1], wt8[:], ALU.mult, ALU.add,
                                      R=[e2, sc[2], wt8], W=[wt8])
                                k.tt('dve', dg[:], cst[:, 0:1, :].to_broadcast([128, NEXP, 128]),
                                     wt8[:, :].unsqueeze(2).to_broadcast([128, NEXP, 128]), ALU.mult,
                                     R=[cst, wt8], W=[dg])
                                for hh in range(2):
                                    ps2 = psum.get()
                                    k.mm(ps2, ps2[:, :], ones_f, dg[:, hh * 4:(hh + 1) * 4, :].rearrange('p e t -> p (e t)'), R=[cst, dg])
                                    c0 = s * 512 + q * 128
                                    k.copy('act', wrow[:, hh * 4:(hh + 1) * 4, c0:c0 + 128],
                                           ps2[:, :].rearrange('p (e t) -> p e t', e=4), R=[ps2], W=[wrow])
                    if moe:
                        passes = []
                        for e_ in range(NEXP):
                            for hh in range(2):
                                passes.append((moe_wg[li, e_], moe_wu[li, e_], moe_wd[li, e_], hh * 1792, 1792, e_))
                    else:
                        passes = [(ffn_wg[li], ffn_wu[li], ffn_wd[li], hh * 1408, 1408, None) for hh in range(2)]
                    for (wg_, wu_, wd_, h0, hn, ex) in passes:
                        HC = hn // 128
                        for cb in range(0, hn, 256):
                            cw = min(256, hn - cb)
                            wg = wgr.get()
                            wu = wur.get()
                            k.dma('pool', wg[:, :, :cw],
                                  wg_[:, h0 + cb:h0 + cb + cw].rearrange('(kc p) n -> p kc n', p=128), R=[Dw], W=[wg])
                            k.dma('pool', wu[:, :, :cw],
                                  wu_[:, h0 + cb:h0 + cb + cw].rearrange('(kc p) n -> p kc n', p=128), R=[Dw], W=[wu])
                            for jj in range(cw // 128):
                                j = cb // 128 + jj
                                for s in range(NS):
                                    sl = slice(s * 512, (s + 1) * 512)
                                    psg = psum.get()
                                    for kc in range(8):
                                        k.mm(psg, psg[:, :], wg[:, kc, jj * 128:(jj + 1) * 128], h2[:, kc, sl],
                                             R=[wg, h2], start=(kc == 0), stop=(kc == 7))
                                    psu = psum.get()
                                    for kc in range(8):
                                        k.mm(psu, psu[:, :], wu[:, kc, jj * 128:(jj + 1) * 128], h2[:, kc, sl],
                                             R=[wu, h2], start=(kc == 0), stop=(kc == 7))
                                    sg = sgr.get()
                                    k.act(sg[:], psg[:, :], AF.Silu, R=[psg], W=[sg])
                                    k.tt('dve', hid[:, j, sl], psu[:, :], sg[:], ALU.mult, R=[psu, sg], W=[hid])
                        for oh in range(2):
                            wd = wdr.get()
                            for c4 in range(0, HC, 7):
                                cn = min(7, HC - c4)
                                k.dma('pool', wd[:, c4:c4 + cn, :],
                                      wd_[h0 + c4 * 128:h0 + (c4 + cn) * 128, oh * 512:(oh + 1) * 512].rearrange(
                                          '(kc p) n -> p kc n', p=128), R=[Dw], W=[wd])
                            for oo in range(4):
                                o = oh * 4 + oo
                                for s in range(NS):
                                    sl = slice(s * 512, (s + 1) * 512)
                                    ps = psum.get()
                                    for j in range(HC):
                                        k.mm(ps, ps[:, :], wd[:, j, oo * 128:(oo + 1) * 128], hid[:, j, sl],
                                             R=[wd, hid], start=(j == 0), stop=(j == HC - 1))
                                    if ex is None:
                                        k.stt('dve', xacc[:, o, sl], ps[:, :], modT[:, 40 + o:41 + o], xacc[:, o, sl],
                                              ALU.mult, ALU.add, R=[ps, modT, xacc], W=[xacc])
                                    else:
                                        t_ = tmp.get()
                                        k.stt('dve', t_[:], ps[:, :], modT[:, 40 + o:41 + o], wrow[:, ex, sl],
                                              ALU.mult, ALU.mult, R=[ps, modT, wrow], W=[t_])
                                        k.tt('pool', xacc[:, o, sl], xacc[:, o, sl], t_[:], ALU.add,
                                             R=[t_, xacc], W=[xacc])
                    k.dma('sp', xdst[:, t0:t0 + TT].rearrange('(kc p) t -> p kc t', p=128), xacc[:], R=[xacc],
                          W=[Dxdst])
                k.barrier()

        def phase_final(xsrc, Dxsrc, ntiles):
            with ExitStack() as ps_:
                xr = Ring([sb("xf%d" % i, [128, 8, 512], F32, stack=ps_) for i in range(2)])
                orr = Ring([sb("of%d" % i, [128, 8, 512], F32, multi=True, stack=ps_) for i in range(2)])
                sq = sb("sqf", [128, 8, 512], F32, stack=ps_)
                for tt in range(ntiles):
                    ts_ = slice(tt * 512, (tt + 1) * 512)
                    xt = xr.get()
                    ot = orr.get()
                    for (a_, b_, sap) in xsrc(tt):
                        k.dma('sp', xt[:, a_:b_, :], sap, R=[Dxsrc], W=[xt])
                    norm_tile(xt, xt[:], sq, lambda kc: ot[:, kc, :], ot, None, None)
                    k.dma('sp', outT[:, ts_].rearrange('(kc p) t -> p kc t', p=128), ot[:], R=[ot], W=[Dout])
                k.barrier()

        def tiles_of(ap):
            return lambda tt: [(0, 8, ap[:, tt * 512:(tt + 1) * 512].rearrange('(kc p) t -> p kc t', p=128))]

        def tiles_of_xg(tt):
            r_, off = (tt * 512) // TH, (tt * 512) % TH
            return [(2 * kb, 2 * kb + 2,
                     xg[kb * 512 + r_ * 256:kb * 512 + r_ * 256 + 256, off:off + 512].rearrange(
                         '(kl p) t -> p kl t', p=128)) for kb in range(4)]

        cur, Dcur = tiles_of(xT_in), Dxin
        fin, Dfin, nfin = cur, Dcur, NT
        for li_, l in enumerate(layers):
            phase_ada(l)
            full, Dfull = None, None
            if 'mix' in stages:
                phase_inproj(l, cur, Dcur)
                phase_mixers(l)
                phase_merge(l, cur, Dcur, x_b, Dx_b)
                cur, Dcur = tiles_of(x_b), Dx_b
                full, Dfull = x_b, Dx_b
                fin, Dfin, nfin = cur, Dcur, NT
            if 'ffn' in stages:
                if full is None:
                    assert not pair
                    full, Dfull = (xT_in, Dxin) if li_ == 0 else (x_a, Dx_a)
                if pair:
                    phase_ffn(l, full, Dfull, xh, Dxh)
                    fin, Dfin, nfin = tiles_of(xh), Dxh, TH // 512
                    if li_ != len(layers) - 1:
                        allgather([(xh[kb * 256:(kb + 1) * 256, :], xg[kb * 512:(kb + 1) * 512, :])
                                   for kb in range(4)], Dxh, Dxg)
                        cur, Dcur = tiles_of_xg, Dxg
                else:
                    phase_ffn(l, full, Dfull, x_a, Dx_a)
                    cur, Dcur = tiles_of(x_a), Dx_a
                    fin, Dfin, nfin = cur, Dcur, NT
        phase_final(fin, Dfin, nfin)
        k.barrier()
        print("instructions:", k.ninst, {kk: v for kk, v in k.cnt.items()})
    return nc


def _consts():
    c = np.zeros((128, 8, 128), np.float32)
    i = np.arange(128)
    c[:, 0, :] = np.eye(128)
    c[:, 1, :] = 1.0
    c[:, 2, :] = (i[:, None] <= i[None, :])
    c[:, 3, :] = np.where(i[None, :] > i[:, None], 1e9, 0.0)
    c[:, 4, :] = (i[None, :] < i[:, None])
    rot = np.zeros((128, 128), np.float32)
    for o in (0, 64):
        for m in range(32):
            rot[o + m + 32, o + m] = -1.0
            rot[o + m, o + m + 32] = 1.0
    c[:, 5, :] = rot
    c[:, 6, :] = np.where(i[None, :] < i[:, None], 1e9, 0.0)
    return c


def _fm(v, nchunk):
    return np.ascontiguousarray(np.asarray(v, np.float32).reshape(nchunk, 128).T)


_PERM_CACHE = {}


def _perm_weights(inp, r):
    if r in _PERM_CACHE:
        return _PERM_CACHE[r]
    perm = [2 * r, 2 * r + 1] + [h for h in range(4) if h not in (2 * r, 2 * r + 1)]
    out = {}
    cols = np.arange(IN_DIM)
    for base in (0, 512, 1024, C_GZ):
        for i, h in enumerate(perm):
            cols[base + i * 128:base + (i + 1) * 128] = np.arange(base + h * 128, base + (h + 1) * 128)
    for base in (C_A, C_B):
        for i, h in enumerate(perm):
            cols[base + i] = base + h
    gp = [r, 1 - r]

    def blocks(arr, base, bs):
        src = arr.copy()
        for i, g_ in enumerate(gp):
            arr[base + i * bs:base + (i + 1) * bs] = src[base + g_ * bs:base + (g_ + 1) * bs]

    for (base, bs) in ((C_SZ, 256), (C_XBC, 256), (C_XBC + 512, 128), (C_XBC + 768, 128), (C_DT, 4)):
        blocks(cols, base, bs)
    out["w_in"] = np.ascontiguousarray(np.asarray(inp["w_in"], np.float32)[:, :, cols])
    sc_ = np.arange(1024)
    for (base, bs) in ((0, 256), (512, 128), (768, 128)):
        blocks(sc_, base, bs)
    out["ssm_conv_w"] = np.ascontiguousarray(np.asarray(inp["ssm_conv_w"], np.float32)[:, :, sc_])
    out["ssm_conv_b"] = np.ascontiguousarray(np.asarray(inp["ssm_conv_b"], np.float32)[:, sc_])
    h8 = np.arange(8)
    blocks(h8, 0, 4)
    for n_ in ("ssm_a_log", "ssm_dt_bias", "ssm_d"):
        out[n_] = np.ascontiguousarray(np.asarray(inp[n_], np.float32)[:, h8])
    n512 = np.arange(512)
    blocks(n512, 0, 256)
    out["ssm_norm_w"] = np.ascontiguousarray(np.asarray(inp["ssm_norm_w"], np.float32)[:, n512])
    cc = np.arange(1536)
    for base in (0, 512, 1024):
        for i, h in enumerate(perm):
            cc[base + i * 128:base + (i + 1) * 128] = np.arange(base + h * 128, base + (h + 1) * 128)
    out["gdn_conv_w"] = np.ascontiguousarray(np.asarray(inp["gdn_conv_w"], np.float32)[:, :, cc])
    out["gdn_a_log"] = np.ascontiguousarray(np.asarray(inp["gdn_a_log"], np.float32)[:, perm])
    out["gdn_dt_bias"] = np.ascontiguousarray(np.asarray(inp["gdn_dt_bias"], np.float32)[:, perm])
    cq = np.concatenate([np.arange(h * 192, (h + 1) * 192) for h in perm])
    ck = np.concatenate([np.arange(h * 128, (h + 1) * 128) for h in perm])
    out["mla_w_uq"] = np.ascontiguousarray(np.asarray(inp["mla_w_uq"], np.float32)[:, :, cq])
    out["mla_w_uk"] = np.ascontiguousarray(np.asarray(inp["mla_w_uk"], np.float32)[:, :, ck])
    out["mla_w_uv"] = np.ascontiguousarray(np.asarray(inp["mla_w_uv"], np.float32)[:, :, ck])
    _PERM_CACHE[r] = out
    return out


def prep_inputs(inp, b, T, r=None):
    if r is not None and r != 0:
        inp = dict(inp)
        inp.update(_perm_weights(inp, r))
    f = lambda a: np.ascontiguousarray(np.asarray(a, np.float32))
    L = DEPTH
    m = {}
    m["xT"] = np.ascontiguousarray(np.asarray(inp["x"][b, :T], np.float32).T)
    m["cT"] = _fm(inp["c"][b], 8)
    m["pos"] = np.ascontiguousarray(np.asarray(inp["positions"][b, :T], np.int32).reshape(1, T))
    m["w_ada"] = f(inp["w_ada"])
    m["b_adaT"] = np.stack([_fm(inp["b_ada"][l], 48) for l in range(L)])
    m["w_in"] = f(inp["w_in"])
    gc = np.asarray(inp["gdn_conv_w"], np.float32)
    m["gdn_convT"] = np.ascontiguousarray(gc.reshape(L, 4, 12, 128).transpose(0, 3, 2, 1))
    rep = lambda a: np.ascontiguousarray(np.broadcast_to(np.asarray(a, np.float32)[:, None, :], (L, 128, a.shape[-1])))
    m["gdn_alog"] = rep(inp["gdn_a_log"])
    m["gdn_dtb"] = rep(inp["gdn_dt_bias"])
    m["gdn_nw"] = np.ascontiguousarray(np.asarray(inp["gdn_norm_w"], np.float32).reshape(L, 128, 1))
    sc = np.asarray(inp["ssm_conv_w"], np.float32)
    m["ssm_convT"] = np.ascontiguousarray(sc.reshape(L, 4, 8, 128).transpose(0, 3, 2, 1))
    m["ssm_convb"] = np.stack([_fm(inp["ssm_conv_b"][l], 8) for l in range(L)])
    m["ssm_alog"] = rep(inp["ssm_a_log"])
    m["ssm_dtb"] = rep(inp["ssm_dt_bias"])
    dexp = np.repeat(np.asarray(inp["ssm_d"], np.float32), 64, axis=1)
    m["ssm_dexp"] = np.stack([_fm(dexp[l], 4) for l in range(L)])
    m["ssm_nw"] = np.stack([_fm(inp["ssm_norm_w"][l], 4) for l in range(L)])
    m["mla_qnw"] = np.stack([_fm(inp["mla_q_norm_w"][l], 4) for l in range(L)])
    m["mla_wuq"] = f(inp["mla_w_uq"])
    m["mla_kvnw"] = np.stack([_fm(inp["mla_kv_norm_w"][l], 2) for l in range(L)])
    m["mla_wuk"] = f(inp["mla_w_uk"])
    m["mla_wuv"] = f(inp["mla_w_uv"])
    for n in ("w_branch_a", "w_branch_b", "w_branch_c", "w_out", "ffn_w_gate", "ffn_w_up", "ffn_w_down",
              "moe_router", "moe_w_gate", "moe_w_up", "moe_w_down"):
        m[n] = f(inp[n])
    m["fnwT"] = _fm(inp["final_norm_w"], 8)
    m["consts"] = _consts()
    m["rsel"] = np.stack([np.ones(128, np.float32), np.zeros(128, np.float32)], 1)
    invf = (10000.0 ** (-np.arange(0, 64, 2, dtype=np.float32) / 64)).astype(np.float32)
    m["invf"] = np.concatenate([invf] * 4).reshape(128, 1).astype(np.float32)
    return m


def kernel(**inputs):
    B, T = inputs["x"].shape[0], inputs["x"].shape[1]
    nc = build_program(T, list(range(DEPTH)), pair=True)
    base = [prep_inputs(inputs, b, T) for b in range(B)]
    in_maps = []
    _PERM_CACHE.clear()
    base1 = [prep_inputs(inputs, b, T, r=1) for b in range(B)]
    for c in range(2 * B):
        m = dict((base, base1)[c % 2][c // 2])
        rs = np.zeros((128, 2), np.float32)
        rs[:, c % 2] = 1.0
        m["rsel"] = rs
        in_maps.append(m)
    res = run_bass_kernel_spmd(nc, in_maps, core_ids=list(range(2 * B)))
    TH = T // 2
    out = np.zeros((B, T, D), np.float32)
    for c in range(2 * B):
        out[c // 2, (c % 2) * TH:(c % 2 + 1) * TH, :] = np.asarray(res.results[c]["outT"], np.float32).T
    return out
```
